# Optimizing a Trainium2 kernel written in Bass

```python
import math
import jax, jax.numpy as jnp
from jax import lax
import numpy as np

D_MODEL = 1024
BATCH = 4
SEQ = 4096
DEPTH = 1
DEC_BATCH = 128
DEC_SEQ = 1
PAST_LEN = 8192
PAGE_SIZE = 128

GDN_HEADS = 4
GDN_DK = 128
GDN_DV = 128
GDN_CONV = 4
GDN_CHUNK = 64
GDN_QK = GDN_HEADS * GDN_DK
GDN_VW = GDN_HEADS * GDN_DV
GDN_CONV_CH = 2 * GDN_QK + GDN_VW
GDN_COLS = GDN_CONV_CH + GDN_VW + 2 * GDN_HEADS

ATT_GROUPS = ((128, 1), (512, 4), (2048, 16))
N_ATT_GROUPS = len(ATT_GROUPS)
ATT_HEADS = 8
ATT_HD = 64
ATT_GROUP_COLS = 3 * ATT_HEADS * ATT_HD
ROT_DIM = ATT_HD // 4
ROPE_THETA = 500000.0

IN_COLS = GDN_COLS + N_ATT_GROUPS * ATT_GROUP_COLS
MIX_WIDTH = GDN_VW + ATT_HEADS * ATT_HD

MOE_GROUPS = 4
MOE_PER_GROUP = 8
MOE_EXPERTS = MOE_GROUPS * MOE_PER_GROUP
MOE_TOPK = 2
MOE_FF = 256
EPS = 1e-6

kernel_name = 'hybrid_gdn_dilated_swa_hmoe_step'


def rmsnorm(x, w):
    xf = x.astype(jnp.float32)
    y = xf * lax.rsqrt(jnp.mean(xf * xf, axis=-1, keepdims=True) + EPS)
    return (y * w.astype(jnp.float32)).astype(x.dtype)


def l2norm(x):
    xf = x.astype(jnp.float32)
    return xf * lax.rsqrt(jnp.sum(xf * xf, axis=-1, keepdims=True) + EPS)


def rope(x, pos):
    half = ROT_DIM // 2
    inv = jnp.exp(-math.log(ROPE_THETA) * jnp.arange(half, dtype=jnp.float32) * (2.0 / ROT_DIM))
    ang = pos.astype(jnp.float32)[:, None] * inv[None, :]
    cos = jnp.cos(ang)[:, None, :]
    sin = jnp.sin(ang)[:, None, :]
    xf = x.astype(jnp.float32)
    x1 = xf[..., :half]
    x2 = xf[..., half:ROT_DIM]
    out = jnp.concatenate([x1 * cos - x2 * sin, x2 * cos + x1 * sin, xf[..., ROT_DIM:]], axis=-1)
    return out.astype(x.dtype)


def causal_conv(u, buf, w):
    full = jnp.concatenate([buf.astype(u.dtype), u], axis=1)
    rhs = jnp.transpose(w).astype(u.dtype)[:, None, :]
    out = lax.conv_general_dilated(full, rhs, window_strides=(1,), padding='VALID',
                                   dimension_numbers=('NWC', 'WIO', 'NWC'),
                                   feature_group_count=u.shape[-1])
    return jax.nn.silu(out), full[:, -(GDN_CONV - 1):]


def gdn_chunked(q, k, v, g, beta, s0):
    f32 = jnp.float32
    b, l, h, _ = k.shape
    dv = v.shape[-1]
    c = min(GDN_CHUNK, l)
    n = -(-l // c)
    pad = n * c - l

    def prep(t):
        t = t.astype(f32)
        t = jnp.pad(t, [(0, 0), (0, pad)] + [(0, 0)] * (t.ndim - 2))
        t = jnp.moveaxis(t.reshape((b, n, c) + t.shape[2:]), 1, 0)
        return jnp.swapaxes(t, 2, 3)

    qc, kc, vc, gc, bc = prep(q), prep(k), prep(v), prep(g), prep(beta)
    gcum = jnp.cumsum(gc, axis=-1)
    causal = jnp.tril(jnp.ones((c, c), dtype=bool))
    strict = jnp.tril(jnp.ones((c, c), dtype=bool), -1)
    decay = jnp.exp(jnp.where(causal, gcum[..., :, None] - gcum[..., None, :], -jnp.inf))
    kb = kc * bc[..., None]
    a_low = jnp.where(strict, jnp.einsum('nbhid,nbhjd->nbhij', kb, kc) * decay, 0.0)
    tmat = a_low + jnp.eye(c, dtype=f32)
    u = lax.linalg.triangular_solve(tmat, vc * bc[..., None], left_side=True, lower=True, unit_diagonal=True)
    w = lax.linalg.triangular_solve(tmat, kb * jnp.exp(gcum)[..., None], left_side=True, lower=True, unit_diagonal=True)
    intra = jnp.where(causal, jnp.einsum('nbhid,nbhjd->nbhij', qc, kc) * decay, 0.0)

    def step(s, inp):
        q_i, k_i, u_i, w_i, a_i, g_i = inp
        v_new = u_i - jnp.einsum('bhck,bhkv->bhcv', w_i, s)
        o_i = (jnp.einsum('bhck,bhkv->bhcv', q_i * jnp.exp(g_i)[..., None], s)
               + jnp.einsum('bhij,bhjv->bhiv', a_i, v_new))
        g_last = g_i[..., -1]
        s = (s * jnp.exp(g_last)[..., None, None]
             + jnp.einsum('bhck,bhcv->bhkv', k_i * jnp.exp(g_last[..., None] - g_i)[..., None], v_new))
        return s, o_i

    s_fin, o = lax.scan(step, s0.astype(f32), (qc, kc, u, w, intra, gcum))
    o = o.transpose(1, 0, 3, 2, 4).reshape(b, n * c, h, dv)[:, :l]
    return o, s_fin


def dilated_attn_prompt(q, k, v, window, dilation):
    b, s, h, d = q.shape
    span = window // dilation
    m = s // dilation
    nb = -(-m // span)
    mp = nb * span

    def sub(t):
        t = t.reshape(b, m, dilation, h, d).transpose(0, 2, 1, 3, 4)
        t = jnp.pad(t, ((0, 0), (0, 0), (0, mp - m), (0, 0), (0, 0)))
        return t.reshape(b, dilation, nb, span, h, d)

    def band(t):
        prev = jnp.pad(t, ((0, 0), (0, 0), (1, 0), (0, 0), (0, 0), (0, 0)))[:, :, :nb]
        return jnp.concatenate([prev, t], axis=3)

    qs, ks, vs = sub(q), sub(k), sub(v)
    kb, vb = band(ks), band(vs)
    scores = jnp.einsum('brnqhd,brnkhd->brnhqk', qs, kb, preferred_element_type=jnp.float32) * (d ** -0.5)
    qi = jnp.arange(span)[:, None]
    kj = jnp.arange(2 * span)[None, :]
    dist = span + qi - kj
    in_band = (dist >= 0) & (dist <= span)
    has_prev = (jnp.arange(nb) > 0)[:, None, None] | (kj >= span)[None]
    mask = in_band[None] & has_prev
    scores = jnp.where(mask[None, None, :, None], scores, -jnp.inf)
    mx = jnp.max(scores, axis=-1, keepdims=True)
    p = jnp.exp(scores - mx)
    den = jnp.sum(p, axis=-1, keepdims=True)
    o = jnp.einsum('brnhqk,brnkhd->brnqhd', p / den, vb.astype(jnp.float32))
    lse = (mx + jnp.log(den))[..., 0]
    o = o.reshape(b, dilation, mp, h, d)[:, :, :m].transpose(0, 2, 1, 3, 4).reshape(b, s, h, d)
    lse = lse.transpose(0, 1, 2, 4, 3).reshape(b, dilation, mp, h)[:, :, :m].transpose(0, 2, 1, 3).reshape(b, s, h)
    return o, lse


def dilated_attn_sample(q, k, v, kv_buf, window, dilation):
    b, l, h, d = q.shape
    wb = kv_buf.shape[1]
    span = window // dilation
    kv_all = jnp.concatenate([kv_buf, jnp.stack([k, v], axis=2).astype(kv_buf.dtype)], axis=1)
    idx = wb + jnp.arange(l)[:, None] - dilation * jnp.arange(span + 1)[None, :]
    valid = idx >= 0
    kv_g = kv_all[:, jnp.maximum(idx, 0)]
    scores = jnp.einsum('blhd,bljhd->blhj', q, kv_g[:, :, :, 0], preferred_element_type=jnp.float32) * (d ** -0.5)
    scores = jnp.where(valid[None, :, None, :], scores, -jnp.inf)
    mx = jnp.max(scores, axis=-1, keepdims=True)
    p = jnp.exp(scores - mx)
    den = jnp.sum(p, axis=-1, keepdims=True)
    o = jnp.einsum('blhj,bljhd->blhd', p / den, kv_g[:, :, :, 1].astype(jnp.float32))
    lse = (mx + jnp.log(den))[..., 0]
    new_buf = kv_all[:, -min(window, wb + l):]
    return o, lse, new_buf


def hier_moe(x, w_router_group, w_router_expert, w_gate_up, w_down):
    b, l, dm = x.shape
    t = x.reshape(b * l, dm)
    nt = b * l
    g_logits = jnp.einsum('td,dg->tg', t, w_router_group, preferred_element_type=jnp.float32)
    g_prob = jax.nn.softmax(g_logits, axis=-1)
    _, g_idx = lax.top_k(g_logits, 1)
    p_group = jnp.take_along_axis(g_prob, g_idx, axis=-1)
    e_logits = jnp.einsum('td,de->te', t, w_router_expert, preferred_element_type=jnp.float32)
    e_logits = e_logits.reshape(nt, MOE_GROUPS, MOE_PER_GROUP)
    e_in = jnp.take_along_axis(e_logits, g_idx[:, :, None], axis=1)[:, 0]
    e_top, e_idx = lax.top_k(e_in, MOE_TOPK)
    weights = jax.nn.softmax(e_top, axis=-1) * p_group
    expert = g_idx * MOE_PER_GROUP + e_idx
    gate = jnp.einsum('tk,tke->te', weights, jax.nn.one_hot(expert, MOE_EXPERTS, dtype=jnp.float32))
    y = jnp.zeros((nt, dm), jnp.float32)
    for e in range(MOE_EXPERTS):
        gu = jnp.dot(t, w_gate_up[e])
        hid = jax.nn.silu(gu[:, :MOE_FF]) * gu[:, MOE_FF:]
        y = y + gate[:, e:e + 1] * jnp.dot(hid, w_down[e], preferred_element_type=jnp.float32)
    return y.astype(x.dtype).reshape(b, l, dm)


def hybrid_layer(x, pos, gdn_s0, conv_buf, kv_bufs, norm1_w, w_in, conv_w, a_log, dt_bias, gdn_norm_w,
                 q_norm_w, k_norm_w, w_out, norm2_w, w_router_group, w_router_expert, w_gate_up, w_down):
    b, l, _ = x.shape
    xn = rmsnorm(x, norm1_w)
    proj = jnp.einsum('bld,dc->blc', xn, w_in)
    c0 = GDN_CONV_CH
    qkv_a = proj[..., :c0]
    z = proj[..., c0:c0 + GDN_VW].reshape(b, l, GDN_HEADS, GDN_DV)
    b_raw = proj[..., c0 + GDN_VW:c0 + GDN_VW + GDN_HEADS]
    a_raw = proj[..., c0 + GDN_VW + GDN_HEADS:GDN_COLS]
    att = proj[..., GDN_COLS:].reshape(b, l, N_ATT_GROUPS, 3, ATT_HEADS, ATT_HD)

    qkv_c, conv_new = causal_conv(qkv_a, conv_buf, conv_w)
    qa = l2norm(qkv_c[..., :GDN_QK].reshape(b, l, GDN_HEADS, GDN_DK)) * (GDN_DK ** -0.5)
    ka = l2norm(qkv_c[..., GDN_QK:2 * GDN_QK].reshape(b, l, GDN_HEADS, GDN_DK))
    va = qkv_c[..., 2 * GDN_QK:].reshape(b, l, GDN_HEADS, GDN_DV)
    beta = jax.nn.sigmoid(b_raw.astype(jnp.float32))
    g = -jnp.exp(a_log.astype(jnp.float32)) * jax.nn.softplus(a_raw.astype(jnp.float32) + dt_bias.astype(jnp.float32))
    oa, s_new = gdn_chunked(qa, ka, va, g, beta, gdn_s0)
    oa = (rmsnorm(oa, gdn_norm_w) * jax.nn.silu(z.astype(jnp.float32))).astype(x.dtype)

    outs, lses, new_bufs = [], [], []
    for gi in range(N_ATT_GROUPS):
        window, dilation = ATT_GROUPS[gi]
        qg = rope(rmsnorm(att[:, :, gi, 0], q_norm_w[gi]), pos)
        kg = rope(rmsnorm(att[:, :, gi, 1], k_norm_w[gi]), pos)
        vg = att[:, :, gi, 2]
        if kv_bufs is None:
            o_g, lse_g = dilated_attn_prompt(qg, kg, vg, window, dilation)
            buf_g = jnp.stack([kg, vg], axis=2)[:, -min(window, l):]
        else:
            o_g, lse_g, buf_g = dilated_attn_sample(qg, kg, vg, kv_bufs[gi], window, dilation)
        outs.append(o_g)
        lses.append(lse_g)
        new_bufs.append(buf_g)
    wgt = jax.nn.softmax(jnp.stack(lses, axis=0), axis=0)
    ob = jnp.sum(wgt[..., None] * jnp.stack(outs, axis=0), axis=0)
    ob = ob.reshape(b, l, ATT_HEADS * ATT_HD).astype(x.dtype)

    mix = jnp.concatenate([oa.reshape(b, l, GDN_VW), ob], axis=-1)
    h = x + jnp.einsum('blm,md->bld', mix, w_out)
    y = h + hier_moe(rmsnorm(h, norm2_w), w_router_group, w_router_expert, w_gate_up, w_down)
    return y, s_new.astype(gdn_s0.dtype), conv_new, new_bufs


def setup_inputs(seed: int = 0) -> dict:
    key = jax.random.key(seed)
    ks = jax.random.split(key, 24)
    f32 = jnp.float32

    def nrm(k, shape, scale):
        return jax.random.normal(k, shape, f32) * scale

    def kv(k, window):
        return nrm(k, (DEPTH, DEC_BATCH, min(window, PAST_LEN), 2, ATT_HEADS, ATT_HD), 1.0)

    dt = jnp.exp(jax.random.uniform(ks[11], (DEPTH, GDN_HEADS), f32, math.log(1e-3), math.log(1e-1)))
    return {
        'x_prompt': nrm(ks[0], (BATCH, SEQ, D_MODEL), 1.0),
        'x_sample': nrm(ks[1], (DEC_BATCH, DEC_SEQ, D_MODEL), 1.0),
        'state_gdn': nrm(ks[2], (DEPTH, DEC_BATCH, GDN_HEADS, GDN_DK, GDN_DV), 0.1),
        'state_conv': nrm(ks[3], (DEPTH, DEC_BATCH, GDN_CONV - 1, GDN_CONV_CH), 1.0),
        'cache_kv_w128': kv(ks[4], ATT_GROUPS[0][0]),
        'cache_kv_w512': kv(ks[5], ATT_GROUPS[1][0]),
        'cache_kv_w2048': kv(ks[6], ATT_GROUPS[2][0]),
        'norm1_w': 1.0 + nrm(ks[7], (DEPTH, D_MODEL), 0.02),
        'w_in': nrm(ks[8], (DEPTH, D_MODEL, IN_COLS), D_MODEL ** -0.5),
        'conv_w': nrm(ks[9], (DEPTH, GDN_CONV_CH, GDN_CONV), GDN_CONV ** -0.5),
        'a_log': jnp.log(jax.random.uniform(ks[10], (DEPTH, GDN_HEADS), f32, 1.0, 16.0)),
        'dt_bias': dt + jnp.log(-jnp.expm1(-dt)),
        'gdn_norm_w': 1.0 + nrm(ks[12], (DEPTH, GDN_DV), 0.02),
        'q_norm_w': 1.0 + nrm(ks[13], (DEPTH, N_ATT_GROUPS, ATT_HD), 0.02),
        'k_norm_w': 1.0 + nrm(ks[14], (DEPTH, N_ATT_GROUPS, ATT_HD), 0.02),
        'w_out': nrm(ks[15], (DEPTH, MIX_WIDTH, D_MODEL), MIX_WIDTH ** -0.5),
        'norm2_w': 1.0 + nrm(ks[16], (DEPTH, D_MODEL), 0.02),
        'w_router_group': nrm(ks[17], (DEPTH, D_MODEL, MOE_GROUPS), D_MODEL ** -0.5),
        'w_router_expert': nrm(ks[18], (DEPTH, D_MODEL, MOE_EXPERTS), D_MODEL ** -0.5),
        'w_gate_up': nrm(ks[19], (DEPTH, MOE_EXPERTS, D_MODEL, 2 * MOE_FF), D_MODEL ** -0.5),
        'w_down': nrm(ks[20], (DEPTH, MOE_EXPERTS, MOE_FF, D_MODEL), MOE_FF ** -0.5),
    }


def reference(x_prompt, x_sample, state_gdn, state_conv, cache_kv_w128, cache_kv_w512, cache_kv_w2048,
              norm1_w, w_in, conv_w, a_log, dt_bias, gdn_norm_w, q_norm_w, k_norm_w, w_out, norm2_w,
              w_router_group, w_router_expert, w_gate_up, w_down):
    bp, lp = x_prompt.shape[0], x_prompt.shape[1]
    ls = x_sample.shape[1]
    pos_p = jnp.arange(lp, dtype=jnp.int32)
    pos_s = PAST_LEN + jnp.arange(ls, dtype=jnp.int32)
    gdn0 = jnp.zeros((bp, GDN_HEADS, GDN_DK, GDN_DV), state_gdn.dtype)
    conv0 = jnp.zeros((bp, GDN_CONV - 1, GDN_CONV_CH), x_prompt.dtype)
    hp, hs = x_prompt, x_sample
    gdn_p, conv_p, w128_p, w512_p, w2048_p = [], [], [], [], []
    gdn_s, conv_s, w128_s, w512_s, w2048_s = [], [], [], [], []
    for li in range(DEPTH):
        wl = (norm1_w[li], w_in[li], conv_w[li], a_log[li], dt_bias[li], gdn_norm_w[li], q_norm_w[li],
              k_norm_w[li], w_out[li], norm2_w[li], w_router_group[li], w_router_expert[li],
              w_gate_up[li], w_down[li])
        hp, sg, sc, bufs = hybrid_layer(hp, pos_p, gdn0, conv0, None, *wl)
        gdn_p.append(sg)
        conv_p.append(sc)
        w128_p.append(bufs[0])
        w512_p.append(bufs[1])
        w2048_p.append(bufs[2])
        hs, sg, sc, bufs = hybrid_layer(hs, pos_s, state_gdn[li], state_conv[li],
                                        (cache_kv_w128[li], cache_kv_w512[li], cache_kv_w2048[li]), *wl)
        gdn_s.append(sg)
        conv_s.append(sc)
        w128_s.append(bufs[0])
        w512_s.append(bufs[1])
        w2048_s.append(bufs[2])
    state_gdn_prompt = jnp.stack(gdn_p)
    state_conv_prompt = jnp.stack(conv_p)
    kv_w128_prompt = jnp.stack(w128_p)
    kv_w512_prompt = jnp.stack(w512_p)
    kv_w2048_prompt = jnp.stack(w2048_p)
    state_gdn_sample = jnp.stack(gdn_s)
    state_conv_sample = jnp.stack(conv_s)
    kv_w128_sample = jnp.stack(w128_s)
    kv_w512_sample = jnp.stack(w512_s)
    kv_w2048_sample = jnp.stack(w2048_s)
    return (hp, hs, state_gdn_prompt, state_conv_prompt, kv_w128_prompt, kv_w512_prompt, kv_w2048_prompt,
            state_gdn_sample, state_conv_sample, kv_w128_sample, kv_w512_sample, kv_w2048_sample)
```

```python
import math
import numpy as np
import concourse.bass as bass
import concourse.mybir as mybir
from concourse.bass_utils import run_bass_kernel_spmd

F32 = mybir.dt.float32
BF16 = mybir.dt.bfloat16
AF = mybir.ActivationFunctionType
ALU = mybir.AluOpType
AX = mybir.AxisListType

NDMA = 24
NCORE = 8
D = 1024
INC = 6664
NTILE = 33
EPS = 1e-6
GROUPS = ((128, 1), (512, 4), (2048, 16))
COLBLK = [(0, 512), (512, 512), (1024, 512), (1536, 512), (2048, 8)]
for _g in range(3):
    _b = 2056 + _g * 1536
    COLBLK += [(_b, 512), (_b + 512, 512), (_b + 1024, 512)]
HALO_SKIP = {3, 5, 8, 11}


class Sched:
    ENG = ['pe', 'act', 'dve', 'pool', 'sp']

    def __init__(self, nc):
        self.nc = nc
        self.ops = {e: [] for e in self.ENG}
        self.esem = {e: nc.alloc_semaphore(name="s_" + e) for e in self.ENG}
        self.ecnt = {e: 0 for e in self.ENG}
        self.dpool = {'sp': list(range(0, 12)), 'pool': list(range(12, 20)), 'act': list(range(20, 24))}
        self.dsem = [nc.alloc_semaphore(name="d%d" % i) for i in range(NDMA)]
        self.dcnt = [0] * NDMA
        self.dnext = {'sp': 0, 'pool': 0, 'act': 0}
        self.last_w = {}
        self.readers = {}
        self.waited = {e: {} for e in self.ENG}

    def _tok_waits(self, e, deps):
        waits = []
        for (sem, val) in deps:
            sid = id(sem)
            if e == 'pe' and sem is self.esem['pe']:
                continue
            if self.waited[e].get(sid, 0) < val:
                self.waited[e][sid] = val
                waits.append((sem, val))
        return waits

    def op(self, e, fn, reads=(), writes=(), dma=False):
        deps = []
        for k in reads:
            if k in self.last_w:
                deps.append(self.last_w[k])
        for k in writes:
            if k in self.last_w:
                deps.append(self.last_w[k])
            for sid, tk in self.readers.get(k, {}).items():
                deps.append(tk)
        if dma:
            pl = self.dpool[e]
            s = pl[self.dnext[e] % len(pl)]
            self.dnext[e] += 1
            if self.dcnt[s] > 0:
                deps.append((self.dsem[s], 16 * self.dcnt[s]))
            self.dcnt[s] += 1
            tok = (self.dsem[s], 16 * self.dcnt[s])
            inc = 16
        else:
            self.ecnt[e] += 1
            tok = (self.esem[e], self.ecnt[e])
            inc = 1
        waits = self._tok_waits(e, deps)
        for k in writes:
            self.last_w[k] = tok
            self.readers[k] = {}
        for k in reads:
            r = self.readers.setdefault(k, {})
            sid = id(tok[0])
            if sid not in r or r[sid][1] < tok[1]:
                r[sid] = tok
        self.ops[e].append((fn, waits, (tok[0], inc)))
        return tok

    def barrier(self):
        toks = [(self.esem[e], self.ecnt[e]) for e in self.ENG if self.ecnt[e] > 0]
        toks += [(self.dsem[s], 16 * self.dcnt[s]) for s in range(NDMA) if self.dcnt[s] > 0]
        for e in self.ENG:
            waits = self._tok_waits(e, [t for t in toks if not (t[0] is self.esem[e])])
            if waits:
                self.ops[e].append((None, waits, None))
        self.last_w = {}
        self.readers = {}

    def emit(self):
        nc = self.nc
        self.barrier()
        with nc.Block() as block:
            def mk(e):
                def body(engine):
                    for (fn, waits, inc) in self.ops[e]:
                        for (sem, val) in waits:
                            engine.wait_ge(sem, val)
                        if fn is not None:
                            ins = fn(engine)
                            ins.then_inc(inc[0], inc[1])
                return body
            block.tensor(mk('pe'))
            block.scalar(mk('act'))
            block.vector(mk('dve'))
            block.gpsimd(mk('pool'))
            block.sync(mk('sp'))


def _shape_view(v, shape):
    if len(shape) == 2:
        return v
    if len(shape) == 3:
        return v.rearrange("p (a b) -> p a b", a=shape[1])
    return v.rearrange("p (a b c) -> p a b c", a=shape[1], b=shape[2])


class Arena:
    def __init__(self, ap, nwords):
        self.ap = ap
        self.n = nwords
        self.off = 0

    def f32(self, shape):
        n = int(np.prod(shape[1:]))
        o = self.off
        self.off += n
        assert self.off <= self.n, ("arena overflow", self.off, self.n)
        return _shape_view(self.ap[0:shape[0], o:o + n], shape)

    def bf16(self, shape):
        n = int(np.prod(shape[1:]))
        nw = (n + 1) // 2
        o = self.off
        self.off += nw
        assert self.off <= self.n, ("arena overflow", self.off, self.n)
        return _shape_view(self.ap[0:shape[0], o:o + nw].bitcast(BF16)[:, 0:n], shape)


def build_program(stop_after=None, debug=False):
    nc = bass.Bass("TRN2", target_bir_lowering=False)

    def din(name, shape):
        return nc.dram_tensor(name, list(shape), F32, kind="ExternalInput").ap()

    def dout(name, shape):
        return nc.dram_tensor(name, list(shape), F32, kind="ExternalOutput").ap()

    def dscr(name, shape):
        return nc.dram_tensor(name, list(shape), F32, kind=("ExternalOutput" if debug else "Internal")).ap()

    x_all = din("x_all", [NTILE * 128, D])
    w_in = din("w_in", [D, INC])
    w_out = din("w_out", [D, D])
    norm1 = din("norm1", [1, D])
    norm2 = din("norm2", [1, D])
    conv_wT = din("conv_wT", [4, 1536])
    a_log = din("a_log", [1, 4])
    dt_bias = din("dt_bias", [1, 4])
    gnw = din("gnw", [1, 128])
    qnw = din("qnw", [3, 64])
    knw = din("knw", [3, 64])
    w_r = din("w_r", [D, 36])
    w_gu = din("w_gu", [32, D, 512])
    w_d = din("w_d", [32, 256, D])
    consts = din("consts", [128, 5, 128])
    rope_t = din("rope_t", [NTILE * 128, 16])
    maskb = din("maskb", [128, 1])
    selh = din("selh", [8, 16, 16])
    st_gdn = din("st_gdn", [16, 4, 128, 128])
    st_conv = din("st_conv", [16, 3, 1536])
    caches = [din("cache%d" % i, [16, GROUPS[i][0], 1024]) for i in range(3)]

    y_own = dout("y_own", [2048, D])
    y_samp = dout("y_samp", [128, D])
    sgp = dout("sgp", [4, 128, 128])
    scp = dout("scp", [3, 1536])
    kvp = [dout("kvp%d" % i, [GROUPS[i][0], 2, 512]) for i in range(3)]
    sgs = dout("sgs", [16, 4, 128, 128])
    scs = dout("scs", [16, 3, 1536])
    kvs = [dout("kvs%d" % i, [16, GROUPS[i][0], 1024]) for i in range(3)]

    proj = dscr("proj", [3 + NTILE * 128, INC])
    gdn_in = dscr("gdn_in", [32, 128, 1544])
    o_gdn = dscr("o_gdn", [2048, 512])
    kn_s = dscr("kn_s", [3, 4096, 512])
    qn_s = dscr("qn_s", [3, 2048, 512])
    att_o = dscr("att_o", [3, 2048, 520])
    mix_d = dscr("mix_d", [17 * 128, D])

    S = Sched(nc)
    AW = 48500
    sb_all = nc.alloc_sbuf_tensor("arena", [128, AW], F32).ap()
    ps_all = nc.alloc_psum_tensor("psarena", [128, 4096], F32).ap()
    A = Arena(sb_all, AW)

    def PB(i):
        return ps_all[:, i * 512:(i + 1) * 512]

    def PBb(i):
        return ps_all[:, i * 512:(i + 1) * 512].bitcast(BF16)

    def pk(i):
        return 'PS%d' % i

    def DMA(eng, out, in_, r=(), w=()):
        S.op(eng, lambda e: e.dma_start(out=out, in_=in_), reads=r, writes=w, dma=True)

    cst = A.f32([128, 5, 128])
    DMA('sp', cst, consts, w=['cst'])
    ident = cst[:, 0, :]
    U_incl = cst[:, 1, :]
    L_incl = cst[:, 2, :]
    U_strict = cst[:, 3, :]
    ones = cst[:, 4, :]
    identb = A.bf16([128, 128])
    S.op('dve', lambda e: e.tensor_copy(out=identb, in_=ident), reads=['cst'], writes=['identb'])
    maskb_t = A.f32([128, 1])
    DMA('sp', maskb_t, maskb, w=['maskb'])
    zero_t = A.f32([128, 1024])
    S.op('pool', lambda e: e.memset(zero_t, 0.0), writes=['zero'])
    base0 = A.off

    for gi in range(3):
        W = GROUPS[gi][0]
        for s in range(16):
            S.op('act', lambda e, gi=gi, s=s, W=W: e.dma_start(out=kvs[gi][s, 0:W - 1, :], in_=caches[gi][s, 1:W, :]),
                 writes=['kvs%d_%d' % (gi, s)], dma=True)
    S.op('act', lambda e: e.dma_start(out=scs[:, 0:2, :], in_=st_conv[:, 1:3, :]), writes=['scs01'], dma=True)

    xnT = A.bf16([128, 8, NTILE * 128])
    w1b = A.f32([128, D])
    DMA('sp', w1b, norm1.partition_broadcast(128), w=['w1b'])
    xt = [A.f32([128, D]) for _ in range(2)]
    sq = A.f32([128, D])
    ss = A.f32([128, 1])
    rstd = A.f32([128, 1])
    xn = A.bf16([128, D])
    for t in range(NTILE):
        xx = xt[t % 2]
        kx = 'xt%d' % (t % 2)
        DMA('sp', xx, x_all[t * 128:(t + 1) * 128, :], w=[kx])
        S.op('act', lambda e, xx=xx: e.activation(out=sq, in_=xx, func=AF.Square, accum_out=ss), reads=[kx], writes=['sq', 'ss'])
        S.op('act', lambda e: e.activation(out=ss, in_=ss, func=AF.Sqrt, scale=1.0 / D, bias=EPS), writes=['ss'])
        S.op('dve', lambda e: e.reciprocal(out=rstd, in_=ss), reads=['ss'], writes=['rstd'])
        S.op('dve', lambda e, xx=xx: e.scalar_tensor_tensor(out=xn, in0=xx, scalar=rstd, in1=w1b, op0=ALU.mult, op1=ALU.mult),
             reads=[kx, 'rstd', 'w1b'], writes=['xn'])
        pb = t % 2

        def tr(e, pb=pb):
            for k in range(8):
                ins = e.transpose(out=PBb(pb)[:, k * 128:(k + 1) * 128], in_=xn[:, k * 128:(k + 1) * 128], identity=identb)
            return ins
        S.op('pe', tr, reads=['xn', 'identb'], writes=[pk(pb)])
        S.op('act', lambda e, t=t, pb=pb: e.activation(out=xnT[:, :, t * 128:(t + 1) * 128],
                                                       in_=PBb(pb).rearrange("p (k t) -> p k t", k=8), func=AF.Copy),
             writes=[pk(pb), 'xnT%d' % t])

    DMA('pool', proj[0:3, 0:1024], zero_t[0:3, :], r=['zero'], w=['projz'])
    DMA('pool', proj[0:3, 1024:1536], zero_t[0:3, 0:512], r=['zero'], w=['projz2'])
    wst = [A.f32([128, 8, 512]) for _ in range(2)]
    wbf = [A.bf16([128, 8, 512]) for _ in range(2)]
    ot = [A.f32([128, 512]) for _ in range(4)]
    oc = 0
    for cb, (c0, ncol) in enumerate(COLBLK):
        wi = cb % 2
        DMA('sp', wst[wi][:, :, 0:ncol], w_in[:, c0:c0 + ncol].rearrange("(k p) c -> p k c", p=128), w=['wst%d' % wi])
        S.op('pool', lambda e, wi=wi, ncol=ncol: e.tensor_copy(out=wbf[wi][:, :, 0:ncol], in_=wst[wi][:, :, 0:ncol]),
             reads=['wst%d' % wi], writes=['wbf%d' % wi])
        for t in range(NTILE):
            if t < 16 and cb in HALO_SKIP:
                continue
            pb = 2 + (oc % 4)
            oi = oc % 4
            oc += 1

            def mm(e, t=t, wi=wi, ncol=ncol, pb=pb):
                for k in range(8):
                    ins = e.matmul(PB(pb)[:, 0:ncol], lhsT=xnT[:, k, t * 128:(t + 1) * 128], rhs=wbf[wi][:, k, 0:ncol],
                                   start=(k == 0), stop=(k == 7))
                return ins
            S.op('pe', mm, reads=['xnT%d' % t, 'wbf%d' % wi], writes=[pk(pb)])
            ee = 'act' if oc % 2 == 0 else 'dve'
            if ee == 'act':
                S.op('act', lambda e, oi=oi, pb=pb, ncol=ncol: e.activation(out=ot[oi][:, 0:ncol], in_=PB(pb)[:, 0:ncol], func=AF.Copy),
                     writes=[pk(pb), 'ot%d' % oi])
            else:
                S.op('dve', lambda e, oi=oi, pb=pb, ncol=ncol: e.tensor_copy(out=ot[oi][:, 0:ncol], in_=PB(pb)[:, 0:ncol]),
                     writes=[pk(pb), 'ot%d' % oi])
            DMA('pool', proj[3 + t * 128:3 + (t + 1) * 128, c0:c0 + ncol], ot[oi][:, 0:ncol], r=['ot%d' % oi], w=['proj'])
    S.barrier()
    DMA('sp', scp, proj[3 + 4096 - 3:3 + 4096, 0:1536], w=['scp'])
    DMA('sp', scs[:, 2, :], proj[3 + 4096:3 + 4096 + 16, 0:1536], w=['scs2'])

    if stop_after == 'B':
        S.emit()
        return nc
    A.off = base0
    cw = A.f32([128, 4, 1536])
    for k in range(4):
        DMA('sp', cw[:, k, :], conv_wT[k:k + 1, :].partition_broadcast(128), w=['cw'])
    negA = A.f32([128, 4])
    dtb = A.f32([128, 4])
    DMA('sp', negA, a_log.partition_broadcast(128), w=['negA'])
    DMA('sp', dtb, dt_bias.partition_broadcast(128), w=['dtb'])
    S.op('act', lambda e: e.activation(out=negA, in_=negA, func=AF.Exp), writes=['negA'])
    S.op('dve', lambda e: e.tensor_scalar(out=negA, in0=negA, scalar1=-1.0, scalar2=None, op0=ALU.mult), writes=['negA'])
    xs = [[A.f32([128, 1536]) for _ in range(4)] for _ in range(2)]
    tmp = [A.f32([128, 1536]) for _ in range(4)]
    cc = A.f32([128, 1536])
    gt = [A.f32([128, 1544]) for _ in range(2)]
    sqc = A.f32([128, 1024])
    ssc = A.f32([128, 8])
    rsc = A.f32([128, 8])
    ba = [A.f32([128, 8]) for _ in range(2)]
    gx = A.f32([128, 4])
    for t in range(32):
        bi = t % 2
        for k in range(4):
            DMA('sp', xs[bi][k], proj[t * 128 + k:t * 128 + k + 128, 0:1536], w=['xs%d_%d' % (bi, k)])
        DMA('sp', ba[bi], proj[3 + t * 128:3 + (t + 1) * 128, 2048:2056], w=['ba%d' % bi])
        for k in range(4):
            S.op('pool', lambda e, bi=bi, k=k: e.tensor_tensor(out=tmp[k], in0=xs[bi][k], in1=cw[:, k, :], op=ALU.mult),
                 reads=['xs%d_%d' % (bi, k), 'cw'], writes=['tmp%d' % k])
        S.op('dve', lambda e: e.tensor_tensor(out=cc, in0=tmp[0], in1=tmp[1], op=ALU.add), reads=['tmp0', 'tmp1'], writes=['cc'])
        S.op('dve', lambda e: e.tensor_tensor(out=cc, in0=cc, in1=tmp[2], op=ALU.add), reads=['tmp2'], writes=['cc'])
        S.op('dve', lambda e: e.tensor_tensor(out=cc, in0=cc, in1=tmp[3], op=ALU.add), reads=['tmp3'], writes=['cc'])
        S.op('act', lambda e: e.activation(out=cc, in_=cc, func=AF.Silu), writes=['cc'])
        g_t = gt[bi]
        kg = 'gt%d' % bi
        S.op('act', lambda e: e.activation(out=sqc, in_=cc[:, 0:1024], func=AF.Square), reads=['cc'], writes=['sqc'])
        S.op('dve', lambda e: e.tensor_reduce(out=ssc, in_=sqc.rearrange("p (h d) -> p h d", h=8), axis=AX.X, op=ALU.add),
             reads=['sqc'], writes=['ssc'])
        S.op('act', lambda e: e.activation(out=ssc, in_=ssc, func=AF.Sqrt, bias=EPS), writes=['ssc'])
        S.op('dve', lambda e: e.reciprocal(out=rsc, in_=ssc), reads=['ssc'], writes=['rsc'])
        S.op('dve', lambda e: e.tensor_scalar(out=rsc[:, 0:4], in0=rsc[:, 0:4], scalar1=128.0 ** -0.5, scalar2=None, op0=ALU.mult), writes=['rsc'])
        S.op('dve', lambda e, g_t=g_t: e.tensor_tensor(out=g_t[:, 0:1024].rearrange("p (h d) -> p h d", h=8),
                                                       in0=cc[:, 0:1024].rearrange("p (h d) -> p h d", h=8),
                                                       in1=rsc.unsqueeze(2).broadcast_to([128, 8, 128]), op=ALU.mult),
             reads=['cc', 'rsc'], writes=[kg])
        S.op('pool', lambda e, g_t=g_t: e.tensor_copy(out=g_t[:, 1024:1536], in_=cc[:, 1024:1536]), reads=['cc'], writes=[kg])
        S.op('act', lambda e, g_t=g_t, bi=bi: e.activation(out=g_t[:, 1536:1540], in_=ba[bi][:, 0:4], func=AF.Sigmoid),
             reads=['ba%d' % bi], writes=[kg])
        S.op('dve', lambda e, bi=bi: e.tensor_tensor(out=gx, in0=ba[bi][:, 4:8], in1=dtb, op=ALU.add), reads=['ba%d' % bi, 'dtb'], writes=['gx'])
        S.op('act', lambda e: e.activation(out=gx, in_=gx, func=AF.Exp), writes=['gx'])
        S.op('act', lambda e: e.activation(out=gx, in_=gx, func=AF.Ln, bias=1.0), writes=['gx'])
        S.op('dve', lambda e, g_t=g_t: e.tensor_tensor(out=g_t[:, 1540:1544], in0=gx, in1=negA, op=ALU.mult), reads=['gx', 'negA'], writes=[kg])
        DMA('pool', gdn_in[t], g_t, r=[kg], w=['gdn_in%d' % t])
    S.barrier()

    if stop_after == 'C':
        S.emit()
        return nc
    A.off = base0
    Sst = A.f32([128, 4, 128])
    S.op('pool', lambda e: e.memset(Sst, 0.0), writes=['Sst'])
    gti = [A.f32([128, 1544]) for _ in range(2)]
    names = ['gc', 'gb', 'mt', 'DT', 'DTs', 'egc', 'egl', 'kds', 'bws', 'kT', 'qgT', 'egb', 'ATn', 'inT', 'Am', 'AT', 'X2', 'XT2',
             'R0', 'R1', 'Bv', 'Bw', 'u', 'wT', 'vn', 'kdec', 'o', 'glast']
    W_ = {}
    for n_ in names:
        if n_ in ('gc', 'egc', 'egl', 'kds', 'bws', 'glast'):
            W_[n_] = A.f32([128, 4])
        else:
            W_[n_] = A.f32([128, 4, 128])
    identbc = ident.unsqueeze(1).broadcast_to([128, 4, 128])

    def P4(i):
        return PB(i).rearrange("p (h d) -> p h d", h=4)

    def evac(eng, out, pbi, w, extra_r=()):
        if eng == 'act':
            S.op('act', lambda e: e.activation(out=out, in_=P4(pbi), func=AF.Copy), reads=list(extra_r), writes=[pk(pbi), w])
        else:
            S.op('dve', lambda e: e.tensor_copy(out=out, in_=P4(pbi)), reads=list(extra_r), writes=[pk(pbi), w])

    for t in range(32):
        bi = t % 2
        G = gti[bi]
        kG = 'gti%d' % bi
        DMA('sp', G, gdn_in[t], w=[kG])
        qv = G[:, 0:512].rearrange("p (h d) -> p h d", h=4)
        kv_ = G[:, 512:1024].rearrange("p (h d) -> p h d", h=4)
        vv = G[:, 1024:1536].rearrange("p (h d) -> p h d", h=4)
        beta = G[:, 1536:1540]
        gg = G[:, 1540:1544]
        S.op('pe', lambda e, gg=gg: e.matmul(PB(0)[:, 0:4], lhsT=U_incl, rhs=gg, start=True, stop=True), reads=[kG, 'cst'], writes=[pk(0)])
        S.op('dve', lambda e: e.tensor_copy(out=W_['gc'], in_=PB(0)[:, 0:4]), writes=[pk(0), 'gc'])
        for h in range(4):
            S.op('dve', lambda e, h=h, gg=gg: e.tensor_scalar(out=W_['gb'][:, h, :], in0=ones, scalar1=gg[:, h:h + 1], scalar2=None, op0=ALU.mult),
                 reads=[kG, 'cst'], writes=['gb'])

        def mm_gb(e):
            for h in range(4):
                ins = e.matmul(P4(1)[:, h, :], lhsT=W_['gb'][:, h, :], rhs=U_incl, start=True, stop=True)
            return ins
        S.op('pe', mm_gb, reads=['gb', 'cst'], writes=[pk(1)])
        for h in range(4):
            S.op('dve', lambda e, h=h: e.tensor_scalar(out=W_['mt'][:, h, :], in0=P4(1)[:, h, :], scalar1=W_['gc'][:, h:h + 1], scalar2=0.0,
                                                       op0=ALU.subtract, op1=ALU.min), reads=['gc'], writes=[pk(1), 'mt'])
        S.op('act', lambda e: e.activation(out=W_['mt'], in_=W_['mt'], func=AF.Exp), writes=['mt'])
        S.op('pool', lambda e: e.tensor_tensor(out=W_['DT'], in0=W_['mt'], in1=U_incl.unsqueeze(1).broadcast_to([128, 4, 128]), op=ALU.mult),
             reads=['mt', 'cst'], writes=['DT'])
        S.op('pool', lambda e: e.tensor_tensor(out=W_['DTs'], in0=W_['mt'], in1=U_strict.unsqueeze(1).broadcast_to([128, 4, 128]), op=ALU.mult),
             reads=['mt', 'cst'], writes=['DTs'])
        S.op('act', lambda e: e.activation(out=W_['egb'], in_=P4(1), func=AF.Exp), writes=[pk(1), 'egb'])
        S.op('dve', lambda e: e.tensor_copy(out=W_['glast'], in_=P4(1)[:, :, 127]), writes=[pk(1), 'glast'])
        S.op('act', lambda e: e.activation(out=W_['egc'], in_=W_['gc'], func=AF.Exp), reads=['gc'], writes=['egc'])
        S.op('act', lambda e: e.activation(out=W_['egl'], in_=W_['glast'], func=AF.Exp), reads=['glast'], writes=['egl'])
        S.op('dve', lambda e: e.tensor_tensor(out=W_['kds'], in0=W_['glast'], in1=W_['gc'], op=ALU.subtract), reads=['glast', 'gc'], writes=['kds'])
        S.op('act', lambda e: e.activation(out=W_['kds'], in_=W_['kds'], func=AF.Exp), writes=['kds'])
        S.op('dve', lambda e, beta=beta: e.tensor_tensor(out=W_['bws'], in0=beta, in1=W_['egc'], op=ALU.mult), reads=[kG, 'egc'], writes=['bws'])
        def tr_k(e, kv_=kv_):
            for h in range(4):
                ins = e.transpose(out=P4(2)[:, h, :], in_=kv_[:, h, :], identity=ident)
            return ins
        S.op('pe', tr_k, reads=[kG, 'cst'], writes=[pk(2)])
        evac('act', W_['kT'], 2, 'kT')

        def tr_q(e, qv=qv):
            for h in range(4):
                ins = e.transpose(out=P4(3)[:, h, :], in_=qv[:, h, :], identity=ident)
            return ins
        S.op('pe', tr_q, reads=[kG, 'cst'], writes=[pk(3)])
        qTd = W_['o']
        evac('dve', qTd, 3, 'o')
        S.op('dve', lambda e, qTd=qTd: e.tensor_tensor(out=W_['qgT'], in0=qTd, in1=W_['egb'], op=ALU.mult), reads=['o', 'egb'], writes=['qgT'])
        def mm_g(e):
            for h in range(4):
                ins = e.matmul(P4(4)[:, h, :], lhsT=W_['kT'][:, h, :], rhs=W_['kT'][:, h, :], start=True, stop=True)
            return ins
        S.op('pe', mm_g, reads=['kT'], writes=[pk(4)])

        def mm_qk(e, qTd=qTd):
            for h in range(4):
                ins = e.matmul(P4(5)[:, h, :], lhsT=W_['kT'][:, h, :], rhs=qTd[:, h, :], start=True, stop=True)
            return ins
        S.op('pe', mm_qk, reads=['kT', 'o'], writes=[pk(5)])
        S.op('dve', lambda e: e.tensor_tensor(out=W_['ATn'], in0=P4(4), in1=W_['DTs'], op=ALU.mult), reads=['DTs'], writes=[pk(4), 'ATn'])
        S.op('dve', lambda e: e.tensor_tensor(out=W_['inT'], in0=P4(5), in1=W_['DT'], op=ALU.mult), reads=['DT'], writes=[pk(5), 'inT'])

        def tr_a(e):
            for h in range(4):
                ins = e.transpose(out=P4(6)[:, h, :], in_=W_['ATn'][:, h, :], identity=ident)
            return ins
        S.op('pe', tr_a, reads=['ATn', 'cst'], writes=[pk(6)])
        S.op('dve', lambda e, beta=beta: e.tensor_tensor(out=W_['Am'], in0=P4(6), in1=beta.unsqueeze(2).broadcast_to([128, 4, 128]), op=ALU.mult),
             reads=[kG], writes=[pk(6), 'Am'])

        def tr_at(e):
            for h in range(4):
                ins = e.transpose(out=P4(7)[:, h, :], in_=W_['Am'][:, h, :], identity=ident)
            return ins
        S.op('pe', tr_at, reads=['Am', 'cst'], writes=[pk(7)])
        evac('act', W_['AT'], 7, 'AT')
        S.op('dve', lambda e: e.tensor_tensor(out=W_['R0'], in0=identbc, in1=W_['AT'], op=ALU.subtract), reads=['AT', 'cst'], writes=['R0'])
        X, XT, kX, kXT = W_['Am'], W_['AT'], 'Am', 'AT'
        Xn, XTn, kXn, kXTn = W_['X2'], W_['XT2'], 'X2', 'XT2'
        Rc, Rn, kRc, kRn = W_['R0'], W_['R1'], 'R0', 'R1'
        for lvl in range(6):
            def mm_x2(e, X=X, XT=XT):
                for h in range(4):
                    ins = e.matmul(P4(2)[:, h, :], lhsT=XT[:, h, :], rhs=X[:, h, :], start=True, stop=True)
                return ins
            S.op('pe', mm_x2, reads=[kX, kXT], writes=[pk(2)])
            if lvl < 5:
                def mm_xt2(e, X=X, XT=XT):
                    for h in range(4):
                        ins = e.matmul(P4(3)[:, h, :], lhsT=X[:, h, :], rhs=XT[:, h, :], start=True, stop=True)
                    return ins
                S.op('pe', mm_xt2, reads=[kX, kXT], writes=[pk(3)])
            evac('act', Xn, 2, kXn)
            if lvl < 5:
                evac('dve', XTn, 3, kXTn)

            def mm_r(e, Xn=Xn, Rc=Rc):
                for h in range(4):
                    ins = e.matmul(P4(4)[:, h, :], lhsT=Xn[:, h, :], rhs=Rc[:, h, :], start=True, stop=True)
                return ins
            S.op('pe', mm_r, reads=[kXn, kRc], writes=[pk(4)])
            S.op('dve', lambda e, Rn=Rn, Rc=Rc: e.tensor_tensor(out=Rn, in0=P4(4), in1=Rc, op=ALU.add), reads=[kRc], writes=[pk(4), kRn])
            X, XT, kX, kXT, Xn, XTn, kXn, kXTn = Xn, XTn, kXn, kXTn, X, XT, kX, kXT
            Rc, Rn, kRc, kRn = Rn, Rc, kRn, kRc
        R, kR = Rc, kRc
        S.op('dve', lambda e, vv=vv, beta=beta: e.tensor_tensor(out=W_['Bv'], in0=vv, in1=beta.unsqueeze(2).broadcast_to([128, 4, 128]), op=ALU.mult),
             reads=[kG], writes=['Bv'])
        S.op('pool', lambda e, kv_=kv_: e.tensor_tensor(out=W_['Bw'], in0=kv_, in1=W_['bws'].unsqueeze(2).broadcast_to([128, 4, 128]), op=ALU.mult),
             reads=[kG, 'bws'], writes=['Bw'])
        S.op('pool', lambda e, kv_=kv_: e.tensor_tensor(out=W_['kdec'], in0=kv_, in1=W_['kds'].unsqueeze(2).broadcast_to([128, 4, 128]), op=ALU.mult),
             reads=[kG, 'kds'], writes=['kdec'])

        def mm_u(e, R=R):
            for h in range(4):
                ins = e.matmul(P4(5)[:, h, :], lhsT=R[:, h, :], rhs=W_['Bv'][:, h, :], start=True, stop=True)
            return ins
        S.op('pe', mm_u, reads=[kR, 'Bv'], writes=[pk(5)])
        evac('act', W_['u'], 5, 'u')

        def mm_w(e, R=R):
            for h in range(4):
                ins = e.matmul(P4(6)[:, h, :], lhsT=W_['Bw'][:, h, :], rhs=R[:, h, :], start=True, stop=True)
            return ins
        S.op('pe', mm_w, reads=[kR, 'Bw'], writes=[pk(6)])
        evac('dve', W_['wT'], 6, 'wT')
        def mm_ws(e):
            for h in range(4):
                ins = e.matmul(P4(7)[:, h, :], lhsT=W_['wT'][:, h, :], rhs=Sst[:, h, :], start=True, stop=True)
            return ins
        S.op('pe', mm_ws, reads=['wT', 'Sst'], writes=[pk(7)])
        S.op('dve', lambda e: e.tensor_tensor(out=W_['vn'], in0=W_['u'], in1=P4(7), op=ALU.subtract), reads=['u'], writes=[pk(7), 'vn'])
        if t >= 16:
            def mm_o(e):
                for h in range(4):
                    e.matmul(P4(0)[:, h, :], lhsT=W_['qgT'][:, h, :], rhs=Sst[:, h, :], start=True, stop=False)
                    ins = e.matmul(P4(0)[:, h, :], lhsT=W_['inT'][:, h, :], rhs=W_['vn'][:, h, :], start=False, stop=True)
                return ins
            S.op('pe', mm_o, reads=['qgT', 'Sst', 'inT', 'vn'], writes=[pk(0)])
            evac('act', W_['o'], 0, 'o')
            DMA('pool', o_gdn[(t - 16) * 128:(t - 15) * 128, :], W_['o'].rearrange("p h d -> p (h d)"), r=['o'], w=['o_gdn'])

        def mm_su(e):
            for h in range(4):
                ins = e.matmul(P4(1)[:, h, :], lhsT=W_['kdec'][:, h, :], rhs=W_['vn'][:, h, :], start=True, stop=True)
            return ins
        S.op('pe', mm_su, reads=['kdec', 'vn'], writes=[pk(1)])
        for h in range(4):
            S.op('dve', lambda e, h=h: e.scalar_tensor_tensor(out=Sst[:, h, :], in0=Sst[:, h, :], scalar=W_['egl'][:, h:h + 1], in1=P4(1)[:, h, :],
                                                              op0=ALU.mult, op1=ALU.add), reads=['egl'], writes=[pk(1), 'Sst'])
    DMA('pool', sgp.rearrange("h k v -> k h v"), Sst, r=['Sst'], w=['sgp'])
    S.barrier()

    if stop_after == 'D':
        S.emit()
        return nc
    A.off = base0
    qkw = A.f32([128, 6, 64])
    for gi in range(3):
        DMA('sp', qkw[:, gi, :], qnw[gi:gi + 1, :].partition_broadcast(128), w=['qkw'])
        DMA('sp', qkw[:, 3 + gi, :], knw[gi:gi + 1, :].partition_broadcast(128), w=['qkw'])
    rin = [A.f32([128, 512]) for _ in range(3)]
    rsq = A.f32([128, 512])
    rss = A.f32([128, 8])
    rrs = A.f32([128, 8])
    rout = [A.f32([128, 512]) for _ in range(3)]
    rp = [A.f32([128, 16]) for _ in range(2)]
    rt = [A.f32([128, 8, 8]) for _ in range(4)]
    cnt = 0
    for gi in range(3):
        W, dil = GROUPS[gi]
        cq = 2056 + gi * 1536
        first_k_tile = 16 - W // 128
        for t in range(first_k_tile, 32):
            DMA('sp', rp[t % 2], rope_t[t * 128:(t + 1) * 128, :], w=['rp%d' % (t % 2)])
            cosb = rp[t % 2][:, 0:8].unsqueeze(1).broadcast_to([128, 8, 8])
            sinb = rp[t % 2][:, 8:16].unsqueeze(1).broadcast_to([128, 8, 8])
            for which in ((1, 0) if t >= 16 else (1,)):
                bi = cnt % 3
                cnt += 1
                src = rin[bi]
                dst = rout[bi]
                ks, kd = 'rin%d' % bi, 'rout%d' % bi
                c0 = cq + which * 512
                DMA('sp', src, proj[3 + t * 128:3 + (t + 1) * 128, c0:c0 + 512], w=[ks])
                S.op('act', lambda e, src=src: e.activation(out=rsq, in_=src, func=AF.Square), reads=[ks], writes=['rsq'])
                S.op('dve', lambda e: e.tensor_reduce(out=rss, in_=rsq.rearrange("p (h d) -> p h d", h=8), axis=AX.X, op=ALU.add),
                     reads=['rsq'], writes=['rss'])
                S.op('act', lambda e: e.activation(out=rss, in_=rss, func=AF.Sqrt, scale=1.0 / 64, bias=EPS), writes=['rss'])
                S.op('dve', lambda e: e.reciprocal(out=rrs, in_=rss), reads=['rss'], writes=['rrs'])
                d3 = dst.rearrange("p (h d) -> p h d", h=8)
                S.op('dve', lambda e, src=src, d3=d3: e.tensor_tensor(out=d3, in0=src.rearrange("p (h d) -> p h d", h=8),
                                                                      in1=rrs.unsqueeze(2).broadcast_to([128, 8, 64]), op=ALU.mult),
                     reads=[ks, 'rrs'], writes=[kd])
                wrow = qkw[:, (3 if which == 1 else 0) + gi, :].unsqueeze(1).broadcast_to([128, 8, 64])
                S.op('pool', lambda e, d3=d3, wrow=wrow: e.tensor_tensor(out=d3, in0=d3, in1=wrow, op=ALU.mult), reads=['qkw'], writes=[kd])
                x1 = d3[:, :, 0:8]
                x2 = d3[:, :, 8:16]
                S.op('dve', lambda e, x1=x1, cosb=cosb: e.tensor_tensor(out=rt[0], in0=x1, in1=cosb, op=ALU.mult), reads=[kd, 'rp%d' % (t % 2)], writes=['rt0'])
                S.op('dve', lambda e, x2=x2, sinb=sinb: e.tensor_tensor(out=rt[1], in0=x2, in1=sinb, op=ALU.mult), reads=[kd, 'rp%d' % (t % 2)], writes=['rt1'])
                S.op('dve', lambda e, x2=x2, cosb=cosb: e.tensor_tensor(out=rt[2], in0=x2, in1=cosb, op=ALU.mult), reads=[kd, 'rp%d' % (t % 2)], writes=['rt2'])
                S.op('dve', lambda e, x1=x1, sinb=sinb: e.tensor_tensor(out=rt[3], in0=x1, in1=sinb, op=ALU.mult), reads=[kd, 'rp%d' % (t % 2)], writes=['rt3'])
                S.op('dve', lambda e, x1=x1: e.tensor_tensor(out=x1, in0=rt[0], in1=rt[1], op=ALU.subtract), reads=['rt0', 'rt1'], writes=[kd])
                S.op('dve', lambda e, x2=x2: e.tensor_tensor(out=x2, in0=rt[2], in1=rt[3], op=ALU.add), reads=['rt2', 'rt3'], writes=[kd])
                if which == 1:
                    DMA('pool', kn_s[gi, t * 128:(t + 1) * 128, :], dst, r=[kd], w=['kn_s'])
                    if t * 128 >= 4096 - W:
                        r0 = t * 128 - (4096 - W)
                        DMA('pool', kvp[gi][r0:r0 + 128, 0, :], dst, r=[kd], w=['kvp'])
                        DMA('pool', kvp[gi][r0:r0 + 128, 1, :], proj[3 + t * 128:3 + (t + 1) * 128, cq + 1024:cq + 1536], w=['kvp'])
                else:
                    DMA('pool', qn_s[gi, (t - 16) * 128:(t - 15) * 128, :], dst, r=[kd], w=['qn_s'])
    S.barrier()

    if stop_after == 'E':
        S.emit()
        return nc
    A.off = base0
    fin = [A.f32([128, 512]) for _ in range(4)]
    fb = [A.bf16([128, 512]) for _ in range(2)]
    qT = A.bf16([128, 4, 128])
    kTs = [A.bf16([128, 4, 128]) for _ in range(2)]
    Vs = [A.bf16([128, 8, 66]) for _ in range(2)]
    for i in range(2):
        S.op('pool', lambda e, i=i: e.memset(Vs[i], 1.0), writes=['Vs%d' % i])
    Pm = [A.bf16([128, 8, 128]) for _ in range(2)]
    Ex = A.bf16([128, 4, 128])
    Osb = A.f32([128, 8, 65])
    fc = 0
    for gi in range(3):
        W, dil = GROUPS[gi]
        nblk = 4096 // dil // 128
        first_q = 2048 // dil // 128
        cv = 2056 + gi * 1536 + 1024
        for r in range(dil):
            for mb in range(first_q - 1, nblk):
                slot = mb % 2
                tok0 = r + dil * mb * 128
                rows = slice(tok0, tok0 + dil * 127 + 1, dil)
                prow = slice(3 + tok0, 3 + tok0 + dil * 127 + 1, dil)
                f1 = fin[fc % 4]; k1 = 'fin%d' % (fc % 4); fc += 1
                DMA('sp', f1, kn_s[gi, rows, :], w=[k1])
                S.op('act', lambda e, f1=f1: e.activation(out=fb[0], in_=f1, func=AF.Copy), reads=[k1], writes=['fb0'])

                def tr1(e):
                    for hp in range(4):
                        ins = e.transpose(out=PBb(0)[:, hp * 128:(hp + 1) * 128], in_=fb[0][:, hp * 128:(hp + 1) * 128], identity=identb)
                    return ins
                S.op('pe', tr1, reads=['fb0', 'identb'], writes=[pk(0)])
                S.op('dve', lambda e, slot=slot: e.tensor_copy(out=kTs[slot], in_=PBb(0)[:, 0:512].rearrange("p (a b) -> p a b", a=4)),
                     writes=[pk(0), 'kT%d' % slot])
                f2 = fin[fc % 4]; k2 = 'fin%d' % (fc % 4); fc += 1
                DMA('sp', f2, proj[prow, cv:cv + 512], w=[k2])
                S.op('pool', lambda e, f2=f2, slot=slot: e.tensor_copy(out=Vs[slot][:, :, 0:64], in_=f2.rearrange("p (h d) -> p h d", h=8)),
                     reads=[k2], writes=['Vs%d' % slot])
                if mb < first_q:
                    continue
                f3 = fin[fc % 4]; k3 = 'fin%d' % (fc % 4); fc += 1
                qrow = slice(tok0 - 2048, tok0 - 2048 + dil * 127 + 1, dil)
                DMA('sp', f3, qn_s[gi, qrow, :], w=[k3])
                S.op('act', lambda e, f3=f3: e.activation(out=fb[1], in_=f3, func=AF.Copy), reads=[k3], writes=['fb1'])

                def tr2(e):
                    for hp in range(4):
                        ins = e.transpose(out=PBb(1)[:, hp * 128:(hp + 1) * 128], in_=fb[1][:, hp * 128:(hp + 1) * 128], identity=identb)
                    return ins
                S.op('pe', tr2, reads=['fb1', 'identb'], writes=[pk(1)])
                S.op('dve', lambda e: e.tensor_copy(out=qT, in_=PBb(1)[:, 0:512].rearrange("p (a b) -> p a b", a=4)), writes=[pk(1), 'qT'])
                halo_prev = (mb == first_q)
                for which, sl in ((0, 1 - slot), (1, slot)):
                    for par in range(2):
                        pbi = 2 + par

                        def mm_s(e, sl=sl, par=par, pbi=pbi):
                            for hp in range(4):
                                lo = par * 64
                                ins = e.matmul(PB(pbi)[:, hp * 128:(hp + 1) * 128], lhsT=kTs[sl][lo:lo + 64, hp, :], rhs=qT[lo:lo + 64, hp, :],
                                               start=True, stop=True)
                            return ins
                        S.op('pe', mm_s, reads=['kT%d' % sl, 'qT'], writes=[pk(pbi)])
                        if which == 0 and halo_prev:
                            S.op('act', lambda e, pbi=pbi: e.activation(out=Ex, in_=P4(pbi), func=AF.Exp, scale=0.125, bias=maskb_t),
                                 reads=['maskb'], writes=[pk(pbi), 'Ex'])
                        else:
                            S.op('act', lambda e, pbi=pbi: e.activation(out=Ex, in_=P4(pbi), func=AF.Exp, scale=0.125), writes=[pk(pbi), 'Ex'])
                        msk = (L_incl if which == 0 else U_incl).unsqueeze(1).broadcast_to([128, 4, 128])
                        S.op('dve', lambda e, which=which, par=par, msk=msk: e.tensor_tensor(out=Pm[which][:, par:8:2, :], in0=Ex, in1=msk, op=ALU.mult),
                             reads=['Ex', 'cst'], writes=['Pm%d_%d' % (which, par)])
                for half in range(2):
                    pbi = 4 + half

                    def mm_pv(e, half=half, pbi=pbi, slot=slot):
                        for hh in range(4):
                            h = half * 4 + hh
                            e.matmul(PB(pbi)[:, hh * 65:(hh + 1) * 65], lhsT=Pm[0][:, h, :], rhs=Vs[1 - slot][:, h, 0:65], start=True, stop=False)
                            ins = e.matmul(PB(pbi)[:, hh * 65:(hh + 1) * 65], lhsT=Pm[1][:, h, :], rhs=Vs[slot][:, h, 0:65], start=False, stop=True)
                        return ins
                    S.op('pe', mm_pv, reads=['Pm0_0', 'Pm0_1', 'Pm1_0', 'Pm1_1', 'Vs0', 'Vs1'], writes=[pk(pbi)])
                    S.op('act', lambda e, half=half, pbi=pbi: e.activation(out=Osb[:, half * 4:(half + 1) * 4, :],
                                                                           in_=PB(pbi)[:, 0:260].rearrange("p (h d) -> p h d", h=4), func=AF.Copy),
                         writes=[pk(pbi), 'Osb%d' % half])
                DMA('pool', att_o[gi, qrow, :], Osb.rearrange("p h d -> p (h d)"), r=['Osb0', 'Osb1'], w=['att_o'])
    S.barrier()

    if stop_after == 'F':
        S.emit()
        return nc
    A.off = base0
    gnb = A.f32([128, 128])
    DMA('sp', gnb, gnw.partition_broadcast(128), w=['gnb'])
    g_o = [A.f32([128, 512]) for _ in range(2)]
    g_z = [A.f32([128, 512]) for _ in range(2)]
    g_a = [[A.f32([128, 520]) for _ in range(3)] for _ in range(2)]
    mixt = [A.f32([128, D]) for _ in range(2)]
    gsq = A.f32([128, 512])
    gss = A.f32([128, 4])
    grs = A.f32([128, 4])
    gden = A.f32([128, 8])
    for t in range(16):
        bi = t % 2
        mx = mixt[bi]
        km = 'mixt%d' % bi
        DMA('sp', g_o[bi], o_gdn[t * 128:(t + 1) * 128, :], w=['g_o%d' % bi])
        DMA('sp', g_z[bi], proj[3 + (16 + t) * 128:3 + (17 + t) * 128, 1536:2048], w=['g_z%d' % bi])
        for gi in range(3):
            DMA('sp', g_a[bi][gi], att_o[gi, t * 128:(t + 1) * 128, :], w=['g_a%d_%d' % (bi, gi)])
        S.op('act', lambda e, bi=bi: e.activation(out=gsq, in_=g_o[bi], func=AF.Square), reads=['g_o%d' % bi], writes=['gsq'])
        S.op('dve', lambda e: e.tensor_reduce(out=gss, in_=gsq.rearrange("p (h d) -> p h d", h=4), axis=AX.X, op=ALU.add), reads=['gsq'], writes=['gss'])
        S.op('act', lambda e: e.activation(out=gss, in_=gss, func=AF.Sqrt, scale=1.0 / 128, bias=EPS), writes=['gss'])
        S.op('dve', lambda e: e.reciprocal(out=grs, in_=gss), reads=['gss'], writes=['grs'])
        m3 = mx[:, 0:512].rearrange("p (h d) -> p h d", h=4)
        S.op('dve', lambda e, bi=bi, m3=m3: e.tensor_tensor(out=m3, in0=g_o[bi].rearrange("p (h d) -> p h d", h=4),
                                                            in1=grs.unsqueeze(2).broadcast_to([128, 4, 128]), op=ALU.mult),
             reads=['g_o%d' % bi, 'grs'], writes=[km])
        S.op('pool', lambda e, m3=m3: e.tensor_tensor(out=m3, in0=m3, in1=gnb.unsqueeze(1).broadcast_to([128, 4, 128]), op=ALU.mult), reads=['gnb'], writes=[km])
        S.op('act', lambda e, bi=bi: e.activation(out=g_z[bi], in_=g_z[bi], func=AF.Silu), writes=['g_z%d' % bi])
        S.op('dve', lambda e, bi=bi, mx=mx: e.tensor_tensor(out=mx[:, 0:512], in0=mx[:, 0:512], in1=g_z[bi], op=ALU.mult), reads=['g_z%d' % bi], writes=[km])
        S.op('pool', lambda e, bi=bi: e.tensor_tensor(out=g_a[bi][0], in0=g_a[bi][0], in1=g_a[bi][1], op=ALU.add), reads=['g_a%d_1' % bi], writes=['g_a%d_0' % bi])
        S.op('pool', lambda e, bi=bi: e.tensor_tensor(out=g_a[bi][0], in0=g_a[bi][0], in1=g_a[bi][2], op=ALU.add), reads=['g_a%d_2' % bi], writes=['g_a%d_0' % bi])
        a3 = g_a[bi][0].rearrange("p (h d) -> p h d", h=8)
        S.op('dve', lambda e, a3=a3: e.reciprocal(out=gden, in_=a3[:, :, 64]), reads=['g_a%d_0' % bi], writes=['gden'])
        S.op('dve', lambda e, a3=a3, mx=mx: e.tensor_tensor(out=mx[:, 512:1024].rearrange("p (h d) -> p h d", h=8), in0=a3[:, :, 0:64],
                                                            in1=gden.unsqueeze(2).broadcast_to([128, 8, 64]), op=ALU.mult),
             reads=['g_a%d_0' % bi, 'gden'], writes=[km])
        DMA('pool', mix_d[t * 128:(t + 1) * 128, :], mx, r=[km], w=['mix_d'])
    S.barrier()

    if stop_after == 'G1':
        S.emit()
        return nc
    sample_path(nc, S, A, base0, locals())

    A.off = base0
    yacc = A.f32([128, 17, D])
    h2T = A.bf16([128, 8, 17 * 128])
    gates = A.f32([128, 17, 32])
    base1 = A.off
    wob = A.bf16([128, 8, D])
    wos = A.f32([128, 8, 512])
    for hf in range(2):
        DMA('sp', wos, w_out[:, hf * 512:(hf + 1) * 512].rearrange("(k p) c -> p k c", p=128), w=['wos'])
        S.op('pool', lambda e, hf=hf: e.tensor_copy(out=wob[:, :, hf * 512:(hf + 1) * 512], in_=wos), reads=['wos'], writes=['wob'])
    wrt = A.f32([128, 8, 36])
    DMA('sp', wrt, w_r.rearrange("(k p) c -> p k c", p=128), w=['wrt'])
    w2b = A.f32([128, D])
    DMA('sp', w2b, norm2.partition_broadcast(128), w=['w2b'])
    mxl = [A.f32([128, D]) for _ in range(2)]
    xl = [A.f32([128, D]) for _ in range(2)]
    mxb = A.bf16([128, D])
    mxT = A.bf16([128, 8, 128])
    hsq = A.f32([128, D])
    hss = A.f32([128, 1])
    hrs = A.f32([128, 1])
    h2n = A.f32([128, D])
    h2Tf = A.f32([128, 8, 128])
    lg = A.f32([128, 36])
    r_ = {n_: A.f32([128, 8]) for n_ in ['gm', 'oh', 'eg', 'sg', 'pg', 'ein', 'm1', 'oh1', 'e2', 'm2', 'oh2', 'w1', 'w2', 'g8', 'tmp']}
    sel = A.f32([128, 4, 8])
    for t in range(17):
        bi = t % 2
        xrow = (16 + t) * 128 if t < 16 else 32 * 128
        DMA('sp', mxl[bi], mix_d[t * 128:(t + 1) * 128, :], w=['mxl%d' % bi])
        DMA('sp', xl[bi], x_all[xrow:xrow + 128, :], w=['xl%d' % bi])
        S.op('act', lambda e, bi=bi: e.activation(out=mxb, in_=mxl[bi], func=AF.Copy), reads=['mxl%d' % bi], writes=['mxb'])

        def trm(e):
            for k in range(8):
                ins = e.transpose(out=PBb(0)[:, k * 128:(k + 1) * 128], in_=mxb[:, k * 128:(k + 1) * 128], identity=identb)
            return ins
        S.op('pe', trm, reads=['mxb', 'identb'], writes=[pk(0)])
        S.op('dve', lambda e: e.tensor_copy(out=mxT, in_=PBb(0).rearrange("p (k t) -> p k t", k=8)), writes=[pk(0), 'mxT'])
        for hf in range(2):
            def mmo(e, hf=hf):
                for k in range(8):
                    ins = e.matmul(PB(1 + hf), lhsT=mxT[:, k, :], rhs=wob[:, k, hf * 512:(hf + 1) * 512], start=(k == 0), stop=(k == 7))
                return ins
            S.op('pe', mmo, reads=['mxT', 'wob'], writes=[pk(1 + hf)])
            S.op('dve', lambda e, hf=hf, t=t, bi=bi: e.tensor_tensor(out=yacc[:, t, hf * 512:(hf + 1) * 512], in0=PB(1 + hf), in1=xl[bi][:, hf * 512:(hf + 1) * 512], op=ALU.add),
                 reads=['xl%d' % bi], writes=[pk(1 + hf), 'yacc%d' % t])
        S.op('act', lambda e, t=t: e.activation(out=hsq, in_=yacc[:, t, :], func=AF.Square, accum_out=hss), reads=['yacc%d' % t], writes=['hsq', 'hss'])
        S.op('act', lambda e: e.activation(out=hss, in_=hss, func=AF.Sqrt, scale=1.0 / D, bias=EPS), writes=['hss'])
        S.op('dve', lambda e: e.reciprocal(out=hrs, in_=hss), reads=['hss'], writes=['hrs'])
        S.op('dve', lambda e, t=t: e.scalar_tensor_tensor(out=h2n, in0=yacc[:, t, :], scalar=hrs, in1=w2b, op0=ALU.mult, op1=ALU.mult),
             reads=['yacc%d' % t, 'hrs', 'w2b'], writes=['h2n'])
        for hf in range(2):
            def trh(e, hf=hf):
                for k in range(4):
                    kk = hf * 4 + k
                    ins = e.transpose(out=PB(3 + hf)[:, k * 128:(k + 1) * 128], in_=h2n[:, kk * 128:(kk + 1) * 128], identity=ident)
                return ins
            S.op('pe', trh, reads=['h2n', 'cst'], writes=[pk(3 + hf)])
            S.op('act', lambda e, hf=hf, t=t: e.activation(out=h2T[:, hf * 4:(hf + 1) * 4, t * 128:(t + 1) * 128], in_=P4(3 + hf), func=AF.Copy),
                 writes=[pk(3 + hf), 'h2T%d' % t])
            S.op('dve', lambda e, hf=hf: e.tensor_copy(out=h2Tf[:, hf * 4:(hf + 1) * 4, :], in_=P4(3 + hf)), writes=[pk(3 + hf), 'h2Tf'])

        def mmr(e):
            for k in range(8):
                ins = e.matmul(PB(5)[:, 0:36], lhsT=h2Tf[:, k, :], rhs=wrt[:, k, :], start=(k == 0), stop=(k == 7))
            return ins
        S.op('pe', mmr, reads=['h2Tf', 'wrt'], writes=[pk(5)])
        S.op('dve', lambda e: e.tensor_copy(out=lg, in_=PB(5)[:, 0:36]), writes=[pk(5), 'lg'])
        R_ = r_
        lgg = lg[:, 0:4]
        le = lg[:, 4:36].rearrange("p (g e) -> p g e", g=4)

        def V(e_, fn, r, w):
            S.op(e_, fn, reads=r, writes=w)
        V('dve', lambda e: e.tensor_reduce(out=R_['gm'][:, 0:1], in_=lgg, axis=AX.X, op=ALU.max), ['lg'], ['gm'])
        V('dve', lambda e: e.tensor_scalar(out=R_['oh'][:, 0:4], in0=lgg, scalar1=R_['gm'][:, 0:1], scalar2=None, op0=ALU.is_equal), ['lg', 'gm'], ['oh'])
        V('dve', lambda e: e.tensor_scalar(out=R_['eg'][:, 0:4], in0=lgg, scalar1=R_['gm'][:, 0:1], scalar2=None, op0=ALU.subtract), ['lg', 'gm'], ['eg'])
        V('act', lambda e: e.activation(out=R_['eg'][:, 0:4], in_=R_['eg'][:, 0:4], func=AF.Exp), [], ['eg'])
        V('dve', lambda e: e.tensor_reduce(out=R_['sg'][:, 0:1], in_=R_['eg'][:, 0:4], axis=AX.X, op=ALU.add), ['eg'], ['sg'])
        V('dve', lambda e: e.reciprocal(out=R_['pg'][:, 0:1], in_=R_['sg'][:, 0:1]), ['sg'], ['pg'])
        V('dve', lambda e: e.tensor_tensor(out=sel, in0=le, in1=R_['oh'][:, 0:4].unsqueeze(2).broadcast_to([128, 4, 8]), op=ALU.mult), ['lg', 'oh'], ['sel'])
        V('dve', lambda e: e.tensor_reduce(out=R_['ein'], in_=sel.rearrange("p g e -> p e g"), axis=AX.X, op=ALU.add), ['sel'], ['ein'])
        V('dve', lambda e: e.tensor_reduce(out=R_['m1'][:, 0:1], in_=R_['ein'], axis=AX.X, op=ALU.max), ['ein'], ['m1'])
        V('dve', lambda e: e.tensor_scalar(out=R_['oh1'], in0=R_['ein'], scalar1=R_['m1'][:, 0:1], scalar2=None, op0=ALU.is_equal), ['ein', 'm1'], ['oh1'])
        V('dve', lambda e: e.scalar_tensor_tensor(out=R_['e2'], in0=R_['oh1'], scalar=-1e30, in1=R_['ein'], op0=ALU.mult, op1=ALU.add), ['oh1', 'ein'], ['e2'])
        V('dve', lambda e: e.tensor_reduce(out=R_['m2'][:, 0:1], in_=R_['e2'], axis=AX.X, op=ALU.max), ['e2'], ['m2'])
        V('dve', lambda e: e.tensor_scalar(out=R_['oh2'], in0=R_['e2'], scalar1=R_['m2'][:, 0:1], scalar2=None, op0=ALU.is_equal), ['e2', 'm2'], ['oh2'])
        V('dve', lambda e: e.tensor_tensor(out=R_['w1'][:, 0:1], in0=R_['m2'][:, 0:1], in1=R_['m1'][:, 0:1], op=ALU.subtract), ['m1', 'm2'], ['w1'])
        V('act', lambda e: e.activation(out=R_['w1'][:, 0:1], in_=R_['w1'][:, 0:1], func=AF.Exp), [], ['w1'])
        V('dve', lambda e: e.tensor_scalar(out=R_['w1'][:, 0:1], in0=R_['w1'][:, 0:1], scalar1=1.0, scalar2=None, op0=ALU.add), [], ['w1'])
        V('dve', lambda e: e.reciprocal(out=R_['w1'][:, 0:1], in_=R_['w1'][:, 0:1]), [], ['w1'])
        V('dve', lambda e: e.tensor_tensor(out=R_['w1'][:, 0:1], in0=R_['w1'][:, 0:1], in1=R_['pg'][:, 0:1], op=ALU.mult), ['pg'], ['w1'])
        V('dve', lambda e: e.tensor_tensor(out=R_['w2'][:, 0:1], in0=R_['pg'][:, 0:1], in1=R_['w1'][:, 0:1], op=ALU.subtract), ['pg', 'w1'], ['w2'])
        V('dve', lambda e: e.tensor_scalar(out=R_['g8'], in0=R_['oh1'], scalar1=R_['w1'][:, 0:1], scalar2=None, op0=ALU.mult), ['oh1', 'w1'], ['g8'])
        V('dve', lambda e: e.scalar_tensor_tensor(out=R_['g8'], in0=R_['oh2'], scalar=R_['w2'][:, 0:1], in1=R_['g8'], op0=ALU.mult, op1=ALU.add), ['oh2', 'w2'], ['g8'])
        V('dve', lambda e, t=t: e.tensor_tensor(out=gates[:, t, :].rearrange("p (g e) -> p g e", g=4),
                                                in0=R_['oh'][:, 0:4].unsqueeze(2).broadcast_to([128, 4, 8]),
                                                in1=R_['g8'].unsqueeze(1).broadcast_to([128, 4, 8]), op=ALU.mult), ['oh', 'g8'], ['gates%d' % t])
    S.barrier()

    if stop_after == 'G2':
        S.emit()
        return nc
    A.off = base1
    gus = [A.f32([128, 8, 512]) for _ in range(2)]
    gub = [A.bf16([128, 8, 512]) for _ in range(2)]
    wds = [A.f32([128, 2, D]) for _ in range(2)]
    wdb = [A.bf16([128, 2, D]) for _ in range(2)]
    sgt = A.f32([128, 2, 512])
    hid = A.bf16([128, 2, 512])
    blocks = [(0, 512), (512, 512), (1024, 512), (1536, 512), (2048, 128)]
    for ex in range(32):
        wi = ex % 2
        DMA('sp', gus[wi], w_gu[ex].rearrange("(k p) c -> p k c", p=128), w=['gus%d' % wi])
        DMA('sp', wds[wi], w_d[ex].rearrange("(k p) c -> p k c", p=128), w=['wds%d' % wi])
        S.op('pool', lambda e, wi=wi: e.tensor_copy(out=gub[wi], in_=gus[wi]), reads=['gus%d' % wi], writes=['gub%d' % wi])
        S.op('pool', lambda e, wi=wi: e.tensor_copy(out=wdb[wi], in_=wds[wi]), reads=['wds%d' % wi], writes=['wdb%d' % wi])
        for (t0, nt) in blocks:
            for c in range(4):
                def mmg(e, c=c, wi=wi, t0=t0, nt=nt):
                    for k in range(8):
                        ins = e.matmul(PB(c)[:, 0:nt], lhsT=gub[wi][:, k, c * 128:(c + 1) * 128], rhs=h2T[:, k, t0:t0 + nt], start=(k == 0), stop=(k == 7))
                    return ins
                S.op('pe', mmg, reads=['gub%d' % wi] + ['h2T%d' % tt for tt in range(t0 // 128, (t0 + nt) // 128)], writes=[pk(c)])
            for c in range(2):
                S.op('act', lambda e, c=c, nt=nt: e.activation(out=sgt[:, c, 0:nt], in_=PB(c)[:, 0:nt], func=AF.Silu), writes=[pk(c), 'sgt%d' % c])
                S.op('dve', lambda e, c=c, nt=nt: e.tensor_tensor(out=hid[:, c, 0:nt], in0=sgt[:, c, 0:nt], in1=PB(2 + c)[:, 0:nt], op=ALU.mult),
                     reads=['sgt%d' % c], writes=[pk(2 + c), 'hid%d' % c])
            for ti in range(nt // 128):
                t = t0 // 128 + ti
                pby = 4 + 2 * (ti % 2)

                def mmd(e, ti=ti, wi=wi, pby=pby):
                    for hf in range(2):
                        for c in range(2):
                            ins = e.matmul(PB(pby + hf), lhsT=hid[:, c, ti * 128:(ti + 1) * 128], rhs=wdb[wi][:, c, hf * 512:(hf + 1) * 512],
                                           start=(c == 0), stop=(c == 1))
                    return ins
                S.op('pe', mmd, reads=['hid0', 'hid1', 'wdb%d' % wi], writes=[pk(pby), pk(pby + 1)])
                S.op('dve', lambda e, t=t, ex=ex, pby=pby: e.scalar_tensor_tensor(out=yacc[:, t, :], in0=ps_all[:, pby * 512:(pby + 2) * 512],
                                                                                 scalar=gates[:, t, ex:ex + 1], in1=yacc[:, t, :], op0=ALU.mult, op1=ALU.add),
                     reads=['gates%d' % t], writes=[pk(pby), pk(pby + 1), 'yacc%d' % t])
    for t in range(16):
        DMA('sp', y_own[t * 128:(t + 1) * 128, :], yacc[:, t, :], r=['yacc%d' % t], w=['y_own'])
    DMA('sp', y_samp, yacc[:, 16, :], r=['yacc16'], w=['y_samp'])
    S.emit()
    return nc


def sample_path(nc, S, A, base0, L):
    g_ = lambda n: L[n]
    proj, mix_d, zero_t, st_gdn, st_conv, caches = g_('proj'), g_('mix_d'), g_('zero_t'), g_('st_gdn'), g_('st_conv'), g_('caches')
    conv_wT, a_log, dt_bias, gnw, qnw, knw, rope_t, selh = g_('conv_wT'), g_('a_log'), g_('dt_bias'), g_('gnw'), g_('qnw'), g_('knw'), g_('rope_t'), g_('selh')
    sgs, kvs = g_('sgs'), g_('kvs')
    ident, ones, PB, pk, ps_all = g_('ident'), g_('ones'), g_('PB'), g_('pk'), g_('ps_all')
    NS = 16
    R0 = 3 + 4096

    def DMA(eng, out, in_, r=(), w=()):
        S.op(eng, lambda e: e.dma_start(out=out, in_=in_), reads=r, writes=w, dma=True)
    A.off = base0
    id16 = ident[0:NS, 0:NS]
    cw = A.f32([NS, 4, 1536])
    for k in range(4):
        DMA('sp', cw[:, k, :], conv_wT[k:k + 1, :].partition_broadcast(NS), w=['s_cw'])
    full = A.f32([NS, 4, 1536])
    DMA('sp', full[:, 0:3, :], st_conv, w=['s_full'])
    DMA('sp', full[:, 3, :], proj[R0:R0 + NS, 0:1536], w=['s_full'])
    zs = A.f32([NS, 512])
    DMA('sp', zs, proj[R0:R0 + NS, 1536:2048], w=['s_z'])
    ba = A.f32([NS, 8])
    DMA('sp', ba, proj[R0:R0 + NS, 2048:2056], w=['s_ba'])
    negA = A.f32([NS, 4])
    dtb = A.f32([NS, 4])
    DMA('sp', negA, a_log.partition_broadcast(NS), w=['s_negA'])
    DMA('sp', dtb, dt_bias.partition_broadcast(NS), w=['s_dtb'])
    gnb = A.f32([NS, 128])
    DMA('sp', gnb, gnw.partition_broadcast(NS), w=['s_gnb'])
    S.op('act', lambda e: e.activation(out=negA, in_=negA, func=AF.Exp), writes=['s_negA'])
    S.op('dve', lambda e: e.tensor_scalar(out=negA, in0=negA, scalar1=-1.0, scalar2=None, op0=ALU.mult), writes=['s_negA'])
    S.op('dve', lambda e: e.tensor_tensor(out=full, in0=full, in1=cw, op=ALU.mult), reads=['s_cw'], writes=['s_full'])
    cc = A.f32([NS, 1536])
    S.op('dve', lambda e: e.tensor_tensor(out=cc, in0=full[:, 0, :], in1=full[:, 1, :], op=ALU.add), reads=['s_full'], writes=['s_cc'])
    S.op('dve', lambda e: e.tensor_tensor(out=cc, in0=cc, in1=full[:, 2, :], op=ALU.add), reads=['s_full'], writes=['s_cc'])
    S.op('dve', lambda e: e.tensor_tensor(out=cc, in0=cc, in1=full[:, 3, :], op=ALU.add), reads=['s_full'], writes=['s_cc'])
    S.op('act', lambda e: e.activation(out=cc, in_=cc, func=AF.Silu), writes=['s_cc'])
    sq = A.f32([NS, 1024])
    ss8 = A.f32([NS, 8])
    rs8 = A.f32([NS, 8])
    qk = A.f32([NS, 8, 128])
    S.op('act', lambda e: e.activation(out=sq, in_=cc[:, 0:1024], func=AF.Square), reads=['s_cc'], writes=['s_sq'])
    S.op('dve', lambda e: e.tensor_reduce(out=ss8, in_=sq.rearrange("p (h d) -> p h d", h=8), axis=AX.X, op=ALU.add), reads=['s_sq'], writes=['s_ss8'])
    S.op('act', lambda e: e.activation(out=ss8, in_=ss8, func=AF.Sqrt, bias=EPS), writes=['s_ss8'])
    S.op('dve', lambda e: e.reciprocal(out=rs8, in_=ss8), reads=['s_ss8'], writes=['s_rs8'])
    S.op('dve', lambda e: e.tensor_scalar(out=rs8[:, 0:4], in0=rs8[:, 0:4], scalar1=128.0 ** -0.5, scalar2=None, op0=ALU.mult), writes=['s_rs8'])
    S.op('dve', lambda e: e.tensor_tensor(out=qk, in0=cc[:, 0:1024].rearrange("p (h d) -> p h d", h=8), in1=rs8.unsqueeze(2).broadcast_to([NS, 8, 128]), op=ALU.mult),
         reads=['s_cc', 's_rs8'], writes=['s_qk'])
    vv = cc[:, 1024:1536].rearrange("p (h d) -> p h d", h=4)
    beta = A.f32([NS, 4])
    gx = A.f32([NS, 4])
    eg = A.f32([NS, 4])
    S.op('act', lambda e: e.activation(out=beta, in_=ba[:, 0:4], func=AF.Sigmoid), reads=['s_ba'], writes=['s_beta'])
    S.op('dve', lambda e: e.tensor_tensor(out=gx, in0=ba[:, 4:8], in1=dtb, op=ALU.add), reads=['s_ba', 's_dtb'], writes=['s_gx'])
    S.op('act', lambda e: e.activation(out=gx, in_=gx, func=AF.Exp), writes=['s_gx'])
    S.op('act', lambda e: e.activation(out=gx, in_=gx, func=AF.Ln, bias=1.0), writes=['s_gx'])
    S.op('dve', lambda e: e.tensor_tensor(out=gx, in0=gx, in1=negA, op=ALU.mult), reads=['s_negA'], writes=['s_gx'])
    S.op('act', lambda e: e.activation(out=eg, in_=gx, func=AF.Exp), reads=['s_gx'], writes=['s_eg'])
    kqT = A.f32([128, 8, NS])

    def trkq(e):
        for j in range(8):
            ins = e.transpose(out=PB(0)[:, j * NS:(j + 1) * NS], in_=qk[:, j, :], identity=id16)
        return ins
    S.op('pe', trkq, reads=['s_qk', 'cst'], writes=[pk(0)])
    S.op('dve', lambda e: e.tensor_copy(out=kqT, in_=PB(0)[:, 0:8 * NS].rearrange("p (j s) -> p j s", j=8)), writes=[pk(0), 's_kqT'])
    egd = A.f32([NS, NS, 4])
    S.op('dve', lambda e: e.tensor_tensor(out=egd, in0=id16.unsqueeze(2).broadcast_to([NS, NS, 4]), in1=eg.unsqueeze(1).broadcast_to([NS, NS, 4]), op=ALU.mult),
         reads=['cst', 's_eg'], writes=['s_egd'])
    S.op('pe', lambda e: e.matmul(PB(1)[:, 0:NS * 4], lhsT=ones[0:NS, :], rhs=egd.rearrange("p s h -> p (s h)"), start=True, stop=True),
         reads=['s_egd', 'cst'], writes=[pk(1)])
    egb = A.f32([128, NS, 4])
    S.op('dve', lambda e: e.tensor_copy(out=egb, in_=PB(1)[:, 0:NS * 4].rearrange("p (s h) -> p s h", s=NS)), writes=[pk(1), 's_egb'])
    Sold = [A.f32([128, 4, 128]) for _ in range(3)]
    Snew = [A.f32([128, 4, 128]) for _ in range(3)]
    kSacc = A.f32([NS, 4, 128])
    oacc = A.f32([NS, 4, 128])
    S.op('pool', lambda e: e.memset(kSacc, 0.0), writes=['s_kSacc'])
    S.op('pool', lambda e: e.memset(oacc, 0.0), writes=['s_oacc'])
    for s_ in range(NS):
        bi = s_ % 3
        DMA('sp', Sold[bi], st_gdn[s_].rearrange("h k v -> k h v"), w=['s_Sold%d' % bi])

        def mm1(e, bi=bi):
            for h in range(4):
                ins = e.matmul(PB(2)[0:NS, h * 128:(h + 1) * 128], lhsT=kqT[:, 4 + h, :], rhs=Sold[bi][:, h, :], start=True, stop=True)
            return ins
        S.op('pe', mm1, reads=['s_kqT', 's_Sold%d' % bi], writes=[pk(2)])
        S.op('dve', lambda e, s_=s_: e.scalar_tensor_tensor(out=kSacc.rearrange("p h d -> p (h d)"), in0=PB(2)[0:NS, :], scalar=id16[:, s_:s_ + 1],
                                                            in1=kSacc.rearrange("p h d -> p (h d)"), op0=ALU.mult, op1=ALU.add),
             reads=['cst'], writes=[pk(2), 's_kSacc'])
    vn = A.f32([NS, 4, 128])
    S.op('dve', lambda e: e.tensor_tensor(out=vn, in0=kSacc, in1=eg.unsqueeze(2).broadcast_to([NS, 4, 128]), op=ALU.mult), reads=['s_kSacc', 's_eg'], writes=['s_vn'])
    S.op('dve', lambda e: e.tensor_tensor(out=vn, in0=vv, in1=vn, op=ALU.subtract), reads=['s_cc'], writes=['s_vn'])
    S.op('dve', lambda e: e.tensor_tensor(out=vn, in0=vn, in1=beta.unsqueeze(2).broadcast_to([NS, 4, 128]), op=ALU.mult), reads=['s_beta'], writes=['s_vn'])
    vm = [A.f32([NS, 4, 128]) for _ in range(2)]
    stmp = [A.f32([128, 4, 128]) for _ in range(2)]
    for s_ in range(NS):
        bi = s_ % 3
        b2 = s_ % 2
        DMA('sp', Sold[bi], st_gdn[s_].rearrange("h k v -> k h v"), w=['s_Sold%d' % bi])
        S.op('dve', lambda e, s_=s_, b2=b2: e.tensor_scalar(out=vm[b2], in0=vn, scalar1=id16[:, s_:s_ + 1], scalar2=None, op0=ALU.mult),
             reads=['s_vn', 'cst'], writes=['s_vm%d' % b2])

        def mm2(e, b2=b2):
            for h in range(4):
                ins = e.matmul(PB(3)[:, h * 128:(h + 1) * 128], lhsT=qk[:, 4 + h, :], rhs=vm[b2][:, h, :], start=True, stop=True)
            return ins
        S.op('pe', mm2, reads=['s_qk', 's_vm%d' % b2], writes=[pk(3)])
        S.op('pool', lambda e, bi=bi, b2=b2, s_=s_: e.tensor_tensor(out=stmp[b2], in0=Sold[bi], in1=egb[:, s_, :].unsqueeze(2).broadcast_to([128, 4, 128]), op=ALU.mult),
             reads=['s_Sold%d' % bi, 's_egb'], writes=['s_stmp%d' % b2])
        S.op('dve', lambda e, bi=bi, b2=b2: e.tensor_tensor(out=Snew[bi], in0=stmp[b2], in1=PB(3).rearrange("p (h d) -> p h d", h=4), op=ALU.add),
             reads=['s_stmp%d' % b2], writes=[pk(3), 's_Snew%d' % bi])
        DMA('pool', sgs[s_].rearrange("h k v -> k h v"), Snew[bi], r=['s_Snew%d' % bi], w=['sgs'])

        def mm3(e, bi=bi):
            for h in range(4):
                ins = e.matmul(PB(4)[0:NS, h * 128:(h + 1) * 128], lhsT=kqT[:, h, :], rhs=Snew[bi][:, h, :], start=True, stop=True)
            return ins
        S.op('pe', mm3, reads=['s_kqT', 's_Snew%d' % bi], writes=[pk(4)])
        S.op('dve', lambda e, s_=s_: e.scalar_tensor_tensor(out=oacc.rearrange("p h d -> p (h d)"), in0=PB(4)[0:NS, :], scalar=id16[:, s_:s_ + 1],
                                                            in1=oacc.rearrange("p h d -> p (h d)"), op0=ALU.mult, op1=ALU.add),
             reads=['cst'], writes=[pk(4), 's_oacc'])
    mixs = A.f32([NS, 1024])
    o4 = A.f32([NS, 4])
    S.op('act', lambda e: e.activation(out=sq[:, 0:512], in_=oacc.rearrange("p h d -> p (h d)"), func=AF.Square), reads=['s_oacc'], writes=['s_sq'])
    S.op('dve', lambda e: e.tensor_reduce(out=o4, in_=sq[:, 0:512].rearrange("p (h d) -> p h d", h=4), axis=AX.X, op=ALU.add), reads=['s_sq'], writes=['s_o4'])
    S.op('act', lambda e: e.activation(out=o4, in_=o4, func=AF.Sqrt, scale=1.0 / 128, bias=EPS), writes=['s_o4'])
    S.op('dve', lambda e: e.reciprocal(out=o4, in_=o4), writes=['s_o4'])
    m3 = mixs[:, 0:512].rearrange("p (h d) -> p h d", h=4)
    S.op('dve', lambda e: e.tensor_tensor(out=m3, in0=oacc, in1=o4.unsqueeze(2).broadcast_to([NS, 4, 128]), op=ALU.mult), reads=['s_oacc', 's_o4'], writes=['s_mix'])
    S.op('dve', lambda e: e.tensor_tensor(out=m3, in0=m3, in1=gnb.unsqueeze(1).broadcast_to([NS, 4, 128]), op=ALU.mult), reads=['s_gnb'], writes=['s_mix'])
    S.op('act', lambda e: e.activation(out=zs, in_=zs, func=AF.Silu), writes=['s_z'])
    S.op('dve', lambda e: e.tensor_tensor(out=mixs[:, 0:512], in0=mixs[:, 0:512], in1=zs, op=ALU.mult), reads=['s_z'], writes=['s_mix'])
    qkw = A.f32([NS, 6, 64])
    for gi in range(3):
        DMA('sp', qkw[:, gi, :], qnw[gi:gi + 1, :].partition_broadcast(NS), w=['s_qkw'])
        DMA('sp', qkw[:, 3 + gi, :], knw[gi:gi + 1, :].partition_broadcast(NS), w=['s_qkw'])
    rp = A.f32([NS, 16])
    DMA('sp', rp, rope_t[4096:4096 + NS, :], w=['s_rp'])
    cosb = rp[:, 0:8].unsqueeze(1).broadcast_to([NS, 8, 8])
    sinb = rp[:, 8:16].unsqueeze(1).broadcast_to([NS, 8, 8])
    qkv = A.f32([NS, 3, 3, 512])
    DMA('sp', qkv.rearrange("p g w c -> p (g w c)"), proj[R0:R0 + NS, 2056:2056 + 4608], w=['s_qkv'])
    s2 = A.f32([NS, 512])
    r8 = A.f32([NS, 8])
    rt = [A.f32([NS, 8, 8]) for _ in range(4)]
    for gi in range(3):
        for which in range(2):
            d3 = qkv[:, gi, which, :].rearrange("p (h d) -> p h d", h=8)
            S.op('act', lambda e, gi=gi, which=which: e.activation(out=s2, in_=qkv[:, gi, which, :], func=AF.Square), reads=['s_qkv'], writes=['s_s2'])
            S.op('dve', lambda e: e.tensor_reduce(out=r8, in_=s2.rearrange("p (h d) -> p h d", h=8), axis=AX.X, op=ALU.add), reads=['s_s2'], writes=['s_r8'])
            S.op('act', lambda e: e.activation(out=r8, in_=r8, func=AF.Sqrt, scale=1.0 / 64, bias=EPS), writes=['s_r8'])
            S.op('dve', lambda e: e.reciprocal(out=r8, in_=r8), writes=['s_r8'])
            S.op('dve', lambda e, d3=d3: e.tensor_tensor(out=d3, in0=d3, in1=r8.unsqueeze(2).broadcast_to([NS, 8, 64]), op=ALU.mult), reads=['s_r8'], writes=['s_qkv'])
            wrow = qkw[:, (3 if which == 1 else 0) + gi, :].unsqueeze(1).broadcast_to([NS, 8, 64])
            S.op('dve', lambda e, d3=d3, wrow=wrow: e.tensor_tensor(out=d3, in0=d3, in1=wrow, op=ALU.mult), reads=['s_qkw'], writes=['s_qkv'])
            x1 = d3[:, :, 0:8]
            x2 = d3[:, :, 8:16]
            S.op('dve', lambda e, x1=x1: e.tensor_tensor(out=rt[0], in0=x1, in1=cosb, op=ALU.mult), reads=['s_qkv', 's_rp'], writes=['s_rt0'])
            S.op('dve', lambda e, x2=x2: e.tensor_tensor(out=rt[1], in0=x2, in1=sinb, op=ALU.mult), reads=['s_qkv', 's_rp'], writes=['s_rt1'])
            S.op('dve', lambda e, x2=x2: e.tensor_tensor(out=rt[2], in0=x2, in1=cosb, op=ALU.mult), reads=['s_qkv', 's_rp'], writes=['s_rt2'])
            S.op('dve', lambda e, x1=x1: e.tensor_tensor(out=rt[3], in0=x1, in1=sinb, op=ALU.mult), reads=['s_qkv', 's_rp'], writes=['s_rt3'])
            S.op('dve', lambda e, x1=x1: e.tensor_tensor(out=x1, in0=rt[0], in1=rt[1], op=ALU.subtract), reads=['s_rt0', 's_rt1'], writes=['s_qkv'])
            S.op('dve', lambda e, x2=x2: e.tensor_tensor(out=x2, in0=rt[2], in1=rt[3], op=ALU.add), reads=['s_rt2', 's_rt3'], writes=['s_qkv'])
        W = GROUPS[gi][0]
        DMA('pool', kvs[gi][:, W - 1, :], qkv[:, gi, 1:3, :].rearrange("p w c -> p (w c)"), r=['s_qkv'], w=['kvs_new%d' % gi])
    selq = A.f32([NS, NS, 128])
    S.op('dve', lambda e: e.tensor_copy(out=selq, in_=id16.unsqueeze(2).broadcast_to([NS, NS, 128])), reads=['cst'], writes=['s_selq'])
    selh_t = A.f32([8, NS, NS])
    DMA('sp', selh_t, selh, w=['s_selh'])
    kvt = [A.f32([128, 1024]) for _ in range(3)]
    prod = A.f32([128, 512])
    sc = A.f32([128, 8])
    pex = A.f32([128, 8])
    Z = A.f32([8, 8, 64])
    Z2 = A.f32([8, 8])
    numS = A.f32([NS, 8, 64])
    denS = A.f32([NS, 8])
    ps_ = A.f32([NS, 8])
    pr16 = A.f32([NS, 8, 64])
    S.op('pool', lambda e: e.memset(numS, 0.0), writes=['s_numS'])
    S.op('pool', lambda e: e.memset(denS, 0.0), writes=['s_denS'])
    cnt = 0
    for gi in range(3):
        W, dil = GROUPS[gi]
        q3 = qkv[:, gi, 0, :].rearrange("p (h d) -> p h d", h=8)
        k3 = qkv[:, gi, 1, :].rearrange("p (h d) -> p h d", h=8)
        v3 = qkv[:, gi, 2, :].rearrange("p (h d) -> p h d", h=8)
        S.op('dve', lambda e, q3=q3, k3=k3: e.tensor_tensor(out=pr16, in0=q3, in1=k3, op=ALU.mult), reads=['s_qkv'], writes=['s_pr16'])
        S.op('dve', lambda e: e.tensor_reduce(out=ps_, in_=pr16, axis=AX.X, op=ALU.add), reads=['s_pr16'], writes=['s_ps'])
        S.op('act', lambda e: e.activation(out=ps_, in_=ps_, func=AF.Exp, scale=0.125), writes=['s_ps'])
        S.op('dve', lambda e: e.tensor_tensor(out=denS, in0=denS, in1=ps_, op=ALU.add), reads=['s_ps'], writes=['s_denS'])
        S.op('dve', lambda e, v3=v3: e.tensor_tensor(out=pr16, in0=v3, in1=ps_.unsqueeze(2).broadcast_to([NS, 8, 64]), op=ALU.mult), reads=['s_qkv', 's_ps'], writes=['s_pr16'])
        S.op('dve', lambda e: e.tensor_tensor(out=numS, in0=numS, in1=pr16, op=ALU.add), reads=['s_pr16'], writes=['s_numS'])
        for s_ in range(NS):
            bi = cnt % 3
            first = (cnt == 0)
            last = (cnt == 3 * NS - 1)
            cnt += 1
            KV = kvt[bi]
            kkv = 's_kvt%d' % bi
            DMA('sp', KV, caches[gi][s_, 0:W - dil + 1:dil, :], w=[kkv])
            S.op('pe', lambda e, s_=s_, gi=gi: e.matmul(PB(0), lhsT=selq[:, s_, :], rhs=qkv[:, gi, 0, :], start=True, stop=True), reads=['s_selq', 's_qkv'], writes=[pk(0)])
            S.op('dve', lambda e, KV=KV: e.tensor_tensor(out=prod, in0=KV[:, 0:512], in1=PB(0), op=ALU.mult), reads=[kkv], writes=[pk(0), 's_prod'])
            S.op('dve', lambda e: e.tensor_reduce(out=sc, in_=prod.rearrange("p (h d) -> p h d", h=8), axis=AX.X, op=ALU.add), reads=['s_prod'], writes=['s_sc'])
            S.op('act', lambda e: e.activation(out=pex, in_=sc, func=AF.Exp, scale=0.125), reads=['s_sc'], writes=['s_pex'])
            S.op('pe', lambda e, KV=KV: e.matmul(PB(1)[0:8, :], lhsT=pex, rhs=KV[:, 512:1024], start=True, stop=True), reads=['s_pex', kkv], writes=[pk(1)])
            S.op('pe', lambda e: e.matmul(PB(2)[0:8, 0:8], lhsT=pex, rhs=ones[:, 0:8], start=True, stop=True), reads=['s_pex', 'cst'], writes=[pk(2)])
            S.op('dve', lambda e: e.tensor_tensor(out=Z, in0=PB(1)[0:8, :].rearrange("p (h d) -> p h d", h=8), in1=ident[0:8, 0:8].unsqueeze(2).broadcast_to([8, 8, 64]), op=ALU.mult),
                 reads=['cst'], writes=[pk(1), 's_Z'])
            S.op('dve', lambda e: e.tensor_tensor(out=Z2, in0=PB(2)[0:8, 0:8], in1=ident[0:8, 0:8], op=ALU.mult), reads=['cst'], writes=[pk(2), 's_Z2'])
            S.op('pe', lambda e, s_=s_, first=first, last=last: e.matmul(PB(6)[0:NS, :], lhsT=selh_t[:, s_, :], rhs=Z.rearrange("p h d -> p (h d)"), start=first, stop=last),
                 reads=['s_selh', 's_Z'], writes=[pk(6)])
            S.op('pe', lambda e, s_=s_, first=first, last=last: e.matmul(PB(7)[0:NS, 0:8], lhsT=selh_t[:, s_, :], rhs=Z2, start=first, stop=last),
                 reads=['s_selh', 's_Z2'], writes=[pk(7)])
    S.op('dve', lambda e: e.tensor_tensor(out=numS, in0=numS, in1=PB(6)[0:NS, :].rearrange("p (h d) -> p h d", h=8), op=ALU.add), writes=[pk(6), 's_numS'])
    S.op('dve', lambda e: e.tensor_tensor(out=denS, in0=denS, in1=PB(7)[0:NS, 0:8], op=ALU.add), writes=[pk(7), 's_denS'])
    S.op('dve', lambda e: e.reciprocal(out=denS, in_=denS), writes=['s_denS'])
    S.op('dve', lambda e: e.tensor_tensor(out=mixs[:, 512:1024].rearrange("p (h d) -> p h d", h=8), in0=numS, in1=denS.unsqueeze(2).broadcast_to([NS, 8, 64]), op=ALU.mult),
         reads=['s_numS', 's_denS'], writes=['s_mix'])
    DMA('pool', mix_d[2048:2048 + NS, :], mixs, r=['s_mix'], w=['mix_d_s'])
    for j in range(2):
        DMA('pool', mix_d[2048 + NS:2176, j * 512:(j + 1) * 512], zero_t[0:128 - NS, 0:512], r=['zero'], w=['mix_d_z%d' % j])
    S.barrier()


_CACHE = {}


def _host_consts():
    c = np.zeros((128, 5, 128), np.float32)
    p = np.arange(128)[:, None]
    f = np.arange(128)[None, :]
    c[:, 0] = (p == f)
    c[:, 1] = (p <= f)
    c[:, 2] = (p >= f)
    c[:, 3] = (p < f)
    c[:, 4] = 1.0
    return c


def _rope_table(pos):
    half = 8
    inv = np.exp(-math.log(500000.0) * np.arange(half, dtype=np.float32) * np.float32(2.0 / 16)).astype(np.float32)
    ang = pos.astype(np.float32)[:, None] * inv[None, :]
    return np.concatenate([np.cos(ang), np.sin(ang)], axis=1).astype(np.float32)


def kernel(x_prompt, x_sample, state_gdn, state_conv, cache_kv_w128, cache_kv_w512, cache_kv_w2048,
           norm1_w, w_in, conv_w, a_log, dt_bias, gdn_norm_w, q_norm_w, k_norm_w, w_out, norm2_w,
           w_router_group, w_router_expert, w_gate_up, w_down):
    f = lambda a: np.ascontiguousarray(np.asarray(a, dtype=np.float32))
    x_prompt, x_sample = f(x_prompt), f(x_sample)
    if 'nc' not in _CACHE:
        _CACHE['nc'] = build_program()
    nc = _CACHE['nc']
    consts = _host_consts()
    w_r = np.concatenate([f(w_router_group)[0], f(w_router_expert)[0]], axis=1)
    shared = {
        "w_in": f(w_in)[0], "w_out": f(w_out)[0], "norm1": f(norm1_w), "norm2": f(norm2_w),
        "conv_wT": np.ascontiguousarray(f(conv_w)[0].T), "a_log": f(a_log), "dt_bias": f(dt_bias), "gnw": f(gdn_norm_w),
        "qnw": f(q_norm_w)[0], "knw": f(k_norm_w)[0], "w_r": np.ascontiguousarray(w_r), "w_gu": f(w_gate_up)[0], "w_d": f(w_down)[0],
        "consts": consts,
        "selh": np.ascontiguousarray(np.broadcast_to(np.eye(16, dtype=np.float32)[None], (8, 16, 16))),
    }
    cachesf = [f(cache_kv_w128)[0], f(cache_kv_w512)[0], f(cache_kv_w2048)[0]]
    sg, sc = f(state_gdn)[0], f(state_conv)[0]
    in_maps = []
    for c in range(NCORE):
        b, half = c // 2, c % 2
        xa = np.zeros((NTILE * 128, D), np.float32)
        if half == 1:
            xa[0:2048] = x_prompt[b, 0:2048]
        xa[2048:4096] = x_prompt[b, half * 2048:(half + 1) * 2048]
        xa[4096:4112] = x_sample[16 * c:16 * c + 16, 0]
        pos = np.concatenate([np.arange(4096) + (half - 1) * 2048, np.full(128, 8192)]).astype(np.float32)
        m = dict(shared)
        m["x_all"] = xa
        m["rope_t"] = _rope_table(pos)
        m["maskb"] = np.full((128, 1), 0.0 if half == 1 else -30000.0, np.float32)
        m["st_gdn"] = np.ascontiguousarray(sg[16 * c:16 * c + 16])
        m["st_conv"] = np.ascontiguousarray(sc[16 * c:16 * c + 16])
        for i in range(3):
            m["cache%d" % i] = np.ascontiguousarray(cachesf[i][16 * c:16 * c + 16].reshape(16, GROUPS[i][0], 1024))
        in_maps.append(m)
    if _CACHE.get('only_maps'):
        return in_maps
    res = run_bass_kernel_spmd(nc, in_maps, core_ids=list(range(NCORE)))
    R = res.results
    y_prompt = np.zeros((4, 4096, D), np.float32)
    for c in range(NCORE):
        y_prompt[c // 2, (c % 2) * 2048:(c % 2 + 1) * 2048] = R[c]["y_own"]
    y_sample = np.concatenate([R[c]["y_samp"][0:16] for c in range(NCORE)], axis=0).reshape(128, 1, D)
    sgp_o = np.stack([R[2 * b + 1]["sgp"] for b in range(4)])[None]
    scp_o = np.stack([R[2 * b + 1]["scp"] for b in range(4)])[None]
    kvp_o = [np.stack([R[2 * b + 1]["kvp%d" % i] for b in range(4)]).reshape(1, 4, GROUPS[i][0], 2, 8, 64) for i in range(3)]
    sgs_o = np.concatenate([R[c]["sgs"] for c in range(NCORE)], axis=0)[None]
    scs_o = np.concatenate([R[c]["scs"] for c in range(NCORE)], axis=0)[None]
    kvs_o = [np.concatenate([R[c]["kvs%d" % i] for c in range(NCORE)], axis=0).reshape(1, 128, GROUPS[i][0], 2, 8, 64) for i in range(3)]
    return (y_prompt, y_sample, sgp_o, scp_o, kvp_o[0], kvp_o[1], kvp_o[2], sgs_o, scs_o, kvs_o[0], kvs_o[1], kvs_o[2])
```

```python
import math
import numpy as np
import concourse.bass as bass
import concourse.mybir as mybir
from concourse.bass_utils import run_bass_kernel_spmd

F32 = mybir.dt.float32
BF16 = mybir.dt.bfloat16
AF = mybir.ActivationFunctionType
ALU = mybir.AluOpType
AX = mybir.AxisListType

NDMA = 32
NCORE = 8
D = 1024
INC = 6664
NTILE = 33
EPS = 1e-6
GROUPS = ((128, 1), (512, 4), (2048, 16))
COLBLK = [(0, 512), (512, 512), (1024, 512), (1536, 512), (2048, 8)]
for _g in range(3):
    _b = 2056 + _g * 1536
    COLBLK += [(_b, 512), (_b + 512, 512), (_b + 1024, 512)]
HALO_SKIP = {3, 5, 8, 11}


class Sched:
    ENG = ['pe', 'act', 'dve', 'pool', 'sp']

    def __init__(self, nc):
        self.nc = nc
        self.ops = {e: [] for e in self.ENG}
        self.esem = {e: nc.alloc_semaphore(name="s_" + e) for e in self.ENG}
        self.ecnt = {e: 0 for e in self.ENG}
        self.dpool = {'sp': list(range(0, 12)), 'pool': list(range(12, 20)), 'act': list(range(20, 24)), 'spc': list(range(24, 32))}
        self.dsem = [nc.alloc_semaphore(name="d%d" % i) for i in range(NDMA)]
        self.dcnt = [0] * NDMA
        self.dnext = {'sp': 0, 'pool': 0, 'act': 0, 'spc': 0}
        self.last_w = {}
        self.readers = {}
        self.waited = {e: {} for e in self.ENG}

    def _tok_waits(self, e, deps):
        waits = []
        for (sem, val) in deps:
            sid = id(sem)
            if e == 'pe' and sem is self.esem['pe']:
                continue
            if self.waited[e].get(sid, 0) < val:
                self.waited[e][sid] = val
                waits.append((sem, val))
        return waits

    def op(self, e, fn, reads=(), writes=(), dma=False, pool=None):
        deps = []
        for k in reads:
            if k in self.last_w:
                deps.append(self.last_w[k])
        for k in writes:
            if k in self.last_w:
                deps.append(self.last_w[k])
            for sid, tk in self.readers.get(k, {}).items():
                deps.append(tk)
        if dma:
            pn = pool or e
            pl = self.dpool[pn]
            s = pl[self.dnext[pn] % len(pl)]
            self.dnext[pn] += 1
            if self.dcnt[s] > 0:
                deps.append((self.dsem[s], 16 * self.dcnt[s]))
            self.dcnt[s] += 1
            tok = (self.dsem[s], 16 * self.dcnt[s])
            inc = 16
        else:
            self.ecnt[e] += 1
            tok = (self.esem[e], self.ecnt[e])
            inc = 1
        waits = self._tok_waits(e, deps)
        for k in writes:
            self.last_w[k] = tok
            self.readers[k] = {}
        for k in reads:
            r = self.readers.setdefault(k, {})
            sid = id(tok[0])
            if sid not in r or r[sid][1] < tok[1]:
                r[sid] = tok
        self.ops[e].append((fn, waits, (tok[0], inc)))
        return tok

    def barrier(self):
        toks = [(self.esem[e], self.ecnt[e]) for e in self.ENG if self.ecnt[e] > 0]
        toks += [(self.dsem[s], 16 * self.dcnt[s]) for s in range(NDMA) if self.dcnt[s] > 0]
        for e in self.ENG:
            waits = self._tok_waits(e, [t for t in toks if not (t[0] is self.esem[e])])
            if waits:
                self.ops[e].append((None, waits, None))
        self.last_w = {}
        self.readers = {}

    def emit(self):
        nc = self.nc
        self.barrier()
        with nc.Block() as block:
            def mk(e):
                def body(engine):
                    for (fn, waits, inc) in self.ops[e]:
                        for (sem, val) in waits:
                            engine.wait_ge(sem, val)
                        if fn is not None:
                            ins = fn(engine)
                            ins.then_inc(inc[0], inc[1])
                return body
            block.tensor(mk('pe'))
            block.scalar(mk('act'))
            block.vector(mk('dve'))
            block.gpsimd(mk('pool'))
            block.sync(mk('sp'))


def _shape_view(v, shape):
    if len(shape) == 2:
        return v
    if len(shape) == 3:
        return v.rearrange("p (a b) -> p a b", a=shape[1])
    return v.rearrange("p (a b c) -> p a b c", a=shape[1], b=shape[2])


class Arena:
    def __init__(self, ap, nwords):
        self.ap = ap
        self.n = nwords
        self.off = 0

    def f32(self, shape):
        n = int(np.prod(shape[1:]))
        o = self.off
        self.off += n
        assert self.off <= self.n, ("arena overflow", self.off, self.n)
        return _shape_view(self.ap[0:shape[0], o:o + n], shape)

    def bf16(self, shape):
        n = int(np.prod(shape[1:]))
        nw = (n + 1) // 2
        o = self.off
        self.off += nw
        assert self.off <= self.n, ("arena overflow", self.off, self.n)
        return _shape_view(self.ap[0:shape[0], o:o + nw].bitcast(BF16)[:, 0:n], shape)


def build_program(stop_after=None, debug=False):
    nc = bass.Bass("TRN2", target_bir_lowering=False)

    def din(name, shape):
        return nc.dram_tensor(name, list(shape), F32, kind="ExternalInput").ap()

    def dout(name, shape):
        return nc.dram_tensor(name, list(shape), F32, kind="ExternalOutput").ap()

    def dscr(name, shape):
        return nc.dram_tensor(name, list(shape), F32, kind=("ExternalOutput" if debug else "Internal")).ap()

    x_all = din("x_all", [NTILE * 128, D])
    w_in = din("w_in", [D, INC])
    w_out = din("w_out", [D, D])
    norm1 = din("norm1", [1, D])
    norm2 = din("norm2", [1, D])
    conv_wT = din("conv_wT", [4, 1536])
    a_log = din("a_log", [1, 4])
    dt_bias = din("dt_bias", [1, 4])
    gnw = din("gnw", [1, 128])
    qnw = din("qnw", [3, 64])
    knw = din("knw", [3, 64])
    w_r = din("w_r", [D, 36])
    w_gu = din("w_gu", [32, D, 512])
    w_d = din("w_d", [32, 256, D])
    consts = din("consts", [128, 5, 128])
    rope_t = din("rope_t", [NTILE * 128, 16])
    maskb = din("maskb", [128, 1])
    selh = din("selh", [8, 16, 16])
    st_gdn = din("st_gdn", [16, 4, 128, 128])
    st_conv = din("st_conv", [16, 3, 1536])
    caches = [din("cache%d" % i, [16, GROUPS[i][0], 1024]) for i in range(3)]

    y_own = dout("y_own", [2048, D])
    y_samp = dout("y_samp", [128, D])
    sgp = dout("sgp", [4, 128, 128])
    scp = dout("scp", [3, 1536])
    kvp = [dout("kvp%d" % i, [GROUPS[i][0], 2, 512]) for i in range(3)]
    sgs = dout("sgs", [16, 4, 128, 128])
    scs = dout("scs", [16, 3, 1536])
    kvs = [dout("kvs%d" % i, [16, GROUPS[i][0], 1024]) for i in range(3)]

    proj = dscr("proj", [3 + NTILE * 128, INC])
    gdn_in = dscr("gdn_in", [32, 128, 1544])
    o_gdn = dscr("o_gdn", [2048, 512])
    kn_s = dscr("kn_s", [3, 4096, 512])
    qn_s = dscr("qn_s", [3, 2048, 512])
    att_o = dscr("att_o", [3, 2048, 520])
    mix_d = dscr("mix_d", [17 * 128, D])

    S = Sched(nc)
    AW = 48500
    sb_all = nc.alloc_sbuf_tensor("arena", [128, AW], F32).ap()
    ps_all = nc.alloc_psum_tensor("psarena", [128, 4096], F32).ap()
    A = Arena(sb_all, AW)

    def PB(i):
        return ps_all[:, i * 512:(i + 1) * 512]

    def PBb(i):
        return ps_all[:, i * 512:(i + 1) * 512].bitcast(BF16)

    def pk(i):
        return 'PS%d' % i

    def DMA(eng, out, in_, r=(), w=()):
        S.op(eng, lambda e: e.dma_start(out=out, in_=in_), reads=r, writes=w, dma=True)

    cst = A.f32([128, 5, 128])
    DMA('sp', cst, consts, w=['cst'])
    ident = cst[:, 0, :]
    U_incl = cst[:, 1, :]
    L_incl = cst[:, 2, :]
    U_strict = cst[:, 3, :]
    ones = cst[:, 4, :]
    identb = A.bf16([128, 128])
    S.op('dve', lambda e: e.tensor_copy(out=identb, in_=ident), reads=['cst'], writes=['identb'])
    maskb_t = A.f32([128, 1])
    DMA('sp', maskb_t, maskb, w=['maskb'])
    zero_t = A.f32([128, 1024])
    S.op('pool', lambda e: e.memset(zero_t, 0.0), writes=['zero'])
    base0 = A.off

    copy_jobs = []
    for s_ in range(16):
        for ch in range(4):
            r0 = ch * 512
            r1 = min(r0 + 512, 2047)
            copy_jobs.append((2, s_, r0, r1))
        copy_jobs.append((1, s_, 0, 511))
        copy_jobs.append((0, s_, 0, 127))

    def issue_copies(n):
        for _ in range(n):
            if not copy_jobs:
                return
            gi, s_, r0, r1 = copy_jobs.pop(0)
            S.op('sp', lambda e, gi=gi, s_=s_, r0=r0, r1=r1: e.dma_start(out=kvs[gi][s_, r0:r1, :], in_=caches[gi][s_, r0 + 1:r1 + 1, :]),
                 writes=[], dma=True, pool='spc')
    S.op('act', lambda e: e.dma_start(out=scs[:, 0:2, :], in_=st_conv[:, 1:3, :]), writes=['scs01'], dma=True)

    xnT = A.bf16([128, 8, NTILE * 128])
    w1b = A.f32([128, D])
    DMA('sp', w1b, norm1.partition_broadcast(128), w=['w1b'])
    xt = [A.f32([128, D]) for _ in range(2)]
    sq = A.f32([128, D])
    ss = A.f32([128, 1])
    rstd = A.f32([128, 1])
    xn = A.bf16([128, D])
    for t in range(NTILE):
        xx = xt[t % 2]
        kx = 'xt%d' % (t % 2)
        DMA('sp', xx, x_all[t * 128:(t + 1) * 128, :], w=[kx])
        S.op('act', lambda e, xx=xx: e.activation(out=sq, in_=xx, func=AF.Square, accum_out=ss), reads=[kx], writes=['sq', 'ss'])
        S.op('act', lambda e: e.activation(out=ss, in_=ss, func=AF.Sqrt, scale=1.0 / D, bias=EPS), writes=['ss'])
        S.op('dve', lambda e: e.reciprocal(out=rstd, in_=ss), reads=['ss'], writes=['rstd'])
        S.op('dve', lambda e, xx=xx: e.scalar_tensor_tensor(out=xn, in0=xx, scalar=rstd, in1=w1b, op0=ALU.mult, op1=ALU.mult),
             reads=[kx, 'rstd', 'w1b'], writes=['xn'])
        pb = t % 2

        def tr(e, pb=pb):
            for k in range(8):
                ins = e.transpose(out=PBb(pb)[:, k * 128:(k + 1) * 128], in_=xn[:, k * 128:(k + 1) * 128], identity=identb)
            return ins
        S.op('pe', tr, reads=['xn', 'identb'], writes=[pk(pb)])
        S.op('act', lambda e, t=t, pb=pb: e.activation(out=xnT[:, :, t * 128:(t + 1) * 128],
                                                       in_=PBb(pb).rearrange("p (k t) -> p k t", k=8), func=AF.Copy),
             writes=[pk(pb), 'xnT%d' % t])

    DMA('pool', proj[0:3, 0:1024], zero_t[0:3, :], r=['zero'], w=['projz'])
    DMA('pool', proj[0:3, 1024:1536], zero_t[0:3, 0:512], r=['zero'], w=['projz2'])
    wst = [A.f32([128, 8, 512]) for _ in range(2)]
    wbf = [A.bf16([128, 8, 512]) for _ in range(2)]
    ot = [A.f32([128, 512]) for _ in range(4)]
    oc = 0
    for cb, (c0, ncol) in enumerate(COLBLK):
        wi = cb % 2
        DMA('sp', wst[wi][:, :, 0:ncol], w_in[:, c0:c0 + ncol].rearrange("(k p) c -> p k c", p=128), w=['wst%d' % wi])
        S.op('pool', lambda e, wi=wi, ncol=ncol: e.tensor_copy(out=wbf[wi][:, :, 0:ncol], in_=wst[wi][:, :, 0:ncol]),
             reads=['wst%d' % wi], writes=['wbf%d' % wi])
        for t in range(NTILE):
            if t < 16 and cb in HALO_SKIP:
                continue
            pb = 2 + (oc % 4)
            oi = oc % 4
            oc += 1

            def mm(e, t=t, wi=wi, ncol=ncol, pb=pb):
                for k in range(8):
                    ins = e.matmul(PB(pb)[:, 0:ncol], lhsT=xnT[:, k, t * 128:(t + 1) * 128], rhs=wbf[wi][:, k, 0:ncol],
                                   start=(k == 0), stop=(k == 7))
                return ins
            S.op('pe', mm, reads=['xnT%d' % t, 'wbf%d' % wi], writes=[pk(pb)])
            ee = 'act' if oc % 2 == 0 else 'dve'
            if ee == 'act':
                S.op('act', lambda e, oi=oi, pb=pb, ncol=ncol: e.activation(out=ot[oi][:, 0:ncol], in_=PB(pb)[:, 0:ncol], func=AF.Copy),
                     writes=[pk(pb), 'ot%d' % oi])
            else:
                S.op('dve', lambda e, oi=oi, pb=pb, ncol=ncol: e.tensor_copy(out=ot[oi][:, 0:ncol], in_=PB(pb)[:, 0:ncol]),
                     writes=[pk(pb), 'ot%d' % oi])
            DMA('pool', proj[3 + t * 128:3 + (t + 1) * 128, c0:c0 + ncol], ot[oi][:, 0:ncol], r=['ot%d' % oi], w=[])
    S.barrier()
    DMA('sp', scp, proj[3 + 4096 - 3:3 + 4096, 0:1536], w=['scp'])
    DMA('sp', scs[:, 2, :], proj[3 + 4096:3 + 4096 + 16, 0:1536], w=['scs2'])

    if stop_after == 'B':
        S.emit()
        return nc
    A.off = base0
    cw = A.f32([128, 4, 1536])
    for k in range(4):
        DMA('sp', cw[:, k, :], conv_wT[k:k + 1, :].partition_broadcast(128), w=['cw'])
    negA = A.f32([128, 4])
    dtb = A.f32([128, 4])
    DMA('sp', negA, a_log.partition_broadcast(128), w=['negA'])
    DMA('sp', dtb, dt_bias.partition_broadcast(128), w=['dtb'])
    S.op('act', lambda e: e.activation(out=negA, in_=negA, func=AF.Exp), writes=['negA'])
    S.op('dve', lambda e: e.tensor_scalar(out=negA, in0=negA, scalar1=-1.0, scalar2=None, op0=ALU.mult), writes=['negA'])
    xs = [[A.f32([128, 1536]) for _ in range(4)] for _ in range(2)]
    tmp = [A.f32([128, 1536]) for _ in range(4)]
    cc = A.f32([128, 1536])
    gt = [A.f32([128, 1544]) for _ in range(2)]
    sqc = A.f32([128, 1024])
    ssc = A.f32([128, 8])
    rsc = A.f32([128, 8])
    ba = [A.f32([128, 8]) for _ in range(2)]
    gx = A.f32([128, 4])
    for t in range(32):
        bi = t % 2
        for k in range(4):
            DMA('sp', xs[bi][k], proj[t * 128 + k:t * 128 + k + 128, 0:1536], w=['xs%d_%d' % (bi, k)])
        DMA('sp', ba[bi], proj[3 + t * 128:3 + (t + 1) * 128, 2048:2056], w=['ba%d' % bi])
        for k in range(4):
            S.op('pool' if k >= 2 else 'dve', lambda e, bi=bi, k=k: e.tensor_tensor(out=tmp[k], in0=xs[bi][k], in1=cw[:, k, :], op=ALU.mult),
                 reads=['xs%d_%d' % (bi, k), 'cw'], writes=['tmp%d' % k])
        S.op('dve', lambda e: e.tensor_tensor(out=cc, in0=tmp[0], in1=tmp[1], op=ALU.add), reads=['tmp0', 'tmp1'], writes=['cc'])
        S.op('dve', lambda e: e.tensor_tensor(out=cc, in0=cc, in1=tmp[2], op=ALU.add), reads=['tmp2'], writes=['cc'])
        S.op('dve', lambda e: e.tensor_tensor(out=cc, in0=cc, in1=tmp[3], op=ALU.add), reads=['tmp3'], writes=['cc'])
        S.op('act', lambda e: e.activation(out=cc, in_=cc, func=AF.Silu), writes=['cc'])
        g_t = gt[bi]
        kg = 'gt%d' % bi
        S.op('act', lambda e: e.activation(out=sqc, in_=cc[:, 0:1024], func=AF.Square), reads=['cc'], writes=['sqc'])
        S.op('dve', lambda e: e.tensor_reduce(out=ssc, in_=sqc.rearrange("p (h d) -> p h d", h=8), axis=AX.X, op=ALU.add),
             reads=['sqc'], writes=['ssc'])
        S.op('act', lambda e: e.activation(out=ssc, in_=ssc, func=AF.Sqrt, bias=EPS), writes=['ssc'])
        S.op('dve', lambda e: e.reciprocal(out=rsc, in_=ssc), reads=['ssc'], writes=['rsc'])
        S.op('dve', lambda e: e.tensor_scalar(out=rsc[:, 0:4], in0=rsc[:, 0:4], scalar1=128.0 ** -0.5, scalar2=None, op0=ALU.mult), writes=['rsc'])
        S.op('dve', lambda e, g_t=g_t: e.tensor_tensor(out=g_t[:, 0:1024].rearrange("p (h d) -> p h d", h=8),
                                                       in0=cc[:, 0:1024].rearrange("p (h d) -> p h d", h=8),
                                                       in1=rsc.unsqueeze(2).broadcast_to([128, 8, 128]), op=ALU.mult),
             reads=['cc', 'rsc'], writes=[kg])
        S.op('pool', lambda e, g_t=g_t: e.tensor_copy(out=g_t[:, 1024:1536], in_=cc[:, 1024:1536]), reads=['cc'], writes=[kg])
        S.op('act', lambda e, g_t=g_t, bi=bi: e.activation(out=g_t[:, 1536:1540], in_=ba[bi][:, 0:4], func=AF.Sigmoid),
             reads=['ba%d' % bi], writes=[kg])
        S.op('dve', lambda e, bi=bi: e.tensor_tensor(out=gx, in0=ba[bi][:, 4:8], in1=dtb, op=ALU.add), reads=['ba%d' % bi, 'dtb'], writes=['gx'])
        S.op('act', lambda e: e.activation(out=gx, in_=gx, func=AF.Exp), writes=['gx'])
        S.op('act', lambda e: e.activation(out=gx, in_=gx, func=AF.Ln, bias=1.0), writes=['gx'])
        S.op('dve', lambda e, g_t=g_t: e.tensor_tensor(out=g_t[:, 1540:1544], in0=gx, in1=negA, op=ALU.mult), reads=['gx', 'negA'], writes=[kg])
        DMA('pool', gdn_in[t], g_t, r=[kg], w=['gdn_in%d' % t])
    S.barrier()

    if stop_after == 'C':
        S.emit()
        return nc
    A.off = base0
    Sst = A.f32([128, 4, 128])
    S.op('pool', lambda e: e.memset(Sst, 0.0), writes=['Sst'])
    gti = [A.f32([128, 1544]) for _ in range(2)]
    names = ['gc', 'gb', 'mt', 'DT', 'DTs', 'egc', 'egl', 'kds', 'bws', 'kT', 'qgT', 'egb', 'ATn', 'inT', 'Am', 'AT', 'X2', 'XT2',
             'R0', 'R1', 'Bv', 'Bw', 'u', 'wT', 'vn', 'kdec', 'o', 'glast']
    W_ = {}
    for n_ in names:
        if n_ in ('gc', 'egc', 'egl', 'kds', 'bws', 'glast'):
            W_[n_] = A.f32([128, 4])
        else:
            W_[n_] = A.f32([128, 4, 128])
    identbc = ident.unsqueeze(1).broadcast_to([128, 4, 128])

    def P4(i):
        return PB(i).rearrange("p (h d) -> p h d", h=4)

    def evac(eng, out, pbi, w, extra_r=()):
        if eng == 'act':
            S.op('act', lambda e: e.activation(out=out, in_=P4(pbi), func=AF.Copy), reads=list(extra_r), writes=[pk(pbi), w])
        else:
            S.op('dve', lambda e: e.tensor_copy(out=out, in_=P4(pbi)), reads=list(extra_r), writes=[pk(pbi), w])

    for t in range(32):
        bi = t % 2
        G = gti[bi]
        kG = 'gti%d' % bi
        DMA('sp', G, gdn_in[t], w=[kG])
        issue_copies(3)
        qv = G[:, 0:512].rearrange("p (h d) -> p h d", h=4)
        kv_ = G[:, 512:1024].rearrange("p (h d) -> p h d", h=4)
        vv = G[:, 1024:1536].rearrange("p (h d) -> p h d", h=4)
        beta = G[:, 1536:1540]
        gg = G[:, 1540:1544]
        S.op('pe', lambda e, gg=gg: e.matmul(PB(0)[:, 0:4], lhsT=U_incl, rhs=gg, start=True, stop=True), reads=[kG, 'cst'], writes=[pk(0)])
        S.op('dve', lambda e: e.tensor_copy(out=W_['gc'], in_=PB(0)[:, 0:4]), writes=[pk(0), 'gc'])
        for h in range(4):
            S.op('dve', lambda e, h=h, gg=gg: e.tensor_scalar(out=W_['gb'][:, h, :], in0=ones, scalar1=gg[:, h:h + 1], scalar2=None, op0=ALU.mult),
                 reads=[kG, 'cst'], writes=['gb'])

        def mm_gb(e):
            for h in range(4):
                ins = e.matmul(P4(1)[:, h, :], lhsT=W_['gb'][:, h, :], rhs=U_incl, start=True, stop=True)
            return ins
        S.op('pe', mm_gb, reads=['gb', 'cst'], writes=[pk(1)])
        for h in range(4):
            S.op('dve', lambda e, h=h: e.tensor_scalar(out=W_['mt'][:, h, :], in0=P4(1)[:, h, :], scalar1=W_['gc'][:, h:h + 1], scalar2=0.0,
                                                       op0=ALU.subtract, op1=ALU.min), reads=['gc'], writes=[pk(1), 'mt'])
        S.op('act', lambda e: e.activation(out=W_['mt'], in_=W_['mt'], func=AF.Exp), writes=['mt'])
        S.op('pool', lambda e: e.tensor_tensor(out=W_['DT'], in0=W_['mt'], in1=U_incl.unsqueeze(1).broadcast_to([128, 4, 128]), op=ALU.mult),
             reads=['mt', 'cst'], writes=['DT'])
        S.op('pool', lambda e: e.tensor_tensor(out=W_['DTs'], in0=W_['mt'], in1=U_strict.unsqueeze(1).broadcast_to([128, 4, 128]), op=ALU.mult),
             reads=['mt', 'cst'], writes=['DTs'])
        if t >= 16:
            S.op('act', lambda e: e.activation(out=W_['egb'], in_=P4(1), func=AF.Exp), writes=[pk(1), 'egb'])
        S.op('dve', lambda e: e.tensor_copy(out=W_['glast'], in_=P4(1)[:, :, 127]), writes=[pk(1), 'glast'])
        S.op('act', lambda e: e.activation(out=W_['egc'], in_=W_['gc'], func=AF.Exp), reads=['gc'], writes=['egc'])
        S.op('act', lambda e: e.activation(out=W_['egl'], in_=W_['glast'], func=AF.Exp), reads=['glast'], writes=['egl'])
        S.op('dve', lambda e: e.tensor_tensor(out=W_['kds'], in0=W_['glast'], in1=W_['gc'], op=ALU.subtract), reads=['glast', 'gc'], writes=['kds'])
        S.op('act', lambda e: e.activation(out=W_['kds'], in_=W_['kds'], func=AF.Exp), writes=['kds'])
        S.op('dve', lambda e, beta=beta: e.tensor_tensor(out=W_['bws'], in0=beta, in1=W_['egc'], op=ALU.mult), reads=[kG, 'egc'], writes=['bws'])
        def tr_k(e, kv_=kv_):
            for h in range(4):
                ins = e.transpose(out=P4(2)[:, h, :], in_=kv_[:, h, :], identity=ident)
            return ins
        S.op('pe', tr_k, reads=[kG, 'cst'], writes=[pk(2)])
        evac('act', W_['kT'], 2, 'kT')

        if t >= 16:
            def tr_q(e, qv=qv):
                for h in range(4):
                    ins = e.transpose(out=P4(3)[:, h, :], in_=qv[:, h, :], identity=ident)
                return ins
            S.op('pe', tr_q, reads=[kG, 'cst'], writes=[pk(3)])
            qTd = W_['o']
            evac('dve', qTd, 3, 'o')
            S.op('dve', lambda e, qTd=qTd: e.tensor_tensor(out=W_['qgT'], in0=qTd, in1=W_['egb'], op=ALU.mult), reads=['o', 'egb'], writes=['qgT'])
        def mm_g(e):
            for h in range(4):
                ins = e.matmul(P4(4)[:, h, :], lhsT=W_['kT'][:, h, :], rhs=W_['kT'][:, h, :], start=True, stop=True)
            return ins
        S.op('pe', mm_g, reads=['kT'], writes=[pk(4)])

        if t >= 16:
            def mm_qk(e, qTd=qTd):
                for h in range(4):
                    ins = e.matmul(P4(5)[:, h, :], lhsT=W_['kT'][:, h, :], rhs=qTd[:, h, :], start=True, stop=True)
                return ins
            S.op('pe', mm_qk, reads=['kT', 'o'], writes=[pk(5)])
        S.op('dve', lambda e: e.tensor_tensor(out=W_['ATn'], in0=P4(4), in1=W_['DTs'], op=ALU.mult), reads=['DTs'], writes=[pk(4), 'ATn'])
        if t >= 16:
            S.op('dve', lambda e: e.tensor_tensor(out=W_['inT'], in0=P4(5), in1=W_['DT'], op=ALU.mult), reads=['DT'], writes=[pk(5), 'inT'])

        def tr_a(e):
            for h in range(4):
                ins = e.transpose(out=P4(6)[:, h, :], in_=W_['ATn'][:, h, :], identity=ident)
            return ins
        S.op('pe', tr_a, reads=['ATn', 'cst'], writes=[pk(6)])
        S.op('dve', lambda e, beta=beta: e.tensor_tensor(out=W_['Am'], in0=P4(6), in1=beta.unsqueeze(2).broadcast_to([128, 4, 128]), op=ALU.mult),
             reads=[kG], writes=[pk(6), 'Am'])

        def tr_at(e):
            for h in range(4):
                ins = e.transpose(out=P4(7)[:, h, :], in_=W_['Am'][:, h, :], identity=ident)
            return ins
        S.op('pe', tr_at, reads=['Am', 'cst'], writes=[pk(7)])
        evac('act', W_['AT'], 7, 'AT')
        S.op('dve', lambda e: e.tensor_tensor(out=W_['R0'], in0=identbc, in1=W_['AT'], op=ALU.subtract), reads=['AT', 'cst'], writes=['R0'])
        X, XT, kX, kXT = W_['Am'], W_['AT'], 'Am', 'AT'
        Xn, XTn, kXn, kXTn = W_['X2'], W_['XT2'], 'X2', 'XT2'
        Rc, Rn, kRc, kRn = W_['R0'], W_['R1'], 'R0', 'R1'
        for lvl in range(6):
            def mm_x2(e, X=X, XT=XT):
                for h in range(4):
                    ins = e.matmul(P4(2)[:, h, :], lhsT=XT[:, h, :], rhs=X[:, h, :], start=True, stop=True)
                return ins
            S.op('pe', mm_x2, reads=[kX, kXT], writes=[pk(2)])
            if lvl < 5:
                def mm_xt2(e, X=X, XT=XT):
                    for h in range(4):
                        ins = e.matmul(P4(3)[:, h, :], lhsT=X[:, h, :], rhs=XT[:, h, :], start=True, stop=True)
                    return ins
                S.op('pe', mm_xt2, reads=[kX, kXT], writes=[pk(3)])
            evac('act', Xn, 2, kXn)
            if lvl < 5:
                evac('dve', XTn, 3, kXTn)

            def mm_r(e, Xn=Xn, Rc=Rc):
                for h in range(4):
                    ins = e.matmul(P4(4)[:, h, :], lhsT=Xn[:, h, :], rhs=Rc[:, h, :], start=True, stop=True)
                return ins
            S.op('pe', mm_r, reads=[kXn, kRc], writes=[pk(4)])
            S.op('dve', lambda e, Rn=Rn, Rc=Rc: e.tensor_tensor(out=Rn, in0=P4(4), in1=Rc, op=ALU.add), reads=[kRc], writes=[pk(4), kRn])
            X, XT, kX, kXT, Xn, XTn, kXn, kXTn = Xn, XTn, kXn, kXTn, X, XT, kX, kXT
            Rc, Rn, kRc, kRn = Rn, Rc, kRn, kRc
        R, kR = Rc, kRc
        S.op('dve', lambda e, vv=vv, beta=beta: e.tensor_tensor(out=W_['Bv'], in0=vv, in1=beta.unsqueeze(2).broadcast_to([128, 4, 128]), op=ALU.mult),
             reads=[kG], writes=['Bv'])
        S.op('pool', lambda e, kv_=kv_: e.tensor_tensor(out=W_['Bw'], in0=kv_, in1=W_['bws'].unsqueeze(2).broadcast_to([128, 4, 128]), op=ALU.mult),
             reads=[kG, 'bws'], writes=['Bw'])
        S.op('pool', lambda e, kv_=kv_: e.tensor_tensor(out=W_['kdec'], in0=kv_, in1=W_['kds'].unsqueeze(2).broadcast_to([128, 4, 128]), op=ALU.mult),
             reads=[kG, 'kds'], writes=['kdec'])

        def mm_u(e, R=R):
            for h in range(4):
                ins = e.matmul(P4(5)[:, h, :], lhsT=R[:, h, :], rhs=W_['Bv'][:, h, :], start=True, stop=True)
            return ins
        S.op('pe', mm_u, reads=[kR, 'Bv'], writes=[pk(5)])
        evac('act', W_['u'], 5, 'u')

        def mm_w(e, R=R):
            for h in range(4):
                ins = e.matmul(P4(6)[:, h, :], lhsT=W_['Bw'][:, h, :], rhs=R[:, h, :], start=True, stop=True)
            return ins
        S.op('pe', mm_w, reads=[kR, 'Bw'], writes=[pk(6)])
        evac('dve', W_['wT'], 6, 'wT')
        def mm_ws(e):
            for h in range(4):
                ins = e.matmul(P4(7)[:, h, :], lhsT=W_['wT'][:, h, :], rhs=Sst[:, h, :], start=True, stop=True)
            return ins
        S.op('pe', mm_ws, reads=['wT', 'Sst'], writes=[pk(7)])
        S.op('dve', lambda e: e.tensor_tensor(out=W_['vn'], in0=W_['u'], in1=P4(7), op=ALU.subtract), reads=['u'], writes=[pk(7), 'vn'])
        if t >= 16:
            def mm_o(e):
                for h in range(4):
                    e.matmul(P4(0)[:, h, :], lhsT=W_['qgT'][:, h, :], rhs=Sst[:, h, :], start=True, stop=False)
                    ins = e.matmul(P4(0)[:, h, :], lhsT=W_['inT'][:, h, :], rhs=W_['vn'][:, h, :], start=False, stop=True)
                return ins
            S.op('pe', mm_o, reads=['qgT', 'Sst', 'inT', 'vn'], writes=[pk(0)])
            evac('act', W_['o'], 0, 'o')
            DMA('pool', o_gdn[(t - 16) * 128:(t - 15) * 128, :], W_['o'].rearrange("p h d -> p (h d)"), r=['o'], w=[])

        def mm_su(e):
            for h in range(4):
                ins = e.matmul(P4(1)[:, h, :], lhsT=W_['kdec'][:, h, :], rhs=W_['vn'][:, h, :], start=True, stop=True)
            return ins
        S.op('pe', mm_su, reads=['kdec', 'vn'], writes=[pk(1)])
        for h in range(4):
            S.op('dve', lambda e, h=h: e.scalar_tensor_tensor(out=Sst[:, h, :], in0=Sst[:, h, :], scalar=W_['egl'][:, h:h + 1], in1=P4(1)[:, h, :],
                                                              op0=ALU.mult, op1=ALU.add), reads=['egl'], writes=[pk(1), 'Sst'])
    issue_copies(1000)
    DMA('pool', sgp.rearrange("h k v -> k h v"), Sst, r=['Sst'], w=['sgp'])
    S.barrier()

    if stop_after == 'D':
        S.emit()
        return nc
    A.off = base0
    qkw = A.f32([128, 6, 64])
    for gi in range(3):
        DMA('sp', qkw[:, gi, :], qnw[gi:gi + 1, :].partition_broadcast(128), w=['qkw'])
        DMA('sp', qkw[:, 3 + gi, :], knw[gi:gi + 1, :].partition_broadcast(128), w=['qkw'])
    rin = [A.f32([128, 512]) for _ in range(3)]
    rsq = A.f32([128, 512])
    rss = A.f32([128, 8])
    rrs = A.f32([128, 8])
    rout = [A.f32([128, 512]) for _ in range(3)]
    rp = [A.f32([128, 16]) for _ in range(2)]
    rt = [A.f32([128, 8, 8]) for _ in range(4)]
    cnt = 0
    for gi in range(3):
        W, dil = GROUPS[gi]
        cq = 2056 + gi * 1536
        first_k_tile = 16 - W // 128
        for t in range(first_k_tile, 32):
            DMA('sp', rp[t % 2], rope_t[t * 128:(t + 1) * 128, :], w=['rp%d' % (t % 2)])
            cosb = rp[t % 2][:, 0:8].unsqueeze(1).broadcast_to([128, 8, 8])
            sinb = rp[t % 2][:, 8:16].unsqueeze(1).broadcast_to([128, 8, 8])
            for which in ((1, 0) if t >= 16 else (1,)):
                bi = cnt % 3
                cnt += 1
                src = rin[bi]
                dst = rout[bi]
                ks, kd = 'rin%d' % bi, 'rout%d' % bi
                c0 = cq + which * 512
                DMA('sp', src, proj[3 + t * 128:3 + (t + 1) * 128, c0:c0 + 512], w=[ks])
                S.op('act', lambda e, src=src: e.activation(out=rsq, in_=src, func=AF.Square), reads=[ks], writes=['rsq'])
                S.op('dve', lambda e: e.tensor_reduce(out=rss, in_=rsq.rearrange("p (h d) -> p h d", h=8), axis=AX.X, op=ALU.add),
                     reads=['rsq'], writes=['rss'])
                S.op('act', lambda e: e.activation(out=rss, in_=rss, func=AF.Sqrt, scale=1.0 / 64, bias=EPS), writes=['rss'])
                S.op('dve', lambda e: e.reciprocal(out=rrs, in_=rss), reads=['rss'], writes=['rrs'])
                d3 = dst.rearrange("p (h d) -> p h d", h=8)
                S.op('dve', lambda e, src=src, d3=d3: e.tensor_tensor(out=d3, in0=src.rearrange("p (h d) -> p h d", h=8),
                                                                      in1=rrs.unsqueeze(2).broadcast_to([128, 8, 64]), op=ALU.mult),
                     reads=[ks, 'rrs'], writes=[kd])
                wrow = qkw[:, (3 if which == 1 else 0) + gi, :].unsqueeze(1).broadcast_to([128, 8, 64])
                S.op('pool', lambda e, d3=d3, wrow=wrow: e.tensor_tensor(out=d3, in0=d3, in1=wrow, op=ALU.mult), reads=['qkw'], writes=[kd])
                x1 = d3[:, :, 0:8]
                x2 = d3[:, :, 8:16]
                S.op('dve', lambda e, x1=x1, cosb=cosb: e.tensor_tensor(out=rt[0], in0=x1, in1=cosb, op=ALU.mult), reads=[kd, 'rp%d' % (t % 2)], writes=['rt0'])
                S.op('dve', lambda e, x2=x2, sinb=sinb: e.tensor_tensor(out=rt[1], in0=x2, in1=sinb, op=ALU.mult), reads=[kd, 'rp%d' % (t % 2)], writes=['rt1'])
                S.op('dve', lambda e, x2=x2, cosb=cosb: e.tensor_tensor(out=rt[2], in0=x2, in1=cosb, op=ALU.mult), reads=[kd, 'rp%d' % (t % 2)], writes=['rt2'])
                S.op('dve', lambda e, x1=x1, sinb=sinb: e.tensor_tensor(out=rt[3], in0=x1, in1=sinb, op=ALU.mult), reads=[kd, 'rp%d' % (t % 2)], writes=['rt3'])
                S.op('dve', lambda e, x1=x1: e.tensor_tensor(out=x1, in0=rt[0], in1=rt[1], op=ALU.subtract), reads=['rt0', 'rt1'], writes=[kd])
                S.op('dve', lambda e, x2=x2: e.tensor_tensor(out=x2, in0=rt[2], in1=rt[3], op=ALU.add), reads=['rt2', 'rt3'], writes=[kd])
                if which == 1:
                    DMA('pool', kn_s[gi, t * 128:(t + 1) * 128, :], dst, r=[kd], w=[])
                    if t * 128 >= 4096 - W:
                        r0 = t * 128 - (4096 - W)
                        DMA('pool', kvp[gi][r0:r0 + 128, 0, :], dst, r=[kd], w=[])
                        DMA('pool', kvp[gi][r0:r0 + 128, 1, :], proj[3 + t * 128:3 + (t + 1) * 128, cq + 1024:cq + 1536], w=[])
                else:
                    DMA('pool', qn_s[gi, (t - 16) * 128:(t - 15) * 128, :], dst, r=[kd], w=[])
    S.barrier()

    if stop_after == 'E':
        S.emit()
        return nc
    A.off = base0
    fin = [A.f32([128, 512]) for _ in range(4)]
    fb = [A.bf16([128, 512]) for _ in range(2)]
    qT = A.bf16([128, 4, 128])
    kTs = [A.bf16([128, 4, 128]) for _ in range(2)]
    Vs = [A.bf16([128, 8, 66]) for _ in range(2)]
    for i in range(2):
        S.op('pool', lambda e, i=i: e.memset(Vs[i], 1.0), writes=['Vs%d' % i])
    Pm = [A.bf16([128, 8, 128]) for _ in range(2)]
    Ex = A.bf16([128, 4, 128])
    Osb = A.f32([128, 8, 65])
    fc = 0
    for gi in range(3):
        W, dil = GROUPS[gi]
        nblk = 4096 // dil // 128
        first_q = 2048 // dil // 128
        cv = 2056 + gi * 1536 + 1024
        for r in range(dil):
            for mb in range(first_q - 1, nblk):
                slot = mb % 2
                tok0 = r + dil * mb * 128
                rows = slice(tok0, tok0 + dil * 127 + 1, dil)
                prow = slice(3 + tok0, 3 + tok0 + dil * 127 + 1, dil)
                f1 = fin[fc % 4]; k1 = 'fin%d' % (fc % 4); fc += 1
                DMA('sp', f1, kn_s[gi, rows, :], w=[k1])
                S.op('act', lambda e, f1=f1: e.activation(out=fb[0], in_=f1, func=AF.Copy), reads=[k1], writes=['fb0'])

                def tr1(e):
                    for hp in range(4):
                        ins = e.transpose(out=PBb(0)[:, hp * 128:(hp + 1) * 128], in_=fb[0][:, hp * 128:(hp + 1) * 128], identity=identb)
                    return ins
                S.op('pe', tr1, reads=['fb0', 'identb'], writes=[pk(0)])
                S.op('dve', lambda e, slot=slot: e.tensor_copy(out=kTs[slot], in_=PBb(0)[:, 0:512].rearrange("p (a b) -> p a b", a=4)),
                     writes=[pk(0), 'kT%d' % slot])
                f2 = fin[fc % 4]; k2 = 'fin%d' % (fc % 4); fc += 1
                DMA('sp', f2, proj[prow, cv:cv + 512], w=[k2])
                S.op('pool', lambda e, f2=f2, slot=slot: e.tensor_copy(out=Vs[slot][:, :, 0:64], in_=f2.rearrange("p (h d) -> p h d", h=8)),
                     reads=[k2], writes=['Vs%d' % slot])
                if mb < first_q:
                    continue
                f3 = fin[fc % 4]; k3 = 'fin%d' % (fc % 4); fc += 1
                qrow = slice(tok0 - 2048, tok0 - 2048 + dil * 127 + 1, dil)
                DMA('sp', f3, qn_s[gi, qrow, :], w=[k3])
                S.op('act', lambda e, f3=f3: e.activation(out=fb[1], in_=f3, func=AF.Copy), reads=[k3], writes=['fb1'])

                def tr2(e):
                    for hp in range(4):
                        ins = e.transpose(out=PBb(1)[:, hp * 128:(hp + 1) * 128], in_=fb[1][:, hp * 128:(hp + 1) * 128], identity=identb)
                    return ins
                S.op('pe', tr2, reads=['fb1', 'identb'], writes=[pk(1)])
                S.op('dve', lambda e: e.tensor_copy(out=qT, in_=PBb(1)[:, 0:512].rearrange("p (a b) -> p a b", a=4)), writes=[pk(1), 'qT'])
                halo_prev = (mb == first_q)
                for which, sl in ((0, 1 - slot), (1, slot)):
                    for par in range(2):
                        pbi = 2 + par

                        def mm_s(e, sl=sl, par=par, pbi=pbi):
                            for hp in range(4):
                                lo = par * 64
                                ins = e.matmul(PB(pbi)[:, hp * 128:(hp + 1) * 128], lhsT=kTs[sl][lo:lo + 64, hp, :], rhs=qT[lo:lo + 64, hp, :],
                                               start=True, stop=True)
                            return ins
                        S.op('pe', mm_s, reads=['kT%d' % sl, 'qT'], writes=[pk(pbi)])
                        if which == 0 and halo_prev:
                            S.op('act', lambda e, pbi=pbi: e.activation(out=Ex, in_=P4(pbi), func=AF.Exp, scale=0.125, bias=maskb_t),
                                 reads=['maskb'], writes=[pk(pbi), 'Ex'])
                        else:
                            S.op('act', lambda e, pbi=pbi: e.activation(out=Ex, in_=P4(pbi), func=AF.Exp, scale=0.125), writes=[pk(pbi), 'Ex'])
                        msk = (L_incl if which == 0 else U_incl).unsqueeze(1).broadcast_to([128, 4, 128])
                        S.op('dve', lambda e, which=which, par=par, msk=msk: e.tensor_tensor(out=Pm[which][:, par:8:2, :], in0=Ex, in1=msk, op=ALU.mult),
                             reads=['Ex', 'cst'], writes=['Pm%d_%d' % (which, par)])
                for half in range(2):
                    pbi = 4 + half

                    def mm_pv(e, half=half, pbi=pbi, slot=slot):
                        for hh in range(4):
                            h = half * 4 + hh
                            e.matmul(PB(pbi)[:, hh * 65:(hh + 1) * 65], lhsT=Pm[0][:, h, :], rhs=Vs[1 - slot][:, h, 0:65], start=True, stop=False)
                            ins = e.matmul(PB(pbi)[:, hh * 65:(hh + 1) * 65], lhsT=Pm[1][:, h, :], rhs=Vs[slot][:, h, 0:65], start=False, stop=True)
                        return ins
                    S.op('pe', mm_pv, reads=['Pm0_0', 'Pm0_1', 'Pm1_0', 'Pm1_1', 'Vs0', 'Vs1'], writes=[pk(pbi)])
                    S.op('act', lambda e, half=half, pbi=pbi: e.activation(out=Osb[:, half * 4:(half + 1) * 4, :],
                                                                           in_=PB(pbi)[:, 0:260].rearrange("p (h d) -> p h d", h=4), func=AF.Copy),
                         writes=[pk(pbi), 'Osb%d' % half])
                DMA('pool', att_o[gi, qrow, :], Osb.rearrange("p h d -> p (h d)"), r=['Osb0', 'Osb1'], w=[])
    S.barrier()

    if stop_after == 'F':
        S.emit()
        return nc
    A.off = base0
    gnb = A.f32([128, 128])
    DMA('sp', gnb, gnw.partition_broadcast(128), w=['gnb'])
    g_o = [A.f32([128, 512]) for _ in range(2)]
    g_z = [A.f32([128, 512]) for _ in range(2)]
    g_a = [[A.f32([128, 520]) for _ in range(3)] for _ in range(2)]
    mixt = [A.f32([128, D]) for _ in range(2)]
    gsq = A.f32([128, 512])
    gss = A.f32([128, 4])
    grs = A.f32([128, 4])
    gden = A.f32([128, 8])
    for t in range(16):
        bi = t % 2
        mx = mixt[bi]
        km = 'mixt%d' % bi
        DMA('sp', g_o[bi], o_gdn[t * 128:(t + 1) * 128, :], w=['g_o%d' % bi])
        DMA('sp', g_z[bi], proj[3 + (16 + t) * 128:3 + (17 + t) * 128, 1536:2048], w=['g_z%d' % bi])
        for gi in range(3):
            DMA('sp', g_a[bi][gi], att_o[gi, t * 128:(t + 1) * 128, :], w=['g_a%d_%d' % (bi, gi)])
        S.op('act', lambda e, bi=bi: e.activation(out=gsq, in_=g_o[bi], func=AF.Square), reads=['g_o%d' % bi], writes=['gsq'])
        S.op('dve', lambda e: e.tensor_reduce(out=gss, in_=gsq.rearrange("p (h d) -> p h d", h=4), axis=AX.X, op=ALU.add), reads=['gsq'], writes=['gss'])
        S.op('act', lambda e: e.activation(out=gss, in_=gss, func=AF.Sqrt, scale=1.0 / 128, bias=EPS), writes=['gss'])
        S.op('dve', lambda e: e.reciprocal(out=grs, in_=gss), reads=['gss'], writes=['grs'])
        m3 = mx[:, 0:512].rearrange("p (h d) -> p h d", h=4)
        S.op('dve', lambda e, bi=bi, m3=m3: e.tensor_tensor(out=m3, in0=g_o[bi].rearrange("p (h d) -> p h d", h=4),
                                                            in1=grs.unsqueeze(2).broadcast_to([128, 4, 128]), op=ALU.mult),
             reads=['g_o%d' % bi, 'grs'], writes=[km])
        S.op('pool', lambda e, m3=m3: e.tensor_tensor(out=m3, in0=m3, in1=gnb.unsqueeze(1).broadcast_to([128, 4, 128]), op=ALU.mult), reads=['gnb'], writes=[km])
        S.op('act', lambda e, bi=bi: e.activation(out=g_z[bi], in_=g_z[bi], func=AF.Silu), writes=['g_z%d' % bi])
        S.op('dve', lambda e, bi=bi, mx=mx: e.tensor_tensor(out=mx[:, 0:512], in0=mx[:, 0:512], in1=g_z[bi], op=ALU.mult), reads=['g_z%d' % bi], writes=[km])
        S.op('pool', lambda e, bi=bi: e.tensor_tensor(out=g_a[bi][0], in0=g_a[bi][0], in1=g_a[bi][1], op=ALU.add), reads=['g_a%d_1' % bi], writes=['g_a%d_0' % bi])
        S.op('pool', lambda e, bi=bi: e.tensor_tensor(out=g_a[bi][0], in0=g_a[bi][0], in1=g_a[bi][2], op=ALU.add), reads=['g_a%d_2' % bi], writes=['g_a%d_0' % bi])
        a3 = g_a[bi][0].rearrange("p (h d) -> p h d", h=8)
        S.op('dve', lambda e, a3=a3: e.reciprocal(out=gden, in_=a3[:, :, 64]), reads=['g_a%d_0' % bi], writes=['gden'])
        S.op('dve', lambda e, a3=a3, mx=mx: e.tensor_tensor(out=mx[:, 512:1024].rearrange("p (h d) -> p h d", h=8), in0=a3[:, :, 0:64],
                                                            in1=gden.unsqueeze(2).broadcast_to([128, 8, 64]), op=ALU.mult),
             reads=['g_a%d_0' % bi, 'gden'], writes=[km])
        DMA('pool', mix_d[t * 128:(t + 1) * 128, :], mx, r=[km], w=[])
    S.barrier()

    if stop_after == 'G1':
        S.emit()
        return nc
    sample_path(nc, S, A, base0, locals())

    A.off = base0
    yacc = A.f32([128, 17, D])
    h2T = A.bf16([128, 8, 17 * 128])
    gates = A.f32([128, 17, 32])
    base1 = A.off
    wob = A.bf16([128, 8, D])
    wos = A.f32([128, 8, 512])
    for hf in range(2):
        DMA('sp', wos, w_out[:, hf * 512:(hf + 1) * 512].rearrange("(k p) c -> p k c", p=128), w=['wos'])
        S.op('pool', lambda e, hf=hf: e.tensor_copy(out=wob[:, :, hf * 512:(hf + 1) * 512], in_=wos), reads=['wos'], writes=['wob'])
    wrt = A.f32([128, 8, 36])
    DMA('sp', wrt, w_r.rearrange("(k p) c -> p k c", p=128), w=['wrt'])
    w2b = A.f32([128, D])
    DMA('sp', w2b, norm2.partition_broadcast(128), w=['w2b'])
    mxl = [A.f32([128, D]) for _ in range(2)]
    xl = [A.f32([128, D]) for _ in range(2)]
    mxb = A.bf16([128, D])
    mxT = A.bf16([128, 8, 128])
    hsq = A.f32([128, D])
    hss = A.f32([128, 1])
    hrs = A.f32([128, 1])
    h2n = A.f32([128, D])
    h2Tf = A.f32([128, 8, 128])
    lg = A.f32([128, 36])
    r_ = {n_: A.f32([128, 8]) for n_ in ['gm', 'oh', 'eg', 'sg', 'pg', 'ein', 'm1', 'oh1', 'e2', 'm2', 'oh2', 'w1', 'w2', 'g8', 'tmp']}
    sel = A.f32([128, 4, 8])
    for t in range(17):
        bi = t % 2
        xrow = (16 + t) * 128 if t < 16 else 32 * 128
        DMA('sp', mxl[bi], mix_d[t * 128:(t + 1) * 128, :], w=['mxl%d' % bi])
        DMA('sp', xl[bi], x_all[xrow:xrow + 128, :], w=['xl%d' % bi])
        S.op('act', lambda e, bi=bi: e.activation(out=mxb, in_=mxl[bi], func=AF.Copy), reads=['mxl%d' % bi], writes=['mxb'])

        def trm(e):
            for k in range(8):
                ins = e.transpose(out=PBb(0)[:, k * 128:(k + 1) * 128], in_=mxb[:, k * 128:(k + 1) * 128], identity=identb)
            return ins
        S.op('pe', trm, reads=['mxb', 'identb'], writes=[pk(0)])
        S.op('dve', lambda e: e.tensor_copy(out=mxT, in_=PBb(0).rearrange("p (k t) -> p k t", k=8)), writes=[pk(0), 'mxT'])
        for hf in range(2):
            def mmo(e, hf=hf):
                for k in range(8):
                    ins = e.matmul(PB(1 + hf), lhsT=mxT[:, k, :], rhs=wob[:, k, hf * 512:(hf + 1) * 512], start=(k == 0), stop=(k == 7))
                return ins
            S.op('pe', mmo, reads=['mxT', 'wob'], writes=[pk(1 + hf)])
            S.op('dve', lambda e, hf=hf, t=t, bi=bi: e.tensor_tensor(out=yacc[:, t, hf * 512:(hf + 1) * 512], in0=PB(1 + hf), in1=xl[bi][:, hf * 512:(hf + 1) * 512], op=ALU.add),
                 reads=['xl%d' % bi], writes=[pk(1 + hf), 'yacc%d' % t])
        S.op('act', lambda e, t=t: e.activation(out=hsq, in_=yacc[:, t, :], func=AF.Square, accum_out=hss), reads=['yacc%d' % t], writes=['hsq', 'hss'])
        S.op('act', lambda e: e.activation(out=hss, in_=hss, func=AF.Sqrt, scale=1.0 / D, bias=EPS), writes=['hss'])
        S.op('dve', lambda e: e.reciprocal(out=hrs, in_=hss), reads=['hss'], writes=['hrs'])
        S.op('dve', lambda e, t=t: e.scalar_tensor_tensor(out=h2n, in0=yacc[:, t, :], scalar=hrs, in1=w2b, op0=ALU.mult, op1=ALU.mult),
             reads=['yacc%d' % t, 'hrs', 'w2b'], writes=['h2n'])
        for hf in range(2):
            def trh(e, hf=hf):
                for k in range(4):
                    kk = hf * 4 + k
                    ins = e.transpose(out=PB(3 + hf)[:, k * 128:(k + 1) * 128], in_=h2n[:, kk * 128:(kk + 1) * 128], identity=ident)
                return ins
            S.op('pe', trh, reads=['h2n', 'cst'], writes=[pk(3 + hf)])
            S.op('act', lambda e, hf=hf, t=t: e.activation(out=h2T[:, hf * 4:(hf + 1) * 4, t * 128:(t + 1) * 128], in_=P4(3 + hf), func=AF.Copy),
                 writes=[pk(3 + hf), 'h2T%d' % t])
            S.op('dve', lambda e, hf=hf: e.tensor_copy(out=h2Tf[:, hf * 4:(hf + 1) * 4, :], in_=P4(3 + hf)), writes=[pk(3 + hf), 'h2Tf'])

        def mmr(e):
            for k in range(8):
                ins = e.matmul(PB(5)[:, 0:36], lhsT=h2Tf[:, k, :], rhs=wrt[:, k, :], start=(k == 0), stop=(k == 7))
            return ins
        S.op('pe', mmr, reads=['h2Tf', 'wrt'], writes=[pk(5)])
        S.op('dve', lambda e: e.tensor_copy(out=lg, in_=PB(5)[:, 0:36]), writes=[pk(5), 'lg'])
        R_ = r_
        lgg = lg[:, 0:4]
        le = lg[:, 4:36].rearrange("p (g e) -> p g e", g=4)

        def V(e_, fn, r, w):
            S.op(e_, fn, reads=r, writes=w)
        V('dve', lambda e: e.tensor_reduce(out=R_['gm'][:, 0:1], in_=lgg, axis=AX.X, op=ALU.max), ['lg'], ['gm'])
        V('dve', lambda e: e.tensor_scalar(out=R_['oh'][:, 0:4], in0=lgg, scalar1=R_['gm'][:, 0:1], scalar2=None, op0=ALU.is_equal), ['lg', 'gm'], ['oh'])
        V('dve', lambda e: e.tensor_scalar(out=R_['eg'][:, 0:4], in0=lgg, scalar1=R_['gm'][:, 0:1], scalar2=None, op0=ALU.subtract), ['lg', 'gm'], ['eg'])
        V('act', lambda e: e.activation(out=R_['eg'][:, 0:4], in_=R_['eg'][:, 0:4], func=AF.Exp), [], ['eg'])
        V('dve', lambda e: e.tensor_reduce(out=R_['sg'][:, 0:1], in_=R_['eg'][:, 0:4], axis=AX.X, op=ALU.add), ['eg'], ['sg'])
        V('dve', lambda e: e.reciprocal(out=R_['pg'][:, 0:1], in_=R_['sg'][:, 0:1]), ['sg'], ['pg'])
        V('dve', lambda e: e.tensor_tensor(out=sel, in0=le, in1=R_['oh'][:, 0:4].unsqueeze(2).broadcast_to([128, 4, 8]), op=ALU.mult), ['lg', 'oh'], ['sel'])
        V('dve', lambda e: e.tensor_reduce(out=R_['ein'], in_=sel.rearrange("p g e -> p e g"), axis=AX.X, op=ALU.add), ['sel'], ['ein'])
        V('dve', lambda e: e.tensor_reduce(out=R_['m1'][:, 0:1], in_=R_['ein'], axis=AX.X, op=ALU.max), ['ein'], ['m1'])
        V('dve', lambda e: e.tensor_scalar(out=R_['oh1'], in0=R_['ein'], scalar1=R_['m1'][:, 0:1], scalar2=None, op0=ALU.is_equal), ['ein', 'm1'], ['oh1'])
        V('dve', lambda e: e.scalar_tensor_tensor(out=R_['e2'], in0=R_['oh1'], scalar=-1e30, in1=R_['ein'], op0=ALU.mult, op1=ALU.add), ['oh1', 'ein'], ['e2'])
        V('dve', lambda e: e.tensor_reduce(out=R_['m2'][:, 0:1], in_=R_['e2'], axis=AX.X, op=ALU.max), ['e2'], ['m2'])
        V('dve', lambda e: e.tensor_scalar(out=R_['oh2'], in0=R_['e2'], scalar1=R_['m2'][:, 0:1], scalar2=None, op0=ALU.is_equal), ['e2', 'm2'], ['oh2'])
        V('dve', lambda e: e.tensor_tensor(out=R_['w1'][:, 0:1], in0=R_['m2'][:, 0:1], in1=R_['m1'][:, 0:1], op=ALU.subtract), ['m1', 'm2'], ['w1'])
        V('act', lambda e: e.activation(out=R_['w1'][:, 0:1], in_=R_['w1'][:, 0:1], func=AF.Exp), [], ['w1'])
        V('dve', lambda e: e.tensor_scalar(out=R_['w1'][:, 0:1], in0=R_['w1'][:, 0:1], scalar1=1.0, scalar2=None, op0=ALU.add), [], ['w1'])
        V('dve', lambda e: e.reciprocal(out=R_['w1'][:, 0:1], in_=R_['w1'][:, 0:1]), [], ['w1'])
        V('dve', lambda e: e.tensor_tensor(out=R_['w1'][:, 0:1], in0=R_['w1'][:, 0:1], in1=R_['pg'][:, 0:1], op=ALU.mult), ['pg'], ['w1'])
        V('dve', lambda e: e.tensor_tensor(out=R_['w2'][:, 0:1], in0=R_['pg'][:, 0:1], in1=R_['w1'][:, 0:1], op=ALU.subtract), ['pg', 'w1'], ['w2'])
        V('dve', lambda e: e.tensor_scalar(out=R_['g8'], in0=R_['oh1'], scalar1=R_['w1'][:, 0:1], scalar2=None, op0=ALU.mult), ['oh1', 'w1'], ['g8'])
        V('dve', lambda e: e.scalar_tensor_tensor(out=R_['g8'], in0=R_['oh2'], scalar=R_['w2'][:, 0:1], in1=R_['g8'], op0=ALU.mult, op1=ALU.add), ['oh2', 'w2'], ['g8'])
        V('dve', lambda e, t=t: e.tensor_tensor(out=gates[:, t, :].rearrange("p (g e) -> p g e", g=4),
                                                in0=R_['oh'][:, 0:4].unsqueeze(2).broadcast_to([128, 4, 8]),
                                                in1=R_['g8'].unsqueeze(1).broadcast_to([128, 4, 8]), op=ALU.mult), ['oh', 'g8'], ['gates%d' % t])
    S.barrier()

    if stop_after == 'G2':
        S.emit()
        return nc
    A.off = base1
    gus = [A.f32([128, 8, 512]) for _ in range(2)]
    gub = [A.bf16([128, 8, 512]) for _ in range(2)]
    wds = [A.f32([128, 2, D]) for _ in range(2)]
    wdb = [A.bf16([128, 2, D]) for _ in range(2)]
    sgt = A.f32([128, 2, 512])
    hid = A.bf16([128, 2, 512])
    blocks = [(0, 512), (512, 512), (1024, 512), (1536, 512), (2048, 128)]
    for ex in range(32):
        wi = ex % 2
        DMA('sp', gus[wi], w_gu[ex].rearrange("(k p) c -> p k c", p=128), w=['gus%d' % wi])
        DMA('sp', wds[wi], w_d[ex].rearrange("(k p) c -> p k c", p=128), w=['wds%d' % wi])
        S.op('pool', lambda e, wi=wi: e.tensor_copy(out=gub[wi], in_=gus[wi]), reads=['gus%d' % wi], writes=['gub%d' % wi])
        S.op('pool', lambda e, wi=wi: e.tensor_copy(out=wdb[wi], in_=wds[wi]), reads=['wds%d' % wi], writes=['wdb%d' % wi])
        for (t0, nt) in blocks:
            for c in range(4):
                def mmg(e, c=c, wi=wi, t0=t0, nt=nt):
                    for k in range(8):
                        ins = e.matmul(PB(c)[:, 0:nt], lhsT=gub[wi][:, k, c * 128:(c + 1) * 128], rhs=h2T[:, k, t0:t0 + nt], start=(k == 0), stop=(k == 7))
                    return ins
                S.op('pe', mmg, reads=['gub%d' % wi] + ['h2T%d' % tt for tt in range(t0 // 128, (t0 + nt) // 128)], writes=[pk(c)])
            for c in range(2):
                S.op('act', lambda e, c=c, nt=nt: e.activation(out=sgt[:, c, 0:nt], in_=PB(c)[:, 0:nt], func=AF.Silu), writes=[pk(c), 'sgt%d' % c])
                S.op('dve', lambda e, c=c, nt=nt: e.tensor_tensor(out=hid[:, c, 0:nt], in0=sgt[:, c, 0:nt], in1=PB(2 + c)[:, 0:nt], op=ALU.mult),
                     reads=['sgt%d' % c], writes=[pk(2 + c), 'hid%d' % c])
            for ti in range(nt // 128):
                t = t0 // 128 + ti
                pby = 4 + 2 * (ti % 2)

                def mmd(e, ti=ti, wi=wi, pby=pby):
                    for hf in range(2):
                        for c in range(2):
                            ins = e.matmul(PB(pby + hf), lhsT=hid[:, c, ti * 128:(ti + 1) * 128], rhs=wdb[wi][:, c, hf * 512:(hf + 1) * 512],
                                           start=(c == 0), stop=(c == 1))
                    return ins
                S.op('pe', mmd, reads=['hid0', 'hid1', 'wdb%d' % wi], writes=[pk(pby), pk(pby + 1)])
                S.op('dve', lambda e, t=t, ex=ex, pby=pby: e.scalar_tensor_tensor(out=yacc[:, t, :], in0=ps_all[:, pby * 512:(pby + 2) * 512],
                                                                                 scalar=gates[:, t, ex:ex + 1], in1=yacc[:, t, :], op0=ALU.mult, op1=ALU.add),
                     reads=['gates%d' % t], writes=[pk(pby), pk(pby + 1), 'yacc%d' % t])
    for t in range(16):
        DMA('sp', y_own[t * 128:(t + 1) * 128, :], yacc[:, t, :], r=['yacc%d' % t], w=[])
    DMA('sp', y_samp, yacc[:, 16, :], r=['yacc16'], w=['y_samp'])
    S.emit()
    return nc


def sample_path(nc, S, A, base0, L):
    g_ = lambda n: L[n]
    proj, mix_d, zero_t, st_gdn, st_conv, caches = g_('proj'), g_('mix_d'), g_('zero_t'), g_('st_gdn'), g_('st_conv'), g_('caches')
    conv_wT, a_log, dt_bias, gnw, qnw, knw, rope_t, selh = g_('conv_wT'), g_('a_log'), g_('dt_bias'), g_('gnw'), g_('qnw'), g_('knw'), g_('rope_t'), g_('selh')
    sgs, kvs = g_('sgs'), g_('kvs')
    ident, ones, PB, pk, ps_all = g_('ident'), g_('ones'), g_('PB'), g_('pk'), g_('ps_all')
    NS = 16
    R0 = 3 + 4096

    def DMA(eng, out, in_, r=(), w=()):
        S.op(eng, lambda e: e.dma_start(out=out, in_=in_), reads=r, writes=w, dma=True)
    A.off = base0
    id16 = ident[0:NS, 0:NS]
    cw = A.f32([NS, 4, 1536])
    for k in range(4):
        DMA('sp', cw[:, k, :], conv_wT[k:k + 1, :].partition_broadcast(NS), w=['s_cw'])
    full = A.f32([NS, 4, 1536])
    DMA('sp', full[:, 0:3, :], st_conv, w=['s_full'])
    DMA('sp', full[:, 3, :], proj[R0:R0 + NS, 0:1536], w=['s_full'])
    zs = A.f32([NS, 512])
    DMA('sp', zs, proj[R0:R0 + NS, 1536:2048], w=['s_z'])
    ba = A.f32([NS, 8])
    DMA('sp', ba, proj[R0:R0 + NS, 2048:2056], w=['s_ba'])
    negA = A.f32([NS, 4])
    dtb = A.f32([NS, 4])
    DMA('sp', negA, a_log.partition_broadcast(NS), w=['s_negA'])
    DMA('sp', dtb, dt_bias.partition_broadcast(NS), w=['s_dtb'])
    gnb = A.f32([NS, 128])
    DMA('sp', gnb, gnw.partition_broadcast(NS), w=['s_gnb'])
    S.op('act', lambda e: e.activation(out=negA, in_=negA, func=AF.Exp), writes=['s_negA'])
    S.op('dve', lambda e: e.tensor_scalar(out=negA, in0=negA, scalar1=-1.0, scalar2=None, op0=ALU.mult), writes=['s_negA'])
    S.op('dve', lambda e: e.tensor_tensor(out=full, in0=full, in1=cw, op=ALU.mult), reads=['s_cw'], writes=['s_full'])
    cc = A.f32([NS, 1536])
    S.op('dve', lambda e: e.tensor_tensor(out=cc, in0=full[:, 0, :], in1=full[:, 1, :], op=ALU.add), reads=['s_full'], writes=['s_cc'])
    S.op('dve', lambda e: e.tensor_tensor(out=cc, in0=cc, in1=full[:, 2, :], op=ALU.add), reads=['s_full'], writes=['s_cc'])
    S.op('dve', lambda e: e.tensor_tensor(out=cc, in0=cc, in1=full[:, 3, :], op=ALU.add), reads=['s_full'], writes=['s_cc'])
    S.op('act', lambda e: e.activation(out=cc, in_=cc, func=AF.Silu), writes=['s_cc'])
    sq = A.f32([NS, 1024])
    ss8 = A.f32([NS, 8])
    rs8 = A.f32([NS, 8])
    qk = A.f32([NS, 8, 128])
    S.op('act', lambda e: e.activation(out=sq, in_=cc[:, 0:1024], func=AF.Square), reads=['s_cc'], writes=['s_sq'])
    S.op('dve', lambda e: e.tensor_reduce(out=ss8, in_=sq.rearrange("p (h d) -> p h d", h=8), axis=AX.X, op=ALU.add), reads=['s_sq'], writes=['s_ss8'])
    S.op('act', lambda e: e.activation(out=ss8, in_=ss8, func=AF.Sqrt, bias=EPS), writes=['s_ss8'])
    S.op('dve', lambda e: e.reciprocal(out=rs8, in_=ss8), reads=['s_ss8'], writes=['s_rs8'])
    S.op('dve', lambda e: e.tensor_scalar(out=rs8[:, 0:4], in0=rs8[:, 0:4], scalar1=128.0 ** -0.5, scalar2=None, op0=ALU.mult), writes=['s_rs8'])
    S.op('dve', lambda e: e.tensor_tensor(out=qk, in0=cc[:, 0:1024].rearrange("p (h d) -> p h d", h=8), in1=rs8.unsqueeze(2).broadcast_to([NS, 8, 128]), op=ALU.mult),
         reads=['s_cc', 's_rs8'], writes=['s_qk'])
    vv = cc[:, 1024:1536].rearrange("p (h d) -> p h d", h=4)
    beta = A.f32([NS, 4])
    gx = A.f32([NS, 4])
    eg = A.f32([NS, 4])
    S.op('act', lambda e: e.activation(out=beta, in_=ba[:, 0:4], func=AF.Sigmoid), reads=['s_ba'], writes=['s_beta'])
    S.op('dve', lambda e: e.tensor_tensor(out=gx, in0=ba[:, 4:8], in1=dtb, op=ALU.add), reads=['s_ba', 's_dtb'], writes=['s_gx'])
    S.op('act', lambda e: e.activation(out=gx, in_=gx, func=AF.Exp), writes=['s_gx'])
    S.op('act', lambda e: e.activation(out=gx, in_=gx, func=AF.Ln, bias=1.0), writes=['s_gx'])
    S.op('dve', lambda e: e.tensor_tensor(out=gx, in0=gx, in1=negA, op=ALU.mult), reads=['s_negA'], writes=['s_gx'])
    S.op('act', lambda e: e.activation(out=eg, in_=gx, func=AF.Exp), reads=['s_gx'], writes=['s_eg'])
    kqT = A.f32([128, 8, NS])

    def trkq(e):
        for j in range(8):
            ins = e.transpose(out=PB(0)[:, j * NS:(j + 1) * NS], in_=qk[:, j, :], identity=id16)
        return ins
    S.op('pe', trkq, reads=['s_qk', 'cst'], writes=[pk(0)])
    S.op('dve', lambda e: e.tensor_copy(out=kqT, in_=PB(0)[:, 0:8 * NS].rearrange("p (j s) -> p j s", j=8)), writes=[pk(0), 's_kqT'])
    egd = A.f32([NS, NS, 4])
    S.op('dve', lambda e: e.tensor_tensor(out=egd, in0=id16.unsqueeze(2).broadcast_to([NS, NS, 4]), in1=eg.unsqueeze(1).broadcast_to([NS, NS, 4]), op=ALU.mult),
         reads=['cst', 's_eg'], writes=['s_egd'])
    S.op('pe', lambda e: e.matmul(PB(1)[:, 0:NS * 4], lhsT=ones[0:NS, :], rhs=egd.rearrange("p s h -> p (s h)"), start=True, stop=True),
         reads=['s_egd', 'cst'], writes=[pk(1)])
    egb = A.f32([128, NS, 4])
    S.op('dve', lambda e: e.tensor_copy(out=egb, in_=PB(1)[:, 0:NS * 4].rearrange("p (s h) -> p s h", s=NS)), writes=[pk(1), 's_egb'])
    Sold = [A.f32([128, 4, 128]) for _ in range(3)]
    Snew = [A.f32([128, 4, 128]) for _ in range(3)]
    kSacc = A.f32([NS, 4, 128])
    oacc = A.f32([NS, 4, 128])
    S.op('pool', lambda e: e.memset(kSacc, 0.0), writes=['s_kSacc'])
    S.op('pool', lambda e: e.memset(oacc, 0.0), writes=['s_oacc'])
    for s_ in range(NS):
        bi = s_ % 3
        DMA('sp', Sold[bi], st_gdn[s_].rearrange("h k v -> k h v"), w=['s_Sold%d' % bi])

        def mm1(e, bi=bi):
            for h in range(4):
                ins = e.matmul(PB(2)[0:NS, h * 128:(h + 1) * 128], lhsT=kqT[:, 4 + h, :], rhs=Sold[bi][:, h, :], start=True, stop=True)
            return ins
        S.op('pe', mm1, reads=['s_kqT', 's_Sold%d' % bi], writes=[pk(2)])
        S.op('dve', lambda e, s_=s_: e.scalar_tensor_tensor(out=kSacc.rearrange("p h d -> p (h d)"), in0=PB(2)[0:NS, :], scalar=id16[:, s_:s_ + 1],
                                                            in1=kSacc.rearrange("p h d -> p (h d)"), op0=ALU.mult, op1=ALU.add),
             reads=['cst'], writes=[pk(2), 's_kSacc'])
    vn = A.f32([NS, 4, 128])
    S.op('dve', lambda e: e.tensor_tensor(out=vn, in0=kSacc, in1=eg.unsqueeze(2).broadcast_to([NS, 4, 128]), op=ALU.mult), reads=['s_kSacc', 's_eg'], writes=['s_vn'])
    S.op('dve', lambda e: e.tensor_tensor(out=vn, in0=vv, in1=vn, op=ALU.subtract), reads=['s_cc'], writes=['s_vn'])
    S.op('dve', lambda e: e.tensor_tensor(out=vn, in0=vn, in1=beta.unsqueeze(2).broadcast_to([NS, 4, 128]), op=ALU.mult), reads=['s_beta'], writes=['s_vn'])
    vm = [A.f32([NS, 4, 128]) for _ in range(2)]
    stmp = [A.f32([128, 4, 128]) for _ in range(2)]
    for s_ in range(NS):
        bi = s_ % 3
        b2 = s_ % 2
        DMA('sp', Sold[bi], st_gdn[s_].rearrange("h k v -> k h v"), w=['s_Sold%d' % bi])
        S.op('dve', lambda e, s_=s_, b2=b2: e.tensor_scalar(out=vm[b2], in0=vn, scalar1=id16[:, s_:s_ + 1], scalar2=None, op0=ALU.mult),
             reads=['s_vn', 'cst'], writes=['s_vm%d' % b2])

        def mm2(e, b2=b2):
            for h in range(4):
                ins = e.matmul(PB(3)[:, h * 128:(h + 1) * 128], lhsT=qk[:, 4 + h, :], rhs=vm[b2][:, h, :], start=True, stop=True)
            return ins
        S.op('pe', mm2, reads=['s_qk', 's_vm%d' % b2], writes=[pk(3)])
        S.op('pool', lambda e, bi=bi, b2=b2, s_=s_: e.tensor_tensor(out=stmp[b2], in0=Sold[bi], in1=egb[:, s_, :].unsqueeze(2).broadcast_to([128, 4, 128]), op=ALU.mult),
             reads=['s_Sold%d' % bi, 's_egb'], writes=['s_stmp%d' % b2])
        S.op('dve', lambda e, bi=bi, b2=b2: e.tensor_tensor(out=Snew[bi], in0=stmp[b2], in1=PB(3).rearrange("p (h d) -> p h d", h=4), op=ALU.add),
             reads=['s_stmp%d' % b2], writes=[pk(3), 's_Snew%d' % bi])
        DMA('pool', sgs[s_].rearrange("h k v -> k h v"), Snew[bi], r=['s_Snew%d' % bi], w=[])

        def mm3(e, bi=bi):
            for h in range(4):
                ins = e.matmul(PB(4)[0:NS, h * 128:(h + 1) * 128], lhsT=kqT[:, h, :], rhs=Snew[bi][:, h, :], start=True, stop=True)
            return ins
        S.op('pe', mm3, reads=['s_kqT', 's_Snew%d' % bi], writes=[pk(4)])
        S.op('dve', lambda e, s_=s_: e.scalar_tensor_tensor(out=oacc.rearrange("p h d -> p (h d)"), in0=PB(4)[0:NS, :], scalar=id16[:, s_:s_ + 1],
                                                            in1=oacc.rearrange("p h d -> p (h d)"), op0=ALU.mult, op1=ALU.add),
             reads=['cst'], writes=[pk(4), 's_oacc'])
    mixs = A.f32([NS, 1024])
    o4 = A.f32([NS, 4])
    S.op('act', lambda e: e.activation(out=sq[:, 0:512], in_=oacc.rearrange("p h d -> p (h d)"), func=AF.Square), reads=['s_oacc'], writes=['s_sq'])
    S.op('dve', lambda e: e.tensor_reduce(out=o4, in_=sq[:, 0:512].rearrange("p (h d) -> p h d", h=4), axis=AX.X, op=ALU.add), reads=['s_sq'], writes=['s_o4'])
    S.op('act', lambda e: e.activation(out=o4, in_=o4, func=AF.Sqrt, scale=1.0 / 128, bias=EPS), writes=['s_o4'])
    S.op('dve', lambda e: e.reciprocal(out=o4, in_=o4), writes=['s_o4'])
    m3 = mixs[:, 0:512].rearrange("p (h d) -> p h d", h=4)
    S.op('dve', lambda e: e.tensor_tensor(out=m3, in0=oacc, in1=o4.unsqueeze(2).broadcast_to([NS, 4, 128]), op=ALU.mult), reads=['s_oacc', 's_o4'], writes=['s_mix'])
    S.op('dve', lambda e: e.tensor_tensor(out=m3, in0=m3, in1=gnb.unsqueeze(1).broadcast_to([NS, 4, 128]), op=ALU.mult), reads=['s_gnb'], writes=['s_mix'])
    S.op('act', lambda e: e.activation(out=zs, in_=zs, func=AF.Silu), writes=['s_z'])
    S.op('dve', lambda e: e.tensor_tensor(out=mixs[:, 0:512], in0=mixs[:, 0:512], in1=zs, op=ALU.mult), reads=['s_z'], writes=['s_mix'])
    qkw = A.f32([NS, 6, 64])
    for gi in range(3):
        DMA('sp', qkw[:, gi, :], qnw[gi:gi + 1, :].partition_broadcast(NS), w=['s_qkw'])
        DMA('sp', qkw[:, 3 + gi, :], knw[gi:gi + 1, :].partition_broadcast(NS), w=['s_qkw'])
    rp = A.f32([NS, 16])
    DMA('sp', rp, rope_t[4096:4096 + NS, :], w=['s_rp'])
    cosb = rp[:, 0:8].unsqueeze(1).broadcast_to([NS, 8, 8])
    sinb = rp[:, 8:16].unsqueeze(1).broadcast_to([NS, 8, 8])
    qkv = A.f32([NS, 3, 3, 512])
    DMA('sp', qkv.rearrange("p g w c -> p (g w c)"), proj[R0:R0 + NS, 2056:2056 + 4608], w=['s_qkv'])
    s2 = A.f32([NS, 512])
    r8 = A.f32([NS, 8])
    rt = [A.f32([NS, 8, 8]) for _ in range(4)]
    for gi in range(3):
        for which in range(2):
            d3 = qkv[:, gi, which, :].rearrange("p (h d) -> p h d", h=8)
            S.op('act', lambda e, gi=gi, which=which: e.activation(out=s2, in_=qkv[:, gi, which, :], func=AF.Square), reads=['s_qkv'], writes=['s_s2'])
            S.op('dve', lambda e: e.tensor_reduce(out=r8, in_=s2.rearrange("p (h d) -> p h d", h=8), axis=AX.X, op=ALU.add), reads=['s_s2'], writes=['s_r8'])
            S.op('act', lambda e: e.activation(out=r8, in_=r8, func=AF.Sqrt, scale=1.0 / 64, bias=EPS), writes=['s_r8'])
            S.op('dve', lambda e: e.reciprocal(out=r8, in_=r8), writes=['s_r8'])
            S.op('dve', lambda e, d3=d3: e.tensor_tensor(out=d3, in0=d3, in1=r8.unsqueeze(2).broadcast_to([NS, 8, 64]), op=ALU.mult), reads=['s_r8'], writes=['s_qkv'])
            wrow = qkw[:, (3 if which == 1 else 0) + gi, :].unsqueeze(1).broadcast_to([NS, 8, 64])
            S.op('dve', lambda e, d3=d3, wrow=wrow: e.tensor_tensor(out=d3, in0=d3, in1=wrow, op=ALU.mult), reads=['s_qkw'], writes=['s_qkv'])
            x1 = d3[:, :, 0:8]
            x2 = d3[:, :, 8:16]
            S.op('dve', lambda e, x1=x1: e.tensor_tensor(out=rt[0], in0=x1, in1=cosb, op=ALU.mult), reads=['s_qkv', 's_rp'], writes=['s_rt0'])
            S.op('dve', lambda e, x2=x2: e.tensor_tensor(out=rt[1], in0=x2, in1=sinb, op=ALU.mult), reads=['s_qkv', 's_rp'], writes=['s_rt1'])
            S.op('dve', lambda e, x2=x2: e.tensor_tensor(out=rt[2], in0=x2, in1=cosb, op=ALU.mult), reads=['s_qkv', 's_rp'], writes=['s_rt2'])
            S.op('dve', lambda e, x1=x1: e.tensor_tensor(out=rt[3], in0=x1, in1=sinb, op=ALU.mult), reads=['s_qkv', 's_rp'], writes=['s_rt3'])
            S.op('dve', lambda e, x1=x1: e.tensor_tensor(out=x1, in0=rt[0], in1=rt[1], op=ALU.subtract), reads=['s_rt0', 's_rt1'], writes=['s_qkv'])
            S.op('dve', lambda e, x2=x2: e.tensor_tensor(out=x2, in0=rt[2], in1=rt[3], op=ALU.add), reads=['s_rt2', 's_rt3'], writes=['s_qkv'])
        W = GROUPS[gi][0]
        DMA('pool', kvs[gi][:, W - 1, :], qkv[:, gi, 1:3, :].rearrange("p w c -> p (w c)"), r=['s_qkv'], w=['kvs_new%d' % gi])
    selq = A.f32([NS, NS, 128])
    S.op('dve', lambda e: e.tensor_copy(out=selq, in_=id16.unsqueeze(2).broadcast_to([NS, NS, 128])), reads=['cst'], writes=['s_selq'])
    selh_t = A.f32([8, NS, NS])
    DMA('sp', selh_t, selh, w=['s_selh'])
    kvt = [A.f32([128, 1024]) for _ in range(3)]
    prod = A.f32([128, 512])
    sc = A.f32([128, 8])
    pex = A.f32([128, 8])
    Z = A.f32([8, 8, 64])
    Z2 = A.f32([8, 8])
    numS = A.f32([NS, 8, 64])
    denS = A.f32([NS, 8])
    ps_ = A.f32([NS, 8])
    pr16 = A.f32([NS, 8, 64])
    S.op('pool', lambda e: e.memset(numS, 0.0), writes=['s_numS'])
    S.op('pool', lambda e: e.memset(denS, 0.0), writes=['s_denS'])
    cnt = 0
    for gi in range(3):
        W, dil = GROUPS[gi]
        q3 = qkv[:, gi, 0, :].rearrange("p (h d) -> p h d", h=8)
        k3 = qkv[:, gi, 1, :].rearrange("p (h d) -> p h d", h=8)
        v3 = qkv[:, gi, 2, :].rearrange("p (h d) -> p h d", h=8)
        S.op('dve', lambda e, q3=q3, k3=k3: e.tensor_tensor(out=pr16, in0=q3, in1=k3, op=ALU.mult), reads=['s_qkv'], writes=['s_pr16'])
        S.op('dve', lambda e: e.tensor_reduce(out=ps_, in_=pr16, axis=AX.X, op=ALU.add), reads=['s_pr16'], writes=['s_ps'])
        S.op('act', lambda e: e.activation(out=ps_, in_=ps_, func=AF.Exp, scale=0.125), writes=['s_ps'])
        S.op('dve', lambda e: e.tensor_tensor(out=denS, in0=denS, in1=ps_, op=ALU.add), reads=['s_ps'], writes=['s_denS'])
        S.op('dve', lambda e, v3=v3: e.tensor_tensor(out=pr16, in0=v3, in1=ps_.unsqueeze(2).broadcast_to([NS, 8, 64]), op=ALU.mult), reads=['s_qkv', 's_ps'], writes=['s_pr16'])
        S.op('dve', lambda e: e.tensor_tensor(out=numS, in0=numS, in1=pr16, op=ALU.add), reads=['s_pr16'], writes=['s_numS'])
        for s_ in range(NS):
            bi = cnt % 3
            first = (cnt == 0)
            last = (cnt == 3 * NS - 1)
            cnt += 1
            KV = kvt[bi]
            kkv = 's_kvt%d' % bi
            DMA('sp', KV, caches[gi][s_, 0:W - dil + 1:dil, :], w=[kkv])
            S.op('pe', lambda e, s_=s_, gi=gi: e.matmul(PB(0), lhsT=selq[:, s_, :], rhs=qkv[:, gi, 0, :], start=True, stop=True), reads=['s_selq', 's_qkv'], writes=[pk(0)])
            S.op('dve', lambda e, KV=KV: e.tensor_tensor(out=prod, in0=KV[:, 0:512], in1=PB(0), op=ALU.mult), reads=[kkv], writes=[pk(0), 's_prod'])
            S.op('dve', lambda e: e.tensor_reduce(out=sc, in_=prod.rearrange("p (h d) -> p h d", h=8), axis=AX.X, op=ALU.add), reads=['s_prod'], writes=['s_sc'])
            S.op('act', lambda e: e.activation(out=pex, in_=sc, func=AF.Exp, scale=0.125), reads=['s_sc'], writes=['s_pex'])
            S.op('pe', lambda e, KV=KV: e.matmul(PB(1)[0:8, :], lhsT=pex, rhs=KV[:, 512:1024], start=True, stop=True), reads=['s_pex', kkv], writes=[pk(1)])
            S.op('pe', lambda e: e.matmul(PB(2)[0:8, 0:8], lhsT=pex, rhs=ones[:, 0:8], start=True, stop=True), reads=['s_pex', 'cst'], writes=[pk(2)])
            S.op('dve', lambda e: e.tensor_tensor(out=Z, in0=PB(1)[0:8, :].rearrange("p (h d) -> p h d", h=8), in1=ident[0:8, 0:8].unsqueeze(2).broadcast_to([8, 8, 64]), op=ALU.mult),
                 reads=['cst'], writes=[pk(1), 's_Z'])
            S.op('dve', lambda e: e.tensor_tensor(out=Z2, in0=PB(2)[0:8, 0:8], in1=ident[0:8, 0:8], op=ALU.mult), reads=['cst'], writes=[pk(2), 's_Z2'])
            S.op('pe', lambda e, s_=s_, first=first, last=last: e.matmul(PB(6)[0:NS, :], lhsT=selh_t[:, s_, :], rhs=Z.rearrange("p h d -> p (h d)"), start=first, stop=last),
                 reads=['s_selh', 's_Z'], writes=[pk(6)])
            S.op('pe', lambda e, s_=s_, first=first, last=last: e.matmul(PB(7)[0:NS, 0:8], lhsT=selh_t[:, s_, :], rhs=Z2, start=first, stop=last),
                 reads=['s_selh', 's_Z2'], writes=[pk(7)])
    S.op('dve', lambda e: e.tensor_tensor(out=numS, in0=numS, in1=PB(6)[0:NS, :].rearrange("p (h d) -> p h d", h=8), op=ALU.add), writes=[pk(6), 's_numS'])
    S.op('dve', lambda e: e.tensor_tensor(out=denS, in0=denS, in1=PB(7)[0:NS, 0:8], op=ALU.add), writes=[pk(7), 's_denS'])
    S.op('dve', lambda e: e.reciprocal(out=denS, in_=denS), writes=['s_denS'])
    S.op('dve', lambda e: e.tensor_tensor(out=mixs[:, 512:1024].rearrange("p (h d) -> p h d", h=8), in0=numS, in1=denS.unsqueeze(2).broadcast_to([NS, 8, 64]), op=ALU.mult),
         reads=['s_numS', 's_denS'], writes=['s_mix'])
    DMA('pool', mix_d[2048:2048 + NS, :], mixs, r=['s_mix'], w=['mix_d_s'])
    for j in range(2):
        DMA('pool', mix_d[2048 + NS:2176, j * 512:(j + 1) * 512], zero_t[0:128 - NS, 0:512], r=['zero'], w=['mix_d_z%d' % j])
    S.barrier()


_CACHE = {}


def _host_consts():
    c = np.zeros((128, 5, 128), np.float32)
    p = np.arange(128)[:, None]
    f = np.arange(128)[None, :]
    c[:, 0] = (p == f)
    c[:, 1] = (p <= f)
    c[:, 2] = (p >= f)
    c[:, 3] = (p < f)
    c[:, 4] = 1.0
    return c


def _rope_table(pos):
    half = 8
    inv = np.exp(-math.log(500000.0) * np.arange(half, dtype=np.float32) * np.float32(2.0 / 16)).astype(np.float32)
    ang = pos.astype(np.float32)[:, None] * inv[None, :]
    return np.concatenate([np.cos(ang), np.sin(ang)], axis=1).astype(np.float32)


def kernel(x_prompt, x_sample, state_gdn, state_conv, cache_kv_w128, cache_kv_w512, cache_kv_w2048,
           norm1_w, w_in, conv_w, a_log, dt_bias, gdn_norm_w, q_norm_w, k_norm_w, w_out, norm2_w,
           w_router_group, w_router_expert, w_gate_up, w_down):
    f = lambda a: np.ascontiguousarray(np.asarray(a, dtype=np.float32))
    x_prompt, x_sample = f(x_prompt), f(x_sample)
    if 'nc' not in _CACHE:
        _CACHE['nc'] = build_program()
    nc = _CACHE['nc']
    consts = _host_consts()
    w_r = np.concatenate([f(w_router_group)[0], f(w_router_expert)[0]], axis=1)
    shared = {
        "w_in": f(w_in)[0], "w_out": f(w_out)[0], "norm1": f(norm1_w), "norm2": f(norm2_w),
        "conv_wT": np.ascontiguousarray(f(conv_w)[0].T), "a_log": f(a_log), "dt_bias": f(dt_bias), "gnw": f(gdn_norm_w),
        "qnw": f(q_norm_w)[0], "knw": f(k_norm_w)[0], "w_r": np.ascontiguousarray(w_r), "w_gu": f(w_gate_up)[0], "w_d": f(w_down)[0],
        "consts": consts,
        "selh": np.ascontiguousarray(np.broadcast_to(np.eye(16, dtype=np.float32)[None], (8, 16, 16))),
    }
    cachesf = [f(cache_kv_w128)[0], f(cache_kv_w512)[0], f(cache_kv_w2048)[0]]
    sg, sc = f(state_gdn)[0], f(state_conv)[0]
    in_maps = []
    for c in range(NCORE):
        b, half = c // 2, c % 2
        xa = np.zeros((NTILE * 128, D), np.float32)
        if half == 1:
            xa[0:2048] = x_prompt[b, 0:2048]
        xa[2048:4096] = x_prompt[b, half * 2048:(half + 1) * 2048]
        xa[4096:4112] = x_sample[16 * c:16 * c + 16, 0]
        pos = np.concatenate([np.arange(4096) + (half - 1) * 2048, np.full(128, 8192)]).astype(np.float32)
        m = dict(shared)
        m["x_all"] = xa
        m["rope_t"] = _rope_table(pos)
        m["maskb"] = np.full((128, 1), 0.0 if half == 1 else -30000.0, np.float32)
        m["st_gdn"] = np.ascontiguousarray(sg[16 * c:16 * c + 16])
        m["st_conv"] = np.ascontiguousarray(sc[16 * c:16 * c + 16])
        for i in range(3):
            m["cache%d" % i] = np.ascontiguousarray(cachesf[i][16 * c:16 * c + 16].reshape(16, GROUPS[i][0], 1024))
        in_maps.append(m)
    if _CACHE.get('only_maps'):
        return in_maps
    res = run_bass_kernel_spmd(nc, in_maps, core_ids=list(range(NCORE)))
    R = res.results
    y_prompt = np.zeros((4, 4096, D), np.float32)
    for c in range(NCORE):
        y_prompt[c // 2, (c % 2) * 2048:(c % 2 + 1) * 2048] = R[c]["y_own"]
    y_sample = np.concatenate([R[c]["y_samp"][0:16] for c in range(NCORE)], axis=0).reshape(128, 1, D)
    sgp_o = np.stack([R[2 * b + 1]["sgp"] for b in range(4)])[None]
    scp_o = np.stack([R[2 * b + 1]["scp"] for b in range(4)])[None]
    kvp_o = [np.stack([R[2 * b + 1]["kvp%d" % i] for b in range(4)]).reshape(1, 4, GROUPS[i][0], 2, 8, 64) for i in range(3)]
    sgs_o = np.concatenate([R[c]["sgs"] for c in range(NCORE)], axis=0)[None]
    scs_o = np.concatenate([R[c]["scs"] for c in range(NCORE)], axis=0)[None]
    kvs_o = [np.concatenate([R[c]["kvs%d" % i] for c in range(NCORE)], axis=0).reshape(1, 128, GROUPS[i][0], 2, 8, 64) for i in range(3)]
    return (y_prompt, y_sample, sgp_o, scp_o, kvp_o[0], kvp_o[1], kvp_o[2], sgs_o, scs_o, kvs_o[0], kvs_o[1], kvs_o[2])
```

```python
import math
import numpy as np
import concourse.bass as bass
import concourse.mybir as mybir
from concourse.bass_utils import run_bass_kernel_spmd

F32 = mybir.dt.float32
BF16 = mybir.dt.bfloat16
AF = mybir.ActivationFunctionType
ALU = mybir.AluOpType
AX = mybir.AxisListType

NDMA = 32
NCORE = 8
D = 1024
INC = 6664
NTILE = 33
EPS = 1e-6
GROUPS = ((128, 1), (512, 4), (2048, 16))
COLBLK = [(0, 512), (512, 512), (1024, 512), (1536, 512), (2048, 8)]
for _g in range(3):
    _b = 2056 + _g * 1536
    COLBLK += [(_b, 512), (_b + 512, 512), (_b + 1024, 512)]
HALO_SKIP = {3, 5, 8, 11}


class Sched:
    ENG = ['pe', 'act', 'dve', 'pool', 'sp']

    def __init__(self, nc):
        self.nc = nc
        self.ops = {e: [] for e in self.ENG}
        self.esem = {e: nc.alloc_semaphore(name="s_" + e) for e in self.ENG}
        self.ecnt = {e: 0 for e in self.ENG}
        self.dpool = {'sp': list(range(0, 12)), 'pool': list(range(12, 20)), 'act': list(range(20, 24)), 'spc': list(range(24, 32))}
        self.dsem = [nc.alloc_semaphore(name="d%d" % i) for i in range(NDMA)]
        self.dcnt = [0] * NDMA
        self.dnext = {'sp': 0, 'pool': 0, 'act': 0, 'spc': 0}
        self.last_w = {}
        self.readers = {}
        self.waited = {e: {} for e in self.ENG}
        self.defer = None

    def _tok_waits(self, e, deps):
        waits = []
        for (sem, val) in deps:
            sid = id(sem)
            if e == 'pe' and sem is self.esem['pe']:
                continue
            if self.waited[e].get(sid, 0) < val:
                self.waited[e][sid] = val
                waits.append((sem, val))
        return waits

    def op(self, e, fn, reads=(), writes=(), dma=False, pool=None):
        if self.defer is not None:
            self.defer.append((e, fn, tuple(reads), tuple(writes), dma, pool))
            return None
        deps = []
        for k in reads:
            if k in self.last_w:
                deps.append(self.last_w[k])
        for k in writes:
            if k in self.last_w:
                deps.append(self.last_w[k])
            for sid, tk in self.readers.get(k, {}).items():
                deps.append(tk)
        if dma:
            pn = pool or e
            pl = self.dpool[pn]
            s = pl[self.dnext[pn] % len(pl)]
            self.dnext[pn] += 1
            if self.dcnt[s] > 0:
                deps.append((self.dsem[s], 16 * self.dcnt[s]))
            self.dcnt[s] += 1
            tok = (self.dsem[s], 16 * self.dcnt[s])
            inc = 16
        else:
            self.ecnt[e] += 1
            tok = (self.esem[e], self.ecnt[e])
            inc = 1
        waits = self._tok_waits(e, deps)
        for k in writes:
            self.last_w[k] = tok
            self.readers[k] = {}
        for k in reads:
            r = self.readers.setdefault(k, {})
            sid = id(tok[0])
            if sid not in r or r[sid][1] < tok[1]:
                r[sid] = tok
        self.ops[e].append((fn, waits, (tok[0], inc)))
        return tok

    def replay(self, lists, weights):
        assert self.defer is None
        pos = [0] * len(lists)
        while any(pos[i] < len(lists[i]) for i in range(len(lists))):
            for i, lst in enumerate(lists):
                for _ in range(weights[i]):
                    if pos[i] < len(lst):
                        self.op(*lst[pos[i]])
                        pos[i] += 1

    def barrier(self):
        toks = [(self.esem[e], self.ecnt[e]) for e in self.ENG if self.ecnt[e] > 0]
        toks += [(self.dsem[s], 16 * self.dcnt[s]) for s in range(NDMA) if self.dcnt[s] > 0]
        for e in self.ENG:
            waits = self._tok_waits(e, [t for t in toks if not (t[0] is self.esem[e])])
            if waits:
                self.ops[e].append((None, waits, None))
        self.last_w = {}
        self.readers = {}

    def emit(self):
        nc = self.nc
        self.barrier()
        with nc.Block() as block:
            def mk(e):
                def body(engine):
                    for (fn, waits, inc) in self.ops[e]:
                        for (sem, val) in waits:
                            engine.wait_ge(sem, val)
                        if fn is not None:
                            ins = fn(engine)
                            ins.then_inc(inc[0], inc[1])
                return body
            block.tensor(mk('pe'))
            block.scalar(mk('act'))
            block.vector(mk('dve'))
            block.gpsimd(mk('pool'))
            block.sync(mk('sp'))


def _shape_view(v, shape):
    if len(shape) == 2:
        return v
    if len(shape) == 3:
        return v.rearrange("p (a b) -> p a b", a=shape[1])
    return v.rearrange("p (a b c) -> p a b c", a=shape[1], b=shape[2])


class Arena:
    def __init__(self, ap, nwords):
        self.ap = ap
        self.n = nwords
        self.off = 0

    def f32(self, shape):
        n = int(np.prod(shape[1:]))
        o = self.off
        self.off += n
        assert self.off <= self.n, ("arena overflow", self.off, self.n)
        return _shape_view(self.ap[0:shape[0], o:o + n], shape)

    def bf16(self, shape):
        n = int(np.prod(shape[1:]))
        nw = (n + 1) // 2
        o = self.off
        self.off += nw
        assert self.off <= self.n, ("arena overflow", self.off, self.n)
        return _shape_view(self.ap[0:shape[0], o:o + nw].bitcast(BF16)[:, 0:n], shape)


def build_program(stop_after=None, debug=False):
    nc = bass.Bass("TRN2", target_bir_lowering=False)

    def din(name, shape):
        return nc.dram_tensor(name, list(shape), F32, kind="ExternalInput").ap()

    def dout(name, shape):
        return nc.dram_tensor(name, list(shape), F32, kind="ExternalOutput").ap()

    def dscr(name, shape):
        return nc.dram_tensor(name, list(shape), F32, kind=("ExternalOutput" if debug else "Internal")).ap()

    x_all = din("x_all", [NTILE * 128, D])
    w_in = din("w_in", [D, INC])
    w_out = din("w_out", [D, D])
    norm1 = din("norm1", [1, D])
    norm2 = din("norm2", [1, D])
    conv_wT = din("conv_wT", [4, 1536])
    a_log = din("a_log", [1, 4])
    dt_bias = din("dt_bias", [1, 4])
    gnw = din("gnw", [1, 128])
    qnw = din("qnw", [3, 64])
    knw = din("knw", [3, 64])
    w_r = din("w_r", [D, 36])
    w_gu = din("w_gu", [32, D, 512])
    w_d = din("w_d", [32, 256, D])
    consts = din("consts", [128, 5, 128])
    rope_t = din("rope_t", [NTILE * 128, 16])
    maskb = din("maskb", [128, 1])
    selh = din("selh", [8, 16, 16])
    st_gdn = din("st_gdn", [16, 4, 128, 128])
    st_conv = din("st_conv", [16, 3, 1536])
    caches = [din("cache%d" % i, [16, GROUPS[i][0], 1024]) for i in range(3)]

    y_own = dout("y_own", [2048, D])
    y_samp = dout("y_samp", [128, D])
    sgp = dout("sgp", [4, 128, 128])
    scp = dout("scp", [3, 1536])
    kvp = [dout("kvp%d" % i, [GROUPS[i][0], 2, 512]) for i in range(3)]
    sgs = dout("sgs", [16, 4, 128, 128])
    scs = dout("scs", [16, 3, 1536])
    kvs = [dout("kvs%d" % i, [16, GROUPS[i][0], 1024]) for i in range(3)]

    proj = dscr("proj", [3 + NTILE * 128, INC])
    gdn_in = dscr("gdn_in", [32, 128, 1544])
    o_gdn = dscr("o_gdn", [2048, 512])
    kn_s = dscr("kn_s", [3, 4096, 512])
    qn_s = dscr("qn_s", [3, 2048, 512])
    att_o = dscr("att_o", [3, 2048, 520])
    mix_d = dscr("mix_d", [17 * 128, D])

    S = Sched(nc)
    AW = 48500
    sb_all = nc.alloc_sbuf_tensor("arena", [128, AW], F32).ap()
    ps_all = nc.alloc_psum_tensor("psarena", [128, 4096], F32).ap()
    A = Arena(sb_all, AW)

    def PB(i):
        return ps_all[:, i * 512:(i + 1) * 512]

    def PBb(i):
        return ps_all[:, i * 512:(i + 1) * 512].bitcast(BF16)

    def pk(i):
        return 'PS%d' % i

    def DMA(eng, out, in_, r=(), w=()):
        S.op(eng, lambda e: e.dma_start(out=out, in_=in_), reads=r, writes=w, dma=True)

    cst = A.f32([128, 5, 128])
    DMA('sp', cst, consts, w=['cst'])
    ident = cst[:, 0, :]
    U_incl = cst[:, 1, :]
    L_incl = cst[:, 2, :]
    U_strict = cst[:, 3, :]
    ones = cst[:, 4, :]
    identb = A.bf16([128, 128])
    S.op('dve', lambda e: e.tensor_copy(out=identb, in_=ident), reads=['cst'], writes=['identb'])
    maskb_t = A.f32([128, 1])
    DMA('sp', maskb_t, maskb, w=['maskb'])
    zero_t = A.f32([128, 1024])
    S.op('pool', lambda e: e.memset(zero_t, 0.0), writes=['zero'])
    base0 = A.off

    copy_jobs = []
    for s_ in range(16):
        for ch in range(4):
            r0 = ch * 512
            r1 = min(r0 + 512, 2047)
            copy_jobs.append((2, s_, r0, r1))
        copy_jobs.append((1, s_, 0, 511))
        copy_jobs.append((0, s_, 0, 127))

    def issue_copies(n):
        for _ in range(n):
            if not copy_jobs:
                return
            gi, s_, r0, r1 = copy_jobs.pop(0)
            S.op('sp', lambda e, gi=gi, s_=s_, r0=r0, r1=r1: e.dma_start(out=kvs[gi][s_, r0:r1, :], in_=caches[gi][s_, r0 + 1:r1 + 1, :]),
                 writes=[], dma=True, pool='spc')
    S.op('act', lambda e: e.dma_start(out=scs[:, 0:2, :], in_=st_conv[:, 1:3, :]), writes=['scs01'], dma=True)

    xnT = A.bf16([128, 8, NTILE * 128])
    w1b = A.f32([128, D])
    DMA('sp', w1b, norm1.partition_broadcast(128), w=['w1b'])
    xt = [A.f32([128, D]) for _ in range(2)]
    sq = A.f32([128, D])
    ss = A.f32([128, 1])
    rstd = A.f32([128, 1])
    xn = A.bf16([128, D])
    for t in range(NTILE):
        xx = xt[t % 2]
        kx = 'xt%d' % (t % 2)
        DMA('sp', xx, x_all[t * 128:(t + 1) * 128, :], w=[kx])
        S.op('act', lambda e, xx=xx: e.activation(out=sq, in_=xx, func=AF.Square, accum_out=ss), reads=[kx], writes=['sq', 'ss'])
        S.op('act', lambda e: e.activation(out=ss, in_=ss, func=AF.Sqrt, scale=1.0 / D, bias=EPS), writes=['ss'])
        S.op('dve', lambda e: e.reciprocal(out=rstd, in_=ss), reads=['ss'], writes=['rstd'])
        S.op('dve', lambda e, xx=xx: e.scalar_tensor_tensor(out=xn, in0=xx, scalar=rstd, in1=w1b, op0=ALU.mult, op1=ALU.mult),
             reads=[kx, 'rstd', 'w1b'], writes=['xn'])
        pb = t % 2

        def tr(e, pb=pb):
            for k in range(8):
                ins = e.transpose(out=PBb(pb)[:, k * 128:(k + 1) * 128], in_=xn[:, k * 128:(k + 1) * 128], identity=identb)
            return ins
        S.op('pe', tr, reads=['xn', 'identb'], writes=[pk(pb)])
        S.op('act', lambda e, t=t, pb=pb: e.activation(out=xnT[:, :, t * 128:(t + 1) * 128],
                                                       in_=PBb(pb).rearrange("p (k t) -> p k t", k=8), func=AF.Copy),
             writes=[pk(pb), 'xnT%d' % t])

    DMA('pool', proj[0:3, 0:1024], zero_t[0:3, :], r=['zero'], w=['projz'])
    DMA('pool', proj[0:3, 1024:1536], zero_t[0:3, 0:512], r=['zero'], w=['projz2'])
    wst = [A.f32([128, 8, 512]) for _ in range(2)]
    wbf = [A.bf16([128, 8, 512]) for _ in range(2)]
    ot = [A.f32([128, 512]) for _ in range(4)]
    oc = 0
    for cb, (c0, ncol) in enumerate(COLBLK):
        wi = cb % 2
        DMA('sp', wst[wi][:, :, 0:ncol], w_in[:, c0:c0 + ncol].rearrange("(k p) c -> p k c", p=128), w=['wst%d' % wi])
        S.op('pool', lambda e, wi=wi, ncol=ncol: e.tensor_copy(out=wbf[wi][:, :, 0:ncol], in_=wst[wi][:, :, 0:ncol]),
             reads=['wst%d' % wi], writes=['wbf%d' % wi])
        for t in range(NTILE):
            if t < 16 and cb in HALO_SKIP:
                continue
            pb = 2 + (oc % 4)
            oi = oc % 4
            oc += 1

            def mm(e, t=t, wi=wi, ncol=ncol, pb=pb):
                for k in range(8):
                    ins = e.matmul(PB(pb)[:, 0:ncol], lhsT=xnT[:, k, t * 128:(t + 1) * 128], rhs=wbf[wi][:, k, 0:ncol],
                                   start=(k == 0), stop=(k == 7))
                return ins
            S.op('pe', mm, reads=['xnT%d' % t, 'wbf%d' % wi], writes=[pk(pb)])
            ee = 'act' if oc % 2 == 0 else 'dve'
            if ee == 'act':
                S.op('act', lambda e, oi=oi, pb=pb, ncol=ncol: e.activation(out=ot[oi][:, 0:ncol], in_=PB(pb)[:, 0:ncol], func=AF.Copy),
                     writes=[pk(pb), 'ot%d' % oi])
            else:
                S.op('dve', lambda e, oi=oi, pb=pb, ncol=ncol: e.tensor_copy(out=ot[oi][:, 0:ncol], in_=PB(pb)[:, 0:ncol]),
                     writes=[pk(pb), 'ot%d' % oi])
            DMA('pool', proj[3 + t * 128:3 + (t + 1) * 128, c0:c0 + ncol], ot[oi][:, 0:ncol], r=['ot%d' % oi], w=[])
    S.barrier()
    DMA('sp', scp, proj[3 + 4096 - 3:3 + 4096, 0:1536], w=['scp'])
    DMA('sp', scs[:, 2, :], proj[3 + 4096:3 + 4096 + 16, 0:1536], w=['scs2'])

    if stop_after == 'B':
        S.emit()
        return nc
    A.off = base0
    cw = A.f32([128, 4, 1536])
    for k in range(4):
        DMA('sp', cw[:, k, :], conv_wT[k:k + 1, :].partition_broadcast(128), w=['cw'])
    negA = A.f32([128, 4])
    dtb = A.f32([128, 4])
    DMA('sp', negA, a_log.partition_broadcast(128), w=['negA'])
    DMA('sp', dtb, dt_bias.partition_broadcast(128), w=['dtb'])
    S.op('act', lambda e: e.activation(out=negA, in_=negA, func=AF.Exp), writes=['negA'])
    S.op('dve', lambda e: e.tensor_scalar(out=negA, in0=negA, scalar1=-1.0, scalar2=None, op0=ALU.mult), writes=['negA'])
    xs = [[A.f32([128, 1536]) for _ in range(4)] for _ in range(2)]
    cc = A.f32([128, 1536])
    gt = [A.f32([128, 1544]) for _ in range(3)]
    sqc = A.f32([128, 1024])
    ssc = A.f32([128, 8])
    rsc = A.f32([128, 8])
    ba = [A.f32([128, 8]) for _ in range(2)]
    gx = A.f32([128, 4])
    def c_tile(t):
            bi = t % 2
            for k in range(4):
                DMA('sp', xs[bi][k], proj[t * 128 + k:t * 128 + k + 128, 0:1536], w=['xs%d_%d' % (bi, k)])
            DMA('sp', ba[bi], proj[3 + t * 128:3 + (t + 1) * 128, 2048:2056], w=['ba%d' % bi])
            for k in range(4):
                S.op('pool' if k >= 2 else 'dve', lambda e, bi=bi, k=k: e.tensor_tensor(out=xs[bi][k], in0=xs[bi][k], in1=cw[:, k, :], op=ALU.mult),
                     reads=['cw'], writes=['xs%d_%d' % (bi, k)])
            S.op('dve', lambda e, bi=bi: e.tensor_tensor(out=cc, in0=xs[bi][0], in1=xs[bi][1], op=ALU.add), reads=['xs%d_0' % bi, 'xs%d_1' % bi], writes=['cc'])
            S.op('dve', lambda e, bi=bi: e.tensor_tensor(out=cc, in0=cc, in1=xs[bi][2], op=ALU.add), reads=['xs%d_2' % bi], writes=['cc'])
            S.op('dve', lambda e, bi=bi: e.tensor_tensor(out=cc, in0=cc, in1=xs[bi][3], op=ALU.add), reads=['xs%d_3' % bi], writes=['cc'])
            S.op('act', lambda e: e.activation(out=cc, in_=cc, func=AF.Silu), writes=['cc'])
            g_t = gt[t % 3]
            kg = 'gt%d' % (t % 3)
            S.op('act', lambda e: e.activation(out=sqc, in_=cc[:, 0:1024], func=AF.Square), reads=['cc'], writes=['sqc'])
            S.op('dve', lambda e: e.tensor_reduce(out=ssc, in_=sqc.rearrange("p (h d) -> p h d", h=8), axis=AX.X, op=ALU.add),
                 reads=['sqc'], writes=['ssc'])
            S.op('act', lambda e: e.activation(out=ssc, in_=ssc, func=AF.Sqrt, bias=EPS), writes=['ssc'])
            S.op('dve', lambda e: e.reciprocal(out=rsc, in_=ssc), reads=['ssc'], writes=['rsc'])
            S.op('dve', lambda e: e.tensor_scalar(out=rsc[:, 0:4], in0=rsc[:, 0:4], scalar1=128.0 ** -0.5, scalar2=None, op0=ALU.mult), writes=['rsc'])
            S.op('dve', lambda e, g_t=g_t: e.tensor_tensor(out=g_t[:, 0:1024].rearrange("p (h d) -> p h d", h=8),
                                                           in0=cc[:, 0:1024].rearrange("p (h d) -> p h d", h=8),
                                                           in1=rsc.unsqueeze(2).broadcast_to([128, 8, 128]), op=ALU.mult),
                 reads=['cc', 'rsc'], writes=[kg])
            S.op('pool', lambda e, g_t=g_t: e.tensor_copy(out=g_t[:, 1024:1536], in_=cc[:, 1024:1536]), reads=['cc'], writes=[kg])
            S.op('act', lambda e, g_t=g_t, bi=bi: e.activation(out=g_t[:, 1536:1540], in_=ba[bi][:, 0:4], func=AF.Sigmoid),
                 reads=['ba%d' % bi], writes=[kg])
            S.op('dve', lambda e, bi=bi: e.tensor_tensor(out=gx, in0=ba[bi][:, 4:8], in1=dtb, op=ALU.add), reads=['ba%d' % bi, 'dtb'], writes=['gx'])
            S.op('act', lambda e: e.activation(out=gx, in_=gx, func=AF.Exp), writes=['gx'])
            S.op('act', lambda e: e.activation(out=gx, in_=gx, func=AF.Ln, bias=1.0), writes=['gx'])
            S.op('dve', lambda e, g_t=g_t: e.tensor_tensor(out=g_t[:, 1540:1544], in0=gx, in1=negA, op=ALU.mult), reads=['gx', 'negA'], writes=[kg])
    Sst = A.f32([128, 4, 128])
    S.op('pool', lambda e: e.memset(Sst, 0.0), writes=['Sst'])
    names = ['gc', 'gb', 'mt', 'DT', 'DTs', 'egc', 'egl', 'kds', 'bws', 'kT', 'qgT', 'egb', 'ATn', 'inT', 'Am', 'AT', 'X2', 'XT2',
             'R0', 'R1', 'Bv', 'Bw', 'u', 'wT', 'vn', 'kdec', 'o', 'glast']
    W_ = {}
    for n_ in names:
        if n_ in ('gc', 'egc', 'egl', 'kds', 'bws', 'glast'):
            W_[n_] = A.f32([128, 4])
        else:
            W_[n_] = A.f32([128, 4, 128])
    identbc = ident.unsqueeze(1).broadcast_to([128, 4, 128])

    def P4(i):
        return PB(i).rearrange("p (h d) -> p h d", h=4)

    def evac(eng, out, pbi, w, extra_r=()):
        if eng == 'act':
            S.op('act', lambda e: e.activation(out=out, in_=P4(pbi), func=AF.Copy), reads=list(extra_r), writes=[pk(pbi), w])
        else:
            S.op('dve', lambda e: e.tensor_copy(out=out, in_=P4(pbi)), reads=list(extra_r), writes=[pk(pbi), w])

    def d_tile(t):
            G = gt[t % 3]
            kG = 'gt%d' % (t % 3)
            issue_copies(3)
            qv = G[:, 0:512].rearrange("p (h d) -> p h d", h=4)
            kv_ = G[:, 512:1024].rearrange("p (h d) -> p h d", h=4)
            vv = G[:, 1024:1536].rearrange("p (h d) -> p h d", h=4)
            beta = G[:, 1536:1540]
            gg = G[:, 1540:1544]
            S.op('pe', lambda e, gg=gg: e.matmul(PB(0)[:, 0:4], lhsT=U_incl, rhs=gg, start=True, stop=True), reads=[kG, 'cst'], writes=[pk(0)])
            S.op('dve', lambda e: e.tensor_copy(out=W_['gc'], in_=PB(0)[:, 0:4]), writes=[pk(0), 'gc'])
            for h in range(4):
                S.op('dve', lambda e, h=h, gg=gg: e.tensor_scalar(out=W_['gb'][:, h, :], in0=ones, scalar1=gg[:, h:h + 1], scalar2=None, op0=ALU.mult),
                     reads=[kG, 'cst'], writes=['gb'])

            def mm_gb(e):
                for h in range(4):
                    ins = e.matmul(P4(1)[:, h, :], lhsT=W_['gb'][:, h, :], rhs=U_incl, start=True, stop=True)
                return ins
            S.op('pe', mm_gb, reads=['gb', 'cst'], writes=[pk(1)])
            for h in range(4):
                S.op('dve', lambda e, h=h: e.tensor_scalar(out=W_['mt'][:, h, :], in0=P4(1)[:, h, :], scalar1=W_['gc'][:, h:h + 1], scalar2=0.0,
                                                           op0=ALU.subtract, op1=ALU.min), reads=['gc'], writes=[pk(1), 'mt'])
            S.op('act', lambda e: e.activation(out=W_['mt'], in_=W_['mt'], func=AF.Exp), writes=['mt'])
            S.op('pool', lambda e: e.tensor_tensor(out=W_['DT'], in0=W_['mt'], in1=U_incl.unsqueeze(1).broadcast_to([128, 4, 128]), op=ALU.mult),
                 reads=['mt', 'cst'], writes=['DT'])
            S.op('pool', lambda e: e.tensor_tensor(out=W_['DTs'], in0=W_['mt'], in1=U_strict.unsqueeze(1).broadcast_to([128, 4, 128]), op=ALU.mult),
                 reads=['mt', 'cst'], writes=['DTs'])
            if t >= 16:
                S.op('act', lambda e: e.activation(out=W_['egb'], in_=P4(1), func=AF.Exp), writes=[pk(1), 'egb'])
            S.op('dve', lambda e: e.tensor_copy(out=W_['glast'], in_=P4(1)[:, :, 127]), writes=[pk(1), 'glast'])
            S.op('act', lambda e: e.activation(out=W_['egc'], in_=W_['gc'], func=AF.Exp), reads=['gc'], writes=['egc'])
            S.op('act', lambda e: e.activation(out=W_['egl'], in_=W_['glast'], func=AF.Exp), reads=['glast'], writes=['egl'])
            S.op('dve', lambda e: e.tensor_tensor(out=W_['kds'], in0=W_['glast'], in1=W_['gc'], op=ALU.subtract), reads=['glast', 'gc'], writes=['kds'])
            S.op('act', lambda e: e.activation(out=W_['kds'], in_=W_['kds'], func=AF.Exp), writes=['kds'])
            S.op('dve', lambda e, beta=beta: e.tensor_tensor(out=W_['bws'], in0=beta, in1=W_['egc'], op=ALU.mult), reads=[kG, 'egc'], writes=['bws'])
            def tr_k(e, kv_=kv_):
                for h in range(4):
                    ins = e.transpose(out=P4(2)[:, h, :], in_=kv_[:, h, :], identity=ident)
                return ins
            S.op('pe', tr_k, reads=[kG, 'cst'], writes=[pk(2)])
            evac('act', W_['kT'], 2, 'kT')

            if t >= 16:
                def tr_q(e, qv=qv):
                    for h in range(4):
                        ins = e.transpose(out=P4(3)[:, h, :], in_=qv[:, h, :], identity=ident)
                    return ins
                S.op('pe', tr_q, reads=[kG, 'cst'], writes=[pk(3)])
                qTd = W_['o']
                evac('dve', qTd, 3, 'o')
                S.op('dve', lambda e, qTd=qTd: e.tensor_tensor(out=W_['qgT'], in0=qTd, in1=W_['egb'], op=ALU.mult), reads=['o', 'egb'], writes=['qgT'])
            def mm_g(e):
                for h in range(4):
                    ins = e.matmul(P4(4)[:, h, :], lhsT=W_['kT'][:, h, :], rhs=W_['kT'][:, h, :], start=True, stop=True)
                return ins
            S.op('pe', mm_g, reads=['kT'], writes=[pk(4)])

            if t >= 16:
                def mm_qk(e, qTd=qTd):
                    for h in range(4):
                        ins = e.matmul(P4(5)[:, h, :], lhsT=W_['kT'][:, h, :], rhs=qTd[:, h, :], start=True, stop=True)
                    return ins
                S.op('pe', mm_qk, reads=['kT', 'o'], writes=[pk(5)])
            S.op('dve', lambda e: e.tensor_tensor(out=W_['ATn'], in0=P4(4), in1=W_['DTs'], op=ALU.mult), reads=['DTs'], writes=[pk(4), 'ATn'])
            if t >= 16:
                S.op('dve', lambda e: e.tensor_tensor(out=W_['inT'], in0=P4(5), in1=W_['DT'], op=ALU.mult), reads=['DT'], writes=[pk(5), 'inT'])

            def tr_a(e):
                for h in range(4):
                    ins = e.transpose(out=P4(6)[:, h, :], in_=W_['ATn'][:, h, :], identity=ident)
                return ins
            S.op('pe', tr_a, reads=['ATn', 'cst'], writes=[pk(6)])
            S.op('dve', lambda e, beta=beta: e.tensor_tensor(out=W_['Am'], in0=P4(6), in1=beta.unsqueeze(2).broadcast_to([128, 4, 128]), op=ALU.mult),
                 reads=[kG], writes=[pk(6), 'Am'])

            def tr_at(e):
                for h in range(4):
                    ins = e.transpose(out=P4(7)[:, h, :], in_=W_['Am'][:, h, :], identity=ident)
                return ins
            S.op('pe', tr_at, reads=['Am', 'cst'], writes=[pk(7)])
            evac('act', W_['AT'], 7, 'AT')
            S.op('dve', lambda e: e.tensor_tensor(out=W_['R0'], in0=identbc, in1=W_['AT'], op=ALU.subtract), reads=['AT', 'cst'], writes=['R0'])
            X, XT, kX, kXT = W_['Am'], W_['AT'], 'Am', 'AT'
            Xn, XTn, kXn, kXTn = W_['X2'], W_['XT2'], 'X2', 'XT2'
            Rc, Rn, kRc, kRn = W_['R0'], W_['R1'], 'R0', 'R1'
            for lvl in range(6):
                def mm_x2(e, X=X, XT=XT):
                    for h in range(4):
                        ins = e.matmul(P4(2)[:, h, :], lhsT=XT[:, h, :], rhs=X[:, h, :], start=True, stop=True)
                    return ins
                S.op('pe', mm_x2, reads=[kX, kXT], writes=[pk(2)])
                if lvl < 5:
                    def mm_xt2(e, X=X, XT=XT):
                        for h in range(4):
                            ins = e.matmul(P4(3)[:, h, :], lhsT=X[:, h, :], rhs=XT[:, h, :], start=True, stop=True)
                        return ins
                    S.op('pe', mm_xt2, reads=[kX, kXT], writes=[pk(3)])
                evac('act', Xn, 2, kXn)
                if lvl < 5:
                    evac('dve', XTn, 3, kXTn)

                def mm_r(e, Xn=Xn, Rc=Rc):
                    for h in range(4):
                        ins = e.matmul(P4(4)[:, h, :], lhsT=Xn[:, h, :], rhs=Rc[:, h, :], start=True, stop=True)
                    return ins
                S.op('pe', mm_r, reads=[kXn, kRc], writes=[pk(4)])
                S.op('dve', lambda e, Rn=Rn, Rc=Rc: e.tensor_tensor(out=Rn, in0=P4(4), in1=Rc, op=ALU.add), reads=[kRc], writes=[pk(4), kRn])
                X, XT, kX, kXT, Xn, XTn, kXn, kXTn = Xn, XTn, kXn, kXTn, X, XT, kX, kXT
                Rc, Rn, kRc, kRn = Rn, Rc, kRn, kRc
            R, kR = Rc, kRc
            S.op('dve', lambda e, vv=vv, beta=beta: e.tensor_tensor(out=W_['Bv'], in0=vv, in1=beta.unsqueeze(2).broadcast_to([128, 4, 128]), op=ALU.mult),
                 reads=[kG], writes=['Bv'])
            S.op('pool', lambda e, kv_=kv_: e.tensor_tensor(out=W_['Bw'], in0=kv_, in1=W_['bws'].unsqueeze(2).broadcast_to([128, 4, 128]), op=ALU.mult),
                 reads=[kG, 'bws'], writes=['Bw'])
            S.op('pool', lambda e, kv_=kv_: e.tensor_tensor(out=W_['kdec'], in0=kv_, in1=W_['kds'].unsqueeze(2).broadcast_to([128, 4, 128]), op=ALU.mult),
                 reads=[kG, 'kds'], writes=['kdec'])

            def mm_u(e, R=R):
                for h in range(4):
                    ins = e.matmul(P4(5)[:, h, :], lhsT=R[:, h, :], rhs=W_['Bv'][:, h, :], start=True, stop=True)
                return ins
            S.op('pe', mm_u, reads=[kR, 'Bv'], writes=[pk(5)])
            evac('act', W_['u'], 5, 'u')

            def mm_w(e, R=R):
                for h in range(4):
                    ins = e.matmul(P4(6)[:, h, :], lhsT=W_['Bw'][:, h, :], rhs=R[:, h, :], start=True, stop=True)
                return ins
            S.op('pe', mm_w, reads=[kR, 'Bw'], writes=[pk(6)])
            evac('dve', W_['wT'], 6, 'wT')
            def mm_ws(e):
                for h in range(4):
                    ins = e.matmul(P4(7)[:, h, :], lhsT=W_['wT'][:, h, :], rhs=Sst[:, h, :], start=True, stop=True)
                return ins
            S.op('pe', mm_ws, reads=['wT', 'Sst'], writes=[pk(7)])
            S.op('dve', lambda e: e.tensor_tensor(out=W_['vn'], in0=W_['u'], in1=P4(7), op=ALU.subtract), reads=['u'], writes=[pk(7), 'vn'])
            if t >= 16:
                def mm_o(e):
                    for h in range(4):
                        e.matmul(P4(0)[:, h, :], lhsT=W_['qgT'][:, h, :], rhs=Sst[:, h, :], start=True, stop=False)
                        ins = e.matmul(P4(0)[:, h, :], lhsT=W_['inT'][:, h, :], rhs=W_['vn'][:, h, :], start=False, stop=True)
                    return ins
                S.op('pe', mm_o, reads=['qgT', 'Sst', 'inT', 'vn'], writes=[pk(0)])
                evac('act', W_['o'], 0, 'o')
                DMA('pool', o_gdn[(t - 16) * 128:(t - 15) * 128, :], W_['o'].rearrange("p h d -> p (h d)"), r=['o'], w=[])

            def mm_su(e):
                for h in range(4):
                    ins = e.matmul(P4(1)[:, h, :], lhsT=W_['kdec'][:, h, :], rhs=W_['vn'][:, h, :], start=True, stop=True)
                return ins
            S.op('pe', mm_su, reads=['kdec', 'vn'], writes=[pk(1)])
            for h in range(4):
                S.op('dve', lambda e, h=h: e.scalar_tensor_tensor(out=Sst[:, h, :], in0=Sst[:, h, :], scalar=W_['egl'][:, h:h + 1], in1=P4(1)[:, h, :],
                                                                  op0=ALU.mult, op1=ALU.add), reads=['egl'], writes=[pk(1), 'Sst'])
    qkw = A.f32([128, 6, 64])
    for gi in range(3):
        DMA('sp', qkw[:, gi, :], qnw[gi:gi + 1, :].partition_broadcast(128), w=['qkw'])
        DMA('sp', qkw[:, 3 + gi, :], knw[gi:gi + 1, :].partition_broadcast(128), w=['qkw'])
    rin = [A.f32([128, 512]) for _ in range(3)]
    rsq = A.f32([128, 512])
    rss = A.f32([128, 8])
    rrs = A.f32([128, 8])
    rout = [A.f32([128, 512]) for _ in range(3)]
    rp = [A.f32([128, 16]) for _ in range(2)]
    rt = [A.f32([128, 8, 8]) for _ in range(4)]
    ecnt = [0]
    def e_tile(gi, t):
        W, dil = GROUPS[gi]
        cq = 2056 + gi * 1536
        if True:
            DMA('sp', rp[t % 2], rope_t[t * 128:(t + 1) * 128, :], w=['rp%d' % (t % 2)])
            cosb = rp[t % 2][:, 0:8].unsqueeze(1).broadcast_to([128, 8, 8])
            sinb = rp[t % 2][:, 8:16].unsqueeze(1).broadcast_to([128, 8, 8])
            for which in ((1, 0) if t >= 16 else (1,)):
                bi = ecnt[0] % 3
                ecnt[0] += 1
                src = rin[bi]
                dst = rout[bi]
                ks, kd = 'rin%d' % bi, 'rout%d' % bi
                c0 = cq + which * 512
                DMA('sp', src, proj[3 + t * 128:3 + (t + 1) * 128, c0:c0 + 512], w=[ks])
                S.op('act', lambda e, src=src: e.activation(out=rsq, in_=src, func=AF.Square), reads=[ks], writes=['rsq'])
                S.op('dve', lambda e: e.tensor_reduce(out=rss, in_=rsq.rearrange("p (h d) -> p h d", h=8), axis=AX.X, op=ALU.add),
                     reads=['rsq'], writes=['rss'])
                S.op('act', lambda e: e.activation(out=rss, in_=rss, func=AF.Sqrt, scale=1.0 / 64, bias=EPS), writes=['rss'])
                S.op('dve', lambda e: e.reciprocal(out=rrs, in_=rss), reads=['rss'], writes=['rrs'])
                d3 = dst.rearrange("p (h d) -> p h d", h=8)
                S.op('dve', lambda e, src=src, d3=d3: e.tensor_tensor(out=d3, in0=src.rearrange("p (h d) -> p h d", h=8),
                                                                      in1=rrs.unsqueeze(2).broadcast_to([128, 8, 64]), op=ALU.mult),
                     reads=[ks, 'rrs'], writes=[kd])
                wrow = qkw[:, (3 if which == 1 else 0) + gi, :].unsqueeze(1).broadcast_to([128, 8, 64])
                S.op('pool', lambda e, d3=d3, wrow=wrow: e.tensor_tensor(out=d3, in0=d3, in1=wrow, op=ALU.mult), reads=['qkw'], writes=[kd])
                x1 = d3[:, :, 0:8]
                x2 = d3[:, :, 8:16]
                S.op('dve', lambda e, x1=x1, cosb=cosb: e.tensor_tensor(out=rt[0], in0=x1, in1=cosb, op=ALU.mult), reads=[kd, 'rp%d' % (t % 2)], writes=['rt0'])
                S.op('dve', lambda e, x2=x2, sinb=sinb: e.tensor_tensor(out=rt[1], in0=x2, in1=sinb, op=ALU.mult), reads=[kd, 'rp%d' % (t % 2)], writes=['rt1'])
                S.op('dve', lambda e, x2=x2, cosb=cosb: e.tensor_tensor(out=rt[2], in0=x2, in1=cosb, op=ALU.mult), reads=[kd, 'rp%d' % (t % 2)], writes=['rt2'])
                S.op('dve', lambda e, x1=x1, sinb=sinb: e.tensor_tensor(out=rt[3], in0=x1, in1=sinb, op=ALU.mult), reads=[kd, 'rp%d' % (t % 2)], writes=['rt3'])
                S.op('dve', lambda e, x1=x1: e.tensor_tensor(out=x1, in0=rt[0], in1=rt[1], op=ALU.subtract), reads=['rt0', 'rt1'], writes=[kd])
                S.op('dve', lambda e, x2=x2: e.tensor_tensor(out=x2, in0=rt[2], in1=rt[3], op=ALU.add), reads=['rt2', 'rt3'], writes=[kd])
                if which == 1:
                    DMA('pool', kn_s[gi, t * 128:(t + 1) * 128, :], dst, r=[kd], w=[])
                    if t * 128 >= 4096 - W:
                        r0 = t * 128 - (4096 - W)
                        DMA('pool', kvp[gi][r0:r0 + 128, 0, :], dst, r=[kd], w=[])
                        DMA('pool', kvp[gi][r0:r0 + 128, 1, :], proj[3 + t * 128:3 + (t + 1) * 128, cq + 1024:cq + 1536], w=[])
                else:
                    DMA('pool', qn_s[gi, (t - 16) * 128:(t - 15) * 128, :], dst, r=[kd], w=[])
    e_units = []
    for gi in range(3):
        for t in range(16 - GROUPS[gi][0] // 128, 32):
            e_units.append((gi, t))
    c_tile(0)
    per = (len(e_units) + 31) // 32
    for t in range(32):
        lc, ld, le = [], [], []
        if t < 31:
            S.defer = lc
            c_tile(t + 1)
        S.defer = ld
        d_tile(t)
        S.defer = le
        for _ in range(per):
            if e_units:
                e_tile(*e_units.pop(0))
        S.defer = None
        S.replay([ld, lc, le], [4, 1, 2])
    issue_copies(1000)
    DMA('pool', sgp.rearrange("h k v -> k h v"), Sst, r=['Sst'], w=['sgp'])
    S.barrier()

    if stop_after == 'E':
        S.emit()
        return nc
    A.off = base0
    fin = [A.f32([128, 512]) for _ in range(4)]
    fb = [A.bf16([128, 512]) for _ in range(2)]
    qT = A.bf16([128, 4, 128])
    kTs = [A.bf16([128, 4, 128]) for _ in range(2)]
    Vs = [A.bf16([128, 8, 66]) for _ in range(2)]
    for i in range(2):
        S.op('pool', lambda e, i=i: e.memset(Vs[i], 1.0), writes=['Vs%d' % i])
    Pm = [A.bf16([128, 8, 128]) for _ in range(2)]
    Ex = A.bf16([128, 4, 128])
    Osb = A.f32([128, 8, 65])
    fc = 0
    for gi in range(3):
        W, dil = GROUPS[gi]
        nblk = 4096 // dil // 128
        first_q = 2048 // dil // 128
        cv = 2056 + gi * 1536 + 1024
        for r in range(dil):
            for mb in range(first_q - 1, nblk):
                slot = mb % 2
                tok0 = r + dil * mb * 128
                rows = slice(tok0, tok0 + dil * 127 + 1, dil)
                prow = slice(3 + tok0, 3 + tok0 + dil * 127 + 1, dil)
                f1 = fin[fc % 4]; k1 = 'fin%d' % (fc % 4); fc += 1
                DMA('sp', f1, kn_s[gi, rows, :], w=[k1])
                S.op('act', lambda e, f1=f1: e.activation(out=fb[0], in_=f1, func=AF.Copy), reads=[k1], writes=['fb0'])

                def tr1(e):
                    for hp in range(4):
                        ins = e.transpose(out=PBb(0)[:, hp * 128:(hp + 1) * 128], in_=fb[0][:, hp * 128:(hp + 1) * 128], identity=identb)
                    return ins
                S.op('pe', tr1, reads=['fb0', 'identb'], writes=[pk(0)])
                S.op('dve', lambda e, slot=slot: e.tensor_copy(out=kTs[slot], in_=PBb(0)[:, 0:512].rearrange("p (a b) -> p a b", a=4)),
                     writes=[pk(0), 'kT%d' % slot])
                f2 = fin[fc % 4]; k2 = 'fin%d' % (fc % 4); fc += 1
                DMA('sp', f2, proj[prow, cv:cv + 512], w=[k2])
                S.op('pool', lambda e, f2=f2, slot=slot: e.tensor_copy(out=Vs[slot][:, :, 0:64], in_=f2.rearrange("p (h d) -> p h d", h=8)),
                     reads=[k2], writes=['Vs%d' % slot])
                if mb < first_q:
                    continue
                f3 = fin[fc % 4]; k3 = 'fin%d' % (fc % 4); fc += 1
                qrow = slice(tok0 - 2048, tok0 - 2048 + dil * 127 + 1, dil)
                DMA('sp', f3, qn_s[gi, qrow, :], w=[k3])
                S.op('act', lambda e, f3=f3: e.activation(out=fb[1], in_=f3, func=AF.Copy), reads=[k3], writes=['fb1'])

                def tr2(e):
                    for hp in range(4):
                        ins = e.transpose(out=PBb(1)[:, hp * 128:(hp + 1) * 128], in_=fb[1][:, hp * 128:(hp + 1) * 128], identity=identb)
                    return ins
                S.op('pe', tr2, reads=['fb1', 'identb'], writes=[pk(1)])
                S.op('dve', lambda e: e.tensor_copy(out=qT, in_=PBb(1)[:, 0:512].rearrange("p (a b) -> p a b", a=4)), writes=[pk(1), 'qT'])
                halo_prev = (mb == first_q)
                for which, sl in ((0, 1 - slot), (1, slot)):
                    for par in range(2):
                        pbi = 2 + par

                        def mm_s(e, sl=sl, par=par, pbi=pbi):
                            for hp in range(4):
                                lo = par * 64
                                ins = e.matmul(PB(pbi)[:, hp * 128:(hp + 1) * 128], lhsT=kTs[sl][lo:lo + 64, hp, :], rhs=qT[lo:lo + 64, hp, :],
                                               start=True, stop=True)
                            return ins
                        S.op('pe', mm_s, reads=['kT%d' % sl, 'qT'], writes=[pk(pbi)])
                        if which == 0 and halo_prev:
                            S.op('act', lambda e, pbi=pbi: e.activation(out=Ex, in_=P4(pbi), func=AF.Exp, scale=0.125, bias=maskb_t),
                                 reads=['maskb'], writes=[pk(pbi), 'Ex'])
                        else:
                            S.op('act', lambda e, pbi=pbi: e.activation(out=Ex, in_=P4(pbi), func=AF.Exp, scale=0.125), writes=[pk(pbi), 'Ex'])
                        msk = (L_incl if which == 0 else U_incl).unsqueeze(1).broadcast_to([128, 4, 128])
                        S.op('dve', lambda e, which=which, par=par, msk=msk: e.tensor_tensor(out=Pm[which][:, par:8:2, :], in0=Ex, in1=msk, op=ALU.mult),
                             reads=['Ex', 'cst'], writes=['Pm%d_%d' % (which, par)])
                for half in range(2):
                    pbi = 4 + half

                    def mm_pv(e, half=half, pbi=pbi, slot=slot):
                        for hh in range(4):
                            h = half * 4 + hh
                            e.matmul(PB(pbi)[:, hh * 65:(hh + 1) * 65], lhsT=Pm[0][:, h, :], rhs=Vs[1 - slot][:, h, 0:65], start=True, stop=False)
                            ins = e.matmul(PB(pbi)[:, hh * 65:(hh + 1) * 65], lhsT=Pm[1][:, h, :], rhs=Vs[slot][:, h, 0:65], start=False, stop=True)
                        return ins
                    S.op('pe', mm_pv, reads=['Pm0_0', 'Pm0_1', 'Pm1_0', 'Pm1_1', 'Vs0', 'Vs1'], writes=[pk(pbi)])
                    S.op('act', lambda e, half=half, pbi=pbi: e.activation(out=Osb[:, half * 4:(half + 1) * 4, :],
                                                                           in_=PB(pbi)[:, 0:260].rearrange("p (h d) -> p h d", h=4), func=AF.Copy),
                         writes=[pk(pbi), 'Osb%d' % half])
                DMA('pool', att_o[gi, qrow, :], Osb.rearrange("p h d -> p (h d)"), r=['Osb0', 'Osb1'], w=[])
    S.barrier()

    if stop_after == 'F':
        S.emit()
        return nc
    A.off = base0
    gnb = A.f32([128, 128])
    DMA('sp', gnb, gnw.partition_broadcast(128), w=['gnb'])
    g_o = [A.f32([128, 512]) for _ in range(2)]
    g_z = [A.f32([128, 512]) for _ in range(2)]
    g_a = [[A.f32([128, 520]) for _ in range(3)] for _ in range(2)]
    mixt = [A.f32([128, D]) for _ in range(2)]
    gsq = A.f32([128, 512])
    gss = A.f32([128, 4])
    grs = A.f32([128, 4])
    gden = A.f32([128, 8])
    for t in range(16):
        bi = t % 2
        mx = mixt[bi]
        km = 'mixt%d' % bi
        DMA('sp', g_o[bi], o_gdn[t * 128:(t + 1) * 128, :], w=['g_o%d' % bi])
        DMA('sp', g_z[bi], proj[3 + (16 + t) * 128:3 + (17 + t) * 128, 1536:2048], w=['g_z%d' % bi])
        for gi in range(3):
            DMA('sp', g_a[bi][gi], att_o[gi, t * 128:(t + 1) * 128, :], w=['g_a%d_%d' % (bi, gi)])
        S.op('act', lambda e, bi=bi: e.activation(out=gsq, in_=g_o[bi], func=AF.Square), reads=['g_o%d' % bi], writes=['gsq'])
        S.op('dve', lambda e: e.tensor_reduce(out=gss, in_=gsq.rearrange("p (h d) -> p h d", h=4), axis=AX.X, op=ALU.add), reads=['gsq'], writes=['gss'])
        S.op('act', lambda e: e.activation(out=gss, in_=gss, func=AF.Sqrt, scale=1.0 / 128, bias=EPS), writes=['gss'])
        S.op('dve', lambda e: e.reciprocal(out=grs, in_=gss), reads=['gss'], writes=['grs'])
        m3 = mx[:, 0:512].rearrange("p (h d) -> p h d", h=4)
        S.op('dve', lambda e, bi=bi, m3=m3: e.tensor_tensor(out=m3, in0=g_o[bi].rearrange("p (h d) -> p h d", h=4),
                                                            in1=grs.unsqueeze(2).broadcast_to([128, 4, 128]), op=ALU.mult),
             reads=['g_o%d' % bi, 'grs'], writes=[km])
        S.op('pool', lambda e, m3=m3: e.tensor_tensor(out=m3, in0=m3, in1=gnb.unsqueeze(1).broadcast_to([128, 4, 128]), op=ALU.mult), reads=['gnb'], writes=[km])
        S.op('act', lambda e, bi=bi: e.activation(out=g_z[bi], in_=g_z[bi], func=AF.Silu), writes=['g_z%d' % bi])
        S.op('dve', lambda e, bi=bi, mx=mx: e.tensor_tensor(out=mx[:, 0:512], in0=mx[:, 0:512], in1=g_z[bi], op=ALU.mult), reads=['g_z%d' % bi], writes=[km])
        S.op('pool', lambda e, bi=bi: e.tensor_tensor(out=g_a[bi][0], in0=g_a[bi][0], in1=g_a[bi][1], op=ALU.add), reads=['g_a%d_1' % bi], writes=['g_a%d_0' % bi])
        S.op('pool', lambda e, bi=bi: e.tensor_tensor(out=g_a[bi][0], in0=g_a[bi][0], in1=g_a[bi][2], op=ALU.add), reads=['g_a%d_2' % bi], writes=['g_a%d_0' % bi])
        a3 = g_a[bi][0].rearrange("p (h d) -> p h d", h=8)
        S.op('dve', lambda e, a3=a3: e.reciprocal(out=gden, in_=a3[:, :, 64]), reads=['g_a%d_0' % bi], writes=['gden'])
        S.op('dve', lambda e, a3=a3, mx=mx: e.tensor_tensor(out=mx[:, 512:1024].rearrange("p (h d) -> p h d", h=8), in0=a3[:, :, 0:64],
                                                            in1=gden.unsqueeze(2).broadcast_to([128, 8, 64]), op=ALU.mult),
             reads=['g_a%d_0' % bi, 'gden'], writes=[km])
        DMA('pool', mix_d[t * 128:(t + 1) * 128, :], mx, r=[km], w=[])
    S.barrier()

    if stop_after == 'G1':
        S.emit()
        return nc
    sample_path(nc, S, A, base0, locals())

    A.off = base0
    yacc = A.f32([128, 17, D])
    h2T = A.bf16([128, 8, 17 * 128])
    gates = A.f32([128, 17, 32])
    base1 = A.off
    wob = A.bf16([128, 8, D])
    wos = A.f32([128, 8, 512])
    for hf in range(2):
        DMA('sp', wos, w_out[:, hf * 512:(hf + 1) * 512].rearrange("(k p) c -> p k c", p=128), w=['wos'])
        S.op('pool', lambda e, hf=hf: e.tensor_copy(out=wob[:, :, hf * 512:(hf + 1) * 512], in_=wos), reads=['wos'], writes=['wob'])
    wrt = A.f32([128, 8, 36])
    DMA('sp', wrt, w_r.rearrange("(k p) c -> p k c", p=128), w=['wrt'])
    w2b = A.f32([128, D])
    DMA('sp', w2b, norm2.partition_broadcast(128), w=['w2b'])
    mxl = [A.f32([128, D]) for _ in range(2)]
    xl = [A.f32([128, D]) for _ in range(2)]
    mxb = A.bf16([128, D])
    mxT = A.bf16([128, 8, 128])
    hsq = A.f32([128, D])
    hss = A.f32([128, 1])
    hrs = A.f32([128, 1])
    h2n = A.f32([128, D])
    h2Tf = A.f32([128, 8, 128])
    lg = A.f32([128, 36])
    r_ = {n_: A.f32([128, 8]) for n_ in ['gm', 'oh', 'eg', 'sg', 'pg', 'ein', 'm1', 'oh1', 'e2', 'm2', 'oh2', 'w1', 'w2', 'g8', 'tmp']}
    sel = A.f32([128, 4, 8])
    for t in range(17):
        bi = t % 2
        xrow = (16 + t) * 128 if t < 16 else 32 * 128
        DMA('sp', mxl[bi], mix_d[t * 128:(t + 1) * 128, :], w=['mxl%d' % bi])
        DMA('sp', xl[bi], x_all[xrow:xrow + 128, :], w=['xl%d' % bi])
        S.op('act', lambda e, bi=bi: e.activation(out=mxb, in_=mxl[bi], func=AF.Copy), reads=['mxl%d' % bi], writes=['mxb'])

        def trm(e):
            for k in range(8):
                ins = e.transpose(out=PBb(0)[:, k * 128:(k + 1) * 128], in_=mxb[:, k * 128:(k + 1) * 128], identity=identb)
            return ins
        S.op('pe', trm, reads=['mxb', 'identb'], writes=[pk(0)])
        S.op('dve', lambda e: e.tensor_copy(out=mxT, in_=PBb(0).rearrange("p (k t) -> p k t", k=8)), writes=[pk(0), 'mxT'])
        for hf in range(2):
            def mmo(e, hf=hf):
                for k in range(8):
                    ins = e.matmul(PB(1 + hf), lhsT=mxT[:, k, :], rhs=wob[:, k, hf * 512:(hf + 1) * 512], start=(k == 0), stop=(k == 7))
                return ins
            S.op('pe', mmo, reads=['mxT', 'wob'], writes=[pk(1 + hf)])
            S.op('dve', lambda e, hf=hf, t=t, bi=bi: e.tensor_tensor(out=yacc[:, t, hf * 512:(hf + 1) * 512], in0=PB(1 + hf), in1=xl[bi][:, hf * 512:(hf + 1) * 512], op=ALU.add),
                 reads=['xl%d' % bi], writes=[pk(1 + hf), 'yacc%d' % t])
        S.op('act', lambda e, t=t: e.activation(out=hsq, in_=yacc[:, t, :], func=AF.Square, accum_out=hss), reads=['yacc%d' % t], writes=['hsq', 'hss'])
        S.op('act', lambda e: e.activation(out=hss, in_=hss, func=AF.Sqrt, scale=1.0 / D, bias=EPS), writes=['hss'])
        S.op('dve', lambda e: e.reciprocal(out=hrs, in_=hss), reads=['hss'], writes=['hrs'])
        S.op('dve', lambda e, t=t: e.scalar_tensor_tensor(out=h2n, in0=yacc[:, t, :], scalar=hrs, in1=w2b, op0=ALU.mult, op1=ALU.mult),
             reads=['yacc%d' % t, 'hrs', 'w2b'], writes=['h2n'])
        for hf in range(2):
            def trh(e, hf=hf):
                for k in range(4):
                    kk = hf * 4 + k
                    ins = e.transpose(out=PB(3 + hf)[:, k * 128:(k + 1) * 128], in_=h2n[:, kk * 128:(kk + 1) * 128], identity=ident)
                return ins
            S.op('pe', trh, reads=['h2n', 'cst'], writes=[pk(3 + hf)])
            S.op('act', lambda e, hf=hf, t=t: e.activation(out=h2T[:, hf * 4:(hf + 1) * 4, t * 128:(t + 1) * 128], in_=P4(3 + hf), func=AF.Copy),
                 writes=[pk(3 + hf), 'h2T%d' % t])
            S.op('dve', lambda e, hf=hf: e.tensor_copy(out=h2Tf[:, hf * 4:(hf + 1) * 4, :], in_=P4(3 + hf)), writes=[pk(3 + hf), 'h2Tf'])

        def mmr(e):
            for k in range(8):
                ins = e.matmul(PB(5)[:, 0:36], lhsT=h2Tf[:, k, :], rhs=wrt[:, k, :], start=(k == 0), stop=(k == 7))
            return ins
        S.op('pe', mmr, reads=['h2Tf', 'wrt'], writes=[pk(5)])
        S.op('dve', lambda e: e.tensor_copy(out=lg, in_=PB(5)[:, 0:36]), writes=[pk(5), 'lg'])
        R_ = r_
        lgg = lg[:, 0:4]
        le = lg[:, 4:36].rearrange("p (g e) -> p g e", g=4)

        def V(e_, fn, r, w):
            S.op(e_, fn, reads=r, writes=w)
        V('dve', lambda e: e.tensor_reduce(out=R_['gm'][:, 0:1], in_=lgg, axis=AX.X, op=ALU.max), ['lg'], ['gm'])
        V('dve', lambda e: e.tensor_scalar(out=R_['oh'][:, 0:4], in0=lgg, scalar1=R_['gm'][:, 0:1], scalar2=None, op0=ALU.is_equal), ['lg', 'gm'], ['oh'])
        V('dve', lambda e: e.tensor_scalar(out=R_['eg'][:, 0:4], in0=lgg, scalar1=R_['gm'][:, 0:1], scalar2=None, op0=ALU.subtract), ['lg', 'gm'], ['eg'])
        V('act', lambda e: e.activation(out=R_['eg'][:, 0:4], in_=R_['eg'][:, 0:4], func=AF.Exp), [], ['eg'])
        V('dve', lambda e: e.tensor_reduce(out=R_['sg'][:, 0:1], in_=R_['eg'][:, 0:4], axis=AX.X, op=ALU.add), ['eg'], ['sg'])
        V('dve', lambda e: e.reciprocal(out=R_['pg'][:, 0:1], in_=R_['sg'][:, 0:1]), ['sg'], ['pg'])
        V('dve', lambda e: e.tensor_tensor(out=sel, in0=le, in1=R_['oh'][:, 0:4].unsqueeze(2).broadcast_to([128, 4, 8]), op=ALU.mult), ['lg', 'oh'], ['sel'])
        V('dve', lambda e: e.tensor_reduce(out=R_['ein'], in_=sel.rearrange("p g e -> p e g"), axis=AX.X, op=ALU.add), ['sel'], ['ein'])
        V('dve', lambda e: e.tensor_reduce(out=R_['m1'][:, 0:1], in_=R_['ein'], axis=AX.X, op=ALU.max), ['ein'], ['m1'])
        V('dve', lambda e: e.tensor_scalar(out=R_['oh1'], in0=R_['ein'], scalar1=R_['m1'][:, 0:1], scalar2=None, op0=ALU.is_equal), ['ein', 'm1'], ['oh1'])
        V('dve', lambda e: e.scalar_tensor_tensor(out=R_['e2'], in0=R_['oh1'], scalar=-1e30, in1=R_['ein'], op0=ALU.mult, op1=ALU.add), ['oh1', 'ein'], ['e2'])
        V('dve', lambda e: e.tensor_reduce(out=R_['m2'][:, 0:1], in_=R_['e2'], axis=AX.X, op=ALU.max), ['e2'], ['m2'])
        V('dve', lambda e: e.tensor_scalar(out=R_['oh2'], in0=R_['e2'], scalar1=R_['m2'][:, 0:1], scalar2=None, op0=ALU.is_equal), ['e2', 'm2'], ['oh2'])
        V('dve', lambda e: e.tensor_tensor(out=R_['w1'][:, 0:1], in0=R_['m2'][:, 0:1], in1=R_['m1'][:, 0:1], op=ALU.subtract), ['m1', 'm2'], ['w1'])
        V('act', lambda e: e.activation(out=R_['w1'][:, 0:1], in_=R_['w1'][:, 0:1], func=AF.Exp), [], ['w1'])
        V('dve', lambda e: e.tensor_scalar(out=R_['w1'][:, 0:1], in0=R_['w1'][:, 0:1], scalar1=1.0, scalar2=None, op0=ALU.add), [], ['w1'])
        V('dve', lambda e: e.reciprocal(out=R_['w1'][:, 0:1], in_=R_['w1'][:, 0:1]), [], ['w1'])
        V('dve', lambda e: e.tensor_tensor(out=R_['w1'][:, 0:1], in0=R_['w1'][:, 0:1], in1=R_['pg'][:, 0:1], op=ALU.mult), ['pg'], ['w1'])
        V('dve', lambda e: e.tensor_tensor(out=R_['w2'][:, 0:1], in0=R_['pg'][:, 0:1], in1=R_['w1'][:, 0:1], op=ALU.subtract), ['pg', 'w1'], ['w2'])
        V('dve', lambda e: e.tensor_scalar(out=R_['g8'], in0=R_['oh1'], scalar1=R_['w1'][:, 0:1], scalar2=None, op0=ALU.mult), ['oh1', 'w1'], ['g8'])
        V('dve', lambda e: e.scalar_tensor_tensor(out=R_['g8'], in0=R_['oh2'], scalar=R_['w2'][:, 0:1], in1=R_['g8'], op0=ALU.mult, op1=ALU.add), ['oh2', 'w2'], ['g8'])
        V('dve', lambda e, t=t: e.tensor_tensor(out=gates[:, t, :].rearrange("p (g e) -> p g e", g=4),
                                                in0=R_['oh'][:, 0:4].unsqueeze(2).broadcast_to([128, 4, 8]),
                                                in1=R_['g8'].unsqueeze(1).broadcast_to([128, 4, 8]), op=ALU.mult), ['oh', 'g8'], ['gates%d' % t])
    S.barrier()

    if stop_after == 'G2':
        S.emit()
        return nc
    A.off = base1
    gus = [A.f32([128, 8, 512]) for _ in range(2)]
    gub = [A.bf16([128, 8, 512]) for _ in range(2)]
    wds = [A.f32([128, 2, D]) for _ in range(2)]
    wdb = [A.bf16([128, 2, D]) for _ in range(2)]
    sgt = A.f32([128, 2, 512])
    hid = A.bf16([128, 2, 512])
    blocks = [(0, 512), (512, 512), (1024, 512), (1536, 512), (2048, 128)]
    for ex in range(32):
        wi = ex % 2
        DMA('sp', gus[wi], w_gu[ex].rearrange("(k p) c -> p k c", p=128), w=['gus%d' % wi])
        DMA('sp', wds[wi], w_d[ex].rearrange("(k p) c -> p k c", p=128), w=['wds%d' % wi])
        S.op('pool', lambda e, wi=wi: e.tensor_copy(out=gub[wi], in_=gus[wi]), reads=['gus%d' % wi], writes=['gub%d' % wi])
        S.op('pool', lambda e, wi=wi: e.tensor_copy(out=wdb[wi], in_=wds[wi]), reads=['wds%d' % wi], writes=['wdb%d' % wi])
        for (t0, nt) in blocks:
            for c in range(4):
                def mmg(e, c=c, wi=wi, t0=t0, nt=nt):
                    for k in range(8):
                        ins = e.matmul(PB(c)[:, 0:nt], lhsT=gub[wi][:, k, c * 128:(c + 1) * 128], rhs=h2T[:, k, t0:t0 + nt], start=(k == 0), stop=(k == 7))
                    return ins
                S.op('pe', mmg, reads=['gub%d' % wi] + ['h2T%d' % tt for tt in range(t0 // 128, (t0 + nt) // 128)], writes=[pk(c)])
            for c in range(2):
                S.op('act', lambda e, c=c, nt=nt: e.activation(out=sgt[:, c, 0:nt], in_=PB(c)[:, 0:nt], func=AF.Silu), writes=[pk(c), 'sgt%d' % c])
                S.op('dve', lambda e, c=c, nt=nt: e.tensor_tensor(out=hid[:, c, 0:nt], in0=sgt[:, c, 0:nt], in1=PB(2 + c)[:, 0:nt], op=ALU.mult),
                     reads=['sgt%d' % c], writes=[pk(2 + c), 'hid%d' % c])
            for ti in range(nt // 128):
                t = t0 // 128 + ti
                pby = 4 + 2 * (ti % 2)

                def mmd(e, ti=ti, wi=wi, pby=pby):
                    for hf in range(2):
                        for c in range(2):
                            ins = e.matmul(PB(pby + hf), lhsT=hid[:, c, ti * 128:(ti + 1) * 128], rhs=wdb[wi][:, c, hf * 512:(hf + 1) * 512],
                                           start=(c == 0), stop=(c == 1))
                    return ins
                S.op('pe', mmd, reads=['hid0', 'hid1', 'wdb%d' % wi], writes=[pk(pby), pk(pby + 1)])
                S.op('dve', lambda e, t=t, ex=ex, pby=pby: e.scalar_tensor_tensor(out=yacc[:, t, :], in0=ps_all[:, pby * 512:(pby + 2) * 512],
                                                                                 scalar=gates[:, t, ex:ex + 1], in1=yacc[:, t, :], op0=ALU.mult, op1=ALU.add),
                     reads=['gates%d' % t], writes=[pk(pby), pk(pby + 1), 'yacc%d' % t])
    for t in range(16):
        DMA('sp', y_own[t * 128:(t + 1) * 128, :], yacc[:, t, :], r=['yacc%d' % t], w=[])
    DMA('sp', y_samp, yacc[:, 16, :], r=['yacc16'], w=['y_samp'])
    S.emit()
    return nc


def sample_path(nc, S, A, base0, L):
    g_ = lambda n: L[n]
    proj, mix_d, zero_t, st_gdn, st_conv, caches = g_('proj'), g_('mix_d'), g_('zero_t'), g_('st_gdn'), g_('st_conv'), g_('caches')
    conv_wT, a_log, dt_bias, gnw, qnw, knw, rope_t, selh = g_('conv_wT'), g_('a_log'), g_('dt_bias'), g_('gnw'), g_('qnw'), g_('knw'), g_('rope_t'), g_('selh')
    sgs, kvs = g_('sgs'), g_('kvs')
    ident, ones, PB, pk, ps_all = g_('ident'), g_('ones'), g_('PB'), g_('pk'), g_('ps_all')
    NS = 16
    R0 = 3 + 4096

    def DMA(eng, out, in_, r=(), w=()):
        S.op(eng, lambda e: e.dma_start(out=out, in_=in_), reads=r, writes=w, dma=True)
    A.off = base0
    id16 = ident[0:NS, 0:NS]
    cw = A.f32([NS, 4, 1536])
    for k in range(4):
        DMA('sp', cw[:, k, :], conv_wT[k:k + 1, :].partition_broadcast(NS), w=['s_cw'])
    full = A.f32([NS, 4, 1536])
    DMA('sp', full[:, 0:3, :], st_conv, w=['s_full'])
    DMA('sp', full[:, 3, :], proj[R0:R0 + NS, 0:1536], w=['s_full'])
    zs = A.f32([NS, 512])
    DMA('sp', zs, proj[R0:R0 + NS, 1536:2048], w=['s_z'])
    ba = A.f32([NS, 8])
    DMA('sp', ba, proj[R0:R0 + NS, 2048:2056], w=['s_ba'])
    negA = A.f32([NS, 4])
    dtb = A.f32([NS, 4])
    DMA('sp', negA, a_log.partition_broadcast(NS), w=['s_negA'])
    DMA('sp', dtb, dt_bias.partition_broadcast(NS), w=['s_dtb'])
    gnb = A.f32([NS, 128])
    DMA('sp', gnb, gnw.partition_broadcast(NS), w=['s_gnb'])
    S.op('act', lambda e: e.activation(out=negA, in_=negA, func=AF.Exp), writes=['s_negA'])
    S.op('dve', lambda e: e.tensor_scalar(out=negA, in0=negA, scalar1=-1.0, scalar2=None, op0=ALU.mult), writes=['s_negA'])
    S.op('dve', lambda e: e.tensor_tensor(out=full, in0=full, in1=cw, op=ALU.mult), reads=['s_cw'], writes=['s_full'])
    cc = A.f32([NS, 1536])
    S.op('dve', lambda e: e.tensor_tensor(out=cc, in0=full[:, 0, :], in1=full[:, 1, :], op=ALU.add), reads=['s_full'], writes=['s_cc'])
    S.op('dve', lambda e: e.tensor_tensor(out=cc, in0=cc, in1=full[:, 2, :], op=ALU.add), reads=['s_full'], writes=['s_cc'])
    S.op('dve', lambda e: e.tensor_tensor(out=cc, in0=cc, in1=full[:, 3, :], op=ALU.add), reads=['s_full'], writes=['s_cc'])
    S.op('act', lambda e: e.activation(out=cc, in_=cc, func=AF.Silu), writes=['s_cc'])
    sq = A.f32([NS, 1024])
    ss8 = A.f32([NS, 8])
    rs8 = A.f32([NS, 8])
    qk = A.f32([NS, 8, 128])
    S.op('act', lambda e: e.activation(out=sq, in_=cc[:, 0:1024], func=AF.Square), reads=['s_cc'], writes=['s_sq'])
    S.op('dve', lambda e: e.tensor_reduce(out=ss8, in_=sq.rearrange("p (h d) -> p h d", h=8), axis=AX.X, op=ALU.add), reads=['s_sq'], writes=['s_ss8'])
    S.op('act', lambda e: e.activation(out=ss8, in_=ss8, func=AF.Sqrt, bias=EPS), writes=['s_ss8'])
    S.op('dve', lambda e: e.reciprocal(out=rs8, in_=ss8), reads=['s_ss8'], writes=['s_rs8'])
    S.op('dve', lambda e: e.tensor_scalar(out=rs8[:, 0:4], in0=rs8[:, 0:4], scalar1=128.0 ** -0.5, scalar2=None, op0=ALU.mult), writes=['s_rs8'])
    S.op('dve', lambda e: e.tensor_tensor(out=qk, in0=cc[:, 0:1024].rearrange("p (h d) -> p h d", h=8), in1=rs8.unsqueeze(2).broadcast_to([NS, 8, 128]), op=ALU.mult),
         reads=['s_cc', 's_rs8'], writes=['s_qk'])
    vv = cc[:, 1024:1536].rearrange("p (h d) -> p h d", h=4)
    beta = A.f32([NS, 4])
    gx = A.f32([NS, 4])
    eg = A.f32([NS, 4])
    S.op('act', lambda e: e.activation(out=beta, in_=ba[:, 0:4], func=AF.Sigmoid), reads=['s_ba'], writes=['s_beta'])
    S.op('dve', lambda e: e.tensor_tensor(out=gx, in0=ba[:, 4:8], in1=dtb, op=ALU.add), reads=['s_ba', 's_dtb'], writes=['s_gx'])
    S.op('act', lambda e: e.activation(out=gx, in_=gx, func=AF.Exp), writes=['s_gx'])
    S.op('act', lambda e: e.activation(out=gx, in_=gx, func=AF.Ln, bias=1.0), writes=['s_gx'])
    S.op('dve', lambda e: e.tensor_tensor(out=gx, in0=gx, in1=negA, op=ALU.mult), reads=['s_negA'], writes=['s_gx'])
    S.op('act', lambda e: e.activation(out=eg, in_=gx, func=AF.Exp), reads=['s_gx'], writes=['s_eg'])
    kqT = A.f32([128, 8, NS])

    def trkq(e):
        for j in range(8):
            ins = e.transpose(out=PB(0)[:, j * NS:(j + 1) * NS], in_=qk[:, j, :], identity=id16)
        return ins
    S.op('pe', trkq, reads=['s_qk', 'cst'], writes=[pk(0)])
    S.op('dve', lambda e: e.tensor_copy(out=kqT, in_=PB(0)[:, 0:8 * NS].rearrange("p (j s) -> p j s", j=8)), writes=[pk(0), 's_kqT'])
    egd = A.f32([NS, NS, 4])
    S.op('dve', lambda e: e.tensor_tensor(out=egd, in0=id16.unsqueeze(2).broadcast_to([NS, NS, 4]), in1=eg.unsqueeze(1).broadcast_to([NS, NS, 4]), op=ALU.mult),
         reads=['cst', 's_eg'], writes=['s_egd'])
    S.op('pe', lambda e: e.matmul(PB(1)[:, 0:NS * 4], lhsT=ones[0:NS, :], rhs=egd.rearrange("p s h -> p (s h)"), start=True, stop=True),
         reads=['s_egd', 'cst'], writes=[pk(1)])
    egb = A.f32([128, NS, 4])
    S.op('dve', lambda e: e.tensor_copy(out=egb, in_=PB(1)[:, 0:NS * 4].rearrange("p (s h) -> p s h", s=NS)), writes=[pk(1), 's_egb'])
    Sold = [A.f32([128, 4, 128]) for _ in range(3)]
    Snew = [A.f32([128, 4, 128]) for _ in range(3)]
    kSacc = A.f32([NS, 4, 128])
    oacc = A.f32([NS, 4, 128])
    S.op('pool', lambda e: e.memset(kSacc, 0.0), writes=['s_kSacc'])
    S.op('pool', lambda e: e.memset(oacc, 0.0), writes=['s_oacc'])
    for s_ in range(NS):
        bi = s_ % 3
        DMA('sp', Sold[bi], st_gdn[s_].rearrange("h k v -> k h v"), w=['s_Sold%d' % bi])

        def mm1(e, bi=bi):
            for h in range(4):
                ins = e.matmul(PB(2)[0:NS, h * 128:(h + 1) * 128], lhsT=kqT[:, 4 + h, :], rhs=Sold[bi][:, h, :], start=True, stop=True)
            return ins
        S.op('pe', mm1, reads=['s_kqT', 's_Sold%d' % bi], writes=[pk(2)])
        S.op('dve', lambda e, s_=s_: e.scalar_tensor_tensor(out=kSacc.rearrange("p h d -> p (h d)"), in0=PB(2)[0:NS, :], scalar=id16[:, s_:s_ + 1],
                                                            in1=kSacc.rearrange("p h d -> p (h d)"), op0=ALU.mult, op1=ALU.add),
             reads=['cst'], writes=[pk(2), 's_kSacc'])
    vn = A.f32([NS, 4, 128])
    S.op('dve', lambda e: e.tensor_tensor(out=vn, in0=kSacc, in1=eg.unsqueeze(2).broadcast_to([NS, 4, 128]), op=ALU.mult), reads=['s_kSacc', 's_eg'], writes=['s_vn'])
    S.op('dve', lambda e: e.tensor_tensor(out=vn, in0=vv, in1=vn, op=ALU.subtract), reads=['s_cc'], writes=['s_vn'])
    S.op('dve', lambda e: e.tensor_tensor(out=vn, in0=vn, in1=beta.unsqueeze(2).broadcast_to([NS, 4, 128]), op=ALU.mult), reads=['s_beta'], writes=['s_vn'])
    vm = [A.f32([NS, 4, 128]) for _ in range(2)]
    stmp = [A.f32([128, 4, 128]) for _ in range(2)]
    for s_ in range(NS):
        bi = s_ % 3
        b2 = s_ % 2
        DMA('sp', Sold[bi], st_gdn[s_].rearrange("h k v -> k h v"), w=['s_Sold%d' % bi])
        S.op('dve', lambda e, s_=s_, b2=b2: e.tensor_scalar(out=vm[b2], in0=vn, scalar1=id16[:, s_:s_ + 1], scalar2=None, op0=ALU.mult),
             reads=['s_vn', 'cst'], writes=['s_vm%d' % b2])

        def mm2(e, b2=b2):
            for h in range(4):
                ins = e.matmul(PB(3)[:, h * 128:(h + 1) * 128], lhsT=qk[:, 4 + h, :], rhs=vm[b2][:, h, :], start=True, stop=True)
            return ins
        S.op('pe', mm2, reads=['s_qk', 's_vm%d' % b2], writes=[pk(3)])
        S.op('pool', lambda e, bi=bi, b2=b2, s_=s_: e.tensor_tensor(out=stmp[b2], in0=Sold[bi], in1=egb[:, s_, :].unsqueeze(2).broadcast_to([128, 4, 128]), op=ALU.mult),
             reads=['s_Sold%d' % bi, 's_egb'], writes=['s_stmp%d' % b2])
        S.op('dve', lambda e, bi=bi, b2=b2: e.tensor_tensor(out=Snew[bi], in0=stmp[b2], in1=PB(3).rearrange("p (h d) -> p h d", h=4), op=ALU.add),
             reads=['s_stmp%d' % b2], writes=[pk(3), 's_Snew%d' % bi])
        DMA('pool', sgs[s_].rearrange("h k v -> k h v"), Snew[bi], r=['s_Snew%d' % bi], w=[])

        def mm3(e, bi=bi):
            for h in range(4):
                ins = e.matmul(PB(4)[0:NS, h * 128:(h + 1) * 128], lhsT=kqT[:, h, :], rhs=Snew[bi][:, h, :], start=True, stop=True)
            return ins
        S.op('pe', mm3, reads=['s_kqT', 's_Snew%d' % bi], writes=[pk(4)])
        S.op('dve', lambda e, s_=s_: e.scalar_tensor_tensor(out=oacc.rearrange("p h d -> p (h d)"), in0=PB(4)[0:NS, :], scalar=id16[:, s_:s_ + 1],
                                                            in1=oacc.rearrange("p h d -> p (h d)"), op0=ALU.mult, op1=ALU.add),
             reads=['cst'], writes=[pk(4), 's_oacc'])
    mixs = A.f32([NS, 1024])
    o4 = A.f32([NS, 4])
    S.op('act', lambda e: e.activation(out=sq[:, 0:512], in_=oacc.rearrange("p h d -> p (h d)"), func=AF.Square), reads=['s_oacc'], writes=['s_sq'])
    S.op('dve', lambda e: e.tensor_reduce(out=o4, in_=sq[:, 0:512].rearrange("p (h d) -> p h d", h=4), axis=AX.X, op=ALU.add), reads=['s_sq'], writes=['s_o4'])
    S.op('act', lambda e: e.activation(out=o4, in_=o4, func=AF.Sqrt, scale=1.0 / 128, bias=EPS), writes=['s_o4'])
    S.op('dve', lambda e: e.reciprocal(out=o4, in_=o4), writes=['s_o4'])
    m3 = mixs[:, 0:512].rearrange("p (h d) -> p h d", h=4)
    S.op('dve', lambda e: e.tensor_tensor(out=m3, in0=oacc, in1=o4.unsqueeze(2).broadcast_to([NS, 4, 128]), op=ALU.mult), reads=['s_oacc', 's_o4'], writes=['s_mix'])
    S.op('dve', lambda e: e.tensor_tensor(out=m3, in0=m3, in1=gnb.unsqueeze(1).broadcast_to([NS, 4, 128]), op=ALU.mult), reads=['s_gnb'], writes=['s_mix'])
    S.op('act', lambda e: e.activation(out=zs, in_=zs, func=AF.Silu), writes=['s_z'])
    S.op('dve', lambda e: e.tensor_tensor(out=mixs[:, 0:512], in0=mixs[:, 0:512], in1=zs, op=ALU.mult), reads=['s_z'], writes=['s_mix'])
    qkw = A.f32([NS, 6, 64])
    for gi in range(3):
        DMA('sp', qkw[:, gi, :], qnw[gi:gi + 1, :].partition_broadcast(NS), w=['s_qkw'])
        DMA('sp', qkw[:, 3 + gi, :], knw[gi:gi + 1, :].partition_broadcast(NS), w=['s_qkw'])
    rp = A.f32([NS, 16])
    DMA('sp', rp, rope_t[4096:4096 + NS, :], w=['s_rp'])
    cosb = rp[:, 0:8].unsqueeze(1).broadcast_to([NS, 8, 8])
    sinb = rp[:, 8:16].unsqueeze(1).broadcast_to([NS, 8, 8])
    qkv = A.f32([NS, 3, 3, 512])
    DMA('sp', qkv.rearrange("p g w c -> p (g w c)"), proj[R0:R0 + NS, 2056:2056 + 4608], w=['s_qkv'])
    s2 = A.f32([NS, 512])
    r8 = A.f32([NS, 8])
    rt = [A.f32([NS, 8, 8]) for _ in range(4)]
    for gi in range(3):
        for which in range(2):
            d3 = qkv[:, gi, which, :].rearrange("p (h d) -> p h d", h=8)
            S.op('act', lambda e, gi=gi, which=which: e.activation(out=s2, in_=qkv[:, gi, which, :], func=AF.Square), reads=['s_qkv'], writes=['s_s2'])
            S.op('dve', lambda e: e.tensor_reduce(out=r8, in_=s2.rearrange("p (h d) -> p h d", h=8), axis=AX.X, op=ALU.add), reads=['s_s2'], writes=['s_r8'])
            S.op('act', lambda e: e.activation(out=r8, in_=r8, func=AF.Sqrt, scale=1.0 / 64, bias=EPS), writes=['s_r8'])
            S.op('dve', lambda e: e.reciprocal(out=r8, in_=r8), writes=['s_r8'])
            S.op('dve', lambda e, d3=d3: e.tensor_tensor(out=d3, in0=d3, in1=r8.unsqueeze(2).broadcast_to([NS, 8, 64]), op=ALU.mult), reads=['s_r8'], writes=['s_qkv'])
            wrow = qkw[:, (3 if which == 1 else 0) + gi, :].unsqueeze(1).broadcast_to([NS, 8, 64])
            S.op('dve', lambda e, d3=d3, wrow=wrow: e.tensor_tensor(out=d3, in0=d3, in1=wrow, op=ALU.mult), reads=['s_qkw'], writes=['s_qkv'])
            x1 = d3[:, :, 0:8]
            x2 = d3[:, :, 8:16]
            S.op('dve', lambda e, x1=x1: e.tensor_tensor(out=rt[0], in0=x1, in1=cosb, op=ALU.mult), reads=['s_qkv', 's_rp'], writes=['s_rt0'])
            S.op('dve', lambda e, x2=x2: e.tensor_tensor(out=rt[1], in0=x2, in1=sinb, op=ALU.mult), reads=['s_qkv', 's_rp'], writes=['s_rt1'])
            S.op('dve', lambda e, x2=x2: e.tensor_tensor(out=rt[2], in0=x2, in1=cosb, op=ALU.mult), reads=['s_qkv', 's_rp'], writes=['s_rt2'])
            S.op('dve', lambda e, x1=x1: e.tensor_tensor(out=rt[3], in0=x1, in1=sinb, op=ALU.mult), reads=['s_qkv', 's_rp'], writes=['s_rt3'])
            S.op('dve', lambda e, x1=x1: e.tensor_tensor(out=x1, in0=rt[0], in1=rt[1], op=ALU.subtract), reads=['s_rt0', 's_rt1'], writes=['s_qkv'])
            S.op('dve', lambda e, x2=x2: e.tensor_tensor(out=x2, in0=rt[2], in1=rt[3], op=ALU.add), reads=['s_rt2', 's_rt3'], writes=['s_qkv'])
        W = GROUPS[gi][0]
        DMA('pool', kvs[gi][:, W - 1, :], qkv[:, gi, 1:3, :].rearrange("p w c -> p (w c)"), r=['s_qkv'], w=['kvs_new%d' % gi])
    selq = A.f32([NS, NS, 128])
    S.op('dve', lambda e: e.tensor_copy(out=selq, in_=id16.unsqueeze(2).broadcast_to([NS, NS, 128])), reads=['cst'], writes=['s_selq'])
    selh_t = A.f32([8, NS, NS])
    DMA('sp', selh_t, selh, w=['s_selh'])
    kvt = [A.f32([128, 1024]) for _ in range(3)]
    prod = A.f32([128, 512])
    sc = A.f32([128, 8])
    pex = A.f32([128, 8])
    Z = A.f32([8, 8, 64])
    Z2 = A.f32([8, 8])
    numS = A.f32([NS, 8, 64])
    denS = A.f32([NS, 8])
    ps_ = A.f32([NS, 8])
    pr16 = A.f32([NS, 8, 64])
    S.op('pool', lambda e: e.memset(numS, 0.0), writes=['s_numS'])
    S.op('pool', lambda e: e.memset(denS, 0.0), writes=['s_denS'])
    cnt = 0
    for gi in range(3):
        W, dil = GROUPS[gi]
        q3 = qkv[:, gi, 0, :].rearrange("p (h d) -> p h d", h=8)
        k3 = qkv[:, gi, 1, :].rearrange("p (h d) -> p h d", h=8)
        v3 = qkv[:, gi, 2, :].rearrange("p (h d) -> p h d", h=8)
        S.op('dve', lambda e, q3=q3, k3=k3: e.tensor_tensor(out=pr16, in0=q3, in1=k3, op=ALU.mult), reads=['s_qkv'], writes=['s_pr16'])
        S.op('dve', lambda e: e.tensor_reduce(out=ps_, in_=pr16, axis=AX.X, op=ALU.add), reads=['s_pr16'], writes=['s_ps'])
        S.op('act', lambda e: e.activation(out=ps_, in_=ps_, func=AF.Exp, scale=0.125), writes=['s_ps'])
        S.op('dve', lambda e: e.tensor_tensor(out=denS, in0=denS, in1=ps_, op=ALU.add), reads=['s_ps'], writes=['s_denS'])
        S.op('dve', lambda e, v3=v3: e.tensor_tensor(out=pr16, in0=v3, in1=ps_.unsqueeze(2).broadcast_to([NS, 8, 64]), op=ALU.mult), reads=['s_qkv', 's_ps'], writes=['s_pr16'])
        S.op('dve', lambda e: e.tensor_tensor(out=numS, in0=numS, in1=pr16, op=ALU.add), reads=['s_pr16'], writes=['s_numS'])
        for s_ in range(NS):
            bi = cnt % 3
            first = (cnt == 0)
            last = (cnt == 3 * NS - 1)
            cnt += 1
            KV = kvt[bi]
            kkv = 's_kvt%d' % bi
            DMA('sp', KV, caches[gi][s_, 0:W - dil + 1:dil, :], w=[kkv])
            S.op('pe', lambda e, s_=s_, gi=gi: e.matmul(PB(0), lhsT=selq[:, s_, :], rhs=qkv[:, gi, 0, :], start=True, stop=True), reads=['s_selq', 's_qkv'], writes=[pk(0)])
            S.op('dve', lambda e, KV=KV: e.tensor_tensor(out=prod, in0=KV[:, 0:512], in1=PB(0), op=ALU.mult), reads=[kkv], writes=[pk(0), 's_prod'])
            S.op('dve', lambda e: e.tensor_reduce(out=sc, in_=prod.rearrange("p (h d) -> p h d", h=8), axis=AX.X, op=ALU.add), reads=['s_prod'], writes=['s_sc'])
            S.op('act', lambda e: e.activation(out=pex, in_=sc, func=AF.Exp, scale=0.125), reads=['s_sc'], writes=['s_pex'])
            S.op('pe', lambda e, KV=KV: e.matmul(PB(1)[0:8, :], lhsT=pex, rhs=KV[:, 512:1024], start=True, stop=True), reads=['s_pex', kkv], writes=[pk(1)])
            S.op('pe', lambda e: e.matmul(PB(2)[0:8, 0:8], lhsT=pex, rhs=ones[:, 0:8], start=True, stop=True), reads=['s_pex', 'cst'], writes=[pk(2)])
            S.op('dve', lambda e: e.tensor_tensor(out=Z, in0=PB(1)[0:8, :].rearrange("p (h d) -> p h d", h=8), in1=ident[0:8, 0:8].unsqueeze(2).broadcast_to([8, 8, 64]), op=ALU.mult),
                 reads=['cst'], writes=[pk(1), 's_Z'])
            S.op('dve', lambda e: e.tensor_tensor(out=Z2, in0=PB(2)[0:8, 0:8], in1=ident[0:8, 0:8], op=ALU.mult), reads=['cst'], writes=[pk(2), 's_Z2'])
            S.op('pe', lambda e, s_=s_, first=first, last=last: e.matmul(PB(6)[0:NS, :], lhsT=selh_t[:, s_, :], rhs=Z.rearrange("p h d -> p (h d)"), start=first, stop=last),
                 reads=['s_selh', 's_Z'], writes=[pk(6)])
            S.op('pe', lambda e, s_=s_, first=first, last=last: e.matmul(PB(7)[0:NS, 0:8], lhsT=selh_t[:, s_, :], rhs=Z2, start=first, stop=last),
                 reads=['s_selh', 's_Z2'], writes=[pk(7)])
    S.op('dve', lambda e: e.tensor_tensor(out=numS, in0=numS, in1=PB(6)[0:NS, :].rearrange("p (h d) -> p h d", h=8), op=ALU.add), writes=[pk(6), 's_numS'])
    S.op('dve', lambda e: e.tensor_tensor(out=denS, in0=denS, in1=PB(7)[0:NS, 0:8], op=ALU.add), writes=[pk(7), 's_denS'])
    S.op('dve', lambda e: e.reciprocal(out=denS, in_=denS), writes=['s_denS'])
    S.op('dve', lambda e: e.tensor_tensor(out=mixs[:, 512:1024].rearrange("p (h d) -> p h d", h=8), in0=numS, in1=denS.unsqueeze(2).broadcast_to([NS, 8, 64]), op=ALU.mult),
         reads=['s_numS', 's_denS'], writes=['s_mix'])
    DMA('pool', mix_d[2048:2048 + NS, :], mixs, r=['s_mix'], w=['mix_d_s'])
    for j in range(2):
        DMA('pool', mix_d[2048 + NS:2176, j * 512:(j + 1) * 512], zero_t[0:128 - NS, 0:512], r=['zero'], w=['mix_d_z%d' % j])
    S.barrier()


_CACHE = {}


def _host_consts():
    c = np.zeros((128, 5, 128), np.float32)
    p = np.arange(128)[:, None]
    f = np.arange(128)[None, :]
    c[:, 0] = (p == f)
    c[:, 1] = (p <= f)
    c[:, 2] = (p >= f)
    c[:, 3] = (p < f)
    c[:, 4] = 1.0
    return c


def _rope_table(pos):
    half = 8
    inv = np.exp(-math.log(500000.0) * np.arange(half, dtype=np.float32) * np.float32(2.0 / 16)).astype(np.float32)
    ang = pos.astype(np.float32)[:, None] * inv[None, :]
    return np.concatenate([np.cos(ang), np.sin(ang)], axis=1).astype(np.float32)


def kernel(x_prompt, x_sample, state_gdn, state_conv, cache_kv_w128, cache_kv_w512, cache_kv_w2048,
           norm1_w, w_in, conv_w, a_log, dt_bias, gdn_norm_w, q_norm_w, k_norm_w, w_out, norm2_w,
           w_router_group, w_router_expert, w_gate_up, w_down):
    f = lambda a: np.ascontiguousarray(np.asarray(a, dtype=np.float32))
    x_prompt, x_sample = f(x_prompt), f(x_sample)
    if 'nc' not in _CACHE:
        _CACHE['nc'] = build_program()
    nc = _CACHE['nc']
    consts = _host_consts()
    w_r = np.concatenate([f(w_router_group)[0], f(w_router_expert)[0]], axis=1)
    shared = {
        "w_in": f(w_in)[0], "w_out": f(w_out)[0], "norm1": f(norm1_w), "norm2": f(norm2_w),
        "conv_wT": np.ascontiguousarray(f(conv_w)[0].T), "a_log": f(a_log), "dt_bias": f(dt_bias), "gnw": f(gdn_norm_w),
        "qnw": f(q_norm_w)[0], "knw": f(k_norm_w)[0], "w_r": np.ascontiguousarray(w_r), "w_gu": f(w_gate_up)[0], "w_d": f(w_down)[0],
        "consts": consts,
        "selh": np.ascontiguousarray(np.broadcast_to(np.eye(16, dtype=np.float32)[None], (8, 16, 16))),
    }
    cachesf = [f(cache_kv_w128)[0], f(cache_kv_w512)[0], f(cache_kv_w2048)[0]]
    sg, sc = f(state_gdn)[0], f(state_conv)[0]
    in_maps = []
    for c in range(NCORE):
        b, half = c // 2, c % 2
        xa = np.zeros((NTILE * 128, D), np.float32)
        if half == 1:
            xa[0:2048] = x_prompt[b, 0:2048]
        xa[2048:4096] = x_prompt[b, half * 2048:(half + 1) * 2048]
        xa[4096:4112] = x_sample[16 * c:16 * c + 16, 0]
        pos = np.concatenate([np.arange(4096) + (half - 1) * 2048, np.full(128, 8192)]).astype(np.float32)
        m = dict(shared)
        m["x_all"] = xa
        m["rope_t"] = _rope_table(pos)
        m["maskb"] = np.full((128, 1), 0.0 if half == 1 else -30000.0, np.float32)
        m["st_gdn"] = np.ascontiguousarray(sg[16 * c:16 * c + 16])
        m["st_conv"] = np.ascontiguousarray(sc[16 * c:16 * c + 16])
        for i in range(3):
            m["cache%d" % i] = np.ascontiguousarray(cachesf[i][16 * c:16 * c + 16].reshape(16, GROUPS[i][0], 1024))
        in_maps.append(m)
    if _CACHE.get('only_maps'):
        return in_maps
    res = run_bass_kernel_spmd(nc, in_maps, core_ids=list(range(NCORE)))
    R = res.results
    y_prompt = np.zeros((4, 4096, D), np.float32)
    for c in range(NCORE):
        y_prompt[c // 2, (c % 2) * 2048:(c % 2 + 1) * 2048] = R[c]["y_own"]
    y_sample = np.concatenate([R[c]["y_samp"][0:16] for c in range(NCORE)], axis=0).reshape(128, 1, D)
    sgp_o = np.stack([R[2 * b + 1]["sgp"] for b in range(4)])[None]
    scp_o = np.stack([R[2 * b + 1]["scp"] for b in range(4)])[None]
    kvp_o = [np.stack([R[2 * b + 1]["kvp%d" % i] for b in range(4)]).reshape(1, 4, GROUPS[i][0], 2, 8, 64) for i in range(3)]
    sgs_o = np.concatenate([R[c]["sgs"] for c in range(NCORE)], axis=0)[None]
    scs_o = np.concatenate([R[c]["scs"] for c in range(NCORE)], axis=0)[None]
    kvs_o = [np.concatenate([R[c]["kvs%d" % i] for c in range(NCORE)], axis=0).reshape(1, 128, GROUPS[i][0], 2, 8, 64) for i in range(3)]
    return (y_prompt, y_sample, sgp_o, scp_o, kvp_o[0], kvp_o[1], kvp_o[2], sgs_o, scs_o, kvs_o[0], kvs_o[1], kvs_o[2])
```

```python
import math
import numpy as np
import concourse.bass as bass
import concourse.mybir as mybir
from concourse.bass_utils import run_bass_kernel_spmd

F32 = mybir.dt.float32
BF16 = mybir.dt.bfloat16
AF = mybir.ActivationFunctionType
ALU = mybir.AluOpType
AX = mybir.AxisListType

NDMA = 32
NCORE = 8
D = 1024
INC = 6664
NTILE = 33
EPS = 1e-6
GROUPS = ((128, 1), (512, 4), (2048, 16))
COLBLK = [(0, 512), (512, 512), (1024, 512), (1536, 512), (2048, 8)]
for _g in range(3):
    _b = 2056 + _g * 1536
    COLBLK += [(_b, 512), (_b + 512, 512), (_b + 1024, 512)]
HALO_SKIP = {3, 5, 8, 11}


class Sched:
    ENG = ['pe', 'act', 'dve', 'pool', 'sp']

    def __init__(self, nc):
        self.nc = nc
        self.ops = {e: [] for e in self.ENG}
        self.esem = {e: nc.alloc_semaphore(name="s_" + e) for e in self.ENG}
        self.ecnt = {e: 0 for e in self.ENG}
        self.dpool = {'sp': list(range(0, 10)), 'pool': list(range(12, 20)), 'act': list(range(20, 24)), 'spc': list(range(24, 29))}
        self.dsem = [nc.alloc_semaphore(name="d%d" % i) for i in range(NDMA)]
        self.dcnt = [0] * NDMA
        self.dnext = {'sp': 0, 'pool': 0, 'act': 0, 'spc': 0}
        self.last_w = {}
        self.readers = {}
        self.waited = {e: {} for e in self.ENG}
        self.defer = None

    def _tok_waits(self, e, deps):
        waits = []
        for (sem, val) in deps:
            sid = id(sem)
            if e == 'pe' and sem is self.esem['pe']:
                continue
            if self.waited[e].get(sid, 0) < val:
                self.waited[e][sid] = val
                waits.append((sem, val))
        return waits

    def op(self, e, fn, reads=(), writes=(), dma=False, pool=None):
        if self.defer is not None:
            self.defer.append((e, fn, tuple(reads), tuple(writes), dma, pool))
            return None
        deps = []
        for k in reads:
            if k in self.last_w:
                deps.append(self.last_w[k])
        for k in writes:
            if k in self.last_w:
                deps.append(self.last_w[k])
            for sid, tk in self.readers.get(k, {}).items():
                deps.append(tk)
        if dma:
            pn = pool or e
            pl = self.dpool[pn]
            s = pl[self.dnext[pn] % len(pl)]
            self.dnext[pn] += 1
            if self.dcnt[s] > 0:
                deps.append((self.dsem[s], 16 * self.dcnt[s]))
            self.dcnt[s] += 1
            tok = (self.dsem[s], 16 * self.dcnt[s])
            inc = 16
        else:
            self.ecnt[e] += 1
            tok = (self.esem[e], self.ecnt[e])
            inc = 1
        waits = self._tok_waits(e, deps)
        for k in writes:
            self.last_w[k] = tok
            self.readers[k] = {}
        for k in reads:
            r = self.readers.setdefault(k, {})
            sid = id(tok[0])
            if sid not in r or r[sid][1] < tok[1]:
                r[sid] = tok
        self.ops[e].append((fn, waits, (tok[0], inc)))
        return tok

    def replay(self, lists, weights):
        assert self.defer is None
        pos = [0] * len(lists)
        while any(pos[i] < len(lists[i]) for i in range(len(lists))):
            for i, lst in enumerate(lists):
                for _ in range(weights[i]):
                    if pos[i] < len(lst):
                        self.op(*lst[pos[i]])
                        pos[i] += 1

    def barrier(self):
        toks = [(self.esem[e], self.ecnt[e]) for e in self.ENG if self.ecnt[e] > 0]
        toks += [(self.dsem[s], 16 * self.dcnt[s]) for s in range(NDMA) if self.dcnt[s] > 0]
        for e in self.ENG:
            waits = self._tok_waits(e, [t for t in toks if not (t[0] is self.esem[e])])
            if waits:
                self.ops[e].append((None, waits, None))
        self.last_w = {}
        self.readers = {}

    def emit(self):
        nc = self.nc
        self.barrier()
        with nc.Block() as block:
            def mk(e):
                def body(engine):
                    for (fn, waits, inc) in self.ops[e]:
                        for (sem, val) in waits:
                            engine.wait_ge(sem, val)
                        if fn is not None:
                            ins = fn(engine)
                            ins.then_inc(inc[0], inc[1])
                return body
            block.tensor(mk('pe'))
            block.scalar(mk('act'))
            block.vector(mk('dve'))
            block.gpsimd(mk('pool'))
            block.sync(mk('sp'))


def _shape_view(v, shape):
    if len(shape) == 2:
        return v
    if len(shape) == 3:
        return v.rearrange("p (a b) -> p a b", a=shape[1])
    return v.rearrange("p (a b c) -> p a b c", a=shape[1], b=shape[2])


class Arena:
    def __init__(self, ap, nwords):
        self.ap = ap
        self.n = nwords
        self.off = 0

    def f32(self, shape):
        n = int(np.prod(shape[1:]))
        o = self.off
        self.off += n
        assert self.off <= self.n, ("arena overflow", self.off, self.n)
        return _shape_view(self.ap[0:shape[0], o:o + n], shape)

    def bf16(self, shape):
        n = int(np.prod(shape[1:]))
        nw = (n + 1) // 2
        o = self.off
        self.off += nw
        assert self.off <= self.n, ("arena overflow", self.off, self.n)
        return _shape_view(self.ap[0:shape[0], o:o + nw].bitcast(BF16)[:, 0:n], shape)


def build_program(stop_after=None, debug=False):
    nc = bass.Bass("TRN2", target_bir_lowering=False)

    def din(name, shape):
        return nc.dram_tensor(name, list(shape), F32, kind="ExternalInput").ap()

    def dout(name, shape):
        return nc.dram_tensor(name, list(shape), F32, kind="ExternalOutput").ap()

    def dscr(name, shape):
        return nc.dram_tensor(name, list(shape), F32, kind=("ExternalOutput" if debug else "Internal")).ap()

    x_all = din("x_all", [NTILE * 128, D])
    w_in = din("w_in", [D, INC])
    w_out = din("w_out", [D, D])
    norm1 = din("norm1", [1, D])
    norm2 = din("norm2", [1, D])
    conv_wT = din("conv_wT", [4, 1536])
    a_log = din("a_log", [1, 4])
    dt_bias = din("dt_bias", [1, 4])
    gnw = din("gnw", [1, 128])
    qnw = din("qnw", [3, 64])
    knw = din("knw", [3, 64])
    w_r = din("w_r", [D, 36])
    w_gu = din("w_gu", [32, D, 512])
    w_d = din("w_d", [32, 256, D])
    consts = din("consts", [128, 5, 128])
    rope_t = din("rope_t", [NTILE * 128, 16])
    maskb = din("maskb", [128, 1])
    selh = din("selh", [8, 16, 16])
    st_gdn = din("st_gdn", [16, 4, 128, 128])
    st_conv = din("st_conv", [16, 3, 1536])
    caches = [din("cache%d" % i, [16, GROUPS[i][0], 1024]) for i in range(3)]

    y_own = dout("y_own", [2048, D])
    y_samp = dout("y_samp", [128, D])
    sgp = dout("sgp", [4, 128, 128])
    scp = dout("scp", [3, 1536])
    kvp = [dout("kvp%d" % i, [GROUPS[i][0], 2, 512]) for i in range(3)]
    sgs = dout("sgs", [16, 4, 128, 128])
    scs = dout("scs", [16, 3, 1536])
    kvs = [dout("kvs%d" % i, [16, GROUPS[i][0], 1024]) for i in range(3)]

    proj = dscr("proj", [3 + NTILE * 128, INC])
    gdn_in = dscr("gdn_in", [32, 128, 1544])
    o_gdn = dscr("o_gdn", [2048, 512])
    kn_s = dscr("kn_s", [3, 4096, 512])
    qn_s = dscr("qn_s", [3, 2048, 512])
    att_o = dscr("att_o", [3, 2048, 520])
    mix_d = dscr("mix_d", [17 * 128, D])

    S = Sched(nc)
    AW = 48500
    sb_all = nc.alloc_sbuf_tensor("arena", [128, AW], F32).ap()
    ps_all = nc.alloc_psum_tensor("psarena", [128, 4096], F32).ap()
    A = Arena(sb_all, AW)

    def PB(i):
        return ps_all[:, i * 512:(i + 1) * 512]

    def PBb(i):
        return ps_all[:, i * 512:(i + 1) * 512].bitcast(BF16)

    def pk(i):
        return 'PS%d' % i

    def DMA(eng, out, in_, r=(), w=()):
        S.op(eng, lambda e: e.dma_start(out=out, in_=in_), reads=r, writes=w, dma=True)

    cst = A.f32([128, 5, 128])
    DMA('sp', cst, consts, w=['cst'])
    ident = cst[:, 0, :]
    U_incl = cst[:, 1, :]
    L_incl = cst[:, 2, :]
    U_strict = cst[:, 3, :]
    ones = cst[:, 4, :]
    identb = A.bf16([128, 128])
    S.op('dve', lambda e: e.tensor_copy(out=identb, in_=ident), reads=['cst'], writes=['identb'])
    maskb_t = A.f32([128, 1])
    DMA('sp', maskb_t, maskb, w=['maskb'])
    zero_t = A.f32([128, 1024])
    S.op('pool', lambda e: e.memset(zero_t, 0.0), writes=['zero'])
    base0 = A.off

    copy_jobs = []
    for s_ in range(16):
        for ch in range(4):
            r0 = ch * 512
            r1 = min(r0 + 512, 2047)
            copy_jobs.append((2, s_, r0, r1))
        copy_jobs.append((1, s_, 0, 511))
        copy_jobs.append((0, s_, 0, 127))

    def issue_copies(n):
        for _ in range(n):
            if not copy_jobs:
                return
            gi, s_, r0, r1 = copy_jobs.pop(0)
            S.op('sp', lambda e, gi=gi, s_=s_, r0=r0, r1=r1: e.dma_start(out=kvs[gi][s_, r0:r1, :], in_=caches[gi][s_, r0 + 1:r1 + 1, :]),
                 writes=[], dma=True, pool='spc')
    S.op('act', lambda e: e.dma_start(out=scs[:, 0:2, :], in_=st_conv[:, 1:3, :]), writes=['scs01'], dma=True)

    xnT = A.bf16([128, 8, NTILE * 128])
    w1b = A.f32([128, D])
    DMA('sp', w1b, norm1.partition_broadcast(128), w=['w1b'])
    xt = [A.f32([128, D]) for _ in range(2)]
    sq = A.f32([128, D])
    ss = A.f32([128, 1])
    rstd = A.f32([128, 1])
    xn = A.bf16([128, D])
    for t in range(NTILE):
        xx = xt[t % 2]
        kx = 'xt%d' % (t % 2)
        DMA('sp', xx, x_all[t * 128:(t + 1) * 128, :], w=[kx])
        S.op('act', lambda e, xx=xx: e.activation(out=sq, in_=xx, func=AF.Square, accum_out=ss), reads=[kx], writes=['sq', 'ss'])
        S.op('act', lambda e: e.activation(out=ss, in_=ss, func=AF.Sqrt, scale=1.0 / D, bias=EPS), writes=['ss'])
        S.op('dve', lambda e: e.reciprocal(out=rstd, in_=ss), reads=['ss'], writes=['rstd'])
        S.op('dve', lambda e, xx=xx: e.scalar_tensor_tensor(out=xn, in0=xx, scalar=rstd, in1=w1b, op0=ALU.mult, op1=ALU.mult),
             reads=[kx, 'rstd', 'w1b'], writes=['xn'])
        pb = t % 2

        def tr(e, pb=pb):
            for k in range(8):
                ins = e.transpose(out=PBb(pb)[:, k * 128:(k + 1) * 128], in_=xn[:, k * 128:(k + 1) * 128], identity=identb)
            return ins
        S.op('pe', tr, reads=['xn', 'identb'], writes=[pk(pb)])
        S.op('act', lambda e, t=t, pb=pb: e.activation(out=xnT[:, :, t * 128:(t + 1) * 128],
                                                       in_=PBb(pb).rearrange("p (k t) -> p k t", k=8), func=AF.Copy),
             writes=[pk(pb), 'xnT%d' % t])

    DMA('pool', proj[0:3, 0:1024], zero_t[0:3, :], r=['zero'], w=['projz'])
    DMA('pool', proj[0:3, 1024:1536], zero_t[0:3, 0:512], r=['zero'], w=['projz2'])
    wst = [A.f32([128, 8, 512]) for _ in range(2)]
    wbf = [A.bf16([128, 8, 512]) for _ in range(2)]
    ot = [A.f32([128, 512]) for _ in range(6)]
    oc = 0
    for cb, (c0, ncol) in enumerate(COLBLK):
        wi = cb % 2
        DMA('sp', wst[wi][:, :, 0:ncol], w_in[:, c0:c0 + ncol].rearrange("(k p) c -> p k c", p=128), w=['wst%d' % wi])
        S.op('pool', lambda e, wi=wi, ncol=ncol: e.tensor_copy(out=wbf[wi][:, :, 0:ncol], in_=wst[wi][:, :, 0:ncol]),
             reads=['wst%d' % wi], writes=['wbf%d' % wi])
        for t in range(NTILE):
            if t < 16 and cb in HALO_SKIP:
                continue
            pb = 2 + (oc % 6)
            oi = oc % 6
            oc += 1

            def mm(e, t=t, wi=wi, ncol=ncol, pb=pb):
                for k in range(8):
                    ins = e.matmul(PB(pb)[:, 0:ncol], lhsT=xnT[:, k, t * 128:(t + 1) * 128], rhs=wbf[wi][:, k, 0:ncol],
                                   start=(k == 0), stop=(k == 7))
                return ins
            S.op('pe', mm, reads=['xnT%d' % t, 'wbf%d' % wi], writes=[pk(pb)])
            ee = 'act' if oc % 2 == 0 else 'dve'
            if ee == 'act':
                S.op('act', lambda e, oi=oi, pb=pb, ncol=ncol: e.activation(out=ot[oi][:, 0:ncol], in_=PB(pb)[:, 0:ncol], func=AF.Copy),
                     writes=[pk(pb), 'ot%d' % oi])
            else:
                S.op('dve', lambda e, oi=oi, pb=pb, ncol=ncol: e.tensor_copy(out=ot[oi][:, 0:ncol], in_=PB(pb)[:, 0:ncol]),
                     writes=[pk(pb), 'ot%d' % oi])
            DMA('pool', proj[3 + t * 128:3 + (t + 1) * 128, c0:c0 + ncol], ot[oi][:, 0:ncol], r=['ot%d' % oi], w=[])
    S.barrier()
    DMA('sp', scp, proj[3 + 4096 - 3:3 + 4096, 0:1536], w=['scp'])
    DMA('sp', scs[:, 2, :], proj[3 + 4096:3 + 4096 + 16, 0:1536], w=['scs2'])

    if stop_after == 'B':
        S.emit()
        return nc
    A.off = base0
    cw = A.f32([128, 4, 1536])
    for k in range(4):
        DMA('sp', cw[:, k, :], conv_wT[k:k + 1, :].partition_broadcast(128), w=['cw'])
    negA = A.f32([128, 4])
    dtb = A.f32([128, 4])
    DMA('sp', negA, a_log.partition_broadcast(128), w=['negA'])
    DMA('sp', dtb, dt_bias.partition_broadcast(128), w=['dtb'])
    S.op('act', lambda e: e.activation(out=negA, in_=negA, func=AF.Exp), writes=['negA'])
    S.op('dve', lambda e: e.tensor_scalar(out=negA, in0=negA, scalar1=-1.0, scalar2=None, op0=ALU.mult), writes=['negA'])
    xs = [[A.f32([128, 1536]) for _ in range(4)] for _ in range(2)]
    cc = A.f32([128, 1536])
    gt = [A.f32([128, 1544]) for _ in range(3)]
    sqc = A.f32([128, 1024])
    ssc = A.f32([128, 8])
    rsc = A.f32([128, 8])
    ba = [A.f32([128, 8]) for _ in range(2)]
    gx = A.f32([128, 4])
    def c_tile(t):
            bi = t % 2
            for k in range(4):
                DMA('sp', xs[bi][k], proj[t * 128 + k:t * 128 + k + 128, 0:1536], w=['xs%d_%d' % (bi, k)])
            DMA('sp', ba[bi], proj[3 + t * 128:3 + (t + 1) * 128, 2048:2056], w=['ba%d' % bi])
            for k in range(4):
                S.op('pool', lambda e, bi=bi, k=k: e.tensor_tensor(out=xs[bi][k], in0=xs[bi][k], in1=cw[:, k, :], op=ALU.mult),
                     reads=['cw'], writes=['xs%d_%d' % (bi, k)])
            S.op('pool', lambda e, bi=bi: e.tensor_tensor(out=cc, in0=xs[bi][0], in1=xs[bi][1], op=ALU.add), reads=['xs%d_0' % bi, 'xs%d_1' % bi], writes=['cc'])
            S.op('pool', lambda e, bi=bi: e.tensor_tensor(out=cc, in0=cc, in1=xs[bi][2], op=ALU.add), reads=['xs%d_2' % bi], writes=['cc'])
            S.op('pool', lambda e, bi=bi: e.tensor_tensor(out=cc, in0=cc, in1=xs[bi][3], op=ALU.add), reads=['xs%d_3' % bi], writes=['cc'])
            S.op('act', lambda e: e.activation(out=cc, in_=cc, func=AF.Silu), writes=['cc'])
            g_t = gt[t % 3]
            kg = 'gt%d' % (t % 3)
            S.op('act', lambda e: e.activation(out=sqc, in_=cc[:, 0:1024], func=AF.Square), reads=['cc'], writes=['sqc'])
            S.op('dve', lambda e: e.tensor_reduce(out=ssc, in_=sqc.rearrange("p (h d) -> p h d", h=8), axis=AX.X, op=ALU.add),
                 reads=['sqc'], writes=['ssc'])
            S.op('act', lambda e: e.activation(out=ssc, in_=ssc, func=AF.Sqrt, bias=EPS), writes=['ssc'])
            S.op('dve', lambda e: e.reciprocal(out=rsc, in_=ssc), reads=['ssc'], writes=['rsc'])
            S.op('dve', lambda e: e.tensor_scalar(out=rsc[:, 0:4], in0=rsc[:, 0:4], scalar1=128.0 ** -0.5, scalar2=None, op0=ALU.mult), writes=['rsc'])
            S.op('dve', lambda e, g_t=g_t: e.tensor_tensor(out=g_t[:, 0:1024].rearrange("p (h d) -> p h d", h=8),
                                                           in0=cc[:, 0:1024].rearrange("p (h d) -> p h d", h=8),
                                                           in1=rsc.unsqueeze(2).broadcast_to([128, 8, 128]), op=ALU.mult),
                 reads=['cc', 'rsc'], writes=[kg])
            S.op('pool', lambda e, g_t=g_t: e.tensor_copy(out=g_t[:, 1024:1536], in_=cc[:, 1024:1536]), reads=['cc'], writes=[kg])
            S.op('act', lambda e, g_t=g_t, bi=bi: e.activation(out=g_t[:, 1536:1540], in_=ba[bi][:, 0:4], func=AF.Sigmoid),
                 reads=['ba%d' % bi], writes=[kg])
            S.op('dve', lambda e, bi=bi: e.tensor_tensor(out=gx, in0=ba[bi][:, 4:8], in1=dtb, op=ALU.add), reads=['ba%d' % bi, 'dtb'], writes=['gx'])
            S.op('act', lambda e: e.activation(out=gx, in_=gx, func=AF.Exp), writes=['gx'])
            S.op('act', lambda e: e.activation(out=gx, in_=gx, func=AF.Ln, bias=1.0), writes=['gx'])
            S.op('dve', lambda e, g_t=g_t: e.tensor_tensor(out=g_t[:, 1540:1544], in0=gx, in1=negA, op=ALU.mult), reads=['gx', 'negA'], writes=[kg])
    Sst = A.f32([128, 4, 128])
    S.op('pool', lambda e: e.memset(Sst, 0.0), writes=['Sst'])
    names = ['gc', 'gb', 'mt', 'DT', 'DTs', 'egc', 'egl', 'kds', 'bws', 'kT', 'qgT', 'egb', 'ATn', 'inT', 'Am', 'AT', 'X2', 'XT2',
             'R0', 'R1', 'Bv', 'Bw', 'u', 'wT', 'vn', 'kdec', 'o', 'glast']
    W_ = {}
    for n_ in names:
        if n_ in ('gc', 'egc', 'egl', 'kds', 'bws', 'glast'):
            W_[n_] = A.f32([128, 4])
        else:
            W_[n_] = A.f32([128, 4, 128])
    identbc = ident.unsqueeze(1).broadcast_to([128, 4, 128])

    def P4(i):
        return PB(i).rearrange("p (h d) -> p h d", h=4)

    def evac(eng, out, pbi, w, extra_r=()):
        if eng == 'act':
            S.op('act', lambda e: e.activation(out=out, in_=P4(pbi), func=AF.Copy), reads=list(extra_r), writes=[pk(pbi), w])
        else:
            S.op('dve', lambda e: e.tensor_copy(out=out, in_=P4(pbi)), reads=list(extra_r), writes=[pk(pbi), w])

    def d_tile(t):
            G = gt[t % 3]
            kG = 'gt%d' % (t % 3)
            issue_copies(3)
            qv = G[:, 0:512].rearrange("p (h d) -> p h d", h=4)
            kv_ = G[:, 512:1024].rearrange("p (h d) -> p h d", h=4)
            vv = G[:, 1024:1536].rearrange("p (h d) -> p h d", h=4)
            beta = G[:, 1536:1540]
            gg = G[:, 1540:1544]
            S.op('pe', lambda e, gg=gg: e.matmul(PB(0)[:, 0:4], lhsT=U_incl, rhs=gg, start=True, stop=True), reads=[kG, 'cst'], writes=[pk(0)])
            S.op('dve', lambda e: e.tensor_copy(out=W_['gc'], in_=PB(0)[:, 0:4]), writes=[pk(0), 'gc'])
            for h in range(4):
                S.op('dve', lambda e, h=h, gg=gg: e.tensor_scalar(out=W_['gb'][:, h, :], in0=ones, scalar1=gg[:, h:h + 1], scalar2=None, op0=ALU.mult),
                     reads=[kG, 'cst'], writes=['gb'])

            def mm_gb(e):
                for h in range(4):
                    ins = e.matmul(P4(1)[:, h, :], lhsT=W_['gb'][:, h, :], rhs=U_incl, start=True, stop=True)
                return ins
            S.op('pe', mm_gb, reads=['gb', 'cst'], writes=[pk(1)])
            for h in range(4):
                S.op('dve', lambda e, h=h: e.tensor_scalar(out=W_['mt'][:, h, :], in0=P4(1)[:, h, :], scalar1=W_['gc'][:, h:h + 1], scalar2=0.0,
                                                           op0=ALU.subtract, op1=ALU.min), reads=['gc'], writes=[pk(1), 'mt'])
            S.op('act', lambda e: e.activation(out=W_['mt'], in_=W_['mt'], func=AF.Exp), writes=['mt'])
            S.op('pool', lambda e: e.tensor_tensor(out=W_['DT'], in0=W_['mt'], in1=U_incl.unsqueeze(1).broadcast_to([128, 4, 128]), op=ALU.mult),
                 reads=['mt', 'cst'], writes=['DT'])
            S.op('pool', lambda e: e.tensor_tensor(out=W_['DTs'], in0=W_['mt'], in1=U_strict.unsqueeze(1).broadcast_to([128, 4, 128]), op=ALU.mult),
                 reads=['mt', 'cst'], writes=['DTs'])
            if t >= 16:
                S.op('act', lambda e: e.activation(out=W_['egb'], in_=P4(1), func=AF.Exp), writes=[pk(1), 'egb'])
            S.op('dve', lambda e: e.tensor_copy(out=W_['glast'], in_=P4(1)[:, :, 127]), writes=[pk(1), 'glast'])
            S.op('act', lambda e: e.activation(out=W_['egc'], in_=W_['gc'], func=AF.Exp), reads=['gc'], writes=['egc'])
            S.op('act', lambda e: e.activation(out=W_['egl'], in_=W_['glast'], func=AF.Exp), reads=['glast'], writes=['egl'])
            S.op('dve', lambda e: e.tensor_tensor(out=W_['kds'], in0=W_['glast'], in1=W_['gc'], op=ALU.subtract), reads=['glast', 'gc'], writes=['kds'])
            S.op('act', lambda e: e.activation(out=W_['kds'], in_=W_['kds'], func=AF.Exp), writes=['kds'])
            S.op('dve', lambda e, beta=beta: e.tensor_tensor(out=W_['bws'], in0=beta, in1=W_['egc'], op=ALU.mult), reads=[kG, 'egc'], writes=['bws'])
            def tr_k(e, kv_=kv_):
                for h in range(4):
                    ins = e.transpose(out=P4(2)[:, h, :], in_=kv_[:, h, :], identity=ident)
                return ins
            S.op('pe', tr_k, reads=[kG, 'cst'], writes=[pk(2)])
            evac('act', W_['kT'], 2, 'kT')

            if t >= 16:
                def tr_q(e, qv=qv):
                    for h in range(4):
                        ins = e.transpose(out=P4(3)[:, h, :], in_=qv[:, h, :], identity=ident)
                    return ins
                S.op('pe', tr_q, reads=[kG, 'cst'], writes=[pk(3)])
                qTd = W_['o']
                evac('dve', qTd, 3, 'o')
                S.op('dve', lambda e, qTd=qTd: e.tensor_tensor(out=W_['qgT'], in0=qTd, in1=W_['egb'], op=ALU.mult), reads=['o', 'egb'], writes=['qgT'])
            def mm_g(e):
                for h in range(4):
                    ins = e.matmul(P4(4)[:, h, :], lhsT=W_['kT'][:, h, :], rhs=W_['kT'][:, h, :], start=True, stop=True)
                return ins
            S.op('pe', mm_g, reads=['kT'], writes=[pk(4)])

            if t >= 16:
                def mm_qk(e, qTd=qTd):
                    for h in range(4):
                        ins = e.matmul(P4(5)[:, h, :], lhsT=W_['kT'][:, h, :], rhs=qTd[:, h, :], start=True, stop=True)
                    return ins
                S.op('pe', mm_qk, reads=['kT', 'o'], writes=[pk(5)])
            S.op('dve', lambda e: e.tensor_tensor(out=W_['ATn'], in0=P4(4), in1=W_['DTs'], op=ALU.mult), reads=['DTs'], writes=[pk(4), 'ATn'])
            if t >= 16:
                S.op('dve', lambda e: e.tensor_tensor(out=W_['inT'], in0=P4(5), in1=W_['DT'], op=ALU.mult), reads=['DT'], writes=[pk(5), 'inT'])

            def tr_a(e):
                for h in range(4):
                    ins = e.transpose(out=P4(6)[:, h, :], in_=W_['ATn'][:, h, :], identity=ident)
                return ins
            S.op('pe', tr_a, reads=['ATn', 'cst'], writes=[pk(6)])
            S.op('dve', lambda e, beta=beta: e.tensor_tensor(out=W_['Am'], in0=P4(6), in1=beta.unsqueeze(2).broadcast_to([128, 4, 128]), op=ALU.mult),
                 reads=[kG], writes=[pk(6), 'Am'])

            def tr_at(e):
                for h in range(4):
                    ins = e.transpose(out=P4(7)[:, h, :], in_=W_['Am'][:, h, :], identity=ident)
                return ins
            S.op('pe', tr_at, reads=['Am', 'cst'], writes=[pk(7)])
            evac('act', W_['AT'], 7, 'AT')
            S.op('dve', lambda e: e.tensor_tensor(out=W_['R0'], in0=identbc, in1=W_['AT'], op=ALU.subtract), reads=['AT', 'cst'], writes=['R0'])
            X, XT, kX, kXT = W_['Am'], W_['AT'], 'Am', 'AT'
            Xn, XTn, kXn, kXTn = W_['X2'], W_['XT2'], 'X2', 'XT2'
            Rc, Rn, kRc, kRn = W_['R0'], W_['R1'], 'R0', 'R1'
            for lvl in range(6):
                def mm_x2(e, X=X, XT=XT):
                    for h in range(4):
                        ins = e.matmul(P4(2)[:, h, :], lhsT=XT[:, h, :], rhs=X[:, h, :], start=True, stop=True)
                    return ins
                S.op('pe', mm_x2, reads=[kX, kXT], writes=[pk(2)])
                if lvl < 5:
                    def mm_xt2(e, X=X, XT=XT):
                        for h in range(4):
                            ins = e.matmul(P4(3)[:, h, :], lhsT=X[:, h, :], rhs=XT[:, h, :], start=True, stop=True)
                        return ins
                    S.op('pe', mm_xt2, reads=[kX, kXT], writes=[pk(3)])
                evac('act', Xn, 2, kXn)
                if lvl < 5:
                    evac('act', XTn, 3, kXTn)

                def mm_r(e, Xn=Xn, Rc=Rc):
                    for h in range(4):
                        ins = e.matmul(P4(4)[:, h, :], lhsT=Xn[:, h, :], rhs=Rc[:, h, :], start=True, stop=True)
                    return ins
                S.op('pe', mm_r, reads=[kXn, kRc], writes=[pk(4)])
                S.op('dve', lambda e, Rn=Rn, Rc=Rc: e.tensor_tensor(out=Rn, in0=P4(4), in1=Rc, op=ALU.add), reads=[kRc], writes=[pk(4), kRn])
                X, XT, kX, kXT, Xn, XTn, kXn, kXTn = Xn, XTn, kXn, kXTn, X, XT, kX, kXT
                Rc, Rn, kRc, kRn = Rn, Rc, kRn, kRc
            R, kR = Rc, kRc
            S.op('dve', lambda e, vv=vv, beta=beta: e.tensor_tensor(out=W_['Bv'], in0=vv, in1=beta.unsqueeze(2).broadcast_to([128, 4, 128]), op=ALU.mult),
                 reads=[kG], writes=['Bv'])
            S.op('pool', lambda e, kv_=kv_: e.tensor_tensor(out=W_['Bw'], in0=kv_, in1=W_['bws'].unsqueeze(2).broadcast_to([128, 4, 128]), op=ALU.mult),
                 reads=[kG, 'bws'], writes=['Bw'])
            S.op('pool', lambda e, kv_=kv_: e.tensor_tensor(out=W_['kdec'], in0=kv_, in1=W_['kds'].unsqueeze(2).broadcast_to([128, 4, 128]), op=ALU.mult),
                 reads=[kG, 'kds'], writes=['kdec'])

            def mm_u(e, R=R):
                for h in range(4):
                    ins = e.matmul(P4(5)[:, h, :], lhsT=R[:, h, :], rhs=W_['Bv'][:, h, :], start=True, stop=True)
                return ins
            S.op('pe', mm_u, reads=[kR, 'Bv'], writes=[pk(5)])
            evac('act', W_['u'], 5, 'u')

            def mm_w(e, R=R):
                for h in range(4):
                    ins = e.matmul(P4(6)[:, h, :], lhsT=W_['Bw'][:, h, :], rhs=R[:, h, :], start=True, stop=True)
                return ins
            S.op('pe', mm_w, reads=[kR, 'Bw'], writes=[pk(6)])
            evac('dve', W_['wT'], 6, 'wT')
            def mm_ws(e):
                for h in range(4):
                    ins = e.matmul(P4(7)[:, h, :], lhsT=W_['wT'][:, h, :], rhs=Sst[:, h, :], start=True, stop=True)
                return ins
            S.op('pe', mm_ws, reads=['wT', 'Sst'], writes=[pk(7)])
            S.op('dve', lambda e: e.tensor_tensor(out=W_['vn'], in0=W_['u'], in1=P4(7), op=ALU.subtract), reads=['u'], writes=[pk(7), 'vn'])
            if t >= 16:
                def mm_o(e):
                    for h in range(4):
                        e.matmul(P4(0)[:, h, :], lhsT=W_['qgT'][:, h, :], rhs=Sst[:, h, :], start=True, stop=False)
                        ins = e.matmul(P4(0)[:, h, :], lhsT=W_['inT'][:, h, :], rhs=W_['vn'][:, h, :], start=False, stop=True)
                    return ins
                S.op('pe', mm_o, reads=['qgT', 'Sst', 'inT', 'vn'], writes=[pk(0)])
                evac('act', W_['o'], 0, 'o')
                DMA('pool', o_gdn[(t - 16) * 128:(t - 15) * 128, :], W_['o'].rearrange("p h d -> p (h d)"), r=['o'], w=[])

            def mm_su(e):
                for h in range(4):
                    ins = e.matmul(P4(1)[:, h, :], lhsT=W_['kdec'][:, h, :], rhs=W_['vn'][:, h, :], start=True, stop=True)
                return ins
            S.op('pe', mm_su, reads=['kdec', 'vn'], writes=[pk(1)])
            for h in range(4):
                S.op('dve', lambda e, h=h: e.scalar_tensor_tensor(out=Sst[:, h, :], in0=Sst[:, h, :], scalar=W_['egl'][:, h:h + 1], in1=P4(1)[:, h, :],
                                                                  op0=ALU.mult, op1=ALU.add), reads=['egl'], writes=[pk(1), 'Sst'])
    qkw = A.f32([128, 6, 64])
    for gi in range(3):
        DMA('sp', qkw[:, gi, :], qnw[gi:gi + 1, :].partition_broadcast(128), w=['qkw'])
        DMA('sp', qkw[:, 3 + gi, :], knw[gi:gi + 1, :].partition_broadcast(128), w=['qkw'])
    rin = [A.f32([128, 512]) for _ in range(3)]
    rsq = A.f32([128, 512])
    rss = A.f32([128, 8])
    rrs = A.f32([128, 8])
    rout = [A.f32([128, 512]) for _ in range(3)]
    rp = [A.f32([128, 16]) for _ in range(2)]
    rt = [A.f32([128, 8, 8]) for _ in range(4)]
    ecnt = [0]
    def e_tile(gi, t):
        W, dil = GROUPS[gi]
        cq = 2056 + gi * 1536
        if True:
            DMA('sp', rp[t % 2], rope_t[t * 128:(t + 1) * 128, :], w=['rp%d' % (t % 2)])
            cosb = rp[t % 2][:, 0:8].unsqueeze(1).broadcast_to([128, 8, 8])
            sinb = rp[t % 2][:, 8:16].unsqueeze(1).broadcast_to([128, 8, 8])
            for which in ((1, 0) if t >= 16 else (1,)):
                bi = ecnt[0] % 3
                ecnt[0] += 1
                src = rin[bi]
                dst = rout[bi]
                ks, kd = 'rin%d' % bi, 'rout%d' % bi
                c0 = cq + which * 512
                DMA('sp', src, proj[3 + t * 128:3 + (t + 1) * 128, c0:c0 + 512], w=[ks])
                S.op('act', lambda e, src=src: e.activation(out=rsq, in_=src, func=AF.Square), reads=[ks], writes=['rsq'])
                S.op('dve', lambda e: e.tensor_reduce(out=rss, in_=rsq.rearrange("p (h d) -> p h d", h=8), axis=AX.X, op=ALU.add),
                     reads=['rsq'], writes=['rss'])
                S.op('act', lambda e: e.activation(out=rss, in_=rss, func=AF.Sqrt, scale=1.0 / 64, bias=EPS), writes=['rss'])
                S.op('dve', lambda e: e.reciprocal(out=rrs, in_=rss), reads=['rss'], writes=['rrs'])
                d3 = dst.rearrange("p (h d) -> p h d", h=8)
                S.op('pool', lambda e, src=src, d3=d3: e.tensor_tensor(out=d3, in0=src.rearrange("p (h d) -> p h d", h=8),
                                                                      in1=rrs.unsqueeze(2).broadcast_to([128, 8, 64]), op=ALU.mult),
                     reads=[ks, 'rrs'], writes=[kd])
                wrow = qkw[:, (3 if which == 1 else 0) + gi, :].unsqueeze(1).broadcast_to([128, 8, 64])
                S.op('pool', lambda e, d3=d3, wrow=wrow: e.tensor_tensor(out=d3, in0=d3, in1=wrow, op=ALU.mult), reads=['qkw'], writes=[kd])
                x1 = d3[:, :, 0:8]
                x2 = d3[:, :, 8:16]
                S.op('dve', lambda e, x1=x1, cosb=cosb: e.tensor_tensor(out=rt[0], in0=x1, in1=cosb, op=ALU.mult), reads=[kd, 'rp%d' % (t % 2)], writes=['rt0'])
                S.op('dve', lambda e, x2=x2, sinb=sinb: e.tensor_tensor(out=rt[1], in0=x2, in1=sinb, op=ALU.mult), reads=[kd, 'rp%d' % (t % 2)], writes=['rt1'])
                S.op('dve', lambda e, x2=x2, cosb=cosb: e.tensor_tensor(out=rt[2], in0=x2, in1=cosb, op=ALU.mult), reads=[kd, 'rp%d' % (t % 2)], writes=['rt2'])
                S.op('dve', lambda e, x1=x1, sinb=sinb: e.tensor_tensor(out=rt[3], in0=x1, in1=sinb, op=ALU.mult), reads=[kd, 'rp%d' % (t % 2)], writes=['rt3'])
                S.op('dve', lambda e, x1=x1: e.tensor_tensor(out=x1, in0=rt[0], in1=rt[1], op=ALU.subtract), reads=['rt0', 'rt1'], writes=[kd])
                S.op('dve', lambda e, x2=x2: e.tensor_tensor(out=x2, in0=rt[2], in1=rt[3], op=ALU.add), reads=['rt2', 'rt3'], writes=[kd])
                if which == 1:
                    DMA('pool', kn_s[gi, t * 128:(t + 1) * 128, :], dst, r=[kd], w=[])
                    if t * 128 >= 4096 - W:
                        r0 = t * 128 - (4096 - W)
                        DMA('pool', kvp[gi][r0:r0 + 128, 0, :], dst, r=[kd], w=[])
                        DMA('pool', kvp[gi][r0:r0 + 128, 1, :], proj[3 + t * 128:3 + (t + 1) * 128, cq + 1024:cq + 1536], w=[])
                else:
                    DMA('pool', qn_s[gi, (t - 16) * 128:(t - 15) * 128, :], dst, r=[kd], w=[])
    e_units = []
    for gi in range(3):
        for t in range(16 - GROUPS[gi][0] // 128, 32):
            e_units.append((gi, t))
    c_tile(0)
    per = (len(e_units) + 31) // 32
    for t in range(32):
        lc, ld, le = [], [], []
        if t < 31:
            S.defer = lc
            c_tile(t + 1)
        S.defer = ld
        d_tile(t)
        S.defer = le
        for _ in range(per):
            if e_units:
                e_tile(*e_units.pop(0))
        S.defer = None
        S.replay([ld, lc, le], [4, 1, 2])
    issue_copies(1000)
    DMA('pool', sgp.rearrange("h k v -> k h v"), Sst, r=['Sst'], w=['sgp'])
    S.barrier()

    if stop_after == 'E':
        S.emit()
        return nc
    A.off = base0
    fin = [A.f32([128, 512]) for _ in range(4)]
    fb = [A.bf16([128, 512]) for _ in range(2)]
    qT = A.bf16([128, 4, 128])
    kTs = [A.bf16([128, 4, 128]) for _ in range(2)]
    Vs = [A.bf16([128, 8, 66]) for _ in range(2)]
    for i in range(2):
        S.op('pool', lambda e, i=i: e.memset(Vs[i], 1.0), writes=['Vs%d' % i])
    Pm = [A.bf16([128, 8, 128]) for _ in range(2)]
    Ex = A.bf16([128, 4, 128])
    Osb = A.f32([128, 8, 65])
    fc = 0
    for gi in range(3):
        W, dil = GROUPS[gi]
        nblk = 4096 // dil // 128
        first_q = 2048 // dil // 128
        cv = 2056 + gi * 1536 + 1024
        for r in range(dil):
            for mb in range(first_q - 1, nblk):
                slot = mb % 2
                tok0 = r + dil * mb * 128
                rows = slice(tok0, tok0 + dil * 127 + 1, dil)
                prow = slice(3 + tok0, 3 + tok0 + dil * 127 + 1, dil)
                f1 = fin[fc % 4]; k1 = 'fin%d' % (fc % 4); fc += 1
                DMA('sp', f1, kn_s[gi, rows, :], w=[k1])
                S.op('act', lambda e, f1=f1: e.activation(out=fb[0], in_=f1, func=AF.Copy), reads=[k1], writes=['fb0'])

                def tr1(e):
                    for hp in range(4):
                        ins = e.transpose(out=PBb(0)[:, hp * 128:(hp + 1) * 128], in_=fb[0][:, hp * 128:(hp + 1) * 128], identity=identb)
                    return ins
                S.op('pe', tr1, reads=['fb0', 'identb'], writes=[pk(0)])
                S.op('dve', lambda e, slot=slot: e.tensor_copy(out=kTs[slot], in_=PBb(0)[:, 0:512].rearrange("p (a b) -> p a b", a=4)),
                     writes=[pk(0), 'kT%d' % slot])
                f2 = fin[fc % 4]; k2 = 'fin%d' % (fc % 4); fc += 1
                DMA('sp', f2, proj[prow, cv:cv + 512], w=[k2])
                S.op('pool', lambda e, f2=f2, slot=slot: e.tensor_copy(out=Vs[slot][:, :, 0:64], in_=f2.rearrange("p (h d) -> p h d", h=8)),
                     reads=[k2], writes=['Vs%d' % slot])
                if mb < first_q:
                    continue
                f3 = fin[fc % 4]; k3 = 'fin%d' % (fc % 4); fc += 1
                qrow = slice(tok0 - 2048, tok0 - 2048 + dil * 127 + 1, dil)
                DMA('sp', f3, qn_s[gi, qrow, :], w=[k3])
                S.op('act', lambda e, f3=f3: e.activation(out=fb[1], in_=f3, func=AF.Copy), reads=[k3], writes=['fb1'])

                def tr2(e):
                    for hp in range(4):
                        ins = e.transpose(out=PBb(1)[:, hp * 128:(hp + 1) * 128], in_=fb[1][:, hp * 128:(hp + 1) * 128], identity=identb)
                    return ins
                S.op('pe', tr2, reads=['fb1', 'identb'], writes=[pk(1)])
                S.op('dve', lambda e: e.tensor_copy(out=qT, in_=PBb(1)[:, 0:512].rearrange("p (a b) -> p a b", a=4)), writes=[pk(1), 'qT'])
                halo_prev = (mb == first_q)
                for which, sl in ((0, 1 - slot), (1, slot)):
                    for par in range(2):
                        pbi = 2 + par

                        def mm_s(e, sl=sl, par=par, pbi=pbi):
                            for hp in range(4):
                                lo = par * 64
                                ins = e.matmul(PB(pbi)[:, hp * 128:(hp + 1) * 128], lhsT=kTs[sl][lo:lo + 64, hp, :], rhs=qT[lo:lo + 64, hp, :],
                                               start=True, stop=True)
                            return ins
                        S.op('pe', mm_s, reads=['kT%d' % sl, 'qT'], writes=[pk(pbi)])
                        if which == 0 and halo_prev:
                            S.op('act', lambda e, pbi=pbi: e.activation(out=Ex, in_=P4(pbi), func=AF.Exp, scale=0.125, bias=maskb_t),
                                 reads=['maskb'], writes=[pk(pbi), 'Ex'])
                        else:
                            S.op('act', lambda e, pbi=pbi: e.activation(out=Ex, in_=P4(pbi), func=AF.Exp, scale=0.125), writes=[pk(pbi), 'Ex'])
                        msk = (L_incl if which == 0 else U_incl).unsqueeze(1).broadcast_to([128, 4, 128])
                        S.op('dve', lambda e, which=which, par=par, msk=msk: e.tensor_tensor(out=Pm[which][:, par:8:2, :], in0=Ex, in1=msk, op=ALU.mult),
                             reads=['Ex', 'cst'], writes=['Pm%d_%d' % (which, par)])
                for half in range(2):
                    pbi = 4 + half

                    def mm_pv(e, half=half, pbi=pbi, slot=slot):
                        for hh in range(4):
                            h = half * 4 + hh
                            e.matmul(PB(pbi)[:, hh * 65:(hh + 1) * 65], lhsT=Pm[0][:, h, :], rhs=Vs[1 - slot][:, h, 0:65], start=True, stop=False)
                            ins = e.matmul(PB(pbi)[:, hh * 65:(hh + 1) * 65], lhsT=Pm[1][:, h, :], rhs=Vs[slot][:, h, 0:65], start=False, stop=True)
                        return ins
                    S.op('pe', mm_pv, reads=['Pm0_0', 'Pm0_1', 'Pm1_0', 'Pm1_1', 'Vs0', 'Vs1'], writes=[pk(pbi)])
                    S.op('act', lambda e, half=half, pbi=pbi: e.activation(out=Osb[:, half * 4:(half + 1) * 4, :],
                                                                           in_=PB(pbi)[:, 0:260].rearrange("p (h d) -> p h d", h=4), func=AF.Copy),
                         writes=[pk(pbi), 'Osb%d' % half])
                DMA('pool', att_o[gi, qrow, :], Osb.rearrange("p h d -> p (h d)"), r=['Osb0', 'Osb1'], w=[])
    S.barrier()

    if stop_after == 'F':
        S.emit()
        return nc
    A.off = base0
    gnb = A.f32([128, 128])
    DMA('sp', gnb, gnw.partition_broadcast(128), w=['gnb'])
    g_o = [A.f32([128, 512]) for _ in range(2)]
    g_z = [A.f32([128, 512]) for _ in range(2)]
    g_a = [[A.f32([128, 520]) for _ in range(3)] for _ in range(2)]
    mixt = [A.f32([128, D]) for _ in range(2)]
    gsq = A.f32([128, 512])
    gss = A.f32([128, 4])
    grs = A.f32([128, 4])
    gden = A.f32([128, 8])
    for t in range(16):
        bi = t % 2
        mx = mixt[bi]
        km = 'mixt%d' % bi
        DMA('sp', g_o[bi], o_gdn[t * 128:(t + 1) * 128, :], w=['g_o%d' % bi])
        DMA('sp', g_z[bi], proj[3 + (16 + t) * 128:3 + (17 + t) * 128, 1536:2048], w=['g_z%d' % bi])
        for gi in range(3):
            DMA('sp', g_a[bi][gi], att_o[gi, t * 128:(t + 1) * 128, :], w=['g_a%d_%d' % (bi, gi)])
        S.op('act', lambda e, bi=bi: e.activation(out=gsq, in_=g_o[bi], func=AF.Square), reads=['g_o%d' % bi], writes=['gsq'])
        S.op('dve', lambda e: e.tensor_reduce(out=gss, in_=gsq.rearrange("p (h d) -> p h d", h=4), axis=AX.X, op=ALU.add), reads=['gsq'], writes=['gss'])
        S.op('act', lambda e: e.activation(out=gss, in_=gss, func=AF.Sqrt, scale=1.0 / 128, bias=EPS), writes=['gss'])
        S.op('dve', lambda e: e.reciprocal(out=grs, in_=gss), reads=['gss'], writes=['grs'])
        m3 = mx[:, 0:512].rearrange("p (h d) -> p h d", h=4)
        S.op('dve', lambda e, bi=bi, m3=m3: e.tensor_tensor(out=m3, in0=g_o[bi].rearrange("p (h d) -> p h d", h=4),
                                                            in1=grs.unsqueeze(2).broadcast_to([128, 4, 128]), op=ALU.mult),
             reads=['g_o%d' % bi, 'grs'], writes=[km])
        S.op('pool', lambda e, m3=m3: e.tensor_tensor(out=m3, in0=m3, in1=gnb.unsqueeze(1).broadcast_to([128, 4, 128]), op=ALU.mult), reads=['gnb'], writes=[km])
        S.op('act', lambda e, bi=bi: e.activation(out=g_z[bi], in_=g_z[bi], func=AF.Silu), writes=['g_z%d' % bi])
        S.op('dve', lambda e, bi=bi, mx=mx: e.tensor_tensor(out=mx[:, 0:512], in0=mx[:, 0:512], in1=g_z[bi], op=ALU.mult), reads=['g_z%d' % bi], writes=[km])
        S.op('pool', lambda e, bi=bi: e.tensor_tensor(out=g_a[bi][0], in0=g_a[bi][0], in1=g_a[bi][1], op=ALU.add), reads=['g_a%d_1' % bi], writes=['g_a%d_0' % bi])
        S.op('pool', lambda e, bi=bi: e.tensor_tensor(out=g_a[bi][0], in0=g_a[bi][0], in1=g_a[bi][2], op=ALU.add), reads=['g_a%d_2' % bi], writes=['g_a%d_0' % bi])
        a3 = g_a[bi][0].rearrange("p (h d) -> p h d", h=8)
        S.op('dve', lambda e, a3=a3: e.reciprocal(out=gden, in_=a3[:, :, 64]), reads=['g_a%d_0' % bi], writes=['gden'])
        S.op('dve', lambda e, a3=a3, mx=mx: e.tensor_tensor(out=mx[:, 512:1024].rearrange("p (h d) -> p h d", h=8), in0=a3[:, :, 0:64],
                                                            in1=gden.unsqueeze(2).broadcast_to([128, 8, 64]), op=ALU.mult),
             reads=['g_a%d_0' % bi, 'gden'], writes=[km])
        DMA('pool', mix_d[t * 128:(t + 1) * 128, :], mx, r=[km], w=[])
    S.barrier()

    if stop_after == 'G1':
        S.emit()
        return nc
    sample_path(nc, S, A, base0, locals())

    A.off = base0
    yacc = A.f32([128, 17, D])
    h2T = A.bf16([128, 8, 17 * 128])
    gates = A.f32([128, 17, 32])
    base1 = A.off
    wob = A.bf16([128, 8, D])
    wos = A.f32([128, 8, 512])
    for hf in range(2):
        DMA('sp', wos, w_out[:, hf * 512:(hf + 1) * 512].rearrange("(k p) c -> p k c", p=128), w=['wos'])
        S.op('pool', lambda e, hf=hf: e.tensor_copy(out=wob[:, :, hf * 512:(hf + 1) * 512], in_=wos), reads=['wos'], writes=['wob'])
    wrt = A.f32([128, 8, 36])
    DMA('sp', wrt, w_r.rearrange("(k p) c -> p k c", p=128), w=['wrt'])
    w2b = A.f32([128, D])
    DMA('sp', w2b, norm2.partition_broadcast(128), w=['w2b'])
    mxl = [A.f32([128, D]) for _ in range(2)]
    xl = [A.f32([128, D]) for _ in range(2)]
    mxb = A.bf16([128, D])
    mxT = A.bf16([128, 8, 128])
    hsq = A.f32([128, D])
    hss = A.f32([128, 1])
    hrs = A.f32([128, 1])
    h2n = A.f32([128, D])
    h2Tf = A.f32([128, 8, 128])
    lg = A.f32([128, 36])
    r_ = {n_: A.f32([128, 8]) for n_ in ['gm', 'oh', 'eg', 'sg', 'pg', 'ein', 'm1', 'oh1', 'e2', 'm2', 'oh2', 'w1', 'w2', 'g8', 'tmp']}
    sel = A.f32([128, 4, 8])
    for t in range(17):
        bi = t % 2
        xrow = (16 + t) * 128 if t < 16 else 32 * 128
        DMA('sp', mxl[bi], mix_d[t * 128:(t + 1) * 128, :], w=['mxl%d' % bi])
        DMA('sp', xl[bi], x_all[xrow:xrow + 128, :], w=['xl%d' % bi])
        S.op('act', lambda e, bi=bi: e.activation(out=mxb, in_=mxl[bi], func=AF.Copy), reads=['mxl%d' % bi], writes=['mxb'])

        def trm(e):
            for k in range(8):
                ins = e.transpose(out=PBb(0)[:, k * 128:(k + 1) * 128], in_=mxb[:, k * 128:(k + 1) * 128], identity=identb)
            return ins
        S.op('pe', trm, reads=['mxb', 'identb'], writes=[pk(0)])
        S.op('dve', lambda e: e.tensor_copy(out=mxT, in_=PBb(0).rearrange("p (k t) -> p k t", k=8)), writes=[pk(0), 'mxT'])
        for hf in range(2):
            def mmo(e, hf=hf):
                for k in range(8):
                    ins = e.matmul(PB(1 + hf), lhsT=mxT[:, k, :], rhs=wob[:, k, hf * 512:(hf + 1) * 512], start=(k == 0), stop=(k == 7))
                return ins
            S.op('pe', mmo, reads=['mxT', 'wob'], writes=[pk(1 + hf)])
            S.op('dve', lambda e, hf=hf, t=t, bi=bi: e.tensor_tensor(out=yacc[:, t, hf * 512:(hf + 1) * 512], in0=PB(1 + hf), in1=xl[bi][:, hf * 512:(hf + 1) * 512], op=ALU.add),
                 reads=['xl%d' % bi], writes=[pk(1 + hf), 'yacc%d' % t])
        S.op('act', lambda e, t=t: e.activation(out=hsq, in_=yacc[:, t, :], func=AF.Square, accum_out=hss), reads=['yacc%d' % t], writes=['hsq', 'hss'])
        S.op('act', lambda e: e.activation(out=hss, in_=hss, func=AF.Sqrt, scale=1.0 / D, bias=EPS), writes=['hss'])
        S.op('dve', lambda e: e.reciprocal(out=hrs, in_=hss), reads=['hss'], writes=['hrs'])
        S.op('dve', lambda e, t=t: e.scalar_tensor_tensor(out=h2n, in0=yacc[:, t, :], scalar=hrs, in1=w2b, op0=ALU.mult, op1=ALU.mult),
             reads=['yacc%d' % t, 'hrs', 'w2b'], writes=['h2n'])
        for hf in range(2):
            def trh(e, hf=hf):
                for k in range(4):
                    kk = hf * 4 + k
                    ins = e.transpose(out=PB(3 + hf)[:, k * 128:(k + 1) * 128], in_=h2n[:, kk * 128:(kk + 1) * 128], identity=ident)
                return ins
            S.op('pe', trh, reads=['h2n', 'cst'], writes=[pk(3 + hf)])
            S.op('act', lambda e, hf=hf, t=t: e.activation(out=h2T[:, hf * 4:(hf + 1) * 4, t * 128:(t + 1) * 128], in_=P4(3 + hf), func=AF.Copy),
                 writes=[pk(3 + hf), 'h2T%d' % t])
            S.op('dve', lambda e, hf=hf: e.tensor_copy(out=h2Tf[:, hf * 4:(hf + 1) * 4, :], in_=P4(3 + hf)), writes=[pk(3 + hf), 'h2Tf'])

        def mmr(e):
            for k in range(8):
                ins = e.matmul(PB(5)[:, 0:36], lhsT=h2Tf[:, k, :], rhs=wrt[:, k, :], start=(k == 0), stop=(k == 7))
            return ins
        S.op('pe', mmr, reads=['h2Tf', 'wrt'], writes=[pk(5)])
        S.op('dve', lambda e: e.tensor_copy(out=lg, in_=PB(5)[:, 0:36]), writes=[pk(5), 'lg'])
        R_ = r_
        lgg = lg[:, 0:4]
        le = lg[:, 4:36].rearrange("p (g e) -> p g e", g=4)

        def V(e_, fn, r, w):
            S.op(e_, fn, reads=r, writes=w)
        V('dve', lambda e: e.tensor_reduce(out=R_['gm'][:, 0:1], in_=lgg, axis=AX.X, op=ALU.max), ['lg'], ['gm'])
        V('dve', lambda e: e.tensor_scalar(out=R_['oh'][:, 0:4], in0=lgg, scalar1=R_['gm'][:, 0:1], scalar2=None, op0=ALU.is_equal), ['lg', 'gm'], ['oh'])
        V('dve', lambda e: e.tensor_scalar(out=R_['eg'][:, 0:4], in0=lgg, scalar1=R_['gm'][:, 0:1], scalar2=None, op0=ALU.subtract), ['lg', 'gm'], ['eg'])
        V('act', lambda e: e.activation(out=R_['eg'][:, 0:4], in_=R_['eg'][:, 0:4], func=AF.Exp), [], ['eg'])
        V('dve', lambda e: e.tensor_reduce(out=R_['sg'][:, 0:1], in_=R_['eg'][:, 0:4], axis=AX.X, op=ALU.add), ['eg'], ['sg'])
        V('dve', lambda e: e.reciprocal(out=R_['pg'][:, 0:1], in_=R_['sg'][:, 0:1]), ['sg'], ['pg'])
        V('dve', lambda e: e.tensor_tensor(out=sel, in0=le, in1=R_['oh'][:, 0:4].unsqueeze(2).broadcast_to([128, 4, 8]), op=ALU.mult), ['lg', 'oh'], ['sel'])
        V('dve', lambda e: e.tensor_reduce(out=R_['ein'], in_=sel.rearrange("p g e -> p e g"), axis=AX.X, op=ALU.add), ['sel'], ['ein'])
        V('dve', lambda e: e.tensor_reduce(out=R_['m1'][:, 0:1], in_=R_['ein'], axis=AX.X, op=ALU.max), ['ein'], ['m1'])
        V('dve', lambda e: e.tensor_scalar(out=R_['oh1'], in0=R_['ein'], scalar1=R_['m1'][:, 0:1], scalar2=None, op0=ALU.is_equal), ['ein', 'm1'], ['oh1'])
        V('dve', lambda e: e.scalar_tensor_tensor(out=R_['e2'], in0=R_['oh1'], scalar=-1e30, in1=R_['ein'], op0=ALU.mult, op1=ALU.add), ['oh1', 'ein'], ['e2'])
        V('dve', lambda e: e.tensor_reduce(out=R_['m2'][:, 0:1], in_=R_['e2'], axis=AX.X, op=ALU.max), ['e2'], ['m2'])
        V('dve', lambda e: e.tensor_scalar(out=R_['oh2'], in0=R_['e2'], scalar1=R_['m2'][:, 0:1], scalar2=None, op0=ALU.is_equal), ['e2', 'm2'], ['oh2'])
        V('dve', lambda e: e.tensor_tensor(out=R_['w1'][:, 0:1], in0=R_['m2'][:, 0:1], in1=R_['m1'][:, 0:1], op=ALU.subtract), ['m1', 'm2'], ['w1'])
        V('act', lambda e: e.activation(out=R_['w1'][:, 0:1], in_=R_['w1'][:, 0:1], func=AF.Exp), [], ['w1'])
        V('dve', lambda e: e.tensor_scalar(out=R_['w1'][:, 0:1], in0=R_['w1'][:, 0:1], scalar1=1.0, scalar2=None, op0=ALU.add), [], ['w1'])
        V('dve', lambda e: e.reciprocal(out=R_['w1'][:, 0:1], in_=R_['w1'][:, 0:1]), [], ['w1'])
        V('dve', lambda e: e.tensor_tensor(out=R_['w1'][:, 0:1], in0=R_['w1'][:, 0:1], in1=R_['pg'][:, 0:1], op=ALU.mult), ['pg'], ['w1'])
        V('dve', lambda e: e.tensor_tensor(out=R_['w2'][:, 0:1], in0=R_['pg'][:, 0:1], in1=R_['w1'][:, 0:1], op=ALU.subtract), ['pg', 'w1'], ['w2'])
        V('dve', lambda e: e.tensor_scalar(out=R_['g8'], in0=R_['oh1'], scalar1=R_['w1'][:, 0:1], scalar2=None, op0=ALU.mult), ['oh1', 'w1'], ['g8'])
        V('dve', lambda e: e.scalar_tensor_tensor(out=R_['g8'], in0=R_['oh2'], scalar=R_['w2'][:, 0:1], in1=R_['g8'], op0=ALU.mult, op1=ALU.add), ['oh2', 'w2'], ['g8'])
        V('dve', lambda e, t=t: e.tensor_tensor(out=gates[:, t, :].rearrange("p (g e) -> p g e", g=4),
                                                in0=R_['oh'][:, 0:4].unsqueeze(2).broadcast_to([128, 4, 8]),
                                                in1=R_['g8'].unsqueeze(1).broadcast_to([128, 4, 8]), op=ALU.mult), ['oh', 'g8'], ['gates%d' % t])
    S.barrier()

    if stop_after == 'G2':
        S.emit()
        return nc
    A.off = base1
    gus = [A.f32([128, 8, 512]) for _ in range(2)]
    gub = [A.bf16([128, 8, 512]) for _ in range(2)]
    wds = [A.f32([128, 2, D]) for _ in range(2)]
    wdb = [A.bf16([128, 2, D]) for _ in range(2)]
    sgt = A.f32([128, 2, 512])
    hid = A.bf16([128, 2, 512])
    blocks = [(0, 512), (512, 512), (1024, 512), (1536, 512), (2048, 128)]
    for ex in range(32):
        wi = ex % 2
        DMA('sp', gus[wi], w_gu[ex].rearrange("(k p) c -> p k c", p=128), w=['gus%d' % wi])
        DMA('sp', wds[wi], w_d[ex].rearrange("(k p) c -> p k c", p=128), w=['wds%d' % wi])
        S.op('pool', lambda e, wi=wi: e.tensor_copy(out=gub[wi], in_=gus[wi]), reads=['gus%d' % wi], writes=['gub%d' % wi])
        S.op('pool', lambda e, wi=wi: e.tensor_copy(out=wdb[wi], in_=wds[wi]), reads=['wds%d' % wi], writes=['wdb%d' % wi])
        for (t0, nt) in blocks:
            for c in range(4):
                def mmg(e, c=c, wi=wi, t0=t0, nt=nt):
                    for k in range(8):
                        ins = e.matmul(PB(c)[:, 0:nt], lhsT=gub[wi][:, k, c * 128:(c + 1) * 128], rhs=h2T[:, k, t0:t0 + nt], start=(k == 0), stop=(k == 7))
                    return ins
                S.op('pe', mmg, reads=['gub%d' % wi] + ['h2T%d' % tt for tt in range(t0 // 128, (t0 + nt) // 128)], writes=[pk(c)])
            for c in range(2):
                S.op('act', lambda e, c=c, nt=nt: e.activation(out=sgt[:, c, 0:nt], in_=PB(c)[:, 0:nt], func=AF.Silu), writes=[pk(c), 'sgt%d' % c])
                S.op('dve', lambda e, c=c, nt=nt: e.tensor_tensor(out=hid[:, c, 0:nt], in0=sgt[:, c, 0:nt], in1=PB(2 + c)[:, 0:nt], op=ALU.mult),
                     reads=['sgt%d' % c], writes=[pk(2 + c), 'hid%d' % c])
            for ti in range(nt // 128):
                t = t0 // 128 + ti
                pby = 4 + 2 * (ti % 2)

                def mmd(e, ti=ti, wi=wi, pby=pby):
                    for hf in range(2):
                        for c in range(2):
                            ins = e.matmul(PB(pby + hf), lhsT=hid[:, c, ti * 128:(ti + 1) * 128], rhs=wdb[wi][:, c, hf * 512:(hf + 1) * 512],
                                           start=(c == 0), stop=(c == 1))
                    return ins
                S.op('pe', mmd, reads=['hid0', 'hid1', 'wdb%d' % wi], writes=[pk(pby), pk(pby + 1)])
                S.op('dve', lambda e, t=t, ex=ex, pby=pby: e.scalar_tensor_tensor(out=yacc[:, t, :], in0=ps_all[:, pby * 512:(pby + 2) * 512],
                                                                                 scalar=gates[:, t, ex:ex + 1], in1=yacc[:, t, :], op0=ALU.mult, op1=ALU.add),
                     reads=['gates%d' % t], writes=[pk(pby), pk(pby + 1), 'yacc%d' % t])
    for t in range(16):
        DMA('sp', y_own[t * 128:(t + 1) * 128, :], yacc[:, t, :], r=['yacc%d' % t], w=[])
    DMA('sp', y_samp, yacc[:, 16, :], r=['yacc16'], w=['y_samp'])
    S.emit()
    return nc


def sample_path(nc, S, A, base0, L):
    g_ = lambda n: L[n]
    proj, mix_d, zero_t, st_gdn, st_conv, caches = g_('proj'), g_('mix_d'), g_('zero_t'), g_('st_gdn'), g_('st_conv'), g_('caches')
    conv_wT, a_log, dt_bias, gnw, qnw, knw, rope_t, selh = g_('conv_wT'), g_('a_log'), g_('dt_bias'), g_('gnw'), g_('qnw'), g_('knw'), g_('rope_t'), g_('selh')
    sgs, kvs = g_('sgs'), g_('kvs')
    ident, ones, PB, pk, ps_all = g_('ident'), g_('ones'), g_('PB'), g_('pk'), g_('ps_all')
    NS = 16
    R0 = 3 + 4096

    def DMA(eng, out, in_, r=(), w=()):
        S.op(eng, lambda e: e.dma_start(out=out, in_=in_), reads=r, writes=w, dma=True)
    A.off = base0
    id16 = ident[0:NS, 0:NS]
    cw = A.f32([NS, 4, 1536])
    for k in range(4):
        DMA('sp', cw[:, k, :], conv_wT[k:k + 1, :].partition_broadcast(NS), w=['s_cw'])
    full = A.f32([NS, 4, 1536])
    DMA('sp', full[:, 0:3, :], st_conv, w=['s_full'])
    DMA('sp', full[:, 3, :], proj[R0:R0 + NS, 0:1536], w=['s_full'])
    zs = A.f32([NS, 512])
    DMA('sp', zs, proj[R0:R0 + NS, 1536:2048], w=['s_z'])
    ba = A.f32([NS, 8])
    DMA('sp', ba, proj[R0:R0 + NS, 2048:2056], w=['s_ba'])
    negA = A.f32([NS, 4])
    dtb = A.f32([NS, 4])
    DMA('sp', negA, a_log.partition_broadcast(NS), w=['s_negA'])
    DMA('sp', dtb, dt_bias.partition_broadcast(NS), w=['s_dtb'])
    gnb = A.f32([NS, 128])
    DMA('sp', gnb, gnw.partition_broadcast(NS), w=['s_gnb'])
    S.op('act', lambda e: e.activation(out=negA, in_=negA, func=AF.Exp), writes=['s_negA'])
    S.op('dve', lambda e: e.tensor_scalar(out=negA, in0=negA, scalar1=-1.0, scalar2=None, op0=ALU.mult), writes=['s_negA'])
    S.op('dve', lambda e: e.tensor_tensor(out=full, in0=full, in1=cw, op=ALU.mult), reads=['s_cw'], writes=['s_full'])
    cc = A.f32([NS, 1536])
    S.op('dve', lambda e: e.tensor_tensor(out=cc, in0=full[:, 0, :], in1=full[:, 1, :], op=ALU.add), reads=['s_full'], writes=['s_cc'])
    S.op('dve', lambda e: e.tensor_tensor(out=cc, in0=cc, in1=full[:, 2, :], op=ALU.add), reads=['s_full'], writes=['s_cc'])
    S.op('dve', lambda e: e.tensor_tensor(out=cc, in0=cc, in1=full[:, 3, :], op=ALU.add), reads=['s_full'], writes=['s_cc'])
    S.op('act', lambda e: e.activation(out=cc, in_=cc, func=AF.Silu), writes=['s_cc'])
    sq = A.f32([NS, 1024])
    ss8 = A.f32([NS, 8])
    rs8 = A.f32([NS, 8])
    qk = A.f32([NS, 8, 128])
    S.op('act', lambda e: e.activation(out=sq, in_=cc[:, 0:1024], func=AF.Square), reads=['s_cc'], writes=['s_sq'])
    S.op('dve', lambda e: e.tensor_reduce(out=ss8, in_=sq.rearrange("p (h d) -> p h d", h=8), axis=AX.X, op=ALU.add), reads=['s_sq'], writes=['s_ss8'])
    S.op('act', lambda e: e.activation(out=ss8, in_=ss8, func=AF.Sqrt, bias=EPS), writes=['s_ss8'])
    S.op('dve', lambda e: e.reciprocal(out=rs8, in_=ss8), reads=['s_ss8'], writes=['s_rs8'])
    S.op('dve', lambda e: e.tensor_scalar(out=rs8[:, 0:4], in0=rs8[:, 0:4], scalar1=128.0 ** -0.5, scalar2=None, op0=ALU.mult), writes=['s_rs8'])
    S.op('dve', lambda e: e.tensor_tensor(out=qk, in0=cc[:, 0:1024].rearrange("p (h d) -> p h d", h=8), in1=rs8.unsqueeze(2).broadcast_to([NS, 8, 128]), op=ALU.mult),
         reads=['s_cc', 's_rs8'], writes=['s_qk'])
    vv = cc[:, 1024:1536].rearrange("p (h d) -> p h d", h=4)
    beta = A.f32([NS, 4])
    gx = A.f32([NS, 4])
    eg = A.f32([NS, 4])
    S.op('act', lambda e: e.activation(out=beta, in_=ba[:, 0:4], func=AF.Sigmoid), reads=['s_ba'], writes=['s_beta'])
    S.op('dve', lambda e: e.tensor_tensor(out=gx, in0=ba[:, 4:8], in1=dtb, op=ALU.add), reads=['s_ba', 's_dtb'], writes=['s_gx'])
    S.op('act', lambda e: e.activation(out=gx, in_=gx, func=AF.Exp), writes=['s_gx'])
    S.op('act', lambda e: e.activation(out=gx, in_=gx, func=AF.Ln, bias=1.0), writes=['s_gx'])
    S.op('dve', lambda e: e.tensor_tensor(out=gx, in0=gx, in1=negA, op=ALU.mult), reads=['s_negA'], writes=['s_gx'])
    S.op('act', lambda e: e.activation(out=eg, in_=gx, func=AF.Exp), reads=['s_gx'], writes=['s_eg'])
    kqT = A.f32([128, 8, NS])

    def trkq(e):
        for j in range(8):
            ins = e.transpose(out=PB(0)[:, j * NS:(j + 1) * NS], in_=qk[:, j, :], identity=id16)
        return ins
    S.op('pe', trkq, reads=['s_qk', 'cst'], writes=[pk(0)])
    S.op('dve', lambda e: e.tensor_copy(out=kqT, in_=PB(0)[:, 0:8 * NS].rearrange("p (j s) -> p j s", j=8)), writes=[pk(0), 's_kqT'])
    egd = A.f32([NS, NS, 4])
    S.op('dve', lambda e: e.tensor_tensor(out=egd, in0=id16.unsqueeze(2).broadcast_to([NS, NS, 4]), in1=eg.unsqueeze(1).broadcast_to([NS, NS, 4]), op=ALU.mult),
         reads=['cst', 's_eg'], writes=['s_egd'])
    S.op('pe', lambda e: e.matmul(PB(1)[:, 0:NS * 4], lhsT=ones[0:NS, :], rhs=egd.rearrange("p s h -> p (s h)"), start=True, stop=True),
         reads=['s_egd', 'cst'], writes=[pk(1)])
    egb = A.f32([128, NS, 4])
    S.op('dve', lambda e: e.tensor_copy(out=egb, in_=PB(1)[:, 0:NS * 4].rearrange("p (s h) -> p s h", s=NS)), writes=[pk(1), 's_egb'])
    Sold = [A.f32([128, 4, 128]) for _ in range(3)]
    Snew = [A.f32([128, 4, 128]) for _ in range(3)]
    kSacc = A.f32([NS, 4, 128])
    oacc = A.f32([NS, 4, 128])
    S.op('pool', lambda e: e.memset(kSacc, 0.0), writes=['s_kSacc'])
    S.op('pool', lambda e: e.memset(oacc, 0.0), writes=['s_oacc'])
    for s_ in range(NS):
        bi = s_ % 3
        DMA('sp', Sold[bi], st_gdn[s_].rearrange("h k v -> k h v"), w=['s_Sold%d' % bi])

        def mm1(e, bi=bi):
            for h in range(4):
                ins = e.matmul(PB(2)[0:NS, h * 128:(h + 1) * 128], lhsT=kqT[:, 4 + h, :], rhs=Sold[bi][:, h, :], start=True, stop=True)
            return ins
        S.op('pe', mm1, reads=['s_kqT', 's_Sold%d' % bi], writes=[pk(2)])
        S.op('dve', lambda e, s_=s_: e.scalar_tensor_tensor(out=kSacc.rearrange("p h d -> p (h d)"), in0=PB(2)[0:NS, :], scalar=id16[:, s_:s_ + 1],
                                                            in1=kSacc.rearrange("p h d -> p (h d)"), op0=ALU.mult, op1=ALU.add),
             reads=['cst'], writes=[pk(2), 's_kSacc'])
    vn = A.f32([NS, 4, 128])
    S.op('dve', lambda e: e.tensor_tensor(out=vn, in0=kSacc, in1=eg.unsqueeze(2).broadcast_to([NS, 4, 128]), op=ALU.mult), reads=['s_kSacc', 's_eg'], writes=['s_vn'])
    S.op('dve', lambda e: e.tensor_tensor(out=vn, in0=vv, in1=vn, op=ALU.subtract), reads=['s_cc'], writes=['s_vn'])
    S.op('dve', lambda e: e.tensor_tensor(out=vn, in0=vn, in1=beta.unsqueeze(2).broadcast_to([NS, 4, 128]), op=ALU.mult), reads=['s_beta'], writes=['s_vn'])
    vm = [A.f32([NS, 4, 128]) for _ in range(2)]
    stmp = [A.f32([128, 4, 128]) for _ in range(2)]
    for s_ in range(NS):
        bi = s_ % 3
        b2 = s_ % 2
        DMA('sp', Sold[bi], st_gdn[s_].rearrange("h k v -> k h v"), w=['s_Sold%d' % bi])
        S.op('dve', lambda e, s_=s_, b2=b2: e.tensor_scalar(out=vm[b2], in0=vn, scalar1=id16[:, s_:s_ + 1], scalar2=None, op0=ALU.mult),
             reads=['s_vn', 'cst'], writes=['s_vm%d' % b2])

        def mm2(e, b2=b2):
            for h in range(4):
                ins = e.matmul(PB(3)[:, h * 128:(h + 1) * 128], lhsT=qk[:, 4 + h, :], rhs=vm[b2][:, h, :], start=True, stop=True)
            return ins
        S.op('pe', mm2, reads=['s_qk', 's_vm%d' % b2], writes=[pk(3)])
        S.op('pool', lambda e, bi=bi, b2=b2, s_=s_: e.tensor_tensor(out=stmp[b2], in0=Sold[bi], in1=egb[:, s_, :].unsqueeze(2).broadcast_to([128, 4, 128]), op=ALU.mult),
             reads=['s_Sold%d' % bi, 's_egb'], writes=['s_stmp%d' % b2])
        S.op('dve', lambda e, bi=bi, b2=b2: e.tensor_tensor(out=Snew[bi], in0=stmp[b2], in1=PB(3).rearrange("p (h d) -> p h d", h=4), op=ALU.add),
             reads=['s_stmp%d' % b2], writes=[pk(3), 's_Snew%d' % bi])
        DMA('pool', sgs[s_].rearrange("h k v -> k h v"), Snew[bi], r=['s_Snew%d' % bi], w=[])

        def mm3(e, bi=bi):
            for h in range(4):
                ins = e.matmul(PB(4)[0:NS, h * 128:(h + 1) * 128], lhsT=kqT[:, h, :], rhs=Snew[bi][:, h, :], start=True, stop=True)
            return ins
        S.op('pe', mm3, reads=['s_kqT', 's_Snew%d' % bi], writes=[pk(4)])
        S.op('dve', lambda e, s_=s_: e.scalar_tensor_tensor(out=oacc.rearrange("p h d -> p (h d)"), in0=PB(4)[0:NS, :], scalar=id16[:, s_:s_ + 1],
                                                            in1=oacc.rearrange("p h d -> p (h d)"), op0=ALU.mult, op1=ALU.add),
             reads=['cst'], writes=[pk(4), 's_oacc'])
    mixs = A.f32([NS, 1024])
    o4 = A.f32([NS, 4])
    S.op('act', lambda e: e.activation(out=sq[:, 0:512], in_=oacc.rearrange("p h d -> p (h d)"), func=AF.Square), reads=['s_oacc'], writes=['s_sq'])
    S.op('dve', lambda e: e.tensor_reduce(out=o4, in_=sq[:, 0:512].rearrange("p (h d) -> p h d", h=4), axis=AX.X, op=ALU.add), reads=['s_sq'], writes=['s_o4'])
    S.op('act', lambda e: e.activation(out=o4, in_=o4, func=AF.Sqrt, scale=1.0 / 128, bias=EPS), writes=['s_o4'])
    S.op('dve', lambda e: e.reciprocal(out=o4, in_=o4), writes=['s_o4'])
    m3 = mixs[:, 0:512].rearrange("p (h d) -> p h d", h=4)
    S.op('dve', lambda e: e.tensor_tensor(out=m3, in0=oacc, in1=o4.unsqueeze(2).broadcast_to([NS, 4, 128]), op=ALU.mult), reads=['s_oacc', 's_o4'], writes=['s_mix'])
    S.op('dve', lambda e: e.tensor_tensor(out=m3, in0=m3, in1=gnb.unsqueeze(1).broadcast_to([NS, 4, 128]), op=ALU.mult), reads=['s_gnb'], writes=['s_mix'])
    S.op('act', lambda e: e.activation(out=zs, in_=zs, func=AF.Silu), writes=['s_z'])
    S.op('dve', lambda e: e.tensor_tensor(out=mixs[:, 0:512], in0=mixs[:, 0:512], in1=zs, op=ALU.mult), reads=['s_z'], writes=['s_mix'])
    qkw = A.f32([NS, 6, 64])
    for gi in range(3):
        DMA('sp', qkw[:, gi, :], qnw[gi:gi + 1, :].partition_broadcast(NS), w=['s_qkw'])
        DMA('sp', qkw[:, 3 + gi, :], knw[gi:gi + 1, :].partition_broadcast(NS), w=['s_qkw'])
    rp = A.f32([NS, 16])
    DMA('sp', rp, rope_t[4096:4096 + NS, :], w=['s_rp'])
    cosb = rp[:, 0:8].unsqueeze(1).broadcast_to([NS, 8, 8])
    sinb = rp[:, 8:16].unsqueeze(1).broadcast_to([NS, 8, 8])
    qkv = A.f32([NS, 3, 3, 512])
    DMA('sp', qkv.rearrange("p g w c -> p (g w c)"), proj[R0:R0 + NS, 2056:2056 + 4608], w=['s_qkv'])
    s2 = A.f32([NS, 512])
    r8 = A.f32([NS, 8])
    rt = [A.f32([NS, 8, 8]) for _ in range(4)]
    for gi in range(3):
        for which in range(2):
            d3 = qkv[:, gi, which, :].rearrange("p (h d) -> p h d", h=8)
            S.op('act', lambda e, gi=gi, which=which: e.activation(out=s2, in_=qkv[:, gi, which, :], func=AF.Square), reads=['s_qkv'], writes=['s_s2'])
            S.op('dve', lambda e: e.tensor_reduce(out=r8, in_=s2.rearrange("p (h d) -> p h d", h=8), axis=AX.X, op=ALU.add), reads=['s_s2'], writes=['s_r8'])
            S.op('act', lambda e: e.activation(out=r8, in_=r8, func=AF.Sqrt, scale=1.0 / 64, bias=EPS), writes=['s_r8'])
            S.op('dve', lambda e: e.reciprocal(out=r8, in_=r8), writes=['s_r8'])
            S.op('dve', lambda e, d3=d3: e.tensor_tensor(out=d3, in0=d3, in1=r8.unsqueeze(2).broadcast_to([NS, 8, 64]), op=ALU.mult), reads=['s_r8'], writes=['s_qkv'])
            wrow = qkw[:, (3 if which == 1 else 0) + gi, :].unsqueeze(1).broadcast_to([NS, 8, 64])
            S.op('dve', lambda e, d3=d3, wrow=wrow: e.tensor_tensor(out=d3, in0=d3, in1=wrow, op=ALU.mult), reads=['s_qkw'], writes=['s_qkv'])
            x1 = d3[:, :, 0:8]
            x2 = d3[:, :, 8:16]
            S.op('dve', lambda e, x1=x1: e.tensor_tensor(out=rt[0], in0=x1, in1=cosb, op=ALU.mult), reads=['s_qkv', 's_rp'], writes=['s_rt0'])
            S.op('dve', lambda e, x2=x2: e.tensor_tensor(out=rt[1], in0=x2, in1=sinb, op=ALU.mult), reads=['s_qkv', 's_rp'], writes=['s_rt1'])
            S.op('dve', lambda e, x2=x2: e.tensor_tensor(out=rt[2], in0=x2, in1=cosb, op=ALU.mult), reads=['s_qkv', 's_rp'], writes=['s_rt2'])
            S.op('dve', lambda e, x1=x1: e.tensor_tensor(out=rt[3], in0=x1, in1=sinb, op=ALU.mult), reads=['s_qkv', 's_rp'], writes=['s_rt3'])
            S.op('dve', lambda e, x1=x1: e.tensor_tensor(out=x1, in0=rt[0], in1=rt[1], op=ALU.subtract), reads=['s_rt0', 's_rt1'], writes=['s_qkv'])
            S.op('dve', lambda e, x2=x2: e.tensor_tensor(out=x2, in0=rt[2], in1=rt[3], op=ALU.add), reads=['s_rt2', 's_rt3'], writes=['s_qkv'])
        W = GROUPS[gi][0]
        DMA('pool', kvs[gi][:, W - 1, :], qkv[:, gi, 1:3, :].rearrange("p w c -> p (w c)"), r=['s_qkv'], w=['kvs_new%d' % gi])
    selq = A.f32([NS, NS, 128])
    S.op('dve', lambda e: e.tensor_copy(out=selq, in_=id16.unsqueeze(2).broadcast_to([NS, NS, 128])), reads=['cst'], writes=['s_selq'])
    selh_t = A.f32([8, NS, NS])
    DMA('sp', selh_t, selh, w=['s_selh'])
    kvt = [A.f32([128, 1024]) for _ in range(3)]
    prod = A.f32([128, 512])
    sc = A.f32([128, 8])
    pex = A.f32([128, 8])
    Z = A.f32([8, 8, 64])
    Z2 = A.f32([8, 8])
    numS = A.f32([NS, 8, 64])
    denS = A.f32([NS, 8])
    ps_ = A.f32([NS, 8])
    pr16 = A.f32([NS, 8, 64])
    S.op('pool', lambda e: e.memset(numS, 0.0), writes=['s_numS'])
    S.op('pool', lambda e: e.memset(denS, 0.0), writes=['s_denS'])
    cnt = 0
    for gi in range(3):
        W, dil = GROUPS[gi]
        q3 = qkv[:, gi, 0, :].rearrange("p (h d) -> p h d", h=8)
        k3 = qkv[:, gi, 1, :].rearrange("p (h d) -> p h d", h=8)
        v3 = qkv[:, gi, 2, :].rearrange("p (h d) -> p h d", h=8)
        S.op('dve', lambda e, q3=q3, k3=k3: e.tensor_tensor(out=pr16, in0=q3, in1=k3, op=ALU.mult), reads=['s_qkv'], writes=['s_pr16'])
        S.op('dve', lambda e: e.tensor_reduce(out=ps_, in_=pr16, axis=AX.X, op=ALU.add), reads=['s_pr16'], writes=['s_ps'])
        S.op('act', lambda e: e.activation(out=ps_, in_=ps_, func=AF.Exp, scale=0.125), writes=['s_ps'])
        S.op('dve', lambda e: e.tensor_tensor(out=denS, in0=denS, in1=ps_, op=ALU.add), reads=['s_ps'], writes=['s_denS'])
        S.op('dve', lambda e, v3=v3: e.tensor_tensor(out=pr16, in0=v3, in1=ps_.unsqueeze(2).broadcast_to([NS, 8, 64]), op=ALU.mult), reads=['s_qkv', 's_ps'], writes=['s_pr16'])
        S.op('dve', lambda e: e.tensor_tensor(out=numS, in0=numS, in1=pr16, op=ALU.add), reads=['s_pr16'], writes=['s_numS'])
        for s_ in range(NS):
            bi = cnt % 3
            first = (cnt == 0)
            last = (cnt == 3 * NS - 1)
            cnt += 1
            KV = kvt[bi]
            kkv = 's_kvt%d' % bi
            DMA('sp', KV, caches[gi][s_, 0:W - dil + 1:dil, :], w=[kkv])
            S.op('pe', lambda e, s_=s_, gi=gi: e.matmul(PB(0), lhsT=selq[:, s_, :], rhs=qkv[:, gi, 0, :], start=True, stop=True), reads=['s_selq', 's_qkv'], writes=[pk(0)])
            S.op('dve', lambda e, KV=KV: e.tensor_tensor(out=prod, in0=KV[:, 0:512], in1=PB(0), op=ALU.mult), reads=[kkv], writes=[pk(0), 's_prod'])
            S.op('dve', lambda e: e.tensor_reduce(out=sc, in_=prod.rearrange("p (h d) -> p h d", h=8), axis=AX.X, op=ALU.add), reads=['s_prod'], writes=['s_sc'])
            S.op('act', lambda e: e.activation(out=pex, in_=sc, func=AF.Exp, scale=0.125), reads=['s_sc'], writes=['s_pex'])
            S.op('pe', lambda e, KV=KV: e.matmul(PB(1)[0:8, :], lhsT=pex, rhs=KV[:, 512:1024], start=True, stop=True), reads=['s_pex', kkv], writes=[pk(1)])
            S.op('pe', lambda e: e.matmul(PB(2)[0:8, 0:8], lhsT=pex, rhs=ones[:, 0:8], start=True, stop=True), reads=['s_pex', 'cst'], writes=[pk(2)])
            S.op('dve', lambda e: e.tensor_tensor(out=Z, in0=PB(1)[0:8, :].rearrange("p (h d) -> p h d", h=8), in1=ident[0:8, 0:8].unsqueeze(2).broadcast_to([8, 8, 64]), op=ALU.mult),
                 reads=['cst'], writes=[pk(1), 's_Z'])
            S.op('dve', lambda e: e.tensor_tensor(out=Z2, in0=PB(2)[0:8, 0:8], in1=ident[0:8, 0:8], op=ALU.mult), reads=['cst'], writes=[pk(2), 's_Z2'])
            S.op('pe', lambda e, s_=s_, first=first, last=last: e.matmul(PB(6)[0:NS, :], lhsT=selh_t[:, s_, :], rhs=Z.rearrange("p h d -> p (h d)"), start=first, stop=last),
                 reads=['s_selh', 's_Z'], writes=[pk(6)])
            S.op('pe', lambda e, s_=s_, first=first, last=last: e.matmul(PB(7)[0:NS, 0:8], lhsT=selh_t[:, s_, :], rhs=Z2, start=first, stop=last),
                 reads=['s_selh', 's_Z2'], writes=[pk(7)])
    S.op('dve', lambda e: e.tensor_tensor(out=numS, in0=numS, in1=PB(6)[0:NS, :].rearrange("p (h d) -> p h d", h=8), op=ALU.add), writes=[pk(6), 's_numS'])
    S.op('dve', lambda e: e.tensor_tensor(out=denS, in0=denS, in1=PB(7)[0:NS, 0:8], op=ALU.add), writes=[pk(7), 's_denS'])
    S.op('dve', lambda e: e.reciprocal(out=denS, in_=denS), writes=['s_denS'])
    S.op('dve', lambda e: e.tensor_tensor(out=mixs[:, 512:1024].rearrange("p (h d) -> p h d", h=8), in0=numS, in1=denS.unsqueeze(2).broadcast_to([NS, 8, 64]), op=ALU.mult),
         reads=['s_numS', 's_denS'], writes=['s_mix'])
    DMA('pool', mix_d[2048:2048 + NS, :], mixs, r=['s_mix'], w=['mix_d_s'])
    for j in range(2):
        DMA('pool', mix_d[2048 + NS:2176, j * 512:(j + 1) * 512], zero_t[0:128 - NS, 0:512], r=['zero'], w=['mix_d_z%d' % j])
    S.barrier()


_CACHE = {}


def _host_consts():
    c = np.zeros((128, 5, 128), np.float32)
    p = np.arange(128)[:, None]
    f = np.arange(128)[None, :]
    c[:, 0] = (p == f)
    c[:, 1] = (p <= f)
    c[:, 2] = (p >= f)
    c[:, 3] = (p < f)
    c[:, 4] = 1.0
    return c


def _rope_table(pos):
    half = 8
    inv = np.exp(-math.log(500000.0) * np.arange(half, dtype=np.float32) * np.float32(2.0 / 16)).astype(np.float32)
    ang = pos.astype(np.float32)[:, None] * inv[None, :]
    return np.concatenate([np.cos(ang), np.sin(ang)], axis=1).astype(np.float32)


def kernel(x_prompt, x_sample, state_gdn, state_conv, cache_kv_w128, cache_kv_w512, cache_kv_w2048,
           norm1_w, w_in, conv_w, a_log, dt_bias, gdn_norm_w, q_norm_w, k_norm_w, w_out, norm2_w,
           w_router_group, w_router_expert, w_gate_up, w_down):
    f = lambda a: np.ascontiguousarray(np.asarray(a, dtype=np.float32))
    x_prompt, x_sample = f(x_prompt), f(x_sample)
    if 'nc' not in _CACHE:
        _CACHE['nc'] = build_program()
    nc = _CACHE['nc']
    consts = _host_consts()
    w_r = np.concatenate([f(w_router_group)[0], f(w_router_expert)[0]], axis=1)
    shared = {
        "w_in": f(w_in)[0], "w_out": f(w_out)[0], "norm1": f(norm1_w), "norm2": f(norm2_w),
        "conv_wT": np.ascontiguousarray(f(conv_w)[0].T), "a_log": f(a_log), "dt_bias": f(dt_bias), "gnw": f(gdn_norm_w),
        "qnw": f(q_norm_w)[0], "knw": f(k_norm_w)[0], "w_r": np.ascontiguousarray(w_r), "w_gu": f(w_gate_up)[0], "w_d": f(w_down)[0],
        "consts": consts,
        "selh": np.ascontiguousarray(np.broadcast_to(np.eye(16, dtype=np.float32)[None], (8, 16, 16))),
    }
    cachesf = [f(cache_kv_w128)[0], f(cache_kv_w512)[0], f(cache_kv_w2048)[0]]
    sg, sc = f(state_gdn)[0], f(state_conv)[0]
    in_maps = []
    for c in range(NCORE):
        b, half = c // 2, c % 2
        xa = np.zeros((NTILE * 128, D), np.float32)
        if half == 1:
            xa[0:2048] = x_prompt[b, 0:2048]
        xa[2048:4096] = x_prompt[b, half * 2048:(half + 1) * 2048]
        xa[4096:4112] = x_sample[16 * c:16 * c + 16, 0]
        pos = np.concatenate([np.arange(4096) + (half - 1) * 2048, np.full(128, 8192)]).astype(np.float32)
        m = dict(shared)
        m["x_all"] = xa
        m["rope_t"] = _rope_table(pos)
        m["maskb"] = np.full((128, 1), 0.0 if half == 1 else -30000.0, np.float32)
        m["st_gdn"] = np.ascontiguousarray(sg[16 * c:16 * c + 16])
        m["st_conv"] = np.ascontiguousarray(sc[16 * c:16 * c + 16])
        for i in range(3):
            m["cache%d" % i] = np.ascontiguousarray(cachesf[i][16 * c:16 * c + 16].reshape(16, GROUPS[i][0], 1024))
        in_maps.append(m)
    if _CACHE.get('only_maps'):
        return in_maps
    res = run_bass_kernel_spmd(nc, in_maps, core_ids=list(range(NCORE)))
    R = res.results
    y_prompt = np.zeros((4, 4096, D), np.float32)
    for c in range(NCORE):
        y_prompt[c // 2, (c % 2) * 2048:(c % 2 + 1) * 2048] = R[c]["y_own"]
    y_sample = np.concatenate([R[c]["y_samp"][0:16] for c in range(NCORE)], axis=0).reshape(128, 1, D)
    sgp_o = np.stack([R[2 * b + 1]["sgp"] for b in range(4)])[None]
    scp_o = np.stack([R[2 * b + 1]["scp"] for b in range(4)])[None]
    kvp_o = [np.stack([R[2 * b + 1]["kvp%d" % i] for b in range(4)]).reshape(1, 4, GROUPS[i][0], 2, 8, 64) for i in range(3)]
    sgs_o = np.concatenate([R[c]["sgs"] for c in range(NCORE)], axis=0)[None]
    scs_o = np.concatenate([R[c]["scs"] for c in range(NCORE)], axis=0)[None]
    kvs_o = [np.concatenate([R[c]["kvs%d" % i] for c in range(NCORE)], axis=0).reshape(1, 128, GROUPS[i][0], 2, 8, 64) for i in range(3)]
    return (y_prompt, y_sample, sgp_o, scp_o, kvp_o[0], kvp_o[1], kvp_o[2], sgs_o, scs_o, kvs_o[0], kvs_o[1], kvs_o[2])
```

```python
import math
import numpy as np
import concourse.bass as bass
import concourse.mybir as mybir
from concourse.bass_utils import run_bass_kernel_spmd

F32 = mybir.dt.float32
BF16 = mybir.dt.bfloat16
AF = mybir.ActivationFunctionType
ALU = mybir.AluOpType
AX = mybir.AxisListType

NDMA = 32
NCORE = 8
D = 1024
INC = 6664
NTILE = 33
EPS = 1e-6
GROUPS = ((128, 1), (512, 4), (2048, 16))
COLBLK = [(0, 512), (512, 512), (1024, 512), (1536, 512), (2048, 8)]
for _g in range(3):
    _b = 2056 + _g * 1536
    COLBLK += [(_b, 512), (_b + 512, 512), (_b + 1024, 512)]
HALO_SKIP = {3, 5, 8, 11}


class Sched:
    ENG = ['pe', 'act', 'dve', 'pool', 'sp']

    def __init__(self, nc):
        self.nc = nc
        self.ops = {e: [] for e in self.ENG}
        self.esem = {e: nc.alloc_semaphore(name="s_" + e) for e in self.ENG}
        self.ecnt = {e: 0 for e in self.ENG}
        self.dpool = {'sp': list(range(0, 10)), 'pool': list(range(12, 20)), 'act': list(range(20, 24)), 'spc': list(range(24, 29))}
        self.dsem = [nc.alloc_semaphore(name="d%d" % i) for i in range(NDMA)]
        self.dcnt = [0] * NDMA
        self.dnext = {'sp': 0, 'pool': 0, 'act': 0, 'spc': 0}
        self.last_w = {}
        self.readers = {}
        self.waited = {e: {} for e in self.ENG}
        self.defer = None

    def _tok_waits(self, e, deps):
        waits = []
        for (sem, val) in deps:
            sid = id(sem)
            if e == 'pe' and sem is self.esem['pe']:
                continue
            if self.waited[e].get(sid, 0) < val:
                self.waited[e][sid] = val
                waits.append((sem, val))
        return waits

    def op(self, e, fn, reads=(), writes=(), dma=False, pool=None):
        if self.defer is not None:
            self.defer.append((e, fn, tuple(reads), tuple(writes), dma, pool))
            return None
        deps = []
        for k in reads:
            if k in self.last_w:
                deps.append(self.last_w[k])
        for k in writes:
            if k in self.last_w:
                deps.append(self.last_w[k])
            for sid, tk in self.readers.get(k, {}).items():
                deps.append(tk)
        if dma:
            pn = pool or e
            pl = self.dpool[pn]
            s = pl[self.dnext[pn] % len(pl)]
            self.dnext[pn] += 1
            if self.dcnt[s] > 0:
                deps.append((self.dsem[s], 16 * self.dcnt[s]))
            self.dcnt[s] += 1
            tok = (self.dsem[s], 16 * self.dcnt[s])
            inc = 16
        else:
            self.ecnt[e] += 1
            tok = (self.esem[e], self.ecnt[e])
            inc = 1
        waits = self._tok_waits(e, deps)
        for k in writes:
            self.last_w[k] = tok
            self.readers[k] = {}
        for k in reads:
            r = self.readers.setdefault(k, {})
            sid = id(tok[0])
            if sid not in r or r[sid][1] < tok[1]:
                r[sid] = tok
        self.ops[e].append((fn, waits, (tok[0], inc)))
        return tok

    def replay(self, lists, weights):
        assert self.defer is None
        pos = [0] * len(lists)
        while any(pos[i] < len(lists[i]) for i in range(len(lists))):
            for i, lst in enumerate(lists):
                for _ in range(weights[i]):
                    if pos[i] < len(lst):
                        self.op(*lst[pos[i]])
                        pos[i] += 1

    def barrier(self):
        toks = [(self.esem[e], self.ecnt[e]) for e in self.ENG if self.ecnt[e] > 0]
        toks += [(self.dsem[s], 16 * self.dcnt[s]) for s in range(NDMA) if self.dcnt[s] > 0]
        for e in self.ENG:
            waits = self._tok_waits(e, [t for t in toks if not (t[0] is self.esem[e])])
            if waits:
                self.ops[e].append((None, waits, None))
        self.last_w = {}
        self.readers = {}

    def emit(self):
        nc = self.nc
        self.barrier()
        with nc.Block() as block:
            def mk(e):
                def body(engine):
                    for (fn, waits, inc) in self.ops[e]:
                        for (sem, val) in waits:
                            engine.wait_ge(sem, val)
                        if fn is not None:
                            ins = fn(engine)
                            ins.then_inc(inc[0], inc[1])
                return body
            block.tensor(mk('pe'))
            block.scalar(mk('act'))
            block.vector(mk('dve'))
            block.gpsimd(mk('pool'))
            block.sync(mk('sp'))


def _shape_view(v, shape):
    if len(shape) == 2:
        return v
    if len(shape) == 3:
        return v.rearrange("p (a b) -> p a b", a=shape[1])
    return v.rearrange("p (a b c) -> p a b c", a=shape[1], b=shape[2])


class Arena:
    def __init__(self, ap, nwords):
        self.ap = ap
        self.n = nwords
        self.off = 0

    def f32(self, shape):
        n = int(np.prod(shape[1:]))
        o = self.off
        self.off += n
        assert self.off <= self.n, ("arena overflow", self.off, self.n)
        return _shape_view(self.ap[0:shape[0], o:o + n], shape)

    def bf16(self, shape):
        n = int(np.prod(shape[1:]))
        nw = (n + 1) // 2
        o = self.off
        self.off += nw
        assert self.off <= self.n, ("arena overflow", self.off, self.n)
        return _shape_view(self.ap[0:shape[0], o:o + nw].bitcast(BF16)[:, 0:n], shape)


def build_program(stop_after=None, debug=False):
    nc = bass.Bass("TRN2", target_bir_lowering=False)

    def din(name, shape):
        return nc.dram_tensor(name, list(shape), F32, kind="ExternalInput").ap()

    def dout(name, shape):
        return nc.dram_tensor(name, list(shape), F32, kind="ExternalOutput").ap()

    def dscr(name, shape):
        return nc.dram_tensor(name, list(shape), F32, kind=("ExternalOutput" if debug else "Internal")).ap()

    x_all = din("x_all", [NTILE * 128, D])
    w_in = din("w_in", [D, INC])
    w_out = din("w_out", [D, D])
    norm1 = din("norm1", [1, D])
    norm2 = din("norm2", [1, D])
    conv_wT = din("conv_wT", [4, 1536])
    a_log = din("a_log", [1, 4])
    dt_bias = din("dt_bias", [1, 4])
    gnw = din("gnw", [1, 128])
    qnw = din("qnw", [3, 64])
    knw = din("knw", [3, 64])
    w_r = din("w_r", [D, 36])
    w_gu = din("w_gu", [32, D, 512])
    w_d = din("w_d", [32, 256, D])
    consts = din("consts", [128, 5, 128])
    rope_t = din("rope_t", [NTILE * 128, 16])
    maskb = din("maskb", [128, 1])
    selh = din("selh", [8, 16, 16])
    st_gdn = din("st_gdn", [16, 4, 128, 128])
    st_conv = din("st_conv", [16, 3, 1536])
    caches = [din("cache%d" % i, [16, GROUPS[i][0], 1024]) for i in range(3)]

    y_own = dout("y_own", [2048, D])
    y_samp = dout("y_samp", [128, D])
    sgp = dout("sgp", [4, 128, 128])
    scp = dout("scp", [3, 1536])
    kvp = [dout("kvp%d" % i, [GROUPS[i][0], 2, 512]) for i in range(3)]
    sgs = dout("sgs", [16, 4, 128, 128])
    scs = dout("scs", [16, 3, 1536])
    kvs = [dout("kvs%d" % i, [16, GROUPS[i][0], 1024]) for i in range(3)]

    proj = dscr("proj", [3 + NTILE * 128, INC])
    gdn_in = dscr("gdn_in", [32, 128, 1544])
    o_gdn = dscr("o_gdn", [2048, 512])
    kn_s = dscr("kn_s", [3, 4096, 512])
    qn_s = dscr("qn_s", [3, 2048, 512])
    att_o = dscr("att_o", [3, 2048, 520])
    mix_d = dscr("mix_d", [17 * 128, D])

    S = Sched(nc)
    AW = 48500
    sb_all = nc.alloc_sbuf_tensor("arena", [128, AW], F32).ap()
    ps_all = nc.alloc_psum_tensor("psarena", [128, 4096], F32).ap()
    A = Arena(sb_all, AW)

    def PB(i):
        return ps_all[:, i * 512:(i + 1) * 512]

    def PBb(i):
        return ps_all[:, i * 512:(i + 1) * 512].bitcast(BF16)

    def pk(i):
        return 'PS%d' % i

    def DMA(eng, out, in_, r=(), w=()):
        S.op(eng, lambda e: e.dma_start(out=out, in_=in_), reads=r, writes=w, dma=True)

    cst = A.f32([128, 5, 128])
    DMA('sp', cst, consts, w=['cst'])
    ident = cst[:, 0, :]
    U_incl = cst[:, 1, :]
    L_incl = cst[:, 2, :]
    U_strict = cst[:, 3, :]
    ones = cst[:, 4, :]
    identb = A.bf16([128, 128])
    S.op('dve', lambda e: e.tensor_copy(out=identb, in_=ident), reads=['cst'], writes=['identb'])
    maskb_t = A.f32([128, 1])
    DMA('sp', maskb_t, maskb, w=['maskb'])
    zero_t = A.f32([128, 1024])
    S.op('pool', lambda e: e.memset(zero_t, 0.0), writes=['zero'])
    base0 = A.off

    copy_jobs = []
    for s_ in range(16):
        for ch in range(4):
            r0 = ch * 512
            r1 = min(r0 + 512, 2047)
            copy_jobs.append((2, s_, r0, r1))
        copy_jobs.append((1, s_, 0, 511))
        copy_jobs.append((0, s_, 0, 127))

    def issue_copies(n):
        for _ in range(n):
            if not copy_jobs:
                return
            gi, s_, r0, r1 = copy_jobs.pop(0)
            S.op('sp', lambda e, gi=gi, s_=s_, r0=r0, r1=r1: e.dma_start(out=kvs[gi][s_, r0:r1, :], in_=caches[gi][s_, r0 + 1:r1 + 1, :]),
                 writes=[], dma=True, pool='spc')
    S.op('act', lambda e: e.dma_start(out=scs[:, 0:2, :], in_=st_conv[:, 1:3, :]), writes=['scs01'], dma=True)

    xnT = A.bf16([128, 8, NTILE * 128])
    w1b = A.f32([128, D])
    DMA('sp', w1b, norm1.partition_broadcast(128), w=['w1b'])
    xt = [A.f32([128, D]) for _ in range(2)]
    sq = A.f32([128, D])
    ss = A.f32([128, 1])
    rstd = A.f32([128, 1])
    xn = A.bf16([128, D])
    for t in range(NTILE):
        xx = xt[t % 2]
        kx = 'xt%d' % (t % 2)
        DMA('sp', xx, x_all[t * 128:(t + 1) * 128, :], w=[kx])
        S.op('act', lambda e, xx=xx: e.activation(out=sq, in_=xx, func=AF.Square, accum_out=ss), reads=[kx], writes=['sq', 'ss'])
        S.op('act', lambda e: e.activation(out=ss, in_=ss, func=AF.Sqrt, scale=1.0 / D, bias=EPS), writes=['ss'])
        S.op('dve', lambda e: e.reciprocal(out=rstd, in_=ss), reads=['ss'], writes=['rstd'])
        S.op('dve', lambda e, xx=xx: e.scalar_tensor_tensor(out=xn, in0=xx, scalar=rstd, in1=w1b, op0=ALU.mult, op1=ALU.mult),
             reads=[kx, 'rstd', 'w1b'], writes=['xn'])
        pb = t % 2

        def tr(e, pb=pb):
            for k in range(8):
                ins = e.transpose(out=PBb(pb)[:, k * 128:(k + 1) * 128], in_=xn[:, k * 128:(k + 1) * 128], identity=identb)
            return ins
        S.op('pe', tr, reads=['xn', 'identb'], writes=[pk(pb)])
        S.op('act', lambda e, t=t, pb=pb: e.activation(out=xnT[:, :, t * 128:(t + 1) * 128],
                                                       in_=PBb(pb).rearrange("p (k t) -> p k t", k=8), func=AF.Copy),
             writes=[pk(pb), 'xnT%d' % t])

    DMA('pool', proj[0:3, 0:1024], zero_t[0:3, :], r=['zero'], w=['projz'])
    DMA('pool', proj[0:3, 1024:1536], zero_t[0:3, 0:512], r=['zero'], w=['projz2'])
    wst = [A.f32([128, 8, 512]) for _ in range(2)]
    wbf = [A.bf16([128, 8, 512]) for _ in range(2)]
    ot = [A.f32([128, 512]) for _ in range(6)]
    oc = 0
    for cb, (c0, ncol) in enumerate(COLBLK):
        wi = cb % 2
        DMA('sp', wst[wi][:, :, 0:ncol], w_in[:, c0:c0 + ncol].rearrange("(k p) c -> p k c", p=128), w=['wst%d' % wi])
        S.op('pool', lambda e, wi=wi, ncol=ncol: e.tensor_copy(out=wbf[wi][:, :, 0:ncol], in_=wst[wi][:, :, 0:ncol]),
             reads=['wst%d' % wi], writes=['wbf%d' % wi])
        for t in range(NTILE):
            if t < 16 and cb in HALO_SKIP:
                continue
            pb = 2 + (oc % 6)
            oi = oc % 6
            oc += 1

            def mm(e, t=t, wi=wi, ncol=ncol, pb=pb):
                for k in range(8):
                    ins = e.matmul(PB(pb)[:, 0:ncol], lhsT=xnT[:, k, t * 128:(t + 1) * 128], rhs=wbf[wi][:, k, 0:ncol],
                                   start=(k == 0), stop=(k == 7))
                return ins
            S.op('pe', mm, reads=['xnT%d' % t, 'wbf%d' % wi], writes=[pk(pb)])
            ee = 'act' if oc % 2 == 0 else 'dve'
            if ee == 'act':
                S.op('act', lambda e, oi=oi, pb=pb, ncol=ncol: e.activation(out=ot[oi][:, 0:ncol], in_=PB(pb)[:, 0:ncol], func=AF.Copy),
                     writes=[pk(pb), 'ot%d' % oi])
            else:
                S.op('dve', lambda e, oi=oi, pb=pb, ncol=ncol: e.tensor_copy(out=ot[oi][:, 0:ncol], in_=PB(pb)[:, 0:ncol]),
                     writes=[pk(pb), 'ot%d' % oi])
            DMA('pool', proj[3 + t * 128:3 + (t + 1) * 128, c0:c0 + ncol], ot[oi][:, 0:ncol], r=['ot%d' % oi], w=[])
    S.barrier()
    DMA('sp', scp, proj[3 + 4096 - 3:3 + 4096, 0:1536], w=['scp'])
    DMA('sp', scs[:, 2, :], proj[3 + 4096:3 + 4096 + 16, 0:1536], w=['scs2'])

    if stop_after == 'B':
        S.emit()
        return nc
    A.off = base0
    cw = A.f32([128, 4, 1536])
    for k in range(4):
        DMA('sp', cw[:, k, :], conv_wT[k:k + 1, :].partition_broadcast(128), w=['cw'])
    negA = A.f32([128, 4])
    dtb = A.f32([128, 4])
    DMA('sp', negA, a_log.partition_broadcast(128), w=['negA'])
    DMA('sp', dtb, dt_bias.partition_broadcast(128), w=['dtb'])
    S.op('act', lambda e: e.activation(out=negA, in_=negA, func=AF.Exp), writes=['negA'])
    S.op('dve', lambda e: e.tensor_scalar(out=negA, in0=negA, scalar1=-1.0, scalar2=None, op0=ALU.mult), writes=['negA'])
    xs = [[A.f32([128, 1536]) for _ in range(4)] for _ in range(2)]
    ccs = [A.f32([128, 1536]) for _ in range(2)]
    gt = [A.f32([128, 1544]) for _ in range(3)]
    sqc = A.f32([128, 1024])
    ssc = A.f32([128, 8])
    rsc = A.f32([128, 8])
    ba = [A.f32([128, 8]) for _ in range(2)]
    gx = A.f32([128, 4])
    def c1_tile(t):
            bi = t % 2
            cc = ccs[t % 2]
            kcc = 'cc%d' % (t % 2)
            for k in range(4):
                DMA('sp', xs[bi][k], proj[t * 128 + k:t * 128 + k + 128, 0:1536], w=['xs%d_%d' % (bi, k)])
            DMA('sp', ba[bi], proj[3 + t * 128:3 + (t + 1) * 128, 2048:2056], w=['ba%d' % bi])
            for k in range(4):
                S.op('pool', lambda e, bi=bi, k=k: e.tensor_tensor(out=xs[bi][k], in0=xs[bi][k], in1=cw[:, k, :], op=ALU.mult),
                     reads=['cw'], writes=['xs%d_%d' % (bi, k)])
            S.op('pool', lambda e, bi=bi: e.tensor_tensor(out=cc, in0=xs[bi][0], in1=xs[bi][1], op=ALU.add), reads=['xs%d_0' % bi, 'xs%d_1' % bi], writes=[kcc])
            S.op('pool', lambda e, bi=bi: e.tensor_tensor(out=cc, in0=cc, in1=xs[bi][2], op=ALU.add), reads=['xs%d_2' % bi], writes=[kcc])
            S.op('pool', lambda e, bi=bi: e.tensor_tensor(out=cc, in0=cc, in1=xs[bi][3], op=ALU.add), reads=['xs%d_3' % bi], writes=[kcc])
    def c2_tile(t):
            bi = t % 2
            cc = ccs[t % 2]
            kcc = 'cc%d' % (t % 2)
            S.op('act', lambda e: e.activation(out=cc, in_=cc, func=AF.Silu), writes=[kcc])
            g_t = gt[t % 3]
            kg = 'gt%d' % (t % 3)
            S.op('act', lambda e: e.activation(out=sqc, in_=cc[:, 0:1024], func=AF.Square), reads=[kcc], writes=['sqc'])
            S.op('dve', lambda e: e.tensor_reduce(out=ssc, in_=sqc.rearrange("p (h d) -> p h d", h=8), axis=AX.X, op=ALU.add),
                 reads=['sqc'], writes=['ssc'])
            S.op('act', lambda e: e.activation(out=ssc, in_=ssc, func=AF.Sqrt, bias=EPS), writes=['ssc'])
            S.op('dve', lambda e: e.reciprocal(out=rsc, in_=ssc), reads=['ssc'], writes=['rsc'])
            S.op('dve', lambda e: e.tensor_scalar(out=rsc[:, 0:4], in0=rsc[:, 0:4], scalar1=128.0 ** -0.5, scalar2=None, op0=ALU.mult), writes=['rsc'])
            S.op('dve', lambda e, g_t=g_t: e.tensor_tensor(out=g_t[:, 0:1024].rearrange("p (h d) -> p h d", h=8),
                                                           in0=cc[:, 0:1024].rearrange("p (h d) -> p h d", h=8),
                                                           in1=rsc.unsqueeze(2).broadcast_to([128, 8, 128]), op=ALU.mult),
                 reads=[kcc, 'rsc'], writes=[kg])
            S.op('pool', lambda e, g_t=g_t: e.tensor_copy(out=g_t[:, 1024:1536], in_=cc[:, 1024:1536]), reads=[kcc], writes=[kg])
            S.op('act', lambda e, g_t=g_t, bi=bi: e.activation(out=g_t[:, 1536:1540], in_=ba[bi][:, 0:4], func=AF.Sigmoid),
                 reads=['ba%d' % bi], writes=[kg])
            S.op('dve', lambda e, bi=bi: e.tensor_tensor(out=gx, in0=ba[bi][:, 4:8], in1=dtb, op=ALU.add), reads=['ba%d' % bi, 'dtb'], writes=['gx'])
            S.op('act', lambda e: e.activation(out=gx, in_=gx, func=AF.Exp), writes=['gx'])
            S.op('act', lambda e: e.activation(out=gx, in_=gx, func=AF.Ln, bias=1.0), writes=['gx'])
            S.op('dve', lambda e, g_t=g_t: e.tensor_tensor(out=g_t[:, 1540:1544], in0=gx, in1=negA, op=ALU.mult), reads=['gx', 'negA'], writes=[kg])
    Sst = A.f32([128, 4, 128])
    S.op('pool', lambda e: e.memset(Sst, 0.0), writes=['Sst'])
    names = ['gc', 'gb', 'mt', 'DT', 'DTs', 'egc', 'egl', 'kds', 'bws', 'kT', 'qgT', 'egb', 'ATn', 'inT', 'Am', 'AT', 'X2', 'XT2',
             'R0', 'R1', 'Bv', 'Bw', 'u', 'wT', 'vn', 'kdec', 'o', 'glast']
    W_ = {}
    for n_ in names:
        if n_ in ('gc', 'egc', 'egl', 'kds', 'bws', 'glast'):
            W_[n_] = A.f32([128, 4])
        else:
            W_[n_] = A.f32([128, 4, 128])
    identbc = ident.unsqueeze(1).broadcast_to([128, 4, 128])

    def P4(i):
        return PB(i).rearrange("p (h d) -> p h d", h=4)

    def evac(eng, out, pbi, w, extra_r=()):
        if eng == 'act':
            S.op('act', lambda e: e.activation(out=out, in_=P4(pbi), func=AF.Copy), reads=list(extra_r), writes=[pk(pbi), w])
        else:
            S.op('dve', lambda e: e.tensor_copy(out=out, in_=P4(pbi)), reads=list(extra_r), writes=[pk(pbi), w])

    def d_tile(t):
            G = gt[t % 3]
            kG = 'gt%d' % (t % 3)
            issue_copies(3)
            qv = G[:, 0:512].rearrange("p (h d) -> p h d", h=4)
            kv_ = G[:, 512:1024].rearrange("p (h d) -> p h d", h=4)
            vv = G[:, 1024:1536].rearrange("p (h d) -> p h d", h=4)
            beta = G[:, 1536:1540]
            gg = G[:, 1540:1544]
            S.op('pe', lambda e, gg=gg: e.matmul(PB(0)[:, 0:4], lhsT=U_incl, rhs=gg, start=True, stop=True), reads=[kG, 'cst'], writes=[pk(0)])
            S.op('dve', lambda e: e.tensor_copy(out=W_['gc'], in_=PB(0)[:, 0:4]), writes=[pk(0), 'gc'])
            for h in range(4):
                S.op('dve', lambda e, h=h, gg=gg: e.tensor_scalar(out=W_['gb'][:, h, :], in0=ones, scalar1=gg[:, h:h + 1], scalar2=None, op0=ALU.mult),
                     reads=[kG, 'cst'], writes=['gb'])

            def mm_gb(e):
                for h in range(4):
                    ins = e.matmul(P4(1)[:, h, :], lhsT=W_['gb'][:, h, :], rhs=U_incl, start=True, stop=True)
                return ins
            S.op('pe', mm_gb, reads=['gb', 'cst'], writes=[pk(1)])
            for h in range(4):
                S.op('dve', lambda e, h=h: e.tensor_scalar(out=W_['mt'][:, h, :], in0=P4(1)[:, h, :], scalar1=W_['gc'][:, h:h + 1], scalar2=0.0,
                                                           op0=ALU.subtract, op1=ALU.min), reads=['gc'], writes=[pk(1), 'mt'])
            S.op('act', lambda e: e.activation(out=W_['mt'], in_=W_['mt'], func=AF.Exp), writes=['mt'])
            S.op('dve', lambda e: e.tensor_tensor(out=W_['DT'], in0=W_['mt'], in1=U_incl.unsqueeze(1).broadcast_to([128, 4, 128]), op=ALU.mult),
                 reads=['mt', 'cst'], writes=['DT'])
            S.op('dve', lambda e: e.tensor_tensor(out=W_['DTs'], in0=W_['mt'], in1=U_strict.unsqueeze(1).broadcast_to([128, 4, 128]), op=ALU.mult),
                 reads=['mt', 'cst'], writes=['DTs'])
            if t >= 16:
                S.op('act', lambda e: e.activation(out=W_['egb'], in_=P4(1), func=AF.Exp), writes=[pk(1), 'egb'])
            S.op('dve', lambda e: e.tensor_copy(out=W_['glast'], in_=P4(1)[:, :, 127]), writes=[pk(1), 'glast'])
            S.op('act', lambda e: e.activation(out=W_['egc'], in_=W_['gc'], func=AF.Exp), reads=['gc'], writes=['egc'])
            S.op('act', lambda e: e.activation(out=W_['egl'], in_=W_['glast'], func=AF.Exp), reads=['glast'], writes=['egl'])
            S.op('dve', lambda e: e.tensor_tensor(out=W_['kds'], in0=W_['glast'], in1=W_['gc'], op=ALU.subtract), reads=['glast', 'gc'], writes=['kds'])
            S.op('act', lambda e: e.activation(out=W_['kds'], in_=W_['kds'], func=AF.Exp), writes=['kds'])
            S.op('dve', lambda e, beta=beta: e.tensor_tensor(out=W_['bws'], in0=beta, in1=W_['egc'], op=ALU.mult), reads=[kG, 'egc'], writes=['bws'])
            def tr_k(e, kv_=kv_):
                for h in range(4):
                    ins = e.transpose(out=P4(2)[:, h, :], in_=kv_[:, h, :], identity=ident)
                return ins
            S.op('pe', tr_k, reads=[kG, 'cst'], writes=[pk(2)])
            evac('act', W_['kT'], 2, 'kT')

            if t >= 16:
                def tr_q(e, qv=qv):
                    for h in range(4):
                        ins = e.transpose(out=P4(3)[:, h, :], in_=qv[:, h, :], identity=ident)
                    return ins
                S.op('pe', tr_q, reads=[kG, 'cst'], writes=[pk(3)])
                qTd = W_['o']
                evac('dve', qTd, 3, 'o')
                S.op('dve', lambda e, qTd=qTd: e.tensor_tensor(out=W_['qgT'], in0=qTd, in1=W_['egb'], op=ALU.mult), reads=['o', 'egb'], writes=['qgT'])
            def mm_g(e):
                for h in range(4):
                    ins = e.matmul(P4(4)[:, h, :], lhsT=W_['kT'][:, h, :], rhs=W_['kT'][:, h, :], start=True, stop=True)
                return ins
            S.op('pe', mm_g, reads=['kT'], writes=[pk(4)])

            if t >= 16:
                def mm_qk(e, qTd=qTd):
                    for h in range(4):
                        ins = e.matmul(P4(5)[:, h, :], lhsT=W_['kT'][:, h, :], rhs=qTd[:, h, :], start=True, stop=True)
                    return ins
                S.op('pe', mm_qk, reads=['kT', 'o'], writes=[pk(5)])
            S.op('dve', lambda e: e.tensor_tensor(out=W_['ATn'], in0=P4(4), in1=W_['DTs'], op=ALU.mult), reads=['DTs'], writes=[pk(4), 'ATn'])
            if t >= 16:
                S.op('dve', lambda e: e.tensor_tensor(out=W_['inT'], in0=P4(5), in1=W_['DT'], op=ALU.mult), reads=['DT'], writes=[pk(5), 'inT'])

            def tr_a(e):
                for h in range(4):
                    ins = e.transpose(out=P4(6)[:, h, :], in_=W_['ATn'][:, h, :], identity=ident)
                return ins
            S.op('pe', tr_a, reads=['ATn', 'cst'], writes=[pk(6)])
            S.op('dve', lambda e, beta=beta: e.tensor_tensor(out=W_['Am'], in0=P4(6), in1=beta.unsqueeze(2).broadcast_to([128, 4, 128]), op=ALU.mult),
                 reads=[kG], writes=[pk(6), 'Am'])

            def tr_at(e):
                for h in range(4):
                    ins = e.transpose(out=P4(7)[:, h, :], in_=W_['Am'][:, h, :], identity=ident)
                return ins
            S.op('pe', tr_at, reads=['Am', 'cst'], writes=[pk(7)])
            evac('act', W_['AT'], 7, 'AT')
            S.op('dve', lambda e: e.tensor_tensor(out=W_['R0'], in0=identbc, in1=W_['AT'], op=ALU.subtract), reads=['AT', 'cst'], writes=['R0'])
            X, XT, kX, kXT = W_['Am'], W_['AT'], 'Am', 'AT'
            Xn, XTn, kXn, kXTn = W_['X2'], W_['XT2'], 'X2', 'XT2'
            Rc, Rn, kRc, kRn = W_['R0'], W_['R1'], 'R0', 'R1'
            for lvl in range(6):
                def mm_x2(e, X=X, XT=XT):
                    for h in range(4):
                        ins = e.matmul(P4(2)[:, h, :], lhsT=XT[:, h, :], rhs=X[:, h, :], start=True, stop=True)
                    return ins
                S.op('pe', mm_x2, reads=[kX, kXT], writes=[pk(2)])
                if lvl < 5:
                    def mm_xt2(e, X=X, XT=XT):
                        for h in range(4):
                            ins = e.matmul(P4(3)[:, h, :], lhsT=X[:, h, :], rhs=XT[:, h, :], start=True, stop=True)
                        return ins
                    S.op('pe', mm_xt2, reads=[kX, kXT], writes=[pk(3)])
                evac('act', Xn, 2, kXn)
                if lvl < 5:
                    evac('act', XTn, 3, kXTn)

                def mm_r(e, Xn=Xn, Rc=Rc):
                    for h in range(4):
                        ins = e.matmul(P4(4)[:, h, :], lhsT=Xn[:, h, :], rhs=Rc[:, h, :], start=True, stop=True)
                    return ins
                S.op('pe', mm_r, reads=[kXn, kRc], writes=[pk(4)])
                S.op('dve', lambda e, Rn=Rn, Rc=Rc: e.tensor_tensor(out=Rn, in0=P4(4), in1=Rc, op=ALU.add), reads=[kRc], writes=[pk(4), kRn])
                X, XT, kX, kXT, Xn, XTn, kXn, kXTn = Xn, XTn, kXn, kXTn, X, XT, kX, kXT
                Rc, Rn, kRc, kRn = Rn, Rc, kRn, kRc
            R, kR = Rc, kRc
            S.op('dve', lambda e, vv=vv, beta=beta: e.tensor_tensor(out=W_['Bv'], in0=vv, in1=beta.unsqueeze(2).broadcast_to([128, 4, 128]), op=ALU.mult),
                 reads=[kG], writes=['Bv'])
            S.op('dve', lambda e, kv_=kv_: e.tensor_tensor(out=W_['Bw'], in0=kv_, in1=W_['bws'].unsqueeze(2).broadcast_to([128, 4, 128]), op=ALU.mult),
                 reads=[kG, 'bws'], writes=['Bw'])
            S.op('dve', lambda e, kv_=kv_: e.tensor_tensor(out=W_['kdec'], in0=kv_, in1=W_['kds'].unsqueeze(2).broadcast_to([128, 4, 128]), op=ALU.mult),
                 reads=[kG, 'kds'], writes=['kdec'])

            def mm_u(e, R=R):
                for h in range(4):
                    ins = e.matmul(P4(5)[:, h, :], lhsT=R[:, h, :], rhs=W_['Bv'][:, h, :], start=True, stop=True)
                return ins
            S.op('pe', mm_u, reads=[kR, 'Bv'], writes=[pk(5)])
            evac('act', W_['u'], 5, 'u')

            def mm_w(e, R=R):
                for h in range(4):
                    ins = e.matmul(P4(6)[:, h, :], lhsT=W_['Bw'][:, h, :], rhs=R[:, h, :], start=True, stop=True)
                return ins
            S.op('pe', mm_w, reads=[kR, 'Bw'], writes=[pk(6)])
            evac('dve', W_['wT'], 6, 'wT')
            def mm_ws(e):
                for h in range(4):
                    ins = e.matmul(P4(7)[:, h, :], lhsT=W_['wT'][:, h, :], rhs=Sst[:, h, :], start=True, stop=True)
                return ins
            S.op('pe', mm_ws, reads=['wT', 'Sst'], writes=[pk(7)])
            S.op('dve', lambda e: e.tensor_tensor(out=W_['vn'], in0=W_['u'], in1=P4(7), op=ALU.subtract), reads=['u'], writes=[pk(7), 'vn'])
            if t >= 16:
                def mm_o(e):
                    for h in range(4):
                        e.matmul(P4(0)[:, h, :], lhsT=W_['qgT'][:, h, :], rhs=Sst[:, h, :], start=True, stop=False)
                        ins = e.matmul(P4(0)[:, h, :], lhsT=W_['inT'][:, h, :], rhs=W_['vn'][:, h, :], start=False, stop=True)
                    return ins
                S.op('pe', mm_o, reads=['qgT', 'Sst', 'inT', 'vn'], writes=[pk(0)])
                evac('act', W_['o'], 0, 'o')
                DMA('pool', o_gdn[(t - 16) * 128:(t - 15) * 128, :], W_['o'].rearrange("p h d -> p (h d)"), r=['o'], w=[])

            def mm_su(e):
                for h in range(4):
                    ins = e.matmul(P4(1)[:, h, :], lhsT=W_['kdec'][:, h, :], rhs=W_['vn'][:, h, :], start=True, stop=True)
                return ins
            S.op('pe', mm_su, reads=['kdec', 'vn'], writes=[pk(1)])
            for h in range(4):
                S.op('dve', lambda e, h=h: e.scalar_tensor_tensor(out=Sst[:, h, :], in0=Sst[:, h, :], scalar=W_['egl'][:, h:h + 1], in1=P4(1)[:, h, :],
                                                                  op0=ALU.mult, op1=ALU.add), reads=['egl'], writes=[pk(1), 'Sst'])
    qkw = A.f32([128, 6, 64])
    for gi in range(3):
        DMA('sp', qkw[:, gi, :], qnw[gi:gi + 1, :].partition_broadcast(128), w=['qkw'])
        DMA('sp', qkw[:, 3 + gi, :], knw[gi:gi + 1, :].partition_broadcast(128), w=['qkw'])
    rin = [A.f32([128, 512]) for _ in range(6)]
    rsq = A.f32([128, 512])
    rss = A.f32([128, 8])
    rrs = A.f32([128, 8])
    rout = [A.f32([128, 512]) for _ in range(6)]
    rp = [A.f32([128, 16]) for _ in range(4)]
    rt = [A.f32([128, 8, 8]) for _ in range(4)]
    ecnt = [0, 0]
    e_ctx = {}

    def e_load(gi, t):
        cq = 2056 + gi * 1536
        ri = ecnt[1] % 4
        ecnt[1] += 1
        DMA('sp', rp[ri], rope_t[t * 128:(t + 1) * 128, :], w=['rp%d' % ri])
        bis = []
        for which in ((1, 0) if t >= 16 else (1,)):
            bi = ecnt[0] % 6
            ecnt[0] += 1
            c0 = cq + which * 512
            DMA('sp', rin[bi], proj[3 + t * 128:3 + (t + 1) * 128, c0:c0 + 512], w=['rin%d' % bi])
            bis.append(bi)
        e_ctx[(gi, t)] = (ri, bis)

    def e_tile(gi, t):
        W, dil = GROUPS[gi]
        cq = 2056 + gi * 1536
        ri, bis = e_ctx[(gi, t)]
        if True:
            cosb = rp[ri][:, 0:8].unsqueeze(1).broadcast_to([128, 8, 8])
            sinb = rp[ri][:, 8:16].unsqueeze(1).broadcast_to([128, 8, 8])
            for wi_, which in enumerate((1, 0) if t >= 16 else (1,)):
                bi = bis[wi_]
                src = rin[bi]
                dst = rout[bi]
                ks, kd = 'rin%d' % bi, 'rout%d' % bi
                c0 = cq + which * 512
                S.op('act', lambda e, src=src: e.activation(out=rsq, in_=src, func=AF.Square), reads=[ks], writes=['rsq'])
                S.op('dve', lambda e: e.tensor_reduce(out=rss, in_=rsq.rearrange("p (h d) -> p h d", h=8), axis=AX.X, op=ALU.add),
                     reads=['rsq'], writes=['rss'])
                S.op('act', lambda e: e.activation(out=rss, in_=rss, func=AF.Sqrt, scale=1.0 / 64, bias=EPS), writes=['rss'])
                S.op('dve', lambda e: e.reciprocal(out=rrs, in_=rss), reads=['rss'], writes=['rrs'])
                d3 = dst.rearrange("p (h d) -> p h d", h=8)
                S.op('dve', lambda e, src=src, d3=d3: e.tensor_tensor(out=d3, in0=src.rearrange("p (h d) -> p h d", h=8),
                                                                      in1=rrs.unsqueeze(2).broadcast_to([128, 8, 64]), op=ALU.mult),
                     reads=[ks, 'rrs'], writes=[kd])
                wrow = qkw[:, (3 if which == 1 else 0) + gi, :].unsqueeze(1).broadcast_to([128, 8, 64])
                S.op('dve', lambda e, d3=d3, wrow=wrow: e.tensor_tensor(out=d3, in0=d3, in1=wrow, op=ALU.mult), reads=['qkw'], writes=[kd])
                x1 = d3[:, :, 0:8]
                x2 = d3[:, :, 8:16]
                S.op('dve', lambda e, x1=x1, cosb=cosb: e.tensor_tensor(out=rt[0], in0=x1, in1=cosb, op=ALU.mult), reads=[kd, 'rp%d' % ri], writes=['rt0'])
                S.op('dve', lambda e, x2=x2, sinb=sinb: e.tensor_tensor(out=rt[1], in0=x2, in1=sinb, op=ALU.mult), reads=[kd, 'rp%d' % ri], writes=['rt1'])
                S.op('dve', lambda e, x2=x2, cosb=cosb: e.tensor_tensor(out=rt[2], in0=x2, in1=cosb, op=ALU.mult), reads=[kd, 'rp%d' % ri], writes=['rt2'])
                S.op('dve', lambda e, x1=x1, sinb=sinb: e.tensor_tensor(out=rt[3], in0=x1, in1=sinb, op=ALU.mult), reads=[kd, 'rp%d' % ri], writes=['rt3'])
                S.op('dve', lambda e, x1=x1: e.tensor_tensor(out=x1, in0=rt[0], in1=rt[1], op=ALU.subtract), reads=['rt0', 'rt1'], writes=[kd])
                S.op('dve', lambda e, x2=x2: e.tensor_tensor(out=x2, in0=rt[2], in1=rt[3], op=ALU.add), reads=['rt2', 'rt3'], writes=[kd])
                if which == 1:
                    DMA('pool', kn_s[gi, t * 128:(t + 1) * 128, :], dst, r=[kd], w=[])
                    if t * 128 >= 4096 - W:
                        r0 = t * 128 - (4096 - W)
                        DMA('pool', kvp[gi][r0:r0 + 128, 0, :], dst, r=[kd], w=[])
                        DMA('pool', kvp[gi][r0:r0 + 128, 1, :], proj[3 + t * 128:3 + (t + 1) * 128, cq + 1024:cq + 1536], w=[])
                else:
                    DMA('pool', qn_s[gi, (t - 16) * 128:(t - 15) * 128, :], dst, r=[kd], w=[])
    e_units = []
    for gi in range(3):
        for t in range(16 - GROUPS[gi][0] // 128, 32):
            e_units.append((gi, t))
    c1_tile(0)
    c1_tile(1)
    c2_tile(0)
    per = (len(e_units) + 31) // 32
    e_sched = [[e_units.pop(0) for _ in range(min(per, len(e_units)))] for _ in range(32)]
    for u in e_sched[0]:
        e_load(*u)
    for t in range(32):
        lc, ld, le = [], [], []
        S.defer = lc
        if t + 1 < 32:
            c2_tile(t + 1)
        if t + 2 < 32:
            c1_tile(t + 2)
        S.defer = ld
        d_tile(t)
        S.defer = le
        for u in e_sched[t]:
            e_tile(*u)
        if t + 1 < 32:
            for u in e_sched[t + 1]:
                e_load(*u)
        S.defer = None
        S.replay([ld, lc, le], [4, 1, 2])
    issue_copies(1000)
    DMA('pool', sgp.rearrange("h k v -> k h v"), Sst, r=['Sst'], w=['sgp'])
    S.barrier()

    if stop_after == 'E':
        S.emit()
        return nc
    A.off = base0
    fin = [A.f32([128, 512]) for _ in range(4)]
    fb = [A.bf16([128, 512]) for _ in range(2)]
    qT = A.bf16([128, 4, 128])
    kTs = [A.bf16([128, 4, 128]) for _ in range(2)]
    Vs = [A.bf16([128, 8, 66]) for _ in range(2)]
    for i in range(2):
        S.op('pool', lambda e, i=i: e.memset(Vs[i], 1.0), writes=['Vs%d' % i])
    Pm = [A.bf16([128, 8, 128]) for _ in range(2)]
    Ex = A.bf16([128, 4, 128])
    Osb = A.f32([128, 8, 65])
    fc = 0
    for gi in range(3):
        W, dil = GROUPS[gi]
        nblk = 4096 // dil // 128
        first_q = 2048 // dil // 128
        cv = 2056 + gi * 1536 + 1024
        for r in range(dil):
            for mb in range(first_q - 1, nblk):
                slot = mb % 2
                tok0 = r + dil * mb * 128
                rows = slice(tok0, tok0 + dil * 127 + 1, dil)
                prow = slice(3 + tok0, 3 + tok0 + dil * 127 + 1, dil)
                f1 = fin[fc % 4]; k1 = 'fin%d' % (fc % 4); fc += 1
                DMA('sp', f1, kn_s[gi, rows, :], w=[k1])
                S.op('act', lambda e, f1=f1: e.activation(out=fb[0], in_=f1, func=AF.Copy), reads=[k1], writes=['fb0'])

                def tr1(e):
                    for hp in range(4):
                        ins = e.transpose(out=PBb(0)[:, hp * 128:(hp + 1) * 128], in_=fb[0][:, hp * 128:(hp + 1) * 128], identity=identb)
                    return ins
                S.op('pe', tr1, reads=['fb0', 'identb'], writes=[pk(0)])
                S.op('dve', lambda e, slot=slot: e.tensor_copy(out=kTs[slot], in_=PBb(0)[:, 0:512].rearrange("p (a b) -> p a b", a=4)),
                     writes=[pk(0), 'kT%d' % slot])
                f2 = fin[fc % 4]; k2 = 'fin%d' % (fc % 4); fc += 1
                DMA('sp', f2, proj[prow, cv:cv + 512], w=[k2])
                S.op('pool', lambda e, f2=f2, slot=slot: e.tensor_copy(out=Vs[slot][:, :, 0:64], in_=f2.rearrange("p (h d) -> p h d", h=8)),
                     reads=[k2], writes=['Vs%d' % slot])
                if mb < first_q:
                    continue
                f3 = fin[fc % 4]; k3 = 'fin%d' % (fc % 4); fc += 1
                qrow = slice(tok0 - 2048, tok0 - 2048 + dil * 127 + 1, dil)
                DMA('sp', f3, qn_s[gi, qrow, :], w=[k3])
                S.op('act', lambda e, f3=f3: e.activation(out=fb[1], in_=f3, func=AF.Copy), reads=[k3], writes=['fb1'])

                def tr2(e):
                    for hp in range(4):
                        ins = e.transpose(out=PBb(1)[:, hp * 128:(hp + 1) * 128], in_=fb[1][:, hp * 128:(hp + 1) * 128], identity=identb)
                    return ins
                S.op('pe', tr2, reads=['fb1', 'identb'], writes=[pk(1)])
                S.op('dve', lambda e: e.tensor_copy(out=qT, in_=PBb(1)[:, 0:512].rearrange("p (a b) -> p a b", a=4)), writes=[pk(1), 'qT'])
                halo_prev = (mb == first_q)
                for which, sl in ((0, 1 - slot), (1, slot)):
                    for par in range(2):
                        pbi = 2 + par

                        def mm_s(e, sl=sl, par=par, pbi=pbi):
                            for hp in range(4):
                                lo = par * 64
                                ins = e.matmul(PB(pbi)[:, hp * 128:(hp + 1) * 128], lhsT=kTs[sl][lo:lo + 64, hp, :], rhs=qT[lo:lo + 64, hp, :],
                                               start=True, stop=True)
                            return ins
                        S.op('pe', mm_s, reads=['kT%d' % sl, 'qT'], writes=[pk(pbi)])
                        if which == 0 and halo_prev:
                            S.op('act', lambda e, pbi=pbi: e.activation(out=Ex, in_=P4(pbi), func=AF.Exp, scale=0.125, bias=maskb_t),
                                 reads=['maskb'], writes=[pk(pbi), 'Ex'])
                        else:
                            S.op('act', lambda e, pbi=pbi: e.activation(out=Ex, in_=P4(pbi), func=AF.Exp, scale=0.125), writes=[pk(pbi), 'Ex'])
                        msk = (L_incl if which == 0 else U_incl).unsqueeze(1).broadcast_to([128, 4, 128])
                        S.op('dve', lambda e, which=which, par=par, msk=msk: e.tensor_tensor(out=Pm[which][:, par:8:2, :], in0=Ex, in1=msk, op=ALU.mult),
                             reads=['Ex', 'cst'], writes=['Pm%d_%d' % (which, par)])
                for half in range(2):
                    pbi = 4 + half

                    def mm_pv(e, half=half, pbi=pbi, slot=slot):
                        for hh in range(4):
                            h = half * 4 + hh
                            e.matmul(PB(pbi)[:, hh * 65:(hh + 1) * 65], lhsT=Pm[0][:, h, :], rhs=Vs[1 - slot][:, h, 0:65], start=True, stop=False)
                            ins = e.matmul(PB(pbi)[:, hh * 65:(hh + 1) * 65], lhsT=Pm[1][:, h, :], rhs=Vs[slot][:, h, 0:65], start=False, stop=True)
                        return ins
                    S.op('pe', mm_pv, reads=['Pm0_0', 'Pm0_1', 'Pm1_0', 'Pm1_1', 'Vs0', 'Vs1'], writes=[pk(pbi)])
                    S.op('act', lambda e, half=half, pbi=pbi: e.activation(out=Osb[:, half * 4:(half + 1) * 4, :],
                                                                           in_=PB(pbi)[:, 0:260].rearrange("p (h d) -> p h d", h=4), func=AF.Copy),
                         writes=[pk(pbi), 'Osb%d' % half])
                DMA('pool', att_o[gi, qrow, :], Osb.rearrange("p h d -> p (h d)"), r=['Osb0', 'Osb1'], w=[])
    S.barrier()

    if stop_after == 'F':
        S.emit()
        return nc
    A.off = base0
    gnb = A.f32([128, 128])
    DMA('sp', gnb, gnw.partition_broadcast(128), w=['gnb'])
    g_o = [A.f32([128, 512]) for _ in range(2)]
    g_z = [A.f32([128, 512]) for _ in range(2)]
    g_a = [[A.f32([128, 520]) for _ in range(3)] for _ in range(2)]
    mixt = [A.f32([128, D]) for _ in range(2)]
    gsq = A.f32([128, 512])
    gss = A.f32([128, 4])
    grs = A.f32([128, 4])
    gden = A.f32([128, 8])
    for t in range(16):
        bi = t % 2
        mx = mixt[bi]
        km = 'mixt%d' % bi
        DMA('sp', g_o[bi], o_gdn[t * 128:(t + 1) * 128, :], w=['g_o%d' % bi])
        DMA('sp', g_z[bi], proj[3 + (16 + t) * 128:3 + (17 + t) * 128, 1536:2048], w=['g_z%d' % bi])
        for gi in range(3):
            DMA('sp', g_a[bi][gi], att_o[gi, t * 128:(t + 1) * 128, :], w=['g_a%d_%d' % (bi, gi)])
        S.op('act', lambda e, bi=bi: e.activation(out=gsq, in_=g_o[bi], func=AF.Square), reads=['g_o%d' % bi], writes=['gsq'])
        S.op('dve', lambda e: e.tensor_reduce(out=gss, in_=gsq.rearrange("p (h d) -> p h d", h=4), axis=AX.X, op=ALU.add), reads=['gsq'], writes=['gss'])
        S.op('act', lambda e: e.activation(out=gss, in_=gss, func=AF.Sqrt, scale=1.0 / 128, bias=EPS), writes=['gss'])
        S.op('dve', lambda e: e.reciprocal(out=grs, in_=gss), reads=['gss'], writes=['grs'])
        m3 = mx[:, 0:512].rearrange("p (h d) -> p h d", h=4)
        S.op('dve', lambda e, bi=bi, m3=m3: e.tensor_tensor(out=m3, in0=g_o[bi].rearrange("p (h d) -> p h d", h=4),
                                                            in1=grs.unsqueeze(2).broadcast_to([128, 4, 128]), op=ALU.mult),
             reads=['g_o%d' % bi, 'grs'], writes=[km])
        S.op('pool', lambda e, m3=m3: e.tensor_tensor(out=m3, in0=m3, in1=gnb.unsqueeze(1).broadcast_to([128, 4, 128]), op=ALU.mult), reads=['gnb'], writes=[km])
        S.op('act', lambda e, bi=bi: e.activation(out=g_z[bi], in_=g_z[bi], func=AF.Silu), writes=['g_z%d' % bi])
        S.op('dve', lambda e, bi=bi, mx=mx: e.tensor_tensor(out=mx[:, 0:512], in0=mx[:, 0:512], in1=g_z[bi], op=ALU.mult), reads=['g_z%d' % bi], writes=[km])
        S.op('pool', lambda e, bi=bi: e.tensor_tensor(out=g_a[bi][0], in0=g_a[bi][0], in1=g_a[bi][1], op=ALU.add), reads=['g_a%d_1' % bi], writes=['g_a%d_0' % bi])
        S.op('pool', lambda e, bi=bi: e.tensor_tensor(out=g_a[bi][0], in0=g_a[bi][0], in1=g_a[bi][2], op=ALU.add), reads=['g_a%d_2' % bi], writes=['g_a%d_0' % bi])
        a3 = g_a[bi][0].rearrange("p (h d) -> p h d", h=8)
        S.op('dve', lambda e, a3=a3: e.reciprocal(out=gden, in_=a3[:, :, 64]), reads=['g_a%d_0' % bi], writes=['gden'])
        S.op('dve', lambda e, a3=a3, mx=mx: e.tensor_tensor(out=mx[:, 512:1024].rearrange("p (h d) -> p h d", h=8), in0=a3[:, :, 0:64],
                                                            in1=gden.unsqueeze(2).broadcast_to([128, 8, 64]), op=ALU.mult),
             reads=['g_a%d_0' % bi, 'gden'], writes=[km])
        DMA('pool', mix_d[t * 128:(t + 1) * 128, :], mx, r=[km], w=[])
    S.barrier()

    if stop_after == 'G1':
        S.emit()
        return nc
    sample_path(nc, S, A, base0, locals())

    A.off = base0
    yacc = A.f32([128, 17, D])
    h2T = A.bf16([128, 8, 17 * 128])
    gates = A.f32([128, 17, 32])
    base1 = A.off
    wob = A.bf16([128, 8, D])
    wos = A.f32([128, 8, 512])
    for hf in range(2):
        DMA('sp', wos, w_out[:, hf * 512:(hf + 1) * 512].rearrange("(k p) c -> p k c", p=128), w=['wos'])
        S.op('pool', lambda e, hf=hf: e.tensor_copy(out=wob[:, :, hf * 512:(hf + 1) * 512], in_=wos), reads=['wos'], writes=['wob'])
    wrt = A.f32([128, 8, 36])
    DMA('sp', wrt, w_r.rearrange("(k p) c -> p k c", p=128), w=['wrt'])
    w2b = A.f32([128, D])
    DMA('sp', w2b, norm2.partition_broadcast(128), w=['w2b'])
    mxl = [A.f32([128, D]) for _ in range(2)]
    xl = [A.f32([128, D]) for _ in range(2)]
    mxb = A.bf16([128, D])
    mxT = A.bf16([128, 8, 128])
    hsq = A.f32([128, D])
    hss = A.f32([128, 1])
    hrs = A.f32([128, 1])
    h2n = A.f32([128, D])
    h2Tf = A.f32([128, 8, 128])
    lg = A.f32([128, 36])
    r_ = {n_: A.f32([128, 8]) for n_ in ['gm', 'oh', 'eg', 'sg', 'pg', 'ein', 'm1', 'oh1', 'e2', 'm2', 'oh2', 'w1', 'w2', 'g8', 'tmp']}
    sel = A.f32([128, 4, 8])
    for t in range(17):
        bi = t % 2
        xrow = (16 + t) * 128 if t < 16 else 32 * 128
        DMA('sp', mxl[bi], mix_d[t * 128:(t + 1) * 128, :], w=['mxl%d' % bi])
        DMA('sp', xl[bi], x_all[xrow:xrow + 128, :], w=['xl%d' % bi])
        S.op('act', lambda e, bi=bi: e.activation(out=mxb, in_=mxl[bi], func=AF.Copy), reads=['mxl%d' % bi], writes=['mxb'])

        def trm(e):
            for k in range(8):
                ins = e.transpose(out=PBb(0)[:, k * 128:(k + 1) * 128], in_=mxb[:, k * 128:(k + 1) * 128], identity=identb)
            return ins
        S.op('pe', trm, reads=['mxb', 'identb'], writes=[pk(0)])
        S.op('dve', lambda e: e.tensor_copy(out=mxT, in_=PBb(0).rearrange("p (k t) -> p k t", k=8)), writes=[pk(0), 'mxT'])
        for hf in range(2):
            def mmo(e, hf=hf):
                for k in range(8):
                    ins = e.matmul(PB(1 + hf), lhsT=mxT[:, k, :], rhs=wob[:, k, hf * 512:(hf + 1) * 512], start=(k == 0), stop=(k == 7))
                return ins
            S.op('pe', mmo, reads=['mxT', 'wob'], writes=[pk(1 + hf)])
            S.op('dve', lambda e, hf=hf, t=t, bi=bi: e.tensor_tensor(out=yacc[:, t, hf * 512:(hf + 1) * 512], in0=PB(1 + hf), in1=xl[bi][:, hf * 512:(hf + 1) * 512], op=ALU.add),
                 reads=['xl%d' % bi], writes=[pk(1 + hf), 'yacc%d' % t])
        S.op('act', lambda e, t=t: e.activation(out=hsq, in_=yacc[:, t, :], func=AF.Square, accum_out=hss), reads=['yacc%d' % t], writes=['hsq', 'hss'])
        S.op('act', lambda e: e.activation(out=hss, in_=hss, func=AF.Sqrt, scale=1.0 / D, bias=EPS), writes=['hss'])
        S.op('dve', lambda e: e.reciprocal(out=hrs, in_=hss), reads=['hss'], writes=['hrs'])
        S.op('dve', lambda e, t=t: e.scalar_tensor_tensor(out=h2n, in0=yacc[:, t, :], scalar=hrs, in1=w2b, op0=ALU.mult, op1=ALU.mult),
             reads=['yacc%d' % t, 'hrs', 'w2b'], writes=['h2n'])
        for hf in range(2):
            def trh(e, hf=hf):
                for k in range(4):
                    kk = hf * 4 + k
                    ins = e.transpose(out=PB(3 + hf)[:, k * 128:(k + 1) * 128], in_=h2n[:, kk * 128:(kk + 1) * 128], identity=ident)
                return ins
            S.op('pe', trh, reads=['h2n', 'cst'], writes=[pk(3 + hf)])
            S.op('act', lambda e, hf=hf, t=t: e.activation(out=h2T[:, hf * 4:(hf + 1) * 4, t * 128:(t + 1) * 128], in_=P4(3 + hf), func=AF.Copy),
                 writes=[pk(3 + hf), 'h2T%d' % t])
            S.op('dve', lambda e, hf=hf: e.tensor_copy(out=h2Tf[:, hf * 4:(hf + 1) * 4, :], in_=P4(3 + hf)), writes=[pk(3 + hf), 'h2Tf'])

        def mmr(e):
            for k in range(8):
                ins = e.matmul(PB(5)[:, 0:36], lhsT=h2Tf[:, k, :], rhs=wrt[:, k, :], start=(k == 0), stop=(k == 7))
            return ins
        S.op('pe', mmr, reads=['h2Tf', 'wrt'], writes=[pk(5)])
        S.op('dve', lambda e: e.tensor_copy(out=lg, in_=PB(5)[:, 0:36]), writes=[pk(5), 'lg'])
        R_ = r_
        lgg = lg[:, 0:4]
        le = lg[:, 4:36].rearrange("p (g e) -> p g e", g=4)

        def V(e_, fn, r, w):
            S.op(e_, fn, reads=r, writes=w)
        V('dve', lambda e: e.tensor_reduce(out=R_['gm'][:, 0:1], in_=lgg, axis=AX.X, op=ALU.max), ['lg'], ['gm'])
        V('dve', lambda e: e.tensor_scalar(out=R_['oh'][:, 0:4], in0=lgg, scalar1=R_['gm'][:, 0:1], scalar2=None, op0=ALU.is_equal), ['lg', 'gm'], ['oh'])
        V('dve', lambda e: e.tensor_scalar(out=R_['eg'][:, 0:4], in0=lgg, scalar1=R_['gm'][:, 0:1], scalar2=None, op0=ALU.subtract), ['lg', 'gm'], ['eg'])
        V('act', lambda e: e.activation(out=R_['eg'][:, 0:4], in_=R_['eg'][:, 0:4], func=AF.Exp), [], ['eg'])
        V('dve', lambda e: e.tensor_reduce(out=R_['sg'][:, 0:1], in_=R_['eg'][:, 0:4], axis=AX.X, op=ALU.add), ['eg'], ['sg'])
        V('dve', lambda e: e.reciprocal(out=R_['pg'][:, 0:1], in_=R_['sg'][:, 0:1]), ['sg'], ['pg'])
        V('dve', lambda e: e.tensor_tensor(out=sel, in0=le, in1=R_['oh'][:, 0:4].unsqueeze(2).broadcast_to([128, 4, 8]), op=ALU.mult), ['lg', 'oh'], ['sel'])
        V('dve', lambda e: e.tensor_reduce(out=R_['ein'], in_=sel.rearrange("p g e -> p e g"), axis=AX.X, op=ALU.add), ['sel'], ['ein'])
        V('dve', lambda e: e.tensor_reduce(out=R_['m1'][:, 0:1], in_=R_['ein'], axis=AX.X, op=ALU.max), ['ein'], ['m1'])
        V('dve', lambda e: e.tensor_scalar(out=R_['oh1'], in0=R_['ein'], scalar1=R_['m1'][:, 0:1], scalar2=None, op0=ALU.is_equal), ['ein', 'm1'], ['oh1'])
        V('dve', lambda e: e.scalar_tensor_tensor(out=R_['e2'], in0=R_['oh1'], scalar=-1e30, in1=R_['ein'], op0=ALU.mult, op1=ALU.add), ['oh1', 'ein'], ['e2'])
        V('dve', lambda e: e.tensor_reduce(out=R_['m2'][:, 0:1], in_=R_['e2'], axis=AX.X, op=ALU.max), ['e2'], ['m2'])
        V('dve', lambda e: e.tensor_scalar(out=R_['oh2'], in0=R_['e2'], scalar1=R_['m2'][:, 0:1], scalar2=None, op0=ALU.is_equal), ['e2', 'm2'], ['oh2'])
        V('dve', lambda e: e.tensor_tensor(out=R_['w1'][:, 0:1], in0=R_['m2'][:, 0:1], in1=R_['m1'][:, 0:1], op=ALU.subtract), ['m1', 'm2'], ['w1'])
        V('act', lambda e: e.activation(out=R_['w1'][:, 0:1], in_=R_['w1'][:, 0:1], func=AF.Exp), [], ['w1'])
        V('dve', lambda e: e.tensor_scalar(out=R_['w1'][:, 0:1], in0=R_['w1'][:, 0:1], scalar1=1.0, scalar2=None, op0=ALU.add), [], ['w1'])
        V('dve', lambda e: e.reciprocal(out=R_['w1'][:, 0:1], in_=R_['w1'][:, 0:1]), [], ['w1'])
        V('dve', lambda e: e.tensor_tensor(out=R_['w1'][:, 0:1], in0=R_['w1'][:, 0:1], in1=R_['pg'][:, 0:1], op=ALU.mult), ['pg'], ['w1'])
        V('dve', lambda e: e.tensor_tensor(out=R_['w2'][:, 0:1], in0=R_['pg'][:, 0:1], in1=R_['w1'][:, 0:1], op=ALU.subtract), ['pg', 'w1'], ['w2'])
        V('dve', lambda e: e.tensor_scalar(out=R_['g8'], in0=R_['oh1'], scalar1=R_['w1'][:, 0:1], scalar2=None, op0=ALU.mult), ['oh1', 'w1'], ['g8'])
        V('dve', lambda e: e.scalar_tensor_tensor(out=R_['g8'], in0=R_['oh2'], scalar=R_['w2'][:, 0:1], in1=R_['g8'], op0=ALU.mult, op1=ALU.add), ['oh2', 'w2'], ['g8'])
        V('dve', lambda e, t=t: e.tensor_tensor(out=gates[:, t, :].rearrange("p (g e) -> p g e", g=4),
                                                in0=R_['oh'][:, 0:4].unsqueeze(2).broadcast_to([128, 4, 8]),
                                                in1=R_['g8'].unsqueeze(1).broadcast_to([128, 4, 8]), op=ALU.mult), ['oh', 'g8'], ['gates%d' % t])
    S.barrier()

    if stop_after == 'G2':
        S.emit()
        return nc
    A.off = base1
    gus = [A.f32([128, 8, 512]) for _ in range(2)]
    gub = [A.bf16([128, 8, 512]) for _ in range(2)]
    wds = [A.f32([128, 2, D]) for _ in range(2)]
    wdb = [A.bf16([128, 2, D]) for _ in range(2)]
    sgt = A.f32([128, 2, 512])
    hid = A.bf16([128, 2, 512])
    blocks = [(0, 512), (512, 512), (1024, 512), (1536, 512), (2048, 128)]
    for ex in range(32):
        wi = ex % 2
        DMA('sp', gus[wi], w_gu[ex].rearrange("(k p) c -> p k c", p=128), w=['gus%d' % wi])
        DMA('sp', wds[wi], w_d[ex].rearrange("(k p) c -> p k c", p=128), w=['wds%d' % wi])
        S.op('pool', lambda e, wi=wi: e.tensor_copy(out=gub[wi], in_=gus[wi]), reads=['gus%d' % wi], writes=['gub%d' % wi])
        S.op('pool', lambda e, wi=wi: e.tensor_copy(out=wdb[wi], in_=wds[wi]), reads=['wds%d' % wi], writes=['wdb%d' % wi])
        for (t0, nt) in blocks:
            for c in range(4):
                def mmg(e, c=c, wi=wi, t0=t0, nt=nt):
                    for k in range(8):
                        ins = e.matmul(PB(c)[:, 0:nt], lhsT=gub[wi][:, k, c * 128:(c + 1) * 128], rhs=h2T[:, k, t0:t0 + nt], start=(k == 0), stop=(k == 7))
                    return ins
                S.op('pe', mmg, reads=['gub%d' % wi] + ['h2T%d' % tt for tt in range(t0 // 128, (t0 + nt) // 128)], writes=[pk(c)])
            for c in range(2):
                S.op('act', lambda e, c=c, nt=nt: e.activation(out=sgt[:, c, 0:nt], in_=PB(c)[:, 0:nt], func=AF.Silu), writes=[pk(c), 'sgt%d' % c])
                S.op('dve', lambda e, c=c, nt=nt: e.tensor_tensor(out=hid[:, c, 0:nt], in0=sgt[:, c, 0:nt], in1=PB(2 + c)[:, 0:nt], op=ALU.mult),
                     reads=['sgt%d' % c], writes=[pk(2 + c), 'hid%d' % c])
            for ti in range(nt // 128):
                t = t0 // 128 + ti
                pby = 4 + 2 * (ti % 2)

                def mmd(e, ti=ti, wi=wi, pby=pby):
                    for hf in range(2):
                        for c in range(2):
                            ins = e.matmul(PB(pby + hf), lhsT=hid[:, c, ti * 128:(ti + 1) * 128], rhs=wdb[wi][:, c, hf * 512:(hf + 1) * 512],
                                           start=(c == 0), stop=(c == 1))
                    return ins
                S.op('pe', mmd, reads=['hid0', 'hid1', 'wdb%d' % wi], writes=[pk(pby), pk(pby + 1)])
                S.op('dve', lambda e, t=t, ex=ex, pby=pby: e.scalar_tensor_tensor(out=yacc[:, t, :], in0=ps_all[:, pby * 512:(pby + 2) * 512],
                                                                                 scalar=gates[:, t, ex:ex + 1], in1=yacc[:, t, :], op0=ALU.mult, op1=ALU.add),
                     reads=['gates%d' % t], writes=[pk(pby), pk(pby + 1), 'yacc%d' % t])
    for t in range(16):
        DMA('sp', y_own[t * 128:(t + 1) * 128, :], yacc[:, t, :], r=['yacc%d' % t], w=[])
    DMA('sp', y_samp, yacc[:, 16, :], r=['yacc16'], w=['y_samp'])
    S.emit()
    return nc


def sample_path(nc, S, A, base0, L):
    g_ = lambda n: L[n]
    proj, mix_d, zero_t, st_gdn, st_conv, caches = g_('proj'), g_('mix_d'), g_('zero_t'), g_('st_gdn'), g_('st_conv'), g_('caches')
    conv_wT, a_log, dt_bias, gnw, qnw, knw, rope_t, selh = g_('conv_wT'), g_('a_log'), g_('dt_bias'), g_('gnw'), g_('qnw'), g_('knw'), g_('rope_t'), g_('selh')
    sgs, kvs = g_('sgs'), g_('kvs')
    ident, ones, PB, pk, ps_all = g_('ident'), g_('ones'), g_('PB'), g_('pk'), g_('ps_all')
    NS = 16
    R0 = 3 + 4096

    def DMA(eng, out, in_, r=(), w=()):
        S.op(eng, lambda e: e.dma_start(out=out, in_=in_), reads=r, writes=w, dma=True)
    A.off = base0
    id16 = ident[0:NS, 0:NS]
    cw = A.f32([NS, 4, 1536])
    for k in range(4):
        DMA('sp', cw[:, k, :], conv_wT[k:k + 1, :].partition_broadcast(NS), w=['s_cw'])
    full = A.f32([NS, 4, 1536])
    DMA('sp', full[:, 0:3, :], st_conv, w=['s_full'])
    DMA('sp', full[:, 3, :], proj[R0:R0 + NS, 0:1536], w=['s_full'])
    zs = A.f32([NS, 512])
    DMA('sp', zs, proj[R0:R0 + NS, 1536:2048], w=['s_z'])
    ba = A.f32([NS, 8])
    DMA('sp', ba, proj[R0:R0 + NS, 2048:2056], w=['s_ba'])
    negA = A.f32([NS, 4])
    dtb = A.f32([NS, 4])
    DMA('sp', negA, a_log.partition_broadcast(NS), w=['s_negA'])
    DMA('sp', dtb, dt_bias.partition_broadcast(NS), w=['s_dtb'])
    gnb = A.f32([NS, 128])
    DMA('sp', gnb, gnw.partition_broadcast(NS), w=['s_gnb'])
    S.op('act', lambda e: e.activation(out=negA, in_=negA, func=AF.Exp), writes=['s_negA'])
    S.op('dve', lambda e: e.tensor_scalar(out=negA, in0=negA, scalar1=-1.0, scalar2=None, op0=ALU.mult), writes=['s_negA'])
    S.op('dve', lambda e: e.tensor_tensor(out=full, in0=full, in1=cw, op=ALU.mult), reads=['s_cw'], writes=['s_full'])
    cc = A.f32([NS, 1536])
    S.op('dve', lambda e: e.tensor_tensor(out=cc, in0=full[:, 0, :], in1=full[:, 1, :], op=ALU.add), reads=['s_full'], writes=['s_cc'])
    S.op('dve', lambda e: e.tensor_tensor(out=cc, in0=cc, in1=full[:, 2, :], op=ALU.add), reads=['s_full'], writes=['s_cc'])
    S.op('dve', lambda e: e.tensor_tensor(out=cc, in0=cc, in1=full[:, 3, :], op=ALU.add), reads=['s_full'], writes=['s_cc'])
    S.op('act', lambda e: e.activation(out=cc, in_=cc, func=AF.Silu), writes=['s_cc'])
    sq = A.f32([NS, 1024])
    ss8 = A.f32([NS, 8])
    rs8 = A.f32([NS, 8])
    qk = A.f32([NS, 8, 128])
    S.op('act', lambda e: e.activation(out=sq, in_=cc[:, 0:1024], func=AF.Square), reads=['s_cc'], writes=['s_sq'])
    S.op('dve', lambda e: e.tensor_reduce(out=ss8, in_=sq.rearrange("p (h d) -> p h d", h=8), axis=AX.X, op=ALU.add), reads=['s_sq'], writes=['s_ss8'])
    S.op('act', lambda e: e.activation(out=ss8, in_=ss8, func=AF.Sqrt, bias=EPS), writes=['s_ss8'])
    S.op('dve', lambda e: e.reciprocal(out=rs8, in_=ss8), reads=['s_ss8'], writes=['s_rs8'])
    S.op('dve', lambda e: e.tensor_scalar(out=rs8[:, 0:4], in0=rs8[:, 0:4], scalar1=128.0 ** -0.5, scalar2=None, op0=ALU.mult), writes=['s_rs8'])
    S.op('dve', lambda e: e.tensor_tensor(out=qk, in0=cc[:, 0:1024].rearrange("p (h d) -> p h d", h=8), in1=rs8.unsqueeze(2).broadcast_to([NS, 8, 128]), op=ALU.mult),
         reads=['s_cc', 's_rs8'], writes=['s_qk'])
    vv = cc[:, 1024:1536].rearrange("p (h d) -> p h d", h=4)
    beta = A.f32([NS, 4])
    gx = A.f32([NS, 4])
    eg = A.f32([NS, 4])
    S.op('act', lambda e: e.activation(out=beta, in_=ba[:, 0:4], func=AF.Sigmoid), reads=['s_ba'], writes=['s_beta'])
    S.op('dve', lambda e: e.tensor_tensor(out=gx, in0=ba[:, 4:8], in1=dtb, op=ALU.add), reads=['s_ba', 's_dtb'], writes=['s_gx'])
    S.op('act', lambda e: e.activation(out=gx, in_=gx, func=AF.Exp), writes=['s_gx'])
    S.op('act', lambda e: e.activation(out=gx, in_=gx, func=AF.Ln, bias=1.0), writes=['s_gx'])
    S.op('dve', lambda e: e.tensor_tensor(out=gx, in0=gx, in1=negA, op=ALU.mult), reads=['s_negA'], writes=['s_gx'])
    S.op('act', lambda e: e.activation(out=eg, in_=gx, func=AF.Exp), reads=['s_gx'], writes=['s_eg'])
    kqT = A.f32([128, 8, NS])

    def trkq(e):
        for j in range(8):
            ins = e.transpose(out=PB(0)[:, j * NS:(j + 1) * NS], in_=qk[:, j, :], identity=id16)
        return ins
    S.op('pe', trkq, reads=['s_qk', 'cst'], writes=[pk(0)])
    S.op('dve', lambda e: e.tensor_copy(out=kqT, in_=PB(0)[:, 0:8 * NS].rearrange("p (j s) -> p j s", j=8)), writes=[pk(0), 's_kqT'])
    egd = A.f32([NS, NS, 4])
    S.op('dve', lambda e: e.tensor_tensor(out=egd, in0=id16.unsqueeze(2).broadcast_to([NS, NS, 4]), in1=eg.unsqueeze(1).broadcast_to([NS, NS, 4]), op=ALU.mult),
         reads=['cst', 's_eg'], writes=['s_egd'])
    S.op('pe', lambda e: e.matmul(PB(1)[:, 0:NS * 4], lhsT=ones[0:NS, :], rhs=egd.rearrange("p s h -> p (s h)"), start=True, stop=True),
         reads=['s_egd', 'cst'], writes=[pk(1)])
    egb = A.f32([128, NS, 4])
    S.op('dve', lambda e: e.tensor_copy(out=egb, in_=PB(1)[:, 0:NS * 4].rearrange("p (s h) -> p s h", s=NS)), writes=[pk(1), 's_egb'])
    Sold = [A.f32([128, 4, 128]) for _ in range(3)]
    Snew = [A.f32([128, 4, 128]) for _ in range(3)]
    kSacc = A.f32([NS, 4, 128])
    oacc = A.f32([NS, 4, 128])
    S.op('pool', lambda e: e.memset(kSacc, 0.0), writes=['s_kSacc'])
    S.op('pool', lambda e: e.memset(oacc, 0.0), writes=['s_oacc'])
    for s_ in range(NS):
        bi = s_ % 3
        DMA('sp', Sold[bi], st_gdn[s_].rearrange("h k v -> k h v"), w=['s_Sold%d' % bi])

        def mm1(e, bi=bi):
            for h in range(4):
                ins = e.matmul(PB(2)[0:NS, h * 128:(h + 1) * 128], lhsT=kqT[:, 4 + h, :], rhs=Sold[bi][:, h, :], start=True, stop=True)
            return ins
        S.op('pe', mm1, reads=['s_kqT', 's_Sold%d' % bi], writes=[pk(2)])
        S.op('dve', lambda e, s_=s_: e.scalar_tensor_tensor(out=kSacc.rearrange("p h d -> p (h d)"), in0=PB(2)[0:NS, :], scalar=id16[:, s_:s_ + 1],
                                                            in1=kSacc.rearrange("p h d -> p (h d)"), op0=ALU.mult, op1=ALU.add),
             reads=['cst'], writes=[pk(2), 's_kSacc'])
    vn = A.f32([NS, 4, 128])
    S.op('dve', lambda e: e.tensor_tensor(out=vn, in0=kSacc, in1=eg.unsqueeze(2).broadcast_to([NS, 4, 128]), op=ALU.mult), reads=['s_kSacc', 's_eg'], writes=['s_vn'])
    S.op('dve', lambda e: e.tensor_tensor(out=vn, in0=vv, in1=vn, op=ALU.subtract), reads=['s_cc'], writes=['s_vn'])
    S.op('dve', lambda e: e.tensor_tensor(out=vn, in0=vn, in1=beta.unsqueeze(2).broadcast_to([NS, 4, 128]), op=ALU.mult), reads=['s_beta'], writes=['s_vn'])
    vm = [A.f32([NS, 4, 128]) for _ in range(2)]
    stmp = [A.f32([128, 4, 128]) for _ in range(2)]
    for s_ in range(NS):
        bi = s_ % 3
        b2 = s_ % 2
        DMA('sp', Sold[bi], st_gdn[s_].rearrange("h k v -> k h v"), w=['s_Sold%d' % bi])
        S.op('dve', lambda e, s_=s_, b2=b2: e.tensor_scalar(out=vm[b2], in0=vn, scalar1=id16[:, s_:s_ + 1], scalar2=None, op0=ALU.mult),
             reads=['s_vn', 'cst'], writes=['s_vm%d' % b2])

        def mm2(e, b2=b2):
            for h in range(4):
                ins = e.matmul(PB(3)[:, h * 128:(h + 1) * 128], lhsT=qk[:, 4 + h, :], rhs=vm[b2][:, h, :], start=True, stop=True)
            return ins
        S.op('pe', mm2, reads=['s_qk', 's_vm%d' % b2], writes=[pk(3)])
        S.op('pool', lambda e, bi=bi, b2=b2, s_=s_: e.tensor_tensor(out=stmp[b2], in0=Sold[bi], in1=egb[:, s_, :].unsqueeze(2).broadcast_to([128, 4, 128]), op=ALU.mult),
             reads=['s_Sold%d' % bi, 's_egb'], writes=['s_stmp%d' % b2])
        S.op('dve', lambda e, bi=bi, b2=b2: e.tensor_tensor(out=Snew[bi], in0=stmp[b2], in1=PB(3).rearrange("p (h d) -> p h d", h=4), op=ALU.add),
             reads=['s_stmp%d' % b2], writes=[pk(3), 's_Snew%d' % bi])
        DMA('pool', sgs[s_].rearrange("h k v -> k h v"), Snew[bi], r=['s_Snew%d' % bi], w=[])

        def mm3(e, bi=bi):
            for h in range(4):
                ins = e.matmul(PB(4)[0:NS, h * 128:(h + 1) * 128], lhsT=kqT[:, h, :], rhs=Snew[bi][:, h, :], start=True, stop=True)
            return ins
        S.op('pe', mm3, reads=['s_kqT', 's_Snew%d' % bi], writes=[pk(4)])
        S.op('dve', lambda e, s_=s_: e.scalar_tensor_tensor(out=oacc.rearrange("p h d -> p (h d)"), in0=PB(4)[0:NS, :], scalar=id16[:, s_:s_ + 1],
                                                            in1=oacc.rearrange("p h d -> p (h d)"), op0=ALU.mult, op1=ALU.add),
             reads=['cst'], writes=[pk(4), 's_oacc'])
    mixs = A.f32([NS, 1024])
    o4 = A.f32([NS, 4])
    S.op('act', lambda e: e.activation(out=sq[:, 0:512], in_=oacc.rearrange("p h d -> p (h d)"), func=AF.Square), reads=['s_oacc'], writes=['s_sq'])
    S.op('dve', lambda e: e.tensor_reduce(out=o4, in_=sq[:, 0:512].rearrange("p (h d) -> p h d", h=4), axis=AX.X, op=ALU.add), reads=['s_sq'], writes=['s_o4'])
    S.op('act', lambda e: e.activation(out=o4, in_=o4, func=AF.Sqrt, scale=1.0 / 128, bias=EPS), writes=['s_o4'])
    S.op('dve', lambda e: e.reciprocal(out=o4, in_=o4), writes=['s_o4'])
    m3 = mixs[:, 0:512].rearrange("p (h d) -> p h d", h=4)
    S.op('dve', lambda e: e.tensor_tensor(out=m3, in0=oacc, in1=o4.unsqueeze(2).broadcast_to([NS, 4, 128]), op=ALU.mult), reads=['s_oacc', 's_o4'], writes=['s_mix'])
    S.op('dve', lambda e: e.tensor_tensor(out=m3, in0=m3, in1=gnb.unsqueeze(1).broadcast_to([NS, 4, 128]), op=ALU.mult), reads=['s_gnb'], writes=['s_mix'])
    S.op('act', lambda e: e.activation(out=zs, in_=zs, func=AF.Silu), writes=['s_z'])
    S.op('dve', lambda e: e.tensor_tensor(out=mixs[:, 0:512], in0=mixs[:, 0:512], in1=zs, op=ALU.mult), reads=['s_z'], writes=['s_mix'])
    qkw = A.f32([NS, 6, 64])
    for gi in range(3):
        DMA('sp', qkw[:, gi, :], qnw[gi:gi + 1, :].partition_broadcast(NS), w=['s_qkw'])
        DMA('sp', qkw[:, 3 + gi, :], knw[gi:gi + 1, :].partition_broadcast(NS), w=['s_qkw'])
    rp = A.f32([NS, 16])
    DMA('sp', rp, rope_t[4096:4096 + NS, :], w=['s_rp'])
    cosb = rp[:, 0:8].unsqueeze(1).broadcast_to([NS, 8, 8])
    sinb = rp[:, 8:16].unsqueeze(1).broadcast_to([NS, 8, 8])
    qkv = A.f32([NS, 3, 3, 512])
    DMA('sp', qkv.rearrange("p g w c -> p (g w c)"), proj[R0:R0 + NS, 2056:2056 + 4608], w=['s_qkv'])
    s2 = A.f32([NS, 512])
    r8 = A.f32([NS, 8])
    rt = [A.f32([NS, 8, 8]) for _ in range(4)]
    for gi in range(3):
        for which in range(2):
            d3 = qkv[:, gi, which, :].rearrange("p (h d) -> p h d", h=8)
            S.op('act', lambda e, gi=gi, which=which: e.activation(out=s2, in_=qkv[:, gi, which, :], func=AF.Square), reads=['s_qkv'], writes=['s_s2'])
            S.op('dve', lambda e: e.tensor_reduce(out=r8, in_=s2.rearrange("p (h d) -> p h d", h=8), axis=AX.X, op=ALU.add), reads=['s_s2'], writes=['s_r8'])
            S.op('act', lambda e: e.activation(out=r8, in_=r8, func=AF.Sqrt, scale=1.0 / 64, bias=EPS), writes=['s_r8'])
            S.op('dve', lambda e: e.reciprocal(out=r8, in_=r8), writes=['s_r8'])
            S.op('dve', lambda e, d3=d3: e.tensor_tensor(out=d3, in0=d3, in1=r8.unsqueeze(2).broadcast_to([NS, 8, 64]), op=ALU.mult), reads=['s_r8'], writes=['s_qkv'])
            wrow = qkw[:, (3 if which == 1 else 0) + gi, :].unsqueeze(1).broadcast_to([NS, 8, 64])
            S.op('dve', lambda e, d3=d3, wrow=wrow: e.tensor_tensor(out=d3, in0=d3, in1=wrow, op=ALU.mult), reads=['s_qkw'], writes=['s_qkv'])
            x1 = d3[:, :, 0:8]
            x2 = d3[:, :, 8:16]
            S.op('dve', lambda e, x1=x1: e.tensor_tensor(out=rt[0], in0=x1, in1=cosb, op=ALU.mult), reads=['s_qkv', 's_rp'], writes=['s_rt0'])
            S.op('dve', lambda e, x2=x2: e.tensor_tensor(out=rt[1], in0=x2, in1=sinb, op=ALU.mult), reads=['s_qkv', 's_rp'], writes=['s_rt1'])
            S.op('dve', lambda e, x2=x2: e.tensor_tensor(out=rt[2], in0=x2, in1=cosb, op=ALU.mult), reads=['s_qkv', 's_rp'], writes=['s_rt2'])
            S.op('dve', lambda e, x1=x1: e.tensor_tensor(out=rt[3], in0=x1, in1=sinb, op=ALU.mult), reads=['s_qkv', 's_rp'], writes=['s_rt3'])
            S.op('dve', lambda e, x1=x1: e.tensor_tensor(out=x1, in0=rt[0], in1=rt[1], op=ALU.subtract), reads=['s_rt0', 's_rt1'], writes=['s_qkv'])
            S.op('dve', lambda e, x2=x2: e.tensor_tensor(out=x2, in0=rt[2], in1=rt[3], op=ALU.add), reads=['s_rt2', 's_rt3'], writes=['s_qkv'])
        W = GROUPS[gi][0]
        DMA('pool', kvs[gi][:, W - 1, :], qkv[:, gi, 1:3, :].rearrange("p w c -> p (w c)"), r=['s_qkv'], w=['kvs_new%d' % gi])
    selq = A.f32([NS, NS, 128])
    S.op('dve', lambda e: e.tensor_copy(out=selq, in_=id16.unsqueeze(2).broadcast_to([NS, NS, 128])), reads=['cst'], writes=['s_selq'])
    selh_t = A.f32([8, NS, NS])
    DMA('sp', selh_t, selh, w=['s_selh'])
    kvt = [A.f32([128, 1024]) for _ in range(3)]
    prod = A.f32([128, 512])
    sc = A.f32([128, 8])
    pex = A.f32([128, 8])
    Z = A.f32([8, 8, 64])
    Z2 = A.f32([8, 8])
    numS = A.f32([NS, 8, 64])
    denS = A.f32([NS, 8])
    ps_ = A.f32([NS, 8])
    pr16 = A.f32([NS, 8, 64])
    S.op('pool', lambda e: e.memset(numS, 0.0), writes=['s_numS'])
    S.op('pool', lambda e: e.memset(denS, 0.0), writes=['s_denS'])
    cnt = 0
    for gi in range(3):
        W, dil = GROUPS[gi]
        q3 = qkv[:, gi, 0, :].rearrange("p (h d) -> p h d", h=8)
        k3 = qkv[:, gi, 1, :].rearrange("p (h d) -> p h d", h=8)
        v3 = qkv[:, gi, 2, :].rearrange("p (h d) -> p h d", h=8)
        S.op('dve', lambda e, q3=q3, k3=k3: e.tensor_tensor(out=pr16, in0=q3, in1=k3, op=ALU.mult), reads=['s_qkv'], writes=['s_pr16'])
        S.op('dve', lambda e: e.tensor_reduce(out=ps_, in_=pr16, axis=AX.X, op=ALU.add), reads=['s_pr16'], writes=['s_ps'])
        S.op('act', lambda e: e.activation(out=ps_, in_=ps_, func=AF.Exp, scale=0.125), writes=['s_ps'])
        S.op('dve', lambda e: e.tensor_tensor(out=denS, in0=denS, in1=ps_, op=ALU.add), reads=['s_ps'], writes=['s_denS'])
        S.op('dve', lambda e, v3=v3: e.tensor_tensor(out=pr16, in0=v3, in1=ps_.unsqueeze(2).broadcast_to([NS, 8, 64]), op=ALU.mult), reads=['s_qkv', 's_ps'], writes=['s_pr16'])
        S.op('dve', lambda e: e.tensor_tensor(out=numS, in0=numS, in1=pr16, op=ALU.add), reads=['s_pr16'], writes=['s_numS'])
        for s_ in range(NS):
            bi = cnt % 3
            first = (cnt == 0)
            last = (cnt == 3 * NS - 1)
            cnt += 1
            KV = kvt[bi]
            kkv = 's_kvt%d' % bi
            DMA('sp', KV, caches[gi][s_, 0:W - dil + 1:dil, :], w=[kkv])
            S.op('pe', lambda e, s_=s_, gi=gi: e.matmul(PB(0), lhsT=selq[:, s_, :], rhs=qkv[:, gi, 0, :], start=True, stop=True), reads=['s_selq', 's_qkv'], writes=[pk(0)])
            S.op('dve', lambda e, KV=KV: e.tensor_tensor(out=prod, in0=KV[:, 0:512], in1=PB(0), op=ALU.mult), reads=[kkv], writes=[pk(0), 's_prod'])
            S.op('dve', lambda e: e.tensor_reduce(out=sc, in_=prod.rearrange("p (h d) -> p h d", h=8), axis=AX.X, op=ALU.add), reads=['s_prod'], writes=['s_sc'])
            S.op('act', lambda e: e.activation(out=pex, in_=sc, func=AF.Exp, scale=0.125), reads=['s_sc'], writes=['s_pex'])
            S.op('pe', lambda e, KV=KV: e.matmul(PB(1)[0:8, :], lhsT=pex, rhs=KV[:, 512:1024], start=True, stop=True), reads=['s_pex', kkv], writes=[pk(1)])
            S.op('pe', lambda e: e.matmul(PB(2)[0:8, 0:8], lhsT=pex, rhs=ones[:, 0:8], start=True, stop=True), reads=['s_pex', 'cst'], writes=[pk(2)])
            S.op('dve', lambda e: e.tensor_tensor(out=Z, in0=PB(1)[0:8, :].rearrange("p (h d) -> p h d", h=8), in1=ident[0:8, 0:8].unsqueeze(2).broadcast_to([8, 8, 64]), op=ALU.mult),
                 reads=['cst'], writes=[pk(1), 's_Z'])
            S.op('dve', lambda e: e.tensor_tensor(out=Z2, in0=PB(2)[0:8, 0:8], in1=ident[0:8, 0:8], op=ALU.mult), reads=['cst'], writes=[pk(2), 's_Z2'])
            S.op('pe', lambda e, s_=s_, first=first, last=last: e.matmul(PB(6)[0:NS, :], lhsT=selh_t[:, s_, :], rhs=Z.rearrange("p h d -> p (h d)"), start=first, stop=last),
                 reads=['s_selh', 's_Z'], writes=[pk(6)])
            S.op('pe', lambda e, s_=s_, first=first, last=last: e.matmul(PB(7)[0:NS, 0:8], lhsT=selh_t[:, s_, :], rhs=Z2, start=first, stop=last),
                 reads=['s_selh', 's_Z2'], writes=[pk(7)])
    S.op('dve', lambda e: e.tensor_tensor(out=numS, in0=numS, in1=PB(6)[0:NS, :].rearrange("p (h d) -> p h d", h=8), op=ALU.add), writes=[pk(6), 's_numS'])
    S.op('dve', lambda e: e.tensor_tensor(out=denS, in0=denS, in1=PB(7)[0:NS, 0:8], op=ALU.add), writes=[pk(7), 's_denS'])
    S.op('dve', lambda e: e.reciprocal(out=denS, in_=denS), writes=['s_denS'])
    S.op('dve', lambda e: e.tensor_tensor(out=mixs[:, 512:1024].rearrange("p (h d) -> p h d", h=8), in0=numS, in1=denS.unsqueeze(2).broadcast_to([NS, 8, 64]), op=ALU.mult),
         reads=['s_numS', 's_denS'], writes=['s_mix'])
    DMA('pool', mix_d[2048:2048 + NS, :], mixs, r=['s_mix'], w=['mix_d_s'])
    for j in range(2):
        DMA('pool', mix_d[2048 + NS:2176, j * 512:(j + 1) * 512], zero_t[0:128 - NS, 0:512], r=['zero'], w=['mix_d_z%d' % j])
    S.barrier()


_CACHE = {}


def _host_consts():
    c = np.zeros((128, 5, 128), np.float32)
    p = np.arange(128)[:, None]
    f = np.arange(128)[None, :]
    c[:, 0] = (p == f)
    c[:, 1] = (p <= f)
    c[:, 2] = (p >= f)
    c[:, 3] = (p < f)
    c[:, 4] = 1.0
    return c


def _rope_table(pos):
    half = 8
    inv = np.exp(-math.log(500000.0) * np.arange(half, dtype=np.float32) * np.float32(2.0 / 16)).astype(np.float32)
    ang = pos.astype(np.float32)[:, None] * inv[None, :]
    return np.concatenate([np.cos(ang), np.sin(ang)], axis=1).astype(np.float32)


def kernel(x_prompt, x_sample, state_gdn, state_conv, cache_kv_w128, cache_kv_w512, cache_kv_w2048,
           norm1_w, w_in, conv_w, a_log, dt_bias, gdn_norm_w, q_norm_w, k_norm_w, w_out, norm2_w,
           w_router_group, w_router_expert, w_gate_up, w_down):
    f = lambda a: np.ascontiguousarray(np.asarray(a, dtype=np.float32))
    x_prompt, x_sample = f(x_prompt), f(x_sample)
    if 'nc' not in _CACHE:
        _CACHE['nc'] = build_program()
    nc = _CACHE['nc']
    consts = _host_consts()
    w_r = np.concatenate([f(w_router_group)[0], f(w_router_expert)[0]], axis=1)
    shared = {
        "w_in": f(w_in)[0], "w_out": f(w_out)[0], "norm1": f(norm1_w), "norm2": f(norm2_w),
        "conv_wT": np.ascontiguousarray(f(conv_w)[0].T), "a_log": f(a_log), "dt_bias": f(dt_bias), "gnw": f(gdn_norm_w),
        "qnw": f(q_norm_w)[0], "knw": f(k_norm_w)[0], "w_r": np.ascontiguousarray(w_r), "w_gu": f(w_gate_up)[0], "w_d": f(w_down)[0],
        "consts": consts,
        "selh": np.ascontiguousarray(np.broadcast_to(np.eye(16, dtype=np.float32)[None], (8, 16, 16))),
    }
    cachesf = [f(cache_kv_w128)[0], f(cache_kv_w512)[0], f(cache_kv_w2048)[0]]
    sg, sc = f(state_gdn)[0], f(state_conv)[0]
    in_maps = []
    for c in range(NCORE):
        b, half = c // 2, c % 2
        xa = np.zeros((NTILE * 128, D), np.float32)
        if half == 1:
            xa[0:2048] = x_prompt[b, 0:2048]
        xa[2048:4096] = x_prompt[b, half * 2048:(half + 1) * 2048]
        xa[4096:4112] = x_sample[16 * c:16 * c + 16, 0]
        pos = np.concatenate([np.arange(4096) + (half - 1) * 2048, np.full(128, 8192)]).astype(np.float32)
        m = dict(shared)
        m["x_all"] = xa
        m["rope_t"] = _rope_table(pos)
        m["maskb"] = np.full((128, 1), 0.0 if half == 1 else -30000.0, np.float32)
        m["st_gdn"] = np.ascontiguousarray(sg[16 * c:16 * c + 16])
        m["st_conv"] = np.ascontiguousarray(sc[16 * c:16 * c + 16])
        for i in range(3):
            m["cache%d" % i] = np.ascontiguousarray(cachesf[i][16 * c:16 * c + 16].reshape(16, GROUPS[i][0], 1024))
        in_maps.append(m)
    if _CACHE.get('only_maps'):
        return in_maps
    res = run_bass_kernel_spmd(nc, in_maps, core_ids=list(range(NCORE)))
    R = res.results
    y_prompt = np.zeros((4, 4096, D), np.float32)
    for c in range(NCORE):
        y_prompt[c // 2, (c % 2) * 2048:(c % 2 + 1) * 2048] = R[c]["y_own"]
    y_sample = np.concatenate([R[c]["y_samp"][0:16] for c in range(NCORE)], axis=0).reshape(128, 1, D)
    sgp_o = np.stack([R[2 * b + 1]["sgp"] for b in range(4)])[None]
    scp_o = np.stack([R[2 * b + 1]["scp"] for b in range(4)])[None]
    kvp_o = [np.stack([R[2 * b + 1]["kvp%d" % i] for b in range(4)]).reshape(1, 4, GROUPS[i][0], 2, 8, 64) for i in range(3)]
    sgs_o = np.concatenate([R[c]["sgs"] for c in range(NCORE)], axis=0)[None]
    scs_o = np.concatenate([R[c]["scs"] for c in range(NCORE)], axis=0)[None]
    kvs_o = [np.concatenate([R[c]["kvs%d" % i] for c in range(NCORE)], axis=0).reshape(1, 128, GROUPS[i][0], 2, 8, 64) for i in range(3)]
    return (y_prompt, y_sample, sgp_o, scp_o, kvp_o[0], kvp_o[1], kvp_o[2], sgs_o, scs_o, kvs_o[0], kvs_o[1], kvs_o[2])
```
